# Optimizing a Trainium2 kernel written in Bass

```python
import numpy as np
import jax
import jax.numpy as jnp
from jax import lax

D_MODEL = 1024
BATCH = 8
SEQ = 4096
DEPTH = 2

BRANCH_WIDTH = D_MODEL // 2
N_BRANCHES = 3
A_HEAD_DIM = 64
A_HEADS = BRANCH_WIDTH // A_HEAD_DIM
IDX_HEADS = 4
IDX_DIM = 64
INDEX_TOPK = 256
B_HEAD_DIM = 64
B_HEADS = BRANCH_WIDTH // B_HEAD_DIM
C_HEAD_DIM = 128
C_HEADS = BRANCH_WIDTH // C_HEAD_DIM
CONV_WIDTH = 4
DELTA_CHUNK = 64
Q_BLOCK = 128
ROPE_THETA = 500000.0
ROT_DIM = A_HEAD_DIM // 4
FFN_DIM = 2 * D_MODEL
NORM_EPS = 1e-6
L2_EPS = 1e-6
IN_SIZES = (
    A_HEADS * A_HEAD_DIM,
    A_HEAD_DIM,
    A_HEAD_DIM,
    IDX_HEADS * IDX_DIM,
    IDX_DIM,
    IDX_HEADS,
    3 * BRANCH_WIDTH,
    B_HEADS,
    3 * BRANCH_WIDTH,
    BRANCH_WIDTH,
    C_HEADS,
    C_HEADS,
    N_BRANCHES * D_MODEL,
)
IN_DIM = 7636

kernel_name = 'hybrid_dsa_fox_gdn_macaron'


def rms_norm(x, gain):
    xf = x.astype(jnp.float32)
    y = xf * lax.rsqrt(jnp.mean(xf * xf, axis=-1, keepdims=True) + NORM_EPS)
    return (y * gain.astype(jnp.float32)).astype(x.dtype)


def l2_normalize(x):
    xf = x.astype(jnp.float32)
    return (xf * lax.rsqrt(jnp.sum(xf * xf, axis=-1, keepdims=True) + L2_EPS)).astype(x.dtype)


def rotary_tables(seq_len):
    pos = jnp.arange(seq_len, dtype=jnp.float32)
    inv_freq = jnp.power(ROPE_THETA, -jnp.arange(0, ROT_DIM, 2, dtype=jnp.float32) / ROT_DIM)
    ang = pos[:, None] * inv_freq[None, :]
    return jnp.cos(ang), jnp.sin(ang)


def apply_partial_rotary(t, cos, sin):
    half = ROT_DIM // 2
    c = cos[None, :, None, :].astype(t.dtype)
    s = sin[None, :, None, :].astype(t.dtype)
    x1 = t[..., :half]
    x2 = t[..., half:ROT_DIM]
    return jnp.concatenate([x1 * c - x2 * s, x2 * c + x1 * s, t[..., ROT_DIM:]], axis=-1)


def swiglu_ffn(h, w_in, w_out):
    gate, up = jnp.split(h @ w_in, 2, axis=-1)
    return (jax.nn.silu(gate) * up) @ w_out


def causal_depthwise_conv(x, w):
    k_width, ch = w.shape
    return lax.conv_general_dilated(
        x, w[:, None, :].astype(x.dtype), window_strides=(1,), padding=[(k_width - 1, 0)],
        dimension_numbers=('NWC', 'WIO', 'NWC'), feature_group_count=ch)


def dsa_sparse_attention(q, k, v, q_idx, k_idx, w_idx, top_k):
    b, s, h, d = q.shape
    nb = s // Q_BLOCK
    pos = jnp.arange(s)
    kv = jnp.concatenate([k, v], axis=-1)

    def to_blocks(t):
        return jnp.swapaxes(t.reshape((b, nb, Q_BLOCK) + t.shape[2:]), 0, 1)

    def block(args):
        qb, qib, wb, pb = args
        rel = jax.nn.relu(jnp.einsum('bqhd,bsd->bqhs', qib, k_idx).astype(jnp.float32) * IDX_DIM ** -0.5)
        score = jnp.einsum('bqhs,bqh->bqs', rel, wb.astype(jnp.float32))
        causal = pos[None, :] <= pb[:, None]
        score = jnp.where(causal[None], score, -jnp.inf)
        _, sel = lax.top_k(score, top_k)
        kv_sel = jax.vmap(lambda kv_b, sel_b: kv_b[sel_b])(kv, sel)
        k_sel = kv_sel[..., :d]
        v_sel = kv_sel[..., d:]
        logits = jnp.einsum('bqhd,bqkd->bhqk', qb, k_sel).astype(jnp.float32) * d ** -0.5
        valid = (sel <= pb[None, :, None])[:, None]
        p = jax.nn.softmax(jnp.where(valid, logits, -jnp.inf), axis=-1).astype(v.dtype)
        return jnp.einsum('bhqk,bqkd->bqhd', p, v_sel)

    out = lax.map(block, (to_blocks(q), to_blocks(q_idx), to_blocks(w_idx), pos.reshape(nb, Q_BLOCK)))
    return jnp.swapaxes(out, 0, 1).reshape(b, s, h, d)


def forgetting_attention(q, k, v, log_f):
    b, s, h, d = q.shape
    nb = s // Q_BLOCK
    pos = jnp.arange(s)
    c = jnp.transpose(jnp.cumsum(log_f, axis=1), (0, 2, 1))

    def block(args):
        qb, cb, pb = args
        logits = (jnp.einsum('bqhd,bshd->bhqs', qb, k).astype(jnp.float32) * d ** -0.5
                  + (cb[..., :, None] - c[..., None, :]))
        causal = pos[None, :] <= pb[:, None]
        p = jax.nn.softmax(jnp.where(causal, logits, -jnp.inf), axis=-1).astype(v.dtype)
        return jnp.einsum('bhqs,bshd->bqhd', p, v)

    q_blocks = jnp.swapaxes(q.reshape(b, nb, Q_BLOCK, h, d), 0, 1)
    c_blocks = jnp.transpose(c.reshape(b, h, nb, Q_BLOCK), (2, 0, 1, 3))
    out = lax.map(block, (q_blocks, c_blocks, pos.reshape(nb, Q_BLOCK)))
    return jnp.swapaxes(out, 0, 1).reshape(b, s, h, d)


def chunked_gated_delta_rule(q, k, v, g, beta):
    b, s, h, dk = q.shape
    dv = v.shape[-1]
    cs = DELTA_CHUNK
    n = s // cs

    def chunks(t):
        t = t.astype(jnp.float32).reshape((b, n, cs, h) + t.shape[3:])
        return jnp.moveaxis(t, (1, 3), (0, 2))

    qc, kc, vc, gc, bc = chunks(q), chunks(k), chunks(v), chunks(g), chunks(beta)
    gc = jnp.cumsum(gc, axis=-1)
    idx = jnp.arange(cs)
    incl = idx[:, None] >= idx[None, :]
    strict = idx[:, None] > idx[None, :]
    decay = jnp.exp(jnp.where(incl, gc[..., :, None] - gc[..., None, :], -jnp.inf))
    kb = kc * bc[..., None]
    a_mat = jnp.where(strict, jnp.einsum('nbhid,nbhjd->nbhij', kb, kc) * decay, 0.0)
    m = a_mat + jnp.eye(cs, dtype=jnp.float32)
    rhs = jnp.concatenate([vc * bc[..., None], kb * jnp.exp(gc)[..., None]], axis=-1)
    sol = lax.linalg.triangular_solve(m, rhs, left_side=True, lower=True, unit_diagonal=True)
    u = sol[..., :dv]
    w = sol[..., dv:]
    qk = jnp.where(incl, jnp.einsum('nbhid,nbhjd->nbhij', qc, kc) * decay, 0.0)

    def step(state, xs):
        q_i, k_i, u_i, w_i, qk_i, g_i = xs
        v_new = u_i - jnp.einsum('bhcd,bhde->bhce', w_i, state)
        o_i = (jnp.einsum('bhcd,bhde->bhce', q_i * jnp.exp(g_i)[..., None], state)
               + jnp.einsum('bhcj,bhje->bhce', qk_i, v_new))
        g_last = g_i[..., -1]
        state = (state * jnp.exp(g_last)[..., None, None]
                 + jnp.einsum('bhcd,bhce->bhde', k_i * jnp.exp(g_last[..., None] - g_i)[..., None], v_new))
        return state, o_i

    state0 = jnp.zeros((b, h, dk, dv), jnp.float32)
    _, o = lax.scan(step, state0, (qc, kc, u, w, qk, gc))
    o = jnp.moveaxis(o, (0, 2), (1, 3)).reshape(b, s, h, dv)
    return o.astype(v.dtype)


def hybrid_mixer(h, cos, sin, top_k, w_in, b_gate, b_forget, conv_w, a_log, dt_bias,
                 delta_norm, w_branch_a, w_branch_b, w_branch_c, w_out):
    b, s, _ = h.shape
    proj = h @ w_in
    (a_q, a_k, a_v, i_q, i_k, i_w, b_qkv, b_f, c_qkv, c_z, c_beta, c_a, gate_logits) = jnp.split(
        proj, np.cumsum(IN_SIZES)[:-1].tolist(), axis=-1)

    q_a = apply_partial_rotary(a_q.reshape(b, s, A_HEADS, A_HEAD_DIM), cos, sin)
    k_a = apply_partial_rotary(a_k[:, :, None, :], cos, sin)[:, :, 0]
    q_i = apply_partial_rotary(i_q.reshape(b, s, IDX_HEADS, IDX_DIM), cos, sin)
    k_i = apply_partial_rotary(i_k[:, :, None, :], cos, sin)[:, :, 0]
    w_i = i_w * IDX_HEADS ** -0.5
    y_a = dsa_sparse_attention(q_a, k_a, a_v, q_i, k_i, w_i, top_k)

    qkv_b = b_qkv.reshape(b, s, 3, B_HEADS, B_HEAD_DIM)
    log_f = jax.nn.log_sigmoid((b_f + b_forget).astype(jnp.float32))
    y_b = forgetting_attention(qkv_b[:, :, 0], qkv_b[:, :, 1], qkv_b[:, :, 2], log_f)

    qkv_c = jax.nn.silu(causal_depthwise_conv(c_qkv, conv_w)).reshape(b, s, 3, C_HEADS, C_HEAD_DIM)
    q_c = l2_normalize(qkv_c[:, :, 0]) * C_HEAD_DIM ** -0.5
    k_c = l2_normalize(qkv_c[:, :, 1])
    v_c = qkv_c[:, :, 2]
    beta = jax.nn.sigmoid(c_beta.astype(jnp.float32))
    g = -jnp.exp(a_log.astype(jnp.float32)) * jax.nn.softplus(c_a.astype(jnp.float32) + dt_bias.astype(jnp.float32))
    o_c = chunked_gated_delta_rule(q_c, k_c, v_c, g, beta)
    y_c = rms_norm(o_c, delta_norm) * jax.nn.silu(c_z.reshape(b, s, C_HEADS, C_HEAD_DIM))

    y_a = y_a.reshape(b, s, BRANCH_WIDTH) @ w_branch_a
    y_b = y_b.reshape(b, s, BRANCH_WIDTH) @ w_branch_b
    y_c = y_c.reshape(b, s, BRANCH_WIDTH) @ w_branch_c
    gates = jax.nn.sigmoid((gate_logits + b_gate).astype(jnp.float32)).astype(h.dtype)
    gates = gates.reshape(b, s, N_BRANCHES, D_MODEL)
    merged = gates[:, :, 0] * y_a + gates[:, :, 1] * y_b + gates[:, :, 2] * y_c
    return merged @ w_out


def setup_inputs(seed: int = 0) -> dict:
    key = jax.random.key(seed)
    ks = jax.random.split(key, 24)
    f32 = jnp.float32

    def nrm(k, shape, scale):
        return jax.random.normal(k, shape, f32) * scale

    def gain(k, shape):
        return 1.0 + 0.02 * jax.random.normal(k, shape, f32)

    dt = jnp.exp(jax.random.uniform(ks[10], (DEPTH, C_HEADS), f32, np.log(0.001), np.log(0.1)))
    return {
        'x': nrm(ks[0], (BATCH, SEQ, D_MODEL), 1.0),
        'ffn1_norm': gain(ks[1], (DEPTH, D_MODEL)),
        'ffn1_w_in': nrm(ks[2], (DEPTH, D_MODEL, 2 * FFN_DIM), D_MODEL ** -0.5),
        'ffn1_w_out': nrm(ks[3], (DEPTH, FFN_DIM, D_MODEL), FFN_DIM ** -0.5),
        'mix_norm': gain(ks[4], (DEPTH, D_MODEL)),
        'w_in': nrm(ks[5], (DEPTH, D_MODEL, IN_DIM), D_MODEL ** -0.5),
        'b_gate': nrm(ks[6], (DEPTH, N_BRANCHES * D_MODEL), 0.1),
        'b_forget': 3.0 + nrm(ks[7], (DEPTH, B_HEADS), 0.5),
        'conv_w': nrm(ks[8], (DEPTH, CONV_WIDTH, 3 * BRANCH_WIDTH), CONV_WIDTH ** -0.5),
        'a_log': jnp.log(jax.random.uniform(ks[9], (DEPTH, C_HEADS), f32, 1.0, 16.0)),
        'dt_bias': dt + jnp.log(-jnp.expm1(-dt)),
        'delta_norm': gain(ks[11], (DEPTH, C_HEAD_DIM)),
        'w_branch_a': nrm(ks[12], (DEPTH, BRANCH_WIDTH, D_MODEL), BRANCH_WIDTH ** -0.5),
        'w_branch_b': nrm(ks[13], (DEPTH, BRANCH_WIDTH, D_MODEL), BRANCH_WIDTH ** -0.5),
        'w_branch_c': nrm(ks[14], (DEPTH, BRANCH_WIDTH, D_MODEL), BRANCH_WIDTH ** -0.5),
        'w_out': nrm(ks[15], (DEPTH, D_MODEL, D_MODEL), D_MODEL ** -0.5),
        'ffn2_norm': gain(ks[16], (DEPTH, D_MODEL)),
        'ffn2_w_in': nrm(ks[17], (DEPTH, D_MODEL, 2 * FFN_DIM), D_MODEL ** -0.5),
        'ffn2_w_out': nrm(ks[18], (DEPTH, FFN_DIM, D_MODEL), FFN_DIM ** -0.5),
        'final_norm': gain(ks[19], (D_MODEL,)),
    }


def reference(x, ffn1_norm, ffn1_w_in, ffn1_w_out, mix_norm, w_in, b_gate, b_forget, conv_w,
              a_log, dt_bias, delta_norm, w_branch_a, w_branch_b, w_branch_c, w_out,
              ffn2_norm, ffn2_w_in, ffn2_w_out, final_norm):
    seq_len = x.shape[1]
    top_k = min(INDEX_TOPK, seq_len // 4)
    cos, sin = rotary_tables(seq_len)
    for l in range(DEPTH):
        x = x + 0.5 * swiglu_ffn(rms_norm(x, ffn1_norm[l]), ffn1_w_in[l], ffn1_w_out[l])
        x = x + hybrid_mixer(rms_norm(x, mix_norm[l]), cos, sin, top_k, w_in[l], b_gate[l],
                             b_forget[l], conv_w[l], a_log[l], dt_bias[l], delta_norm[l],
                             w_branch_a[l], w_branch_b[l], w_branch_c[l], w_out[l])
        x = x + 0.5 * swiglu_ffn(rms_norm(x, ffn2_norm[l]), ffn2_w_in[l], ffn2_w_out[l])
    return rms_norm(x, final_norm)
```

```python
from contextlib import ExitStack
import numpy as np
import concourse.bass as bass
import concourse.mybir as mybir
from concourse.bass_utils import run_bass_kernel_spmd

F32 = mybir.dt.float32
BF16 = mybir.dt.bfloat16
ALU = mybir.AluOpType
AF = mybir.ActivationFunctionType
AX = mybir.AxisListType

D = 1024
DEPTH = 2
NB = 8
IN_SIZES = (512, 64, 64, 256, 64, 4, 1536, 8, 1536, 512, 4, 4, 3072)
OFF = np.concatenate([[0], np.cumsum(IN_SIZES)]).tolist()
(O_AQ, O_AK, O_AV, O_IQ, O_IK, O_IW, O_BQKV, O_BF, O_CQKV, O_CZ, O_CB, O_CA, O_G) = OFF[:13]
NEG = -32768.0


class Buf:
    __slots__ = ("t", "last_w", "readers", "name")

    def __init__(self, t=None, name=""):
        self.t = t
        self.last_w = None
        self.readers = {}
        self.name = name

    def __getitem__(self, k):
        return self.t[k]


class Eng:
    def __init__(self, name, handle, sem):
        self.name = name
        self.h = handle
        self.sem = sem
        self.count = 0
        self.seen = {}


class Ctx:
    SAME_ENGINE_SYNC = True
    RAW_ONLY_SAME_ENGINE = False

    def __init__(self, nc, n_dma_sems=10):
        self.nc = nc
        self.sems = {}
        self.eng = {}
        for nm, h in (("pe", nc.tensor), ("act", nc.scalar), ("dve", nc.vector),
                      ("pool", nc.gpsimd), ("sp", nc.sync)):
            self.sems["s_" + nm] = nc.alloc_semaphore("s_" + nm)
            self.eng[nm] = Eng(nm, h, "s_" + nm)
        self.dma_pool = {}
        for q in ("sp", "pool", "act"):
            lst = []
            for i in range(n_dma_sems):
                k = f"d_{q}{i}"
                self.sems[k] = nc.alloc_semaphore(k)
                lst.append([k, 0])
            self.dma_pool[q] = [lst, 0]
        self.n_instr = 0
        self.n_wait = 0
        self.uid = 0

    def sb(self, es, name, shape, dt):
        self.uid += 1
        nm = f"{name}_{self.uid}"
        return Buf(es.enter_context(self.nc.sbuf_tensor(nm, list(shape), dt)), nm)

    def _need(self, reads, writes, own=None):
        need = {}

        def add(ev, raw):
            if ev is None:
                return
            k, v = ev
            if k == own and not raw and self.RAW_ONLY_SAME_ENGINE:
                return
            if need.get(k, 0) < v:
                need[k] = v
        for b in reads:
            add(b.last_w, True)
        for b in writes:
            add(b.last_w, False)
            for k, v in b.readers.items():
                add((k, v), False)
        return need

    def _emit_waits(self, e, need):
        for k, v in need.items():
            if k == e.sem and (e.name == "pe" or not self.SAME_ENGINE_SYNC):
                continue
            if e.seen.get(k, 0) >= v:
                continue
            e.h.wait_ge(self.sems[k], v)
            e.seen[k] = v
            self.n_wait += 1

    def _record(self, ev, reads, writes):
        k, v = ev
        for b in writes:
            b.last_w = ev
            b.readers = {}
        for b in reads:
            if b.readers.get(k, 0) < v:
                b.readers[k] = v

    def op(self, en, fn, reads=(), writes=(), sig=True):
        e = self.eng[en]
        self._emit_waits(e, self._need(reads, writes, e.sem))
        ins = fn(e.h)
        self.n_instr += 1
        if sig:
            ins.then_inc(self.sems[e.sem], 1)
            e.count += 1
            ev = (e.sem, e.count)
        else:
            ev = (e.sem, e.count + 1)
        self._record(ev, reads, writes)
        return ins

    def dma(self, q, out, in_, reads=(), writes=(), **kw):
        e = self.eng[q]
        lst, idx = self.dma_pool[q]
        ent = lst[idx % len(lst)]
        self.dma_pool[q][1] = idx + 1
        need = self._need(reads, writes, None)
        if ent[1] > 0 and need.get(ent[0], 0) < ent[1]:
            need[ent[0]] = ent[1]
        self._emit_waits(e, need)
        ins = e.h.dma_start(out=out, in_=in_, **kw)
        ent[1] += 16
        ins.then_inc(self.sems[ent[0]], 16)
        self.n_instr += 1
        self._record((ent[0], ent[1]), reads, writes)
        return ins

    def barrier(self):
        for e in self.eng.values():
            need = {}
            for f in self.eng.values():
                if f is not e and f.count > 0:
                    need[f.sem] = f.count
            for q in self.dma_pool:
                for k, v in self.dma_pool[q][0]:
                    if v > 0:
                        need[k] = v
            self._emit_waits(e, need)


class Prog:
    def __init__(self, S, ext=None):
        self.S = S
        self.NT = S // 128
        self.NG = S // 512
        self.nc = bass.Bass("TRN2", target_bir_lowering=False)
        self.c = Ctx(self.nc)
        self.ext = ext or {}
        self.dram = {}
        self.dbuf = {}
        nc = self.nc
        self.psall = nc.alloc_psum_tensor("psall", [128, 8 * 512], F32)
        self.ps = [Buf(self.psall[:, i * 512:(i + 1) * 512], f"ps{i}") for i in range(8)]

    def dr(self, name, shape, dt, kind=None):
        if kind is None:
            kind = {"in": "ExternalInput", "out": "ExternalOutput"}.get(self.ext.get(name), "Internal")
        t = self.nc.dram_tensor(name, list(shape), dt, kind=kind)
        self.dram[name] = t.ap()
        return self.dram[name]

    def db(self, name, idx=0):
        k = (name, idx)
        if k not in self.dbuf:
            self.dbuf[k] = Buf(name=f"{name}{idx}")
        return self.dbuf[k]

    def setup_consts(self, es):
        c, nc = self.c, self.nc
        cm = self.dr("cmat", [7, 128, 128], F32, kind="ExternalInput")
        self.ident_b = c.sb(es, "identb", [128, 128], BF16)
        self.ones_b = c.sb(es, "onesb", [128, 128], BF16)
        self.ones_f = c.sb(es, "onesf", [128, 128], F32)
        self.triu_f = c.sb(es, "triuf", [128, 128], F32)
        self.ident_f = c.sb(es, "identf", [128, 128], F32)
        self.negm_f = c.sb(es, "negmf", [128, 128], F32)
        self.tri01_b = c.sb(es, "tri01b", [128, 128], BF16)
        cb = self.db("cmat")
        c.dma("pool", self.ident_b[:, :], cm[0], reads=[cb], writes=[self.ident_b])
        c.dma("pool", self.ones_b[:, :], cm[1], reads=[cb], writes=[self.ones_b])
        c.dma("sp", self.ones_f[:, :], cm[1], reads=[cb], writes=[self.ones_f])
        c.dma("sp", self.triu_f[:, :], cm[2], reads=[cb], writes=[self.triu_f])
        c.dma("sp", self.ident_f[:, :], cm[0], reads=[cb], writes=[self.ident_f])
        c.dma("sp", self.negm_f[:, :], cm[3], reads=[cb], writes=[self.negm_f])
        c.dma("pool", self.tri01_b[:, :], cm[2], reads=[cb], writes=[self.tri01_b])
        self.NCV = DEPTH * CV_PER_LAYER + 8
        cv = self.dr("cvec", [128, self.NCV], F32, kind="ExternalInput")
        self.cvec = c.sb(es, "cvec", [128, self.NCV], F32)
        c.dma("sp", self.cvec[:, :], cv[:, :], reads=[self.db("cvec")], writes=[self.cvec])

    def rmsnorm_group(self, xg, sq, hT_ap_fn, hT_buf, rstd, gcol, psb, nch=8, ncols=512):
        c = self.c
        c.op("act", lambda h: h.activation(out=sq[:, :, :], in_=xg[:, :, :], func=AF.Square), reads=[xg], writes=[sq])
        for k in range(nch):
            c.op("pe", lambda h, k=k: h.matmul(psb[:, :ncols], lhsT=self.ones_b[:, :], rhs=sq[:, k, :], start=(k == 0), stop=(k == nch - 1)),
                 reads=[self.ones_b, sq], writes=[psb], sig=(k == nch - 1))
        c.op("dve", lambda h: h.tensor_scalar(out=rstd[:, :], in0=psb[:, :ncols], scalar1=1.0 / (nch * 128), scalar2=1e-6, op0=ALU.mult, op1=ALU.add),
             reads=[psb], writes=[rstd])
        c.op("act", lambda h: h.activation(out=rstd[:, :], in_=rstd[:, :], func=AF.Ln), reads=[rstd], writes=[rstd])
        c.op("act", lambda h: h.activation(out=rstd[:, :], in_=rstd[:, :], func=AF.Exp, scale=-0.5), reads=[rstd], writes=[rstd])
        for k in range(nch):
            c.op("dve", lambda h, k=k: h.scalar_tensor_tensor(out=hT_ap_fn(k), in0=xg[:, k, :], scalar=self.cvec[:, gcol + k:gcol + k + 1],
                                                            in1=rstd[:, :], op0=ALU.mult, op1=ALU.mult),
                 reads=[xg, rstd, self.cvec], writes=[hT_buf])

    def ffn_phase(self, xsrc, xdst, w_in, w_out, gcol):
        c, S = self.c, self.S
        with ExitStack() as es:
            win = c.sb(es, "win", [128, 8, 4096], BF16)
            wout = c.sb(es, "wout", [128, 16, 1024], BF16)
            xgs = [c.sb(es, "xg", [128, 8, 512], F32) for _ in range(2)]
            sq = c.sb(es, "sq", [128, 8, 512], BF16)
            hT = c.sb(es, "hT", [128, 8, 512], BF16)
            act = c.sb(es, "actT", [128, 16, 512], BF16)
            rstd = c.sb(es, "rstd", [128, 512], F32)
            sgs = [c.sb(es, "sg", [128, 512], F32) for _ in range(2)]
            wb = self.db("w")
            for k in range(8):
                c.dma("pool", win[:, k, :], w_in[k * 128:(k + 1) * 128, :], reads=[wb], writes=[win])
            for k in range(16):
                c.dma("pool", wout[:, k, :], w_out[k * 128:(k + 1) * 128, :], reads=[wb], writes=[wout])
            xs3 = xsrc.rearrange("(c p) t -> p c t", p=128)
            xd3 = xdst.rearrange("(c p) t -> p c t", p=128)
            ps = self.ps
            for g in range(self.NG):
                xg = xgs[g % 2]
                tsl = slice(g * 512, (g + 1) * 512)
                c.dma("sp", xg[:, :, :], xs3[:, :, tsl], reads=[self.db(xsrc.name, g)], writes=[xg])
                self.rmsnorm_group(xg, sq, lambda k: hT[:, k, :], hT, rstd, gcol, ps[0])
                for j in range(16):
                    pg, pu = ps[1 + 2 * (j % 2)], ps[2 + 2 * (j % 2)]
                    for k in range(8):
                        c.op("pe", lambda h, k=k, j=j, pg=pg: h.matmul(pg[:, :], lhsT=win[:, k, j * 128:(j + 1) * 128], rhs=hT[:, k, :], start=(k == 0), stop=(k == 7)),
                             reads=[win, hT], writes=[pg], sig=(k == 7))
                    for k in range(8):
                        c.op("pe", lambda h, k=k, j=j, pu=pu: h.matmul(pu[:, :], lhsT=win[:, k, 2048 + j * 128:2048 + (j + 1) * 128], rhs=hT[:, k, :], start=(k == 0), stop=(k == 7)),
                             reads=[win, hT], writes=[pu], sig=(k == 7))
                    sg = sgs[j % 2]
                    c.op("act", lambda h, pg=pg, sg=sg: h.activation(out=sg[:, :], in_=pg[:, :], func=AF.Silu), reads=[pg], writes=[sg])
                    c.op("dve", lambda h, pu=pu, sg=sg, j=j: h.tensor_tensor(out=act[:, j, :], in0=sg[:, :], in1=pu[:, :], op=ALU.mult), reads=[sg, pu], writes=[act])
                for d in range(8):
                    po = ps[5 + d % 2]
                    for j in range(16):
                        c.op("pe", lambda h, d=d, j=j, po=po: h.matmul(po[:, :], lhsT=wout[:, j, d * 128:(d + 1) * 128], rhs=act[:, j, :], start=(j == 0), stop=(j == 15)),
                             reads=[wout, act], writes=[po], sig=(j == 15))
                    c.op("dve", lambda h, d=d, po=po, xg=xg: h.scalar_tensor_tensor(out=xg[:, d, :], in0=po[:, :], scalar=0.5, in1=xg[:, d, :], op0=ALU.mult, op1=ALU.add),
                         reads=[po, xg], writes=[xg])
                c.dma("sp", xd3[:, :, tsl], xg[:, :, :], reads=[xg], writes=[self.db(xdst.name, g)])
            c.barrier()


CV_FFN1, CV_MIX, CV_FFN2, CV_BG, CV_CONV, CV_DN, CV_BF, CV_ALOG, CV_DT = 0, 8, 16, 24, 48, 96, 97, 105, 109
CV_PER_LAYER = 113


def build_cvec(inp):
    cv = np.zeros((128, DEPTH * CV_PER_LAYER + 8), np.float32)
    for l in range(DEPTH):
        b = l * CV_PER_LAYER
        cv[:, b + CV_FFN1:b + CV_FFN1 + 8] = inp["ffn1_norm"][l].reshape(8, 128).T
        cv[:, b + CV_MIX:b + CV_MIX + 8] = inp["mix_norm"][l].reshape(8, 128).T
        cv[:, b + CV_FFN2:b + CV_FFN2 + 8] = inp["ffn2_norm"][l].reshape(8, 128).T
        cv[:, b + CV_BG:b + CV_BG + 24] = inp["b_gate"][l].reshape(24, 128).T
        cv[:, b + CV_CONV:b + CV_CONV + 48] = inp["conv_w"][l].reshape(4, 12, 128).transpose(2, 1, 0).reshape(128, 48)
        cv[:, b + CV_DN] = inp["delta_norm"][l]
        cv[:, b + CV_BF:b + CV_BF + 8] = inp["b_forget"][l][None, :]
        cv[:, b + CV_ALOG:b + CV_ALOG + 4] = inp["a_log"][l][None, :]
        cv[:, b + CV_DT:b + CV_DT + 4] = inp["dt_bias"][l][None, :]
    cv[:, DEPTH * CV_PER_LAYER:] = inp["final_norm"].reshape(8, 128).T
    return cv


def build_cmat():
    i = np.arange(128)
    ident = np.eye(128, dtype=np.float32)
    ones = np.ones((128, 128), np.float32)
    triu = (i[:, None] <= i[None, :]).astype(np.float32)
    negm = np.where(i[None, :] >= i[:, None], 0.0, -1e4).astype(np.float32)
    negc = np.where(i[None, :] <= i[:, None], 0.0, -1e30).astype(np.float32)
    z = np.zeros((128, 128), np.float32)
    strictu = (i[:, None] < i[None, :]).astype(np.float32)
    return np.stack([ident, ones, triu, negm, negc, strictu, z])


def build_pow2(nit=26):
    return np.tile((0.5 ** np.arange(1, nit + 1)).astype(np.float32)[None, :], (128, 1))


def swap_cols():
    idx = []
    for base, n in ((O_AQ, 512), (O_AK, 64), (O_IQ, 256), (O_IK, 64)):
        for j in range(n):
            d = j % 64
            hb = base + (j // 64) * 64
            if d < 8:
                idx.append(hb + d + 8)
            elif d < 16:
                idx.append(hb + d - 8)
            else:
                idx.append(hb + d)
    return np.array(idx)


def small_cols():
    return np.concatenate([np.arange(O_AV, O_AV + 64), np.arange(O_IW, O_IW + 4), np.arange(O_BF, O_BF + 8),
                           np.arange(O_CB, O_CB + 4), np.arange(O_CA, O_CA + 4)])


def build_rot(S):
    pos = np.arange(S, dtype=np.float32)
    inv = np.power(np.float32(500000.0), -np.arange(0, 16, 2, dtype=np.float32) / np.float32(16)).astype(np.float32)
    ang = (pos[:, None] * inv[None, :]).astype(np.float32)
    cos, sin = np.cos(ang).astype(np.float32), np.sin(ang).astype(np.float32)
    C = np.ones((128, S), np.float32)
    Sg = np.zeros((128, S), np.float32)
    for p in range(128):
        d = p % 64
        if d < 8:
            C[p] = cos[:, d]
            Sg[p] = -sin[:, d]
        elif d < 16:
            C[p] = cos[:, d - 8]
            Sg[p] = sin[:, d - 8]
    return C, Sg


class ProgM1(Prog):
    def declare_scratch(self):
        S = self.S
        d = self.dr
        self.qaT = d("qaT", [512, S], BF16); self.kaT = d("kaT", [64, S], BF16)
        self.qiT = d("qiT", [256, S], BF16); self.kiT = d("kiT", [64, S], BF16)
        self.vA = d("vA", [S, 64], BF16)
        self.qbT = d("qbT", [512, S], BF16); self.kbT = d("kbT", [512, S], BF16); self.vB = d("vB", [S, 512], BF16)
        self.qcT = d("qcT", [512, S], BF16); self.kcT = d("kcT", [512, S], BF16)
        self.kC = d("kC", [S, 512], BF16); self.vC = d("vC", [S, 512], BF16)
        self.szT = d("szT", [512, S], BF16)
        self.smallD = d("smallD", [S, 24], F32)
        self.gT = d("gT", [3072, S], BF16)
        self.yT = d("yT", [3, 512, S], BF16)

    def rot(self):
        self._rot = (getattr(self, "_rot", -1) + 1) % 8
        return self.ps[self._rot]

    def m1_phase(self, xT, w_in, w_sw, w_small, cvb):
        c, S, NG = self.c, self.S, self.NG
        rotC = self.dram["rotC"]; rotS = self.dram["rotS"]
        with ExitStack() as es:
            hT = c.sb(es, "hTall", [128, 8, S], BF16)
            wts = [c.sb(es, "wt", [128, 8, 512], BF16) for _ in range(3)]
            wti = [0]
            wb = self.db("w")

            def load_w(src_list):
                wt = wts[wti[0] % 3]
                wti[0] += 1
                for (ap, c0, n) in src_list:
                    c.dma("pool", wt[:, :, c0:c0 + n], ap.rearrange("(k p) n -> p k n", p=128), reads=[wb], writes=[wt])
                return wt

            def fm(wt, c0, M, g, psb, rows0=0):
                tsl = slice(g * 512, (g + 1) * 512)
                for k in range(8):
                    c.op("pe", lambda h, k=k: h.matmul(psb[rows0:rows0 + M, :], lhsT=wt[:, k, c0:c0 + M], rhs=hT[:, k, tsl], start=(k == 0), stop=(k == 7)),
                         reads=[wt, hT], writes=[psb], sig=(k == 7))

            with ExitStack() as es2:
                xgs = [c.sb(es2, "xg", [128, 8, 512], F32) for _ in range(2)]
                sq = c.sb(es2, "sq", [128, 8, 512], BF16)
                rstd = c.sb(es2, "rstd", [128, 512], F32)
                x3 = xT.rearrange("(c p) t -> p c t", p=128)
                for g in range(NG):
                    xg = xgs[g % 2]
                    tsl = slice(g * 512, (g + 1) * 512)
                    c.dma("sp", xg[:, :, :], x3[:, :, tsl], reads=[self.db(xT.name, g)], writes=[xg])
                    self.rmsnorm_group(xg, sq, lambda k, tsl=tsl: hT[:, k, tsl], hT, rstd, cvb + CV_MIX, self.rot())
                c.barrier()

            with ExitStack() as es2:
                stg = [c.sb(es2, "stg", [128, 4, 512], BF16) for _ in range(2)]
                stgi = [0]
                t1s = [c.sb(es2, "t1", [128, 512], F32) for _ in range(2)]
                t2s = [c.sb(es2, "t2", [128, 512], F32) for _ in range(2)]
                rc = [c.sb(es2, "rc", [128, 512], F32) for _ in range(2)]
                rs = [c.sb(es2, "rs", [128, 512], F32) for _ in range(2)]

                def nstg():
                    stgi[0] += 1
                    return stg[stgi[0] % 2]

                def load_rot(g):
                    tsl = slice(g * 512, (g + 1) * 512)
                    c.dma("sp", rc[g % 2][:, :], rotC[:, tsl], reads=[self.db("rot")], writes=[rc[g % 2]])
                    c.dma("sp", rs[g % 2][:, :], rotS[:, tsl], reads=[self.db("rot")], writes=[rs[g % 2]])

                def rotary(pn, psw, g, out_ap, out_buf, i):
                    t1, t2 = t1s[i % 2], t2s[i % 2]
                    c.op("dve", lambda h: h.tensor_tensor(out=t1[:, :], in0=pn[:, :], in1=rc[g % 2][:, :], op=ALU.mult), reads=[pn, rc[g % 2]], writes=[t1])
                    c.op("dve", lambda h: h.tensor_tensor(out=t2[:, :], in0=psw[:, :], in1=rs[g % 2][:, :], op=ALU.mult), reads=[psw, rs[g % 2]], writes=[t2])
                    c.op("pool", lambda h: h.tensor_tensor(out=out_ap, in0=t1[:, :], in1=t2[:, :], op=ALU.add), reads=[t1, t2], writes=[out_buf])

                wn = load_w([(w_in[:, O_AQ:O_AQ + 512], 0, 512)])
                ws = load_w([(w_sw[:, 0:512], 0, 512)])
                for g in range(NG):
                    tsl = slice(g * 512, (g + 1) * 512)
                    load_rot(g)
                    so = nstg()
                    for ch in range(4):
                        pn, psw = self.rot(), self.rot()
                        fm(wn, ch * 128, 128, g, pn)
                        fm(ws, ch * 128, 128, g, psw)
                        rotary(pn, psw, g, so[:, ch, :], so, ch)
                    c.dma("sp", self.qaT.rearrange("(c p) t -> p c t", p=128)[:, :, tsl], so[:, :, :], reads=[so], writes=[self.db("qaT", g)])
                wn = load_w([(w_in[:, O_IQ:O_IQ + 256], 0, 256), (w_in[:, O_AK:O_AK + 64], 256, 64), (w_in[:, O_IK:O_IK + 64], 320, 64)])
                ws = load_w([(w_sw[:, 576:832], 0, 256), (w_sw[:, 512:576], 256, 64), (w_sw[:, 832:896], 320, 64)])
                for g in range(NG):
                    tsl = slice(g * 512, (g + 1) * 512)
                    load_rot(g)
                    so = nstg()
                    for ch in range(3):
                        pn, psw = self.rot(), self.rot()
                        fm(wn, ch * 128, 128, g, pn)
                        fm(ws, ch * 128, 128, g, psw)
                        rotary(pn, psw, g, so[:, ch, :], so, ch)
                    c.dma("sp", self.qiT.rearrange("(c p) t -> p c t", p=128)[:, :, tsl], so[:, 0:2, :], reads=[so], writes=[self.db("qiT", g)])
                    c.dma("sp", self.kaT[:, tsl], so[0:64, 2, :], reads=[so], writes=[self.db("kaT", g)])
                    c.dma("sp", self.kiT[:, tsl], so[64:128, 2, :], reads=[so], writes=[self.db("kiT", g)])
                for (c0, dst) in ((O_BQKV, self.qbT), (O_BQKV + 512, self.kbT)):
                    wn = load_w([(w_in[:, c0:c0 + 512], 0, 512)])
                    for g in range(NG):
                        tsl = slice(g * 512, (g + 1) * 512)
                        so = nstg()
                        for ch in range(4):
                            pn = self.rot()
                            fm(wn, ch * 128, 128, g, pn)
                            c.op("act", lambda h, pn=pn, ch=ch, so=so: h.activation(out=so[:, ch, :], in_=pn[:, :], func=AF.Copy), reads=[pn], writes=[so])
                        c.dma("sp", dst.rearrange("(c p) t -> p c t", p=128)[:, :, tsl], so[:, :, :], reads=[so], writes=[self.db(dst.name, g)])
                wn = load_w([(w_in[:, O_BQKV + 1024:O_BQKV + 1536], 0, 512)])
                for g in range(NG):
                    so = nstg()
                    for tt in range(4):
                        pn = self.rot()
                        t0 = g * 512 + tt * 128
                        for k in range(8):
                            c.op("pe", lambda h, k=k, pn=pn, t0=t0: h.matmul(pn[:, :], lhsT=hT[:, k, t0:t0 + 128], rhs=wn[:, k, :], start=(k == 0), stop=(k == 7)),
                                 reads=[wn, hT], writes=[pn], sig=(k == 7))
                        c.op("act", lambda h, pn=pn, tt=tt, so=so: h.activation(out=so[:, tt, :], in_=pn[:, :], func=AF.Copy), reads=[pn], writes=[so])
                    c.dma("sp", self.vB.rearrange("(n p) d -> p n d", p=128)[:, g * 4:(g + 1) * 4, :], so[:, :, :], reads=[so], writes=[self.db("vB", g)])
                with ExitStack() as es3:
                    xcs = [c.sb(es3, "xc", [128, 515], F32) for _ in range(4)]
                    accs = [c.sb(es3, "acc", [128, 512], F32) for _ in range(2)]
                    sls = [c.sb(es3, "sl", [128, 512], F32) for _ in range(2)]
                    sqb = [c.sb(es3, "sqb", [128, 512], BF16) for _ in range(2)]
                    rr = [c.sb(es3, "rr", [128, 512], F32) for _ in range(2)]
                    tok = [c.sb(es3, "tok", [128, 4, 512], BF16) for _ in range(2)]
                    for sec, (dstT, dstTok) in enumerate(((self.qcT, None), (self.kcT, self.kC), (None, self.vC))):
                        c0 = O_CQKV + sec * 512
                        wn = load_w([(w_in[:, c0:c0 + 512], 0, 512)])
                        for j in range(4):
                            c.op("pool", lambda h, j=j: h.memset(xcs[j][:, 0:3], 0.0), writes=[xcs[j]])
                        for g in range(NG):
                            tsl = slice(g * 512, (g + 1) * 512)
                            so = nstg()
                            for j in range(4):
                                ch = sec * 4 + j
                                pn = self.rot()
                                fm(wn, j * 128, 128, g, pn)
                                xc = xcs[j]
                                acc, sl = accs[j % 2], sls[j % 2]
                                wc = cvb + CV_CONV + ch * 4
                                c.op("act", lambda h, pn=pn, xc=xc: h.activation(out=xc[:, 3:515], in_=pn[:, :], func=AF.Copy), reads=[pn], writes=[xc])
                                c.op("dve", lambda h, xc=xc, acc=acc, wc=wc: h.tensor_scalar(out=acc[:, :], in0=xc[:, 3:515], scalar1=self.cvec[:, wc + 3:wc + 4], scalar2=None, op0=ALU.mult),
                                     reads=[xc, self.cvec], writes=[acc])
                                for tap in (2, 1, 0):
                                    c.op("dve", lambda h, xc=xc, acc=acc, wc=wc, tap=tap: h.scalar_tensor_tensor(out=acc[:, :], in0=xc[:, tap:tap + 512], scalar=self.cvec[:, wc + tap:wc + tap + 1],
                                                                                                         in1=acc[:, :], op0=ALU.mult, op1=ALU.add),
                                         reads=[xc, acc, self.cvec], writes=[acc])
                                c.op("pool", lambda h, xc=xc: h.tensor_copy(out=xc[:, 0:3], in_=xc[:, 512:515]), reads=[xc], writes=[xc])
                                if sec == 2:
                                    c.op("act", lambda h, acc=acc, so=so, j=j: h.activation(out=so[:, j, :], in_=acc[:, :], func=AF.Silu), reads=[acc], writes=[so])
                                else:
                                    c.op("act", lambda h, acc=acc, sl=sl: h.activation(out=sl[:, :], in_=acc[:, :], func=AF.Silu), reads=[acc], writes=[sl])
                                    sb_, r_ = sqb[j % 2], rr[j % 2]
                                    c.op("act", lambda h, sl=sl, sb_=sb_: h.activation(out=sb_[:, :], in_=sl[:, :], func=AF.Square), reads=[sl], writes=[sb_])
                                    p2 = self.rot()
                                    c.op("pe", lambda h, p2=p2, sb_=sb_: h.matmul(p2[:, :], lhsT=self.ones_b[:, :], rhs=sb_[:, :], start=True, stop=True), reads=[self.ones_b, sb_], writes=[p2])
                                    c.op("dve", lambda h, p2=p2, r_=r_: h.tensor_scalar(out=r_[:, :], in0=p2[:, :], scalar1=1e-6, scalar2=None, op0=ALU.add), reads=[p2], writes=[r_])
                                    c.op("act", lambda h, r_=r_: h.activation(out=r_[:, :], in_=r_[:, :], func=AF.Ln), reads=[r_], writes=[r_])
                                    c.op("act", lambda h, r_=r_: h.activation(out=r_[:, :], in_=r_[:, :], func=AF.Exp, scale=-0.5), reads=[r_], writes=[r_])
                                    qs = float(128 ** -0.5) if sec == 0 else 1.0
                                    c.op("dve", lambda h, sl=sl, r_=r_, so=so, j=j, qs=qs: h.scalar_tensor_tensor(out=so[:, j, :], in0=sl[:, :], scalar=qs, in1=r_[:, :], op0=ALU.mult, op1=ALU.mult),
                                         reads=[sl, r_], writes=[so])
                            if dstT is not None:
                                c.dma("sp", dstT.rearrange("(c p) t -> p c t", p=128)[:, :, tsl], so[:, :, :], reads=[so], writes=[self.db(dstT.name, g)])
                            if dstTok is not None:
                                tk = tok[g % 2]
                                for tt in range(4):
                                    pt = self.rot()
                                    for j in range(4):
                                        c.op("pe", lambda h, pt=pt, j=j, tt=tt, so=so: h.matmul(pt[:, j * 128:(j + 1) * 128], lhsT=so[:, j, tt * 128:(tt + 1) * 128], rhs=self.ident_b[:, :], start=True, stop=True),
                                             reads=[so, self.ident_b], writes=[pt], sig=(j == 3))
                                    c.op("act", lambda h, pt=pt, tk=tk, tt=tt: h.activation(out=tk[:, tt, :], in_=pt[:, :], func=AF.Copy), reads=[pt], writes=[tk])
                                c.dma("sp", dstTok.rearrange("(n p) d -> p n d", p=128)[:, g * 4:(g + 1) * 4, :], tk[:, :, :], reads=[tk], writes=[self.db(dstTok.name, g)])
                wn = load_w([(w_in[:, O_CZ:O_CZ + 512], 0, 512)])
                for g in range(NG):
                    tsl = slice(g * 512, (g + 1) * 512)
                    so = nstg()
                    for ch in range(4):
                        pn = self.rot()
                        fm(wn, ch * 128, 128, g, pn)
                        c.op("act", lambda h, pn=pn, ch=ch, so=so: h.activation(out=so[:, ch, :], in_=pn[:, :], func=AF.Silu), reads=[pn], writes=[so])
                    c.dma("sp", self.szT.rearrange("(c p) t -> p c t", p=128)[:, :, tsl], so[:, :, :], reads=[so], writes=[self.db("szT", g)])
                for sec in range(6):
                    wn = load_w([(w_in[:, O_G + sec * 512:O_G + (sec + 1) * 512], 0, 512)])
                    for g in range(NG):
                        tsl = slice(g * 512, (g + 1) * 512)
                        so = nstg()
                        for ch in range(4):
                            pn = self.rot()
                            fm(wn, ch * 128, 128, g, pn)
                            bc = cvb + CV_BG + sec * 4 + ch
                            c.op("act", lambda h, pn=pn, ch=ch, so=so, bc=bc: h.activation(out=so[:, ch, :], in_=pn[:, :], func=AF.Sigmoid, bias=self.cvec[:, bc:bc + 1]),
                                 reads=[pn, self.cvec], writes=[so])
                        c.dma("sp", self.gT.rearrange("(c p) t -> p c t", p=128)[:, sec * 4:(sec + 1) * 4, tsl], so[:, :, :], reads=[so], writes=[self.db("gT", sec * 100 + g)])
                with ExitStack() as es3:
                    wsm = c.sb(es3, "wsm", [128, 8, 84], BF16)
                    c.dma("pool", wsm[:, :, :], w_small.rearrange("(k p) n -> p k n", p=128), reads=[wb], writes=[wsm])
                    negA = c.sb(es3, "negA", [128, 4], F32)
                    c.op("act", lambda h: h.activation(out=negA[:, :], in_=self.cvec[:, cvb + CV_ALOG:cvb + CV_ALOG + 4], func=AF.Exp), reads=[self.cvec], writes=[negA])
                    c.op("dve", lambda h: h.tensor_scalar(out=negA[:, :], in0=negA[:, :], scalar1=-1.0, scalar2=None, op0=ALU.mult), reads=[negA], writes=[negA])
                    sms = [c.sb(es3, "sm", [128, 4, 24], F32) for _ in range(2)]
                    vas = [c.sb(es3, "vas", [128, 4, 64], BF16) for _ in range(2)]
                    tmp = [c.sb(es3, "tmps", [128, 16], F32) for _ in range(2)]
                    for g in range(NG):
                        sm, va = sms[g % 2], vas[g % 2]
                        for tt in range(4):
                            pn = self.rot()
                            t0 = g * 512 + tt * 128
                            tp = tmp[tt % 2]
                            for k in range(8):
                                c.op("pe", lambda h, k=k, pn=pn, t0=t0: h.matmul(pn[:, 0:84], lhsT=hT[:, k, t0:t0 + 128], rhs=wsm[:, k, :], start=(k == 0), stop=(k == 7)),
                                     reads=[wsm, hT], writes=[pn], sig=(k == 7))
                            c.op("act", lambda h, pn=pn, va=va, tt=tt: h.activation(out=va[:, tt, :], in_=pn[:, 0:64], func=AF.Copy), reads=[pn], writes=[va])
                            c.op("dve", lambda h, pn=pn, sm=sm, tt=tt: h.tensor_scalar(out=sm[:, tt, 0:4], in0=pn[:, 64:68], scalar1=1.0 / 16.0, scalar2=None, op0=ALU.mult), reads=[pn], writes=[sm])
                            c.op("dve", lambda h, pn=pn, tp=tp: h.tensor_tensor(out=tp[:, 0:8], in0=pn[:, 68:76], in1=self.cvec[:, cvb + CV_BF:cvb + CV_BF + 8], op=ALU.add), reads=[pn, self.cvec], writes=[tp])
                            c.op("dve", lambda h, pn=pn, tp=tp: h.tensor_tensor(out=tp[:, 8:12], in0=pn[:, 80:84], in1=self.cvec[:, cvb + CV_DT:cvb + CV_DT + 4], op=ALU.add), reads=[pn, self.cvec, tp], writes=[tp])
                            c.op("act", lambda h, tp=tp: h.activation(out=tp[:, 0:8], in_=tp[:, 0:8], func=AF.Exp, scale=-1.0), reads=[tp], writes=[tp])
                            c.op("act", lambda h, tp=tp: h.activation(out=tp[:, 8:12], in_=tp[:, 8:12], func=AF.Exp), reads=[tp], writes=[tp])
                            c.op("act", lambda h, tp=tp: h.activation(out=tp[:, 0:12], in_=tp[:, 0:12], func=AF.Ln, bias=1.0), reads=[tp], writes=[tp])
                            c.op("dve", lambda h, tp=tp, sm=sm, tt=tt: h.tensor_scalar(out=sm[:, tt, 4:12], in0=tp[:, 0:8], scalar1=-1.0, scalar2=None, op0=ALU.mult), reads=[tp], writes=[sm])
                            c.op("dve", lambda h, tp=tp, sm=sm, tt=tt: h.tensor_tensor(out=sm[:, tt, 16:20], in0=tp[:, 8:12], in1=negA[:, :], op=ALU.mult), reads=[tp, negA, sm], writes=[sm])
                            c.op("act", lambda h, pn=pn, sm=sm, tt=tt: h.activation(out=sm[:, tt, 12:16], in_=pn[:, 76:80], func=AF.Sigmoid), reads=[pn, sm], writes=[sm])
                        c.dma("sp", self.smallD.rearrange("(n p) c -> p n c", p=128)[:, g * 4:(g + 1) * 4, 0:20], sm[:, :, 0:20], reads=[sm], writes=[self.db("smallD", g)])
                        c.dma("sp", self.vA.rearrange("(n p) d -> p n d", p=128)[:, g * 4:(g + 1) * 4, :], va[:, :, :], reads=[va], writes=[self.db("vA", g)])
                c.barrier()


class ProgAB(ProgM1):
    def rotset(self, key, banks):
        d = self.__dict__.setdefault("_rs", {})
        d[key] = (d.get(key, -1) + 1) % len(banks)
        return self.ps[banks[d[key]]]

    def _b_setup(self, es, lbanks, pbanks):
        c, S, NT, NG = self.c, self.S, self.NT, self.NG
        qbh = [c.sb(es, "qbh", [128, S], BF16) for _ in range(2)]
        kbh = [c.sb(es, "kbh", [128, S], BF16) for _ in range(2)]
        vext = [c.sb(es, "vext", [128, NT, 128], BF16) for _ in range(2)]
        lf = c.sb(es, "lf", [128, NT, 8], F32)
        lfacc = c.sb(es, "lfacc", [128, NT + 1, 8], F32)
        csb = c.sb(es, "csb", [128, NT, 8], F32)
        carry = c.sb(es, "carry", [128, NT, 8], F32)
        npair = NT * (NT + 1) // 2
        bias = c.sb(es, "biasall", [128, npair, 8], F32)
        pts = [c.sb(es, "pt", [128, 512], BF16) for _ in range(4)]
        ptq = [[Buf(name=f"ptq{a}_{b}") for b in range(4)] for a in range(4)]
        rsum = [c.sb(es, "rsum", [64, 512], F32) for _ in range(2)]
        nums = [c.sb(es, "bnum", [64, 512], F32) for _ in range(2)]
        yst = [c.sb(es, "yst", [64, 512], BF16) for _ in range(2)]
        allg = lambda nm: [self.db(nm, g) for g in range(NG)]
        c.dma("sp", lf[:, :, :], self.smallD.rearrange("(n p) c -> p n c", p=128)[:, :, 4:12], reads=allg("smallD"), writes=[lf])
        for v in vext:
            c.op("pool", lambda h, v=v: h.memset(v[:, :, 64:128], 1.0), writes=[v])
        c.op("pool", lambda h: h.memset(lfacc[:, 0, :], 0.0), writes=[lfacc])
        for n in range(NT):
            c.op("dve", lambda h, n=n: h.tensor_tensor(out=lfacc[:, n + 1, :], in0=lfacc[:, n, :], in1=lf[:, n, :], op=ALU.add), reads=[lfacc, lf], writes=[lfacc])
        for n in range(NT):
            pb = self.rotset("bpl", lbanks)
            c.op("pe", lambda h, n=n, pb=pb: h.matmul(pb[:, 0:8], lhsT=self.triu_f[:, :], rhs=lf[:, n, :], start=True, stop=False), reads=[self.triu_f, lf], writes=[pb], sig=False)
            c.op("pe", lambda h, n=n, pb=pb: h.matmul(pb[:, 0:8], lhsT=self.ones_f[:, :], rhs=lfacc[:, n, :], start=False, stop=True), reads=[self.ones_f, lfacc], writes=[pb])
            c.op("act", lambda h, n=n, pb=pb: h.activation(out=csb[:, n, :], in_=pb[:, 0:8], func=AF.Copy), reads=[pb], writes=[csb])
            pb = self.rotset("bpl", lbanks)
            c.op("pe", lambda h, n=n, pb=pb: h.matmul(pb[:, 0:8], lhsT=self.ones_f[:, :], rhs=lfacc[:, n, :], start=True, stop=True), reads=[self.ones_f, lfacc], writes=[pb])
            c.op("act", lambda h, n=n, pb=pb: h.activation(out=carry[:, n, :], in_=pb[:, 0:8], func=AF.Copy), reads=[pb], writes=[carry])
        pidx = {}
        pi = 0
        for i in range(NT):
            for j in range(i + 1):
                pidx[(i, j)] = pi
                c.op("pool", lambda h, i=i, j=j, pi=pi: h.tensor_tensor(out=bias[:, pi, :], in0=carry[:, i, :], in1=csb[:, j, :], op=ALU.subtract), reads=[carry, csb], writes=[bias])
                pi += 1
        vB3 = self.vB.rearrange("(n p) d -> p n d", p=128)
        yb = self.yT[1]
        steps = [(hh, g, j) for hh in range(8) for g in range(NG) for j in range(4 * g + 4)]
        pls = {}
        loaded = set()

        def load_pair(hp):
            if hp in loaded or hp >= 4:
                return
            loaded.add(hp)
            c.dma("sp", qbh[hp % 2][:, :], self.qbT[hp * 128:(hp + 1) * 128, :], reads=allg("qbT"), writes=[qbh[hp % 2]])
            c.dma("sp", kbh[hp % 2][:, :], self.kbT[hp * 128:(hp + 1) * 128, :], reads=allg("kbT"), writes=[kbh[hp % 2]])

        def logits(k):
            hh, g, j = steps[k]
            hb, hp = (hh % 2) * 64, hh // 2
            load_pair(hp)
            qb, kb = qbh[hp % 2], kbh[hp % 2]
            col0 = max(j - 4 * g, 0) * 128
            pl = self.rotset("bpl", lbanks)
            pls[k] = pl
            c.op("pe", lambda h: h.matmul(pl[:, col0:512], lhsT=kb[hb:hb + 64, j * 128:(j + 1) * 128],
                                          rhs=qb[hb:hb + 64, g * 512 + col0:(g + 1) * 512], start=True, stop=True),
                 reads=[kb, qb], writes=[pl])

        def gen():
            po = None
            logits(0)
            for k, (hh, g, j) in enumerate(steps):
                ve = vext[hh % 2]
                if g == 0 and j == 0:
                    c.dma("sp", ve[:, :, 0:64], vB3[:, :, hh * 64:(hh + 1) * 64], reads=allg("vB"), writes=[ve])
                if j == 0:
                    po = self.rotset("bpo", pbanks)
                if k + 1 < len(steps):
                    logits(k + 1)
                nj = 4 * g + 4
                r = j - 4 * g
                col0 = max(r, 0) * 128
                pl = pls.pop(k)
                pt = pts[j % 4]
                for qq in range(max(r, 0), 4):
                    pi = pidx[(4 * g + qq, j)]
                    qs_ = slice(qq * 128, (qq + 1) * 128)
                    c.op("act", lambda h, pl=pl, pt=pt, qs_=qs_, pi=pi, hh=hh: h.activation(out=pt[:, qs_], in_=pl[:, qs_], func=AF.Exp, scale=0.125, bias=bias[:, pi, hh:hh + 1]),
                         reads=[pl, bias], writes=[ptq[j % 4][qq]])
                if r >= 0:
                    c.op("pool", lambda h, pt=pt, col0=col0: h.tensor_tensor(out=pt[:, col0:col0 + 128], in0=pt[:, col0:col0 + 128], in1=self.tri01_b[:, :], op=ALU.mult),
                         reads=[ptq[j % 4][r], self.tri01_b], writes=[ptq[j % 4][r]])
                c.op("pe", lambda h, po=po, pt=pt, j=j, col0=col0, nj=nj, ve=ve: h.matmul(po[:, col0:512], lhsT=ve[:, j, :], rhs=pt[:, col0:512], start=(j == 0), stop=(j == nj - 1)),
                     reads=[ve] + ptq[j % 4][max(r, 0):4], writes=[po], sig=(j == nj - 1))
                if j == nj - 1:
                    rs_, ys_, nm_ = rsum[g % 2], yst[g % 2], nums[g % 2]
                    c.op("act", lambda h, po=po, rs_=rs_: h.activation(out=rs_[:, :], in_=po[64:128, :], func=AF.Copy), reads=[po], writes=[rs_])
                    c.op("act", lambda h, po=po, nm_=nm_: h.activation(out=nm_[:, :], in_=po[0:64, :], func=AF.Copy), reads=[po], writes=[nm_])
                    c.op("act", lambda h, rs_=rs_: h.activation(out=rs_[:, :], in_=rs_[:, :], func=AF.Ln), reads=[rs_], writes=[rs_])
                    c.op("act", lambda h, rs_=rs_: h.activation(out=rs_[:, :], in_=rs_[:, :], func=AF.Exp, scale=-1.0), reads=[rs_], writes=[rs_])
                    c.op("pool", lambda h, nm_=nm_, rs_=rs_, ys_=ys_: h.tensor_tensor(out=ys_[:, :], in0=nm_[:, :], in1=rs_[:, :], op=ALU.mult), reads=[nm_, rs_], writes=[ys_])
                    c.dma("sp", yb[hh * 64:(hh + 1) * 64, g * 512:(g + 1) * 512], ys_[:, :], reads=[ys_], writes=[self.db("yT1", hh * 100 + g)])
                yield k
        return gen(), len(steps)

    def b_phase(self):
        with ExitStack() as es:
            g, n = self._b_setup(es, [0, 1, 2, 3, 4, 5], [6, 7])
            for _ in g:
                pass
            self.c.barrier()

    NITER = 16
    TIE = True

    def _a_setup(self, es, sbanks, lpairs, pvpair):
        c, S, NT, NG = self.c, self.S, self.NT, self.NG
        NIT = self.NITER
        qi = c.sb(es, "qi", [128, 2, S], BF16)
        ki = c.sb(es, "ki", [128, S], BF16)
        ka = c.sb(es, "ka", [64, S], BF16)
        vext = c.sb(es, "vexta", [128, NT, 128], BF16)
        wi = c.sb(es, "wi", [128, NT, 4], F32)
        qat = [c.sb(es, "qat", [64, 1024], BF16) for _ in range(2)]
        score = c.sb(es, "score", [128, S], F32)
        isz = c.sb(es, "isz", [128, S], BF16)
        zrk = c.sb(es, "zrk", [128, S], BF16)
        maskb = [c.sb(es, "maskb", [128, S], BF16) for _ in range(2)]
        irep = c.sb(es, "irep", [128, 512], BF16)
        negc = c.sb(es, "negc", [128, 128], F32)
        pow2 = c.sb(es, "pow2", [128, NIT], F32)
        rts = [c.sb(es, "rt", [128, 512], F32) for _ in range(3)]
        pts = [c.sb(es, "pta", [128, 1024], BF16) for _ in range(3)]
        sm = c.sb(es, "bis", [128, 16], F32)
        steps = c.sb(es, "steps", [128, NIT], F32)
        rsum = c.sb(es, "rsuma", [64, 1024], F32)
        yst = [c.sb(es, "ysta", [64, 1024], BF16) for _ in range(2)]
        cm = self.dram["cmat"]
        cb = self.db("cmat")
        for r in range(4):
            c.dma("pool", irep[:, r * 128:(r + 1) * 128], cm[0], reads=[cb], writes=[irep])
        c.dma("sp", negc[:, :], cm[4], reads=[cb], writes=[negc])
        c.dma("sp", pow2[:, :], self.dram["pow2"][:, 0:NIT], reads=[self.db("pow2")], writes=[pow2])
        allg = lambda nm: [self.db(nm, g) for g in range(NG)]
        c.dma("sp", qi[:, :, :], self.qiT.rearrange("(hp p) t -> p hp t", p=128), reads=allg("qiT"), writes=[qi])
        c.dma("sp", ki[0:64, :], self.kiT[:, :], reads=allg("kiT"), writes=[ki])
        c.dma("sp", ki[64:128, :], self.kiT[:, :], reads=allg("kiT"), writes=[ki])
        c.dma("sp", ka[:, :], self.kaT[:, :], reads=allg("kaT"), writes=[ka])
        c.dma("sp", vext[:, :, 0:64], self.vA.rearrange("(n p) d -> p n d", p=128), reads=allg("vA"), writes=[vext])
        c.op("pool", lambda h: h.memset(vext[:, :, 64:128], 1.0), writes=[vext])
        c.dma("sp", wi[:, :, :], self.smallD.rearrange("(n p) c -> p n c", p=128)[:, :, 0:4], reads=allg("smallD"), writes=[wi])
        qa3 = self.qaT.rearrange("(h d) t -> d h t", d=64)
        ya = self.yT[0].rearrange("(h d) t -> d h t", d=64)
        NEGM = -32768.0

        def stage1(i):
            ncols = (i + 1) * 128
            qt = qat[i % 2]
            mb = maskb[i % 2]
            c.dma("sp", qt[:, :].rearrange("d (h t) -> d h t", h=8), qa3[:, :, i * 128:(i + 1) * 128], reads=allg("qaT"), writes=[qt])
            for s0 in range(0, ncols, 512):
                w = min(512, ncols - s0)
                for hd in range(4):
                    pl = self.rotset("apl", sbanks)
                    hb, hp = (hd % 2) * 64, hd // 2
                    c.op("pe", lambda h, pl=pl, hb=hb, hp=hp, s0=s0, w=w: h.matmul(pl[:, 0:w], lhsT=qi[hb:hb + 64, hp, i * 128:(i + 1) * 128], rhs=ki[hb:hb + 64, s0:s0 + w], start=True, stop=True),
                         reads=[qi, ki], writes=[pl])
                    if hd == 0:
                        c.op("dve", lambda h, pl=pl, s0=s0, w=w: h.tensor_scalar(out=score[:, s0:s0 + w], in0=pl[:, 0:w], scalar1=0.0, scalar2=wi[:, i, 0:1], op0=ALU.max, op1=ALU.mult),
                             reads=[pl, wi], writes=[score])
                    else:
                        rt = rts[hd - 1]
                        c.op("act", lambda h, pl=pl, rt=rt, w=w: h.activation(out=rt[:, 0:w], in_=pl[:, 0:w], func=AF.Relu), reads=[pl], writes=[rt])
                        c.op("dve", lambda h, rt=rt, hd=hd, s0=s0, w=w: h.scalar_tensor_tensor(out=score[:, s0:s0 + w], in0=rt[:, 0:w], scalar=wi[:, i, hd:hd + 1], in1=score[:, s0:s0 + w],
                                                                                            op0=ALU.mult, op1=ALU.add),
                             reads=[rt, wi, score], writes=[score])
            sc = score[:, 0:ncols]
            c.op("dve", lambda h: h.tensor_reduce(out=sm[:, 5:6], in_=sc, axis=AX.X, op=ALU.max), reads=[score], writes=[sm])
            c.op("dve", lambda h: h.tensor_reduce(out=sm[:, 6:7], in_=sc, axis=AX.X, op=ALU.min), reads=[score, sm], writes=[sm])
            c.op("dve", lambda h: h.tensor_tensor(out=score[:, i * 128:ncols], in0=score[:, i * 128:ncols], in1=negc[:, :], op=ALU.add), reads=[score, negc], writes=[score])
            c.op("dve", lambda h: h.tensor_scalar(out=sm[:, 0:1], in0=sm[:, 6:7], scalar1=-1.0, scalar2=None, op0=ALU.add), reads=[sm], writes=[sm])
            c.op("dve", lambda h: h.scalar_tensor_tensor(out=sm[:, 1:2], in0=sm[:, 5:6], scalar=1.0, in1=sm[:, 0:1], op0=ALU.add, op1=ALU.subtract), reads=[sm], writes=[sm])
            c.op("dve", lambda h: h.tensor_scalar(out=steps[:, :], in0=pow2[:, :], scalar1=sm[:, 1:2], scalar2=None, op0=ALU.mult), reads=[sm, pow2], writes=[steps])
            for k in range(NIT):
                c.op("dve", lambda h, k=k: h.tensor_tensor(out=sm[:, 2:3], in0=sm[:, 0:1], in1=steps[:, k:k + 1], op=ALU.add), reads=[sm, steps], writes=[sm])
                c.op("dve", lambda h: h.tensor_scalar(out=isz[:, 0:ncols], in0=sc, scalar1=sm[:, 2:3], scalar2=0.0, op0=ALU.is_ge, op1=ALU.add, accum_out=sm[:, 3:4]),
                     reads=[score, sm], writes=[isz, sm])
                c.op("dve", lambda h, k=k: h.scalar_tensor_tensor(out=sm[:, 4:5], in0=sm[:, 3:4], scalar=255.5, in1=steps[:, k:k + 1], op0=ALU.is_ge, op1=ALU.mult), reads=[sm, steps], writes=[sm])
                c.op("dve", lambda h: h.tensor_tensor(out=sm[:, 0:1], in0=sm[:, 0:1], in1=sm[:, 4:5], op=ALU.add), reads=[sm], writes=[sm])
            c.op("dve", lambda h: h.tensor_scalar(out=isz[:, 0:ncols], in0=sc, scalar1=0.0, scalar2=0.0, op0=ALU.is_gt, op1=ALU.add, accum_out=sm[:, 7:8]), reads=[score, sm], writes=[isz, sm])
            c.op("dve", lambda h: h.tensor_scalar(out=isz[:, 0:ncols], in0=sc, scalar1=0.0, scalar2=0.0, op0=ALU.is_equal, op1=ALU.add, accum_out=sm[:, 8:9]), reads=[score, sm], writes=[isz, sm])
            c.op("dve", lambda h: h.tensor_tensor(out=sm[:, 8:9], in0=sm[:, 8:9], in1=sm[:, 7:8], op=ALU.add), reads=[sm], writes=[sm])
            c.op("dve", lambda h: h.tensor_scalar(out=sm[:, 13:14], in0=sm[:, 7:8], scalar1=255.5, scalar2=None, op0=ALU.is_lt), reads=[sm], writes=[sm])
            c.op("dve", lambda h: h.scalar_tensor_tensor(out=sm[:, 9:10], in0=sm[:, 8:9], scalar=255.5, in1=sm[:, 13:14], op0=ALU.is_ge, op1=ALU.mult), reads=[sm], writes=[sm])
            c.op("dve", lambda h: h.tensor_scalar(out=sm[:, 10:11], in0=sm[:, 7:8], scalar1=-1.0, scalar2=256.5, op0=ALU.mult, op1=ALU.add), reads=[sm], writes=[sm])
            c.op("dve", lambda h: h.tensor_scalar(out=sm[:, 13:14], in0=sm[:, 9:10], scalar1=-1.0, scalar2=1.0, op0=ALU.mult, op1=ALU.add), reads=[sm], writes=[sm])
            c.op("dve", lambda h: h.tensor_tensor(out=sm[:, 13:14], in0=sm[:, 13:14], in1=sm[:, 0:1], op=ALU.mult), reads=[sm], writes=[sm])
            c.op("dve", lambda h: h.scalar_tensor_tensor(out=sm[:, 11:12], in0=sm[:, 9:10], scalar=1e-30, in1=sm[:, 13:14], op0=ALU.mult, op1=ALU.add), reads=[sm], writes=[sm])
            c.op("dve", lambda h: h.tensor_scalar(out=sm[:, 12:13], in0=sm[:, 9:10], scalar1=-NEGM, scalar2=None, op0=ALU.mult), reads=[sm], writes=[sm])
            c.op("dve", lambda h: h.tensor_tensor_scan(out=zrk[:, 0:ncols], data0=isz[:, 0:ncols], data1=isz[:, 0:ncols], initial=0.0, op0=ALU.add, op1=ALU.max), reads=[isz], writes=[zrk])
            c.op("dve", lambda h: h.scalar_tensor_tensor(out=isz[:, 0:ncols], in0=zrk[:, 0:ncols], scalar=sm[:, 10:11], in1=isz[:, 0:ncols], op0=ALU.is_le, op1=ALU.mult), reads=[zrk, sm, isz], writes=[isz])
            c.op("dve", lambda h, mb=mb: h.tensor_scalar(out=mb[:, 0:ncols], in0=sc, scalar1=sm[:, 11:12], scalar2=NEGM, op0=ALU.is_lt, op1=ALU.mult), reads=[score, sm], writes=[mb])
            if self.TIE:
                c.op("dve", lambda h, mb=mb: h.scalar_tensor_tensor(out=mb[:, 0:ncols], in0=isz[:, 0:ncols], scalar=sm[:, 12:13], in1=mb[:, 0:ncols], op0=ALU.mult, op1=ALU.add), reads=[isz, sm, mb], writes=[mb])

        def stage2(i):
            qt = qat[i % 2]
            mb = maskb[i % 2]
            po = (self.ps[pvpair[0]], self.ps[pvpair[1]])
            lb = [b_ for pr in lpairs for b_ in pr]
            nlb = len(lb)
            hsteps = [(j, half) for j in range(i + 1) for half in range(2)]

            def alog(k):
                j, half = hsteps[k]
                pl = self.ps[lb[k % nlb]]
                c.op("pe", lambda h: h.matmul(pl[:, :], lhsT=ka[:, j * 128:(j + 1) * 128], rhs=qt[:, half * 512:(half + 1) * 512], start=True, stop=False),
                     reads=[ka, qt], writes=[pl], sig=False)
                c.op("pe", lambda h: h.matmul(pl[:, :], lhsT=mb[:, j * 128:(j + 1) * 128], rhs=irep[:, :], start=False, stop=True),
                     reads=[mb, irep], writes=[pl])

            ptb = [[Buf(name=f"apt{a_}_{b_}") for b_ in range(2)] for a_ in range(3)]
            la = min(nlb - 1, 3)
            for k in range(min(la, len(hsteps))):
                alog(k)
            for k, (j, half) in enumerate(hsteps):
                if k + la < len(hsteps):
                    alog(k + la)
                pl = self.ps[lb[k % nlb]]
                pt = pts[j % 3]
                c.op("act", lambda h, pl=pl, half=half, pt=pt: h.activation(out=pt[:, half * 512:(half + 1) * 512], in_=pl[:, :], func=AF.Exp, scale=0.125), reads=[pl], writes=[self._aptb(pt, half)])
                c.op("pe", lambda h, half=half, j=j, pt=pt, po=po: h.matmul(po[half][:, :], lhsT=vext[:, j, :], rhs=pt[:, half * 512:(half + 1) * 512], start=(j == 0), stop=(j == i)),
                     reads=[vext, self._aptb(pt, half)], writes=[po[half]], sig=(j == i))
            ys_ = yst[i % 2]
            for half in range(2):
                hs = slice(half * 512, (half + 1) * 512)
                c.op("act", lambda h, half=half, hs=hs: h.activation(out=rsum[:, hs], in_=po[half][64:128, :], func=AF.Copy), reads=[po[half]], writes=[rsum])
                c.op("act", lambda h, hs=hs: h.activation(out=rsum[:, hs], in_=rsum[:, hs], func=AF.Ln), reads=[rsum], writes=[rsum])
                c.op("act", lambda h, hs=hs: h.activation(out=rsum[:, hs], in_=rsum[:, hs], func=AF.Exp, scale=-1.0), reads=[rsum], writes=[rsum])
                c.op("dve", lambda h, half=half, hs=hs, ys_=ys_: h.tensor_tensor(out=ys_[:, hs], in0=po[half][0:64, :], in1=rsum[:, hs], op=ALU.mult), reads=[po[half], rsum], writes=[ys_])
            c.dma("sp", ya[:, :, i * 128:(i + 1) * 128], ys_[:, :].rearrange("d (h t) -> d h t", h=8), reads=[ys_], writes=[self.db("yT0", i)])

        return stage1, stage2

    def _aptb(self, pt, half):
        d = self.__dict__.setdefault("_aptbufs", {})
        k = (id(pt), half)
        if k not in d:
            d[k] = Buf(name=f"aptb{len(d)}")
        return d[k]

    def a_phase(self):
        NT = self.NT
        with ExitStack() as es:
            stage1, stage2 = self._a_setup(es, [0, 1], [(2, 3), (4, 5)], (6, 7))
            stage1(0)
            for i in range(NT):
                if i + 1 < NT:
                    stage1(i + 1)
                stage2(i)
            self.c.barrier()

    def ab_phase(self):
        NT = self.NT
        with ExitStack() as es:
            stage1, stage2 = self._a_setup(es, [0], [(1, 2)], (3, 4))
            bgen, nb = self._b_setup(es, [5, 6], [7])
            done = 0

            def advance(upto):
                nonlocal done
                while done < min(upto, nb):
                    next(bgen)
                    done += 1
            stage1(0)
            for i in range(NT):
                if i + 1 < NT:
                    stage1(i + 1)
                stage2(i)
                advance((nb * (i + 1) + NT - 1) // NT)
            advance(nb)
            self.c.barrier()


def build_gmask():
    i = np.arange(128)
    out = np.zeros((14, 128, 512), np.float32)
    for l in range(7):
        b = 1 << l
        t, tp = i[:, None], i[None, :]
        m = ((t // (2 * b)) == (tp // (2 * b))) & ((t % (2 * b)) >= b) & ((tp % (2 * b)) < b)
        m = m.astype(np.float32)
        out[l] = np.tile(m, (1, 4))
        out[7 + l] = np.tile(m.T, (1, 4))
    return out


class ProgC(ProgAB):
    def c_phase(self, cvb):
        c, S, NT = self.c, self.S, self.NT
        with ExitStack() as es:
            def T(name, shape, dt, n=1):
                return [c.sb(es, name, shape, dt) for _ in range(n)]
            gm = T("gm", [128, 14, 512], BF16)[0]
            identrep = T("identrep", [128, 512], BF16)[0]
            negm4 = T("negm4", [128, 512], F32)[0]
            strict4 = T("strict4", [128, 512], BF16)[0]
            cm = self.dram["cmat"]
            cb = self.db("cmat")
            c.dma("pool", gm[:, :, :], self.dram["gmask"].rearrange("l p n -> p l n"), reads=[self.db("gmask")], writes=[gm])
            for r in range(4):
                c.dma("pool", identrep[:, r * 128:(r + 1) * 128], cm[0], reads=[cb], writes=[identrep])
                c.dma("sp", negm4[:, r * 128:(r + 1) * 128], cm[3], reads=[cb], writes=[negm4])
                c.dma("pool", strict4[:, r * 128:(r + 1) * 128], cm[5], reads=[cb], writes=[strict4])
            kTs = T("kTc", [128, 4, 128], BF16, 2); qTs = T("qTc", [128, 4, 128], BF16, 2)
            ktoks = T("ktok", [128, 512], BF16, 2); vtoks = T("vtok", [128, 512], BF16, 2)
            szs = T("szc", [128, 4, 128], BF16, 2); gbs = T("gb", [128, 8], F32, 2)
            gtri = T("gtri", [128, 4, 128], F32)[0]; ngtri = T("ngtri", [128, 4, 128], F32)[0]; bdiag = T("bdiag", [128, 4, 128], F32)[0]
            dm = T("dm", [128, 512], F32)[0]; decT = T("decT", [128, 512], F32)[0]; egcb = T("egcb", [128, 512], F32)[0]
            bbc = T("bbc", [128, 512], F32)[0]; dsb = T("dsb", [128, 512], F32)[0]
            ngb = T("ngb", [128, 4], F32)[0]
            gcs = T("gcs", [128, 8], F32)[0]; sm = T("csm", [128, 16], F32)[0]
            ATb = T("ATb", [128, 4, 128], BF16)[0]; Ab = T("Ab", [128, 4, 128], BF16)[0]; qkb = T("qkb", [128, 4, 128], BF16)[0]
            Ts = T("Tm", [128, 4, 128], BF16, 2); Us = T("Um", [128, 4, 128], BF16, 2)
            Xb = T("Xb", [128, 4, 128], BF16)[0]; Xpb = T("Xpb", [128, 4, 128], BF16)[0]; tmpb = T("tmpb", [128, 512], BF16, 2)
            kbg = T("kbg", [128, 512], BF16)[0]; vb = T("vb", [128, 512], BF16)[0]; kdec = T("kdec", [128, 512], BF16)[0]
            qg = T("qg", [128, 4, 128], BF16)[0]; nwT = T("nwT", [128, 4, 128], BF16)[0]; vnew = T("vnew", [128, 512], BF16)[0]
            state_f = T("state_f", [128, 4, 128], F32)[0]; state_b = T("state_b", [128, 4, 128], BF16)[0]
            osq = T("osq", [128, 4, 128], BF16)[0]; rstd = T("crstd", [128, 512], F32)[0]; yn = T("yn", [128, 512], F32)[0]
            yst = T("ystc", [128, 4, 128], BF16, 2)
            c.op("pool", lambda h: h.memset(state_f[:, :, :], 0.0), writes=[state_f])
            c.op("pool", lambda h: h.memset(state_b[:, :, :], 0.0), writes=[state_b])
            qc3 = self.qcT.rearrange("(h d) t -> d h t", d=128); kc3 = self.kcT.rearrange("(h d) t -> d h t", d=128)
            sz3 = self.szT.rearrange("(h d) t -> d h t", d=128); yc3 = self.yT[2].rearrange("(h d) t -> d h t", d=128)
            allg = lambda nm: [self.db(nm, g) for g in range(self.NG)]
            f2 = lambda b: b[:, :, :].rearrange("p h t -> p (h t)")
            for n in range(NT):
                cs = slice(n * 128, (n + 1) * 128)
                kT, qT, ktok, vtok, sz, gb = kTs[n % 2], qTs[n % 2], ktoks[n % 2], vtoks[n % 2], szs[n % 2], gbs[n % 2]
                c.dma("sp", kT[:, :, :], kc3[:, :, cs], reads=allg("kcT"), writes=[kT])
                c.dma("sp", qT[:, :, :], qc3[:, :, cs], reads=allg("qcT"), writes=[qT])
                c.dma("sp", sz[:, :, :], sz3[:, :, cs], reads=allg("szT"), writes=[sz])
                c.dma("sp", ktok[:, :], self.kC[cs, :], reads=allg("kC"), writes=[ktok])
                c.dma("sp", vtok[:, :], self.vC[cs, :], reads=allg("vC"), writes=[vtok])
                c.dma("sp", gb[:, :], self.smallD[cs, 12:20], reads=allg("smallD"), writes=[gb])
                c.op("dve", lambda h: h.tensor_scalar(out=ngb[:, :], in0=gb[:, 4:8], scalar1=-1.0, scalar2=None, op0=ALU.mult), reads=[gb], writes=[ngb])
                for hd in range(4):
                    c.op("dve", lambda h, hd=hd: h.tensor_scalar(out=gtri[:, hd, :], in0=self.triu_f[:, :], scalar1=gb[:, 4 + hd:5 + hd], scalar2=None, op0=ALU.mult), reads=[self.triu_f, gb], writes=[gtri])
                    c.op("act", lambda h, hd=hd: h.activation(out=ngtri[:, hd, :], in_=self.triu_f[:, :], func=AF.Copy, scale=ngb[:, hd:hd + 1]), reads=[self.triu_f, ngb], writes=[ngtri])
                    c.op("act", lambda h, hd=hd: h.activation(out=bdiag[:, hd, :], in_=self.ident_f[:, :], func=AF.Copy, scale=gb[:, hd:hd + 1]), reads=[self.ident_f, gb], writes=[bdiag])
                pD, pG, pB, pS = self.rot(), self.rot(), self.rot(), self.rot()
                for hd in range(4):
                    hs = slice(hd * 128, (hd + 1) * 128)
                    c.op("pe", lambda h, hd=hd, hs=hs: h.matmul(pD[:, hs], lhsT=self.ones_f[:, :], rhs=gtri[:, hd, :], start=True, stop=False), reads=[self.ones_f, gtri], writes=[pD], sig=False)
                    c.op("pe", lambda h, hd=hd, hs=hs: h.matmul(pD[:, hs], lhsT=ngtri[:, hd, :], rhs=self.ones_f[:, :], start=False, stop=True), reads=[self.ones_f, ngtri], writes=[pD], sig=(hd == 3))
                for hd in range(4):
                    hs = slice(hd * 128, (hd + 1) * 128)
                    c.op("pe", lambda h, hd=hd, hs=hs: h.matmul(pG[:, hs], lhsT=self.ones_f[:, :], rhs=gtri[:, hd, :], start=True, stop=True), reads=[self.ones_f, gtri], writes=[pG], sig=(hd == 3))
                for hd in range(4):
                    hs = slice(hd * 128, (hd + 1) * 128)
                    c.op("pe", lambda h, hd=hd, hs=hs: h.matmul(pB[:, hs], lhsT=self.ones_f[:, :], rhs=bdiag[:, hd, :], start=True, stop=True), reads=[self.ones_f, bdiag], writes=[pB], sig=(hd == 3))
                c.op("pe", lambda h: h.matmul(pS[:, 0:4], lhsT=self.triu_f[:, :], rhs=gb[:, 4:8], start=True, stop=True), reads=[self.triu_f, gb], writes=[pS], sig=False)
                c.op("pe", lambda h: h.matmul(pS[:, 4:8], lhsT=self.ones_f[:, :], rhs=gb[:, 4:8], start=True, stop=True), reads=[self.ones_f, gb], writes=[pS])
                c.op("dve", lambda h: h.scalar_tensor_tensor(out=dm[:, :], in0=pD[:, :], scalar=0.0, in1=negm4[:, :], op0=ALU.min, op1=ALU.add), reads=[pD, negm4], writes=[dm])
                c.op("act", lambda h: h.activation(out=decT[:, :], in_=dm[:, :], func=AF.Exp), reads=[dm], writes=[decT])
                c.op("act", lambda h: h.activation(out=egcb[:, :], in_=pG[:, :], func=AF.Exp), reads=[pG], writes=[egcb])
                c.op("act", lambda h: h.activation(out=bbc[:, :], in_=pB[:, :], func=AF.Copy), reads=[pB], writes=[bbc])
                c.op("act", lambda h: h.activation(out=gcs[:, :], in_=pS[:, 0:8], func=AF.Copy), reads=[pS], writes=[gcs])
                c.op("act", lambda h: h.activation(out=sm[:, 0:4], in_=gcs[:, 0:4], func=AF.Exp), reads=[gcs], writes=[sm])
                c.op("dve", lambda h: h.tensor_tensor(out=sm[:, 0:4], in0=sm[:, 0:4], in1=gb[:, 0:4], op=ALU.mult), reads=[sm, gb], writes=[sm])
                c.op("dve", lambda h: h.tensor_tensor(out=sm[:, 12:16], in0=gcs[:, 4:8], in1=gcs[:, 0:4], op=ALU.subtract), reads=[gcs, sm], writes=[sm])
                c.op("act", lambda h: h.activation(out=sm[:, 4:8], in_=sm[:, 12:16], func=AF.Exp), reads=[sm], writes=[sm])
                c.op("act", lambda h: h.activation(out=sm[:, 8:12], in_=gcs[:, 4:8], func=AF.Exp), reads=[gcs, sm], writes=[sm])
                pK, pQ = self.rot(), self.rot()
                for hd in range(4):
                    hs = slice(hd * 128, (hd + 1) * 128)
                    c.op("pe", lambda h, hd=hd, hs=hs: h.matmul(pK[:, hs], lhsT=kT[:, hd, :], rhs=kT[:, hd, :], start=True, stop=True), reads=[kT], writes=[pK], sig=(hd == 3))
                for hd in range(4):
                    hs = slice(hd * 128, (hd + 1) * 128)
                    c.op("pe", lambda h, hd=hd, hs=hs: h.matmul(pQ[:, hs], lhsT=kT[:, hd, :], rhs=qT[:, hd, :], start=True, stop=True), reads=[kT, qT], writes=[pQ], sig=(hd == 3))
                c.op("dve", lambda h: h.tensor_tensor(out=dsb[:, :], in0=decT[:, :], in1=bbc[:, :], op=ALU.mult), reads=[decT, bbc], writes=[dsb])
                c.op("dve", lambda h: h.tensor_tensor(out=dsb[:, :], in0=dsb[:, :], in1=strict4[:, :], op=ALU.mult), reads=[dsb, strict4], writes=[dsb])
                c.op("dve", lambda h: h.tensor_tensor(out=f2(ATb), in0=pK[:, :], in1=dsb[:, :], op=ALU.mult), reads=[pK, dsb], writes=[ATb])
                c.op("dve", lambda h: h.tensor_tensor(out=f2(qkb), in0=pQ[:, :], in1=decT[:, :], op=ALU.mult), reads=[pQ, decT], writes=[qkb])
                pA = self.rot()
                for hd in range(4):
                    hs = slice(hd * 128, (hd + 1) * 128)
                    c.op("pe", lambda h, hd=hd, hs=hs: h.matmul(pA[:, hs], lhsT=ATb[:, hd, :], rhs=self.ident_b[:, :], start=True, stop=True), reads=[ATb, self.ident_b], writes=[pA], sig=(hd == 3))
                c.op("act", lambda h: h.activation(out=f2(Ab), in_=pA[:, :], func=AF.Copy), reads=[pA], writes=[Ab])
                Tc, Uc = Ts[0], Us[0]
                c.op("dve", lambda h: h.tensor_tensor(out=tmpb[0][:, :], in0=f2(Ab), in1=gm[:, 0, :], op=ALU.mult), reads=[Ab, gm], writes=[tmpb[0]])
                c.op("dve", lambda h: h.tensor_tensor(out=f2(Tc), in0=identrep[:, :], in1=tmpb[0][:, :], op=ALU.subtract), reads=[tmpb[0], identrep], writes=[Tc])
                c.op("dve", lambda h: h.tensor_tensor(out=tmpb[1][:, :], in0=f2(ATb), in1=gm[:, 7, :], op=ALU.mult), reads=[ATb, gm], writes=[tmpb[1]])
                c.op("dve", lambda h: h.tensor_tensor(out=f2(Uc), in0=identrep[:, :], in1=tmpb[1][:, :], op=ALU.subtract), reads=[tmpb[1], identrep], writes=[Uc])
                for l in range(1, 7):
                    Tn, Un = Ts[l % 2], Us[l % 2]
                    pXp = self.rot()
                    for hd in range(4):
                        hs = slice(hd * 128, (hd + 1) * 128)
                        c.op("pe", lambda h, hd=hd, hs=hs, Uc=Uc: h.matmul(pXp[:, hs], lhsT=Ab[:, hd, :], rhs=Uc[:, hd, :], start=True, stop=True), reads=[Ab, Uc], writes=[pXp], sig=(hd == 3))
                    c.op("dve", lambda h, l=l: h.tensor_tensor(out=f2(Xpb), in0=pXp[:, :], in1=gm[:, 7 + l, :], op=ALU.mult), reads=[pXp, gm], writes=[Xpb])
                    if l < 6:
                        pX = self.rot()
                        for hd in range(4):
                            hs = slice(hd * 128, (hd + 1) * 128)
                            c.op("pe", lambda h, hd=hd, hs=hs, Tc=Tc: h.matmul(pX[:, hs], lhsT=ATb[:, hd, :], rhs=Tc[:, hd, :], start=True, stop=True), reads=[ATb, Tc], writes=[pX], sig=(hd == 3))
                        c.op("dve", lambda h, l=l: h.tensor_tensor(out=f2(Xb), in0=pX[:, :], in1=gm[:, l, :], op=ALU.mult), reads=[pX, gm], writes=[Xb])
                    pYp = self.rot()
                    for hd in range(4):
                        hs = slice(hd * 128, (hd + 1) * 128)
                        c.op("pe", lambda h, hd=hd, hs=hs, Tc=Tc: h.matmul(pYp[:, hs], lhsT=Tc[:, hd, :], rhs=Xpb[:, hd, :], start=True, stop=True), reads=[Tc, Xpb], writes=[pYp], sig=(hd == 3))
                    c.op("dve", lambda h, Uc=Uc, Un=Un: h.tensor_tensor(out=f2(Un), in0=f2(Uc), in1=pYp[:, :], op=ALU.subtract), reads=[Uc, pYp], writes=[Un])
                    if l < 6:
                        pY = self.rot()
                        for hd in range(4):
                            hs = slice(hd * 128, (hd + 1) * 128)
                            c.op("pe", lambda h, hd=hd, hs=hs, Uc=Uc: h.matmul(pY[:, hs], lhsT=Uc[:, hd, :], rhs=Xb[:, hd, :], start=True, stop=True), reads=[Uc, Xb], writes=[pY], sig=(hd == 3))
                        c.op("dve", lambda h, Tc=Tc, Tn=Tn: h.tensor_tensor(out=f2(Tn), in0=f2(Tc), in1=pY[:, :], op=ALU.subtract), reads=[Tc, pY], writes=[Tn])
                    Tc, Uc = Tn, Un
                U = Uc
                for hd in range(4):
                    hs = slice(hd * 128, (hd + 1) * 128)
                    c.op("act", lambda h, hd=hd, hs=hs: h.activation(out=kbg[:, hs], in_=ktok[:, hs], func=AF.Copy, scale=sm[:, hd:hd + 1]), reads=[ktok, sm], writes=[kbg])
                    c.op("act", lambda h, hd=hd, hs=hs: h.activation(out=vb[:, hs], in_=vtok[:, hs], func=AF.Copy, scale=gb[:, hd:hd + 1]), reads=[vtok, gb], writes=[vb])
                    c.op("act", lambda h, hd=hd, hs=hs: h.activation(out=kdec[:, hs], in_=ktok[:, hs], func=AF.Copy, scale=sm[:, 4 + hd:5 + hd]), reads=[ktok, sm], writes=[kdec])
                c.op("dve", lambda h: h.tensor_tensor(out=f2(qg), in0=f2(qT), in1=egcb[:, :], op=ALU.mult), reads=[qT, egcb], writes=[qg])
                pW = self.rot()
                for hd in range(4):
                    hs = slice(hd * 128, (hd + 1) * 128)
                    c.op("pe", lambda h, hd=hd, hs=hs: h.matmul(pW[:, hs], lhsT=kbg[:, hs], rhs=U[:, hd, :], start=True, stop=True), reads=[kbg, U], writes=[pW], sig=(hd == 3))
                c.op("act", lambda h: h.activation(out=f2(nwT), in_=pW[:, :], func=AF.Copy, scale=-1.0), reads=[pW], writes=[nwT])
                pV = self.rot()
                for hd in range(4):
                    hs = slice(hd * 128, (hd + 1) * 128)
                    c.op("pe", lambda h, hd=hd, hs=hs: h.matmul(pV[:, hs], lhsT=U[:, hd, :], rhs=vb[:, hs], start=True, stop=False), reads=[U, vb], writes=[pV], sig=False)
                    c.op("pe", lambda h, hd=hd, hs=hs: h.matmul(pV[:, hs], lhsT=nwT[:, hd, :], rhs=state_b[:, hd, :], start=False, stop=True), reads=[nwT, state_b], writes=[pV], sig=(hd == 3))
                c.op("act", lambda h: h.activation(out=vnew[:, :], in_=pV[:, :], func=AF.Copy), reads=[pV], writes=[vnew])
                pO = self.rot()
                for hd in range(4):
                    hs = slice(hd * 128, (hd + 1) * 128)
                    c.op("pe", lambda h, hd=hd, hs=hs: h.matmul(pO[:, hs], lhsT=state_b[:, hd, :], rhs=qg[:, hd, :], start=True, stop=False), reads=[state_b, qg], writes=[pO], sig=False)
                    c.op("pe", lambda h, hd=hd, hs=hs: h.matmul(pO[:, hs], lhsT=vnew[:, hs], rhs=qkb[:, hd, :], start=False, stop=True), reads=[vnew, qkb], writes=[pO], sig=(hd == 3))
                pS2 = self.rot()
                for hd in range(4):
                    hs = slice(hd * 128, (hd + 1) * 128)
                    c.op("pe", lambda h, hd=hd, hs=hs: h.matmul(pS2[:, hs], lhsT=kdec[:, hs], rhs=vnew[:, hs], start=True, stop=True), reads=[kdec, vnew], writes=[pS2], sig=(hd == 3))
                for hd in range(4):
                    hs = slice(hd * 128, (hd + 1) * 128)
                    c.op("dve", lambda h, hd=hd, hs=hs: h.scalar_tensor_tensor(out=state_f[:, hd, :], in0=state_f[:, hd, :], scalar=sm[:, 8 + hd:9 + hd], in1=pS2[:, hs], op0=ALU.mult, op1=ALU.add),
                         reads=[state_f, sm, pS2], writes=[state_f])
                c.op("act", lambda h: h.activation(out=f2(state_b), in_=f2(state_f), func=AF.Copy), reads=[state_f], writes=[state_b])
                c.op("act", lambda h: h.activation(out=f2(osq), in_=pO[:, :], func=AF.Square), reads=[pO], writes=[osq])
                pN = self.rot()
                for hd in range(4):
                    hs = slice(hd * 128, (hd + 1) * 128)
                    c.op("pe", lambda h, hd=hd, hs=hs: h.matmul(pN[:, hs], lhsT=self.ones_b[:, :], rhs=osq[:, hd, :], start=True, stop=True), reads=[self.ones_b, osq], writes=[pN], sig=(hd == 3))
                c.op("dve", lambda h: h.tensor_scalar(out=rstd[:, :], in0=pN[:, :], scalar1=1.0 / 128, scalar2=1e-6, op0=ALU.mult, op1=ALU.add), reads=[pN], writes=[rstd])
                c.op("act", lambda h: h.activation(out=rstd[:, :], in_=rstd[:, :], func=AF.Ln), reads=[rstd], writes=[rstd])
                c.op("act", lambda h: h.activation(out=rstd[:, :], in_=rstd[:, :], func=AF.Exp, scale=-0.5), reads=[rstd], writes=[rstd])
                c.op("dve", lambda h: h.tensor_tensor(out=yn[:, :], in0=pO[:, :], in1=rstd[:, :], op=ALU.mult), reads=[pO, rstd], writes=[yn])
                ys_ = yst[n % 2]
                c.op("dve", lambda h, ys_=ys_: h.scalar_tensor_tensor(out=f2(ys_), in0=yn[:, :], scalar=self.cvec[:, cvb + CV_DN:cvb + CV_DN + 1], in1=f2(sz), op0=ALU.mult, op1=ALU.mult),
                     reads=[yn, self.cvec, sz], writes=[ys_])
                c.dma("sp", yc3[:, :, cs], ys_[:, :, :], reads=[ys_], writes=[self.db("yT2", n)])
            c.barrier()


class ProgFull(ProgC):
    def merge_phase(self, xsrc, xdst, wbr, w_out):
        c, S, NG = self.c, self.S, self.NG
        with ExitStack() as es:
            wbs = [c.sb(es, "wbr", [128, 4, 1024], BF16) for _ in range(3)]
            wo = c.sb(es, "wo", [128, 8, 1024], BF16)
            wb = self.db("w")
            for i in range(3):
                c.dma("pool", wbs[i][:, :, :], wbr[i].rearrange("(k p) n -> p k n", p=128), reads=[wb], writes=[wbs[i]])
            c.dma("pool", wo[:, :, :], w_out.rearrange("(k p) n -> p k n", p=128), reads=[wb], writes=[wo])
            ys = [c.sb(es, "ymg", [128, 3, 4, 512], BF16) for _ in range(2)]
            gts = [c.sb(es, "gmg", [128, 24, 512], BF16) for _ in range(2)]
            xgs = [c.sb(es, "xmg", [128, 8, 512], F32) for _ in range(2)]
            mg = c.sb(es, "mg", [128, 8, 512], BF16)
            t1 = [c.sb(es, "mt1", [128, 512], F32) for _ in range(2)]
            t2 = [c.sb(es, "mt2", [128, 512], F32) for _ in range(2)]
            xs3 = xsrc.rearrange("(c p) t -> p c t", p=128)
            xd3 = xdst.rearrange("(c p) t -> p c t", p=128)
            for g in range(NG):
                tsl = slice(g * 512, (g + 1) * 512)
                y, gt, xg = ys[g % 2], gts[g % 2], xgs[g % 2]
                deps = [self.db("yT0", i) for i in range(g * 4, g * 4 + 4)] + [self.db("yT1", hh * 100 + g) for hh in range(8)] + [self.db("yT2", n) for n in range(g * 4, g * 4 + 4)]
                for br in range(3):
                    c.dma("sp", y[:, br, :, :], self.yT[br].rearrange("(k p) t -> p k t", p=128)[:, :, tsl], reads=deps, writes=[y])
                c.dma("sp", gt[:, :, :], self.gT.rearrange("(k p) t -> p k t", p=128)[:, :, tsl], reads=[self.db("gT", sec * 100 + g) for sec in range(6)], writes=[gt])
                c.dma("sp", xg[:, :, :], xs3[:, :, tsl], reads=[self.db(xsrc.name, g)], writes=[xg])
                for d in range(8):
                    pbs = []
                    for br in range(3):
                        pb = self.rot()
                        pbs.append(pb)
                        for k in range(4):
                            c.op("pe", lambda h, br=br, k=k, d=d, pb=pb: h.matmul(pb[:, :], lhsT=wbs[br][:, k, d * 128:(d + 1) * 128], rhs=y[:, br, k, :], start=(k == 0), stop=(k == 3)),
                                 reads=[wbs[br], y], writes=[pb], sig=(k == 3))
                    a, b = t1[d % 2], t2[d % 2]
                    c.op("dve", lambda h, d=d, a=a: h.tensor_tensor(out=a[:, :], in0=pbs[0][:, :], in1=gt[:, d, :], op=ALU.mult), reads=[pbs[0], gt], writes=[a])
                    c.op("dve", lambda h, d=d, b=b: h.tensor_tensor(out=b[:, :], in0=pbs[1][:, :], in1=gt[:, 8 + d, :], op=ALU.mult), reads=[pbs[1], gt], writes=[b])
                    c.op("dve", lambda h, a=a, b=b: h.tensor_tensor(out=a[:, :], in0=a[:, :], in1=b[:, :], op=ALU.add), reads=[a, b], writes=[a])
                    c.op("dve", lambda h, d=d, b=b: h.tensor_tensor(out=b[:, :], in0=pbs[2][:, :], in1=gt[:, 16 + d, :], op=ALU.mult), reads=[pbs[2], gt], writes=[b])
                    c.op("dve", lambda h, d=d, a=a, b=b: h.tensor_tensor(out=mg[:, d, :], in0=a[:, :], in1=b[:, :], op=ALU.add), reads=[a, b], writes=[mg])
                for d in range(8):
                    po = self.rot()
                    for k in range(8):
                        c.op("pe", lambda h, d=d, k=k, po=po: h.matmul(po[:, :], lhsT=wo[:, k, d * 128:(d + 1) * 128], rhs=mg[:, k, :], start=(k == 0), stop=(k == 7)),
                             reads=[wo, mg], writes=[po], sig=(k == 7))
                    c.op("dve", lambda h, d=d, po=po, xg=xg: h.tensor_tensor(out=xg[:, d, :], in0=po[:, :], in1=xg[:, d, :], op=ALU.add), reads=[po, xg], writes=[xg])
                c.dma("sp", xd3[:, :, tsl], xg[:, :, :], reads=[xg], writes=[self.db(xdst.name, g)])
            c.barrier()

    def final_phase(self, xsrc, out):
        c, S, NG = self.c, self.S, self.NG
        with ExitStack() as es:
            xgs = [c.sb(es, "xf", [128, 8, 512], F32) for _ in range(2)]
            ogs = [c.sb(es, "of", [128, 8, 512], F32) for _ in range(2)]
            sq = c.sb(es, "sqf", [128, 8, 512], BF16)
            rstd = c.sb(es, "rstdf", [128, 512], F32)
            xs3 = xsrc.rearrange("(c p) t -> p c t", p=128)
            o3 = out.rearrange("(c p) t -> p c t", p=128)
            for g in range(NG):
                tsl = slice(g * 512, (g + 1) * 512)
                xg, og = xgs[g % 2], ogs[g % 2]
                c.dma("sp", xg[:, :, :], xs3[:, :, tsl], reads=[self.db(xsrc.name, g)], writes=[xg])
                self.rmsnorm_group(xg, sq, lambda k, og=og: og[:, k, :], og, rstd, DEPTH * CV_PER_LAYER, self.rot())
                c.dma("sp", o3[:, :, tsl], og[:, :, :], reads=[og], writes=[self.db("out", g)])
            c.barrier()


def build_full(S=4096):
    P = ProgFull(S)
    es = ExitStack()
    P.setup_consts(es)
    P.declare_scratch()
    d = P.dr
    EI = "ExternalInput"
    d("rotC", [128, S], F32, kind=EI); d("rotS", [128, S], F32, kind=EI)
    d("pow2", [128, 26], F32, kind=EI); d("gmask", [14, 128, 512], F32, kind=EI)
    xin = d("xT_in", [1024, S], F32, kind=EI)
    out = d("outT", [1024, S], F32, kind="ExternalOutput")
    xa = d("xTa", [1024, S], F32); xb = d("xTb", [1024, S], F32)
    f1i = d("ffn1_w_in", [DEPTH, 1024, 4096], F32, kind=EI); f1o = d("ffn1_w_out", [DEPTH, 2048, 1024], F32, kind=EI)
    f2i = d("ffn2_w_in", [DEPTH, 1024, 4096], F32, kind=EI); f2o = d("ffn2_w_out", [DEPTH, 2048, 1024], F32, kind=EI)
    w = d("w_in", [DEPTH, 1024, 7636], F32, kind=EI); wsw = d("w_sw", [DEPTH, 1024, 896], F32, kind=EI); wsm = d("w_small", [DEPTH, 1024, 84], F32, kind=EI)
    wba = d("w_branch_a", [DEPTH, 512, 1024], F32, kind=EI); wbb = d("w_branch_b", [DEPTH, 512, 1024], F32, kind=EI); wbc = d("w_branch_c", [DEPTH, 512, 1024], F32, kind=EI)
    wo = d("w_out", [DEPTH, 1024, 1024], F32, kind=EI)
    cur = xin
    for l in range(DEPTH):
        cvb = l * CV_PER_LAYER
        P.ffn_phase(cur, xa, f1i[l], f1o[l], cvb + CV_FFN1)
        P.m1_phase(xa, w[l], wsw[l], wsm[l], cvb)
        P.ab_phase()
        P.c_phase(cvb)
        P.merge_phase(xa, xb, [wba[l], wbb[l], wbc[l]], wo[l])
        P.ffn_phase(xb, xa, f2i[l], f2o[l], cvb + CV_FFN2)
        cur = xa
    P.final_phase(xa, out)
    es.close()
    return P


_CACHE = {}


def kernel(**inputs):
    S = 4096
    inp = {k: np.asarray(v) for k, v in inputs.items()}
    if "prog" not in _CACHE:
        _CACHE["prog"] = build_full(S)
    P = _CACHE["prog"]
    rotC, rotS = build_rot(S)
    shared = {
        "cmat": build_cmat(), "cvec": build_cvec(inp), "rotC": rotC, "rotS": rotS, "pow2": build_pow2(), "gmask": build_gmask(),
        "ffn1_w_in": inp["ffn1_w_in"], "ffn1_w_out": inp["ffn1_w_out"], "ffn2_w_in": inp["ffn2_w_in"], "ffn2_w_out": inp["ffn2_w_out"],
        "w_in": inp["w_in"], "w_sw": np.ascontiguousarray(inp["w_in"][:, :, swap_cols()]), "w_small": np.ascontiguousarray(inp["w_in"][:, :, small_cols()]),
        "w_branch_a": inp["w_branch_a"], "w_branch_b": inp["w_branch_b"], "w_branch_c": inp["w_branch_c"], "w_out": inp["w_out"],
    }
    in_maps = []
    for b in range(NB):
        m = dict(shared)
        m["xT_in"] = np.ascontiguousarray(inp["x"][b].T)
        in_maps.append(m)
    res = run_bass_kernel_spmd(P.nc, in_maps, core_ids=list(range(NB)))
    out = np.stack([np.ascontiguousarray(r["outT"].T) for r in res.results], axis=0)
    return out.astype(np.float32)
```

```python
from contextlib import ExitStack
import numpy as np
import concourse.bass as bass
import concourse.mybir as mybir
from concourse.bass_utils import run_bass_kernel_spmd

F32 = mybir.dt.float32
BF16 = mybir.dt.bfloat16
ALU = mybir.AluOpType
AF = mybir.ActivationFunctionType
AX = mybir.AxisListType

D = 1024
DEPTH = 2
NB = 8
IN_SIZES = (512, 64, 64, 256, 64, 4, 1536, 8, 1536, 512, 4, 4, 3072)
OFF = np.concatenate([[0], np.cumsum(IN_SIZES)]).tolist()
(O_AQ, O_AK, O_AV, O_IQ, O_IK, O_IW, O_BQKV, O_BF, O_CQKV, O_CZ, O_CB, O_CA, O_G) = OFF[:13]
NEG = -32768.0


class Buf:
    __slots__ = ("t", "last_w", "readers", "name")

    def __init__(self, t=None, name=""):
        self.t = t
        self.last_w = None
        self.readers = {}
        self.name = name

    def __getitem__(self, k):
        return self.t[k]


class Eng:
    def __init__(self, name, handle, sem):
        self.name = name
        self.h = handle
        self.sem = sem
        self.count = 0
        self.seen = {}


class Ctx:
    SAME_ENGINE_SYNC = True
    RAW_ONLY_SAME_ENGINE = False

    def __init__(self, nc, n_dma_sems=10):
        self.nc = nc
        self.sems = {}
        self.eng = {}
        for nm, h in (("pe", nc.tensor), ("act", nc.scalar), ("dve", nc.vector),
                      ("pool", nc.gpsimd), ("sp", nc.sync)):
            self.sems["s_" + nm] = nc.alloc_semaphore("s_" + nm)
            self.eng[nm] = Eng(nm, h, "s_" + nm)
        self.dma_pool = {}
        for q in ("sp", "pool", "act"):
            lst = []
            for i in range(n_dma_sems):
                k = f"d_{q}{i}"
                self.sems[k] = nc.alloc_semaphore(k)
                lst.append([k, 0])
            self.dma_pool[q] = [lst, 0]
        self.n_instr = 0
        self.n_wait = 0
        self.uid = 0

    def sb(self, es, name, shape, dt):
        self.uid += 1
        nm = f"{name}_{self.uid}"
        return Buf(es.enter_context(self.nc.sbuf_tensor(nm, list(shape), dt)), nm)

    def _need(self, reads, writes, own=None):
        need = {}

        def add(ev, raw):
            if ev is None:
                return
            k, v = ev
            if k == own and not raw and self.RAW_ONLY_SAME_ENGINE:
                return
            if need.get(k, 0) < v:
                need[k] = v
        for b in reads:
            add(b.last_w, True)
        for b in writes:
            add(b.last_w, False)
            for k, v in b.readers.items():
                add((k, v), False)
        return need

    def _emit_waits(self, e, need):
        for k, v in need.items():
            if k == e.sem and (e.name == "pe" or not self.SAME_ENGINE_SYNC):
                continue
            if e.seen.get(k, 0) >= v:
                continue
            e.h.wait_ge(self.sems[k], v)
            e.seen[k] = v
            self.n_wait += 1

    def _record(self, ev, reads, writes):
        k, v = ev
        for b in writes:
            b.last_w = ev
            b.readers = {}
        for b in reads:
            if b.readers.get(k, 0) < v:
                b.readers[k] = v

    def op(self, en, fn, reads=(), writes=(), sig=True):
        e = self.eng[en]
        self._emit_waits(e, self._need(reads, writes, e.sem))
        ins = fn(e.h)
        self.n_instr += 1
        if sig:
            ins.then_inc(self.sems[e.sem], 1)
            e.count += 1
            ev = (e.sem, e.count)
        else:
            ev = (e.sem, e.count + 1)
        self._record(ev, reads, writes)
        return ins

    def dma(self, q, out, in_, reads=(), writes=(), **kw):
        e = self.eng[q]
        lst, idx = self.dma_pool[q]
        ent = lst[idx % len(lst)]
        self.dma_pool[q][1] = idx + 1
        need = self._need(reads, writes, None)
        if ent[1] > 0 and need.get(ent[0], 0) < ent[1]:
            need[ent[0]] = ent[1]
        self._emit_waits(e, need)
        ins = e.h.dma_start(out=out, in_=in_, **kw)
        ent[1] += 16
        ins.then_inc(self.sems[ent[0]], 16)
        self.n_instr += 1
        self._record((ent[0], ent[1]), reads, writes)
        return ins

    def barrier(self):
        for e in self.eng.values():
            need = {}
            for f in self.eng.values():
                if f is not e and f.count > 0:
                    need[f.sem] = f.count
            for q in self.dma_pool:
                for k, v in self.dma_pool[q][0]:
                    if v > 0:
                        need[k] = v
            self._emit_waits(e, need)


class Prog:
    def __init__(self, S, ext=None):
        self.S = S
        self.NT = S // 128
        self.NG = S // 512
        self.nc = bass.Bass("TRN2", target_bir_lowering=False)
        self.c = Ctx(self.nc)
        self.ext = ext or {}
        self.dram = {}
        self.dbuf = {}
        nc = self.nc
        self.psall = nc.alloc_psum_tensor("psall", [128, 8 * 512], F32)
        self.ps = [Buf(self.psall[:, i * 512:(i + 1) * 512], f"ps{i}") for i in range(8)]

    def dr(self, name, shape, dt, kind=None):
        if kind is None:
            kind = {"in": "ExternalInput", "out": "ExternalOutput"}.get(self.ext.get(name), "Internal")
        t = self.nc.dram_tensor(name, list(shape), dt, kind=kind)
        self.dram[name] = t.ap()
        return self.dram[name]

    def db(self, name, idx=0):
        k = (name, idx)
        if k not in self.dbuf:
            self.dbuf[k] = Buf(name=f"{name}{idx}")
        return self.dbuf[k]

    def setup_consts(self, es):
        c, nc = self.c, self.nc
        cm = self.dr("cmat", [7, 128, 128], F32, kind="ExternalInput")
        self.ident_b = c.sb(es, "identb", [128, 128], BF16)
        self.ones_b = c.sb(es, "onesb", [128, 128], BF16)
        self.ones_f = c.sb(es, "onesf", [128, 128], F32)
        self.triu_f = c.sb(es, "triuf", [128, 128], F32)
        self.ident_f = c.sb(es, "identf", [128, 128], F32)
        self.negm_f = c.sb(es, "negmf", [128, 128], F32)
        self.tri01_b = c.sb(es, "tri01b", [128, 128], BF16)
        cb = self.db("cmat")
        c.dma("pool", self.ident_b[:, :], cm[0], reads=[cb], writes=[self.ident_b])
        c.dma("pool", self.ones_b[:, :], cm[1], reads=[cb], writes=[self.ones_b])
        c.dma("sp", self.ones_f[:, :], cm[1], reads=[cb], writes=[self.ones_f])
        c.dma("sp", self.triu_f[:, :], cm[2], reads=[cb], writes=[self.triu_f])
        c.dma("sp", self.ident_f[:, :], cm[0], reads=[cb], writes=[self.ident_f])
        c.dma("sp", self.negm_f[:, :], cm[3], reads=[cb], writes=[self.negm_f])
        c.dma("pool", self.tri01_b[:, :], cm[2], reads=[cb], writes=[self.tri01_b])
        self.NCV = DEPTH * CV_PER_LAYER + 8
        cv = self.dr("cvec", [128, self.NCV], F32, kind="ExternalInput")
        self.cvec = c.sb(es, "cvec", [128, self.NCV], F32)
        c.dma("sp", self.cvec[:, :], cv[:, :], reads=[self.db("cvec")], writes=[self.cvec])

    def rmsnorm_group(self, xg, sq, hT_ap_fn, hT_buf, rstd, gcol, psb, nch=8, ncols=512):
        c = self.c
        c.op("act", lambda h: h.activation(out=sq[:, :, :], in_=xg[:, :, :], func=AF.Square), reads=[xg], writes=[sq])
        for k in range(nch):
            c.op("pe", lambda h, k=k: h.matmul(psb[:, :ncols], lhsT=self.ones_b[:, :], rhs=sq[:, k, :], start=(k == 0), stop=(k == nch - 1)),
                 reads=[self.ones_b, sq], writes=[psb], sig=(k == nch - 1))
        c.op("dve", lambda h: h.tensor_scalar(out=rstd[:, :], in0=psb[:, :ncols], scalar1=1.0 / (nch * 128), scalar2=1e-6, op0=ALU.mult, op1=ALU.add),
             reads=[psb], writes=[rstd])
        c.op("act", lambda h: h.activation(out=rstd[:, :], in_=rstd[:, :], func=AF.Ln), reads=[rstd], writes=[rstd])
        c.op("act", lambda h: h.activation(out=rstd[:, :], in_=rstd[:, :], func=AF.Exp, scale=-0.5), reads=[rstd], writes=[rstd])
        for k in range(nch):
            c.op("dve", lambda h, k=k: h.scalar_tensor_tensor(out=hT_ap_fn(k), in0=xg[:, k, :], scalar=self.cvec[:, gcol + k:gcol + k + 1],
                                                            in1=rstd[:, :], op0=ALU.mult, op1=ALU.mult),
                 reads=[xg, rstd, self.cvec], writes=[hT_buf])

    def ffn_phase(self, xsrc, xdst, w_in, w_out, gcol):
        c, S = self.c, self.S
        with ExitStack() as es:
            win = c.sb(es, "win", [128, 8, 4096], BF16)
            wout = c.sb(es, "wout", [128, 16, 1024], BF16)
            xgs = [c.sb(es, "xg", [128, 8, 512], F32) for _ in range(2)]
            sq = c.sb(es, "sq", [128, 8, 512], BF16)
            hT = c.sb(es, "hT", [128, 8, 512], BF16)
            act = c.sb(es, "actT", [128, 16, 512], BF16)
            rstd = c.sb(es, "rstd", [128, 512], F32)
            sgs = [c.sb(es, "sg", [128, 512], F32) for _ in range(2)]
            wb = self.db("w")
            for k in range(8):
                c.dma("pool", win[:, k, :], w_in[k * 128:(k + 1) * 128, :], reads=[wb], writes=[win])
            for k in range(16):
                c.dma("pool", wout[:, k, :], w_out[k * 128:(k + 1) * 128, :], reads=[wb], writes=[wout])
            xs3 = xsrc.rearrange("(c p) t -> p c t", p=128)
            xd3 = xdst.rearrange("(c p) t -> p c t", p=128)
            ps = self.ps
            for g in range(self.NG):
                xg = xgs[g % 2]
                tsl = slice(g * 512, (g + 1) * 512)
                c.dma("sp", xg[:, :, :], xs3[:, :, tsl], reads=[self.db(xsrc.name, g)], writes=[xg])
                self.rmsnorm_group(xg, sq, lambda k: hT[:, k, :], hT, rstd, gcol, ps[0])
                for j in range(16):
                    pg, pu = ps[1 + 2 * (j % 2)], ps[2 + 2 * (j % 2)]
                    for k in range(8):
                        c.op("pe", lambda h, k=k, j=j, pg=pg: h.matmul(pg[:, :], lhsT=win[:, k, j * 128:(j + 1) * 128], rhs=hT[:, k, :], start=(k == 0), stop=(k == 7)),
                             reads=[win, hT], writes=[pg], sig=(k == 7))
                    for k in range(8):
                        c.op("pe", lambda h, k=k, j=j, pu=pu: h.matmul(pu[:, :], lhsT=win[:, k, 2048 + j * 128:2048 + (j + 1) * 128], rhs=hT[:, k, :], start=(k == 0), stop=(k == 7)),
                             reads=[win, hT], writes=[pu], sig=(k == 7))
                    sg = sgs[j % 2]
                    c.op("act", lambda h, pg=pg, sg=sg: h.activation(out=sg[:, :], in_=pg[:, :], func=AF.Silu), reads=[pg], writes=[sg])
                    c.op("dve", lambda h, pu=pu, sg=sg, j=j: h.tensor_tensor(out=act[:, j, :], in0=sg[:, :], in1=pu[:, :], op=ALU.mult), reads=[sg, pu], writes=[act])
                for d in range(8):
                    po = ps[5 + d % 2]
                    for j in range(16):
                        c.op("pe", lambda h, d=d, j=j, po=po: h.matmul(po[:, :], lhsT=wout[:, j, d * 128:(d + 1) * 128], rhs=act[:, j, :], start=(j == 0), stop=(j == 15)),
                             reads=[wout, act], writes=[po], sig=(j == 15))
                    c.op("dve", lambda h, d=d, po=po, xg=xg: h.scalar_tensor_tensor(out=xg[:, d, :], in0=po[:, :], scalar=0.5, in1=xg[:, d, :], op0=ALU.mult, op1=ALU.add),
                         reads=[po, xg], writes=[xg])
                c.dma("sp", xd3[:, :, tsl], xg[:, :, :], reads=[xg], writes=[self.db(xdst.name, g)])
            c.barrier()


CV_FFN1, CV_MIX, CV_FFN2, CV_BG, CV_CONV, CV_DN, CV_BF, CV_ALOG, CV_DT = 0, 8, 16, 24, 48, 96, 97, 105, 109
CV_PER_LAYER = 113


def build_cvec(inp):
    cv = np.zeros((128, DEPTH * CV_PER_LAYER + 8), np.float32)
    for l in range(DEPTH):
        b = l * CV_PER_LAYER
        cv[:, b + CV_FFN1:b + CV_FFN1 + 8] = inp["ffn1_norm"][l].reshape(8, 128).T
        cv[:, b + CV_MIX:b + CV_MIX + 8] = inp["mix_norm"][l].reshape(8, 128).T
        cv[:, b + CV_FFN2:b + CV_FFN2 + 8] = inp["ffn2_norm"][l].reshape(8, 128).T
        cv[:, b + CV_BG:b + CV_BG + 24] = inp["b_gate"][l].reshape(24, 128).T
        cv[:, b + CV_CONV:b + CV_CONV + 48] = inp["conv_w"][l].reshape(4, 12, 128).transpose(2, 1, 0).reshape(128, 48)
        cv[:, b + CV_DN] = inp["delta_norm"][l]
        cv[:, b + CV_BF:b + CV_BF + 8] = inp["b_forget"][l][None, :]
        cv[:, b + CV_ALOG:b + CV_ALOG + 4] = inp["a_log"][l][None, :]
        cv[:, b + CV_DT:b + CV_DT + 4] = inp["dt_bias"][l][None, :]
    cv[:, DEPTH * CV_PER_LAYER:] = inp["final_norm"].reshape(8, 128).T
    return cv


def build_cmat():
    i = np.arange(128)
    ident = np.eye(128, dtype=np.float32)
    ones = np.ones((128, 128), np.float32)
    triu = (i[:, None] <= i[None, :]).astype(np.float32)
    negm = np.where(i[None, :] >= i[:, None], 0.0, -1e4).astype(np.float32)
    negc = np.where(i[None, :] <= i[:, None], 0.0, -1e30).astype(np.float32)
    z = np.zeros((128, 128), np.float32)
    strictu = (i[:, None] < i[None, :]).astype(np.float32)
    return np.stack([ident, ones, triu, negm, negc, strictu, z])


def build_pow2(nit=26):
    return np.tile((0.5 ** np.arange(1, nit + 1)).astype(np.float32)[None, :], (128, 1))


def swap_cols():
    idx = []
    for base, n in ((O_AQ, 512), (O_AK, 64), (O_IQ, 256), (O_IK, 64)):
        for j in range(n):
            d = j % 64
            hb = base + (j // 64) * 64
            if d < 8:
                idx.append(hb + d + 8)
            elif d < 16:
                idx.append(hb + d - 8)
            else:
                idx.append(hb + d)
    return np.array(idx)


def small_cols():
    return np.concatenate([np.arange(O_AV, O_AV + 64), np.arange(O_IW, O_IW + 4), np.arange(O_BF, O_BF + 8),
                           np.arange(O_CB, O_CB + 4), np.arange(O_CA, O_CA + 4)])


def build_rot(S):
    pos = np.arange(S, dtype=np.float32)
    inv = np.power(np.float32(500000.0), -np.arange(0, 16, 2, dtype=np.float32) / np.float32(16)).astype(np.float32)
    ang = (pos[:, None] * inv[None, :]).astype(np.float32)
    cos, sin = np.cos(ang).astype(np.float32), np.sin(ang).astype(np.float32)
    C = np.ones((128, S), np.float32)
    Sg = np.zeros((128, S), np.float32)
    for p in range(128):
        d = p % 64
        if d < 8:
            C[p] = cos[:, d]
            Sg[p] = -sin[:, d]
        elif d < 16:
            C[p] = cos[:, d - 8]
            Sg[p] = sin[:, d - 8]
    return C, Sg


class ProgM1(Prog):
    def declare_scratch(self):
        S = self.S
        d = self.dr
        self.qaT = d("qaT", [512, S], BF16); self.kaT = d("kaT", [64, S], BF16)
        self.qiT = d("qiT", [256, S], BF16); self.kiT = d("kiT", [64, S], BF16)
        self.vA = d("vA", [S, 64], BF16)
        self.qbT = d("qbT", [512, S], BF16); self.kbT = d("kbT", [512, S], BF16); self.vB = d("vB", [S, 512], BF16)
        self.qcT = d("qcT", [512, S], BF16); self.kcT = d("kcT", [512, S], BF16)
        self.kC = d("kC", [S, 512], BF16); self.vC = d("vC", [S, 512], BF16)
        self.szT = d("szT", [512, S], BF16)
        self.smallD = d("smallD", [S, 24], F32)
        self.gT = d("gT", [3072, S], BF16)
        self.yT = d("yT", [3, 512, S], BF16)

    def rot(self):
        self._rot = (getattr(self, "_rot", -1) + 1) % 8
        return self.ps[self._rot]

    def m1_phase(self, xT, w_in, w_sw, w_small, cvb):
        c, S, NG = self.c, self.S, self.NG
        rotC = self.dram["rotC"]; rotS = self.dram["rotS"]
        with ExitStack() as es:
            hT = c.sb(es, "hTall", [128, 8, S], BF16)
            wts = [c.sb(es, "wt", [128, 8, 512], BF16) for _ in range(3)]
            wti = [0]
            wb = self.db("w")

            def load_w(src_list):
                wt = wts[wti[0] % 3]
                wti[0] += 1
                for (ap, c0, n) in src_list:
                    c.dma("pool", wt[:, :, c0:c0 + n], ap.rearrange("(k p) n -> p k n", p=128), reads=[wb], writes=[wt])
                return wt

            def fm(wt, c0, M, g, psb, rows0=0):
                tsl = slice(g * 512, (g + 1) * 512)
                for k in range(8):
                    c.op("pe", lambda h, k=k: h.matmul(psb[rows0:rows0 + M, :], lhsT=wt[:, k, c0:c0 + M], rhs=hT[:, k, tsl], start=(k == 0), stop=(k == 7)),
                         reads=[wt, hT], writes=[psb], sig=(k == 7))

            with ExitStack() as es2:
                xgs = [c.sb(es2, "xg", [128, 8, 512], F32) for _ in range(2)]
                sq = c.sb(es2, "sq", [128, 8, 512], BF16)
                rstd = c.sb(es2, "rstd", [128, 512], F32)
                x3 = xT.rearrange("(c p) t -> p c t", p=128)
                for g in range(NG):
                    xg = xgs[g % 2]
                    tsl = slice(g * 512, (g + 1) * 512)
                    c.dma("sp", xg[:, :, :], x3[:, :, tsl], reads=[self.db(xT.name, g)], writes=[xg])
                    self.rmsnorm_group(xg, sq, lambda k, tsl=tsl: hT[:, k, tsl], hT, rstd, cvb + CV_MIX, self.rot())
                c.barrier()

            with ExitStack() as es2:
                stg = [c.sb(es2, "stg", [128, 4, 512], BF16) for _ in range(2)]
                stgi = [0]
                t1s = [c.sb(es2, "t1", [128, 512], F32) for _ in range(2)]
                t2s = [c.sb(es2, "t2", [128, 512], F32) for _ in range(2)]
                rc = [c.sb(es2, "rc", [128, 512], F32) for _ in range(2)]
                rs = [c.sb(es2, "rs", [128, 512], F32) for _ in range(2)]

                def nstg():
                    stgi[0] += 1
                    return stg[stgi[0] % 2]

                def load_rot(g):
                    tsl = slice(g * 512, (g + 1) * 512)
                    c.dma("sp", rc[g % 2][:, :], rotC[:, tsl], reads=[self.db("rot")], writes=[rc[g % 2]])
                    c.dma("sp", rs[g % 2][:, :], rotS[:, tsl], reads=[self.db("rot")], writes=[rs[g % 2]])

                def rotary(pn, psw, g, out_ap, out_buf, i):
                    t1, t2 = t1s[i % 2], t2s[i % 2]
                    c.op("dve", lambda h: h.tensor_tensor(out=t1[:, :], in0=pn[:, :], in1=rc[g % 2][:, :], op=ALU.mult), reads=[pn, rc[g % 2]], writes=[t1])
                    c.op("dve", lambda h: h.tensor_tensor(out=t2[:, :], in0=psw[:, :], in1=rs[g % 2][:, :], op=ALU.mult), reads=[psw, rs[g % 2]], writes=[t2])
                    c.op("pool", lambda h: h.tensor_tensor(out=out_ap, in0=t1[:, :], in1=t2[:, :], op=ALU.add), reads=[t1, t2], writes=[out_buf])

                wn = load_w([(w_in[:, O_AQ:O_AQ + 512], 0, 512)])
                ws = load_w([(w_sw[:, 0:512], 0, 512)])
                for g in range(NG):
                    tsl = slice(g * 512, (g + 1) * 512)
                    load_rot(g)
                    so = nstg()
                    for ch in range(4):
                        pn, psw = self.rot(), self.rot()
                        fm(wn, ch * 128, 128, g, pn)
                        fm(ws, ch * 128, 128, g, psw)
                        rotary(pn, psw, g, so[:, ch, :], so, ch)
                    c.dma("sp", self.qaT.rearrange("(c p) t -> p c t", p=128)[:, :, tsl], so[:, :, :], reads=[so], writes=[self.db("qaT", g)])
                wn = load_w([(w_in[:, O_IQ:O_IQ + 256], 0, 256), (w_in[:, O_AK:O_AK + 64], 256, 64), (w_in[:, O_IK:O_IK + 64], 320, 64)])
                ws = load_w([(w_sw[:, 576:832], 0, 256), (w_sw[:, 512:576], 256, 64), (w_sw[:, 832:896], 320, 64)])
                for g in range(NG):
                    tsl = slice(g * 512, (g + 1) * 512)
                    load_rot(g)
                    so = nstg()
                    for ch in range(3):
                        pn, psw = self.rot(), self.rot()
                        fm(wn, ch * 128, 128, g, pn)
                        fm(ws, ch * 128, 128, g, psw)
                        rotary(pn, psw, g, so[:, ch, :], so, ch)
                    c.dma("sp", self.qiT.rearrange("(c p) t -> p c t", p=128)[:, :, tsl], so[:, 0:2, :], reads=[so], writes=[self.db("qiT", g)])
                    c.dma("sp", self.kaT[:, tsl], so[0:64, 2, :], reads=[so], writes=[self.db("kaT", g)])
                    c.dma("sp", self.kiT[:, tsl], so[64:128, 2, :], reads=[so], writes=[self.db("kiT", g)])
                for (c0, dst) in ((O_BQKV, self.qbT), (O_BQKV + 512, self.kbT)):
                    wn = load_w([(w_in[:, c0:c0 + 512], 0, 512)])
                    for g in range(NG):
                        tsl = slice(g * 512, (g + 1) * 512)
                        so = nstg()
                        for ch in range(4):
                            pn = self.rot()
                            fm(wn, ch * 128, 128, g, pn)
                            c.op("act", lambda h, pn=pn, ch=ch, so=so: h.activation(out=so[:, ch, :], in_=pn[:, :], func=AF.Copy), reads=[pn], writes=[so])
                        c.dma("sp", dst.rearrange("(c p) t -> p c t", p=128)[:, :, tsl], so[:, :, :], reads=[so], writes=[self.db(dst.name, g)])
                wn = load_w([(w_in[:, O_BQKV + 1024:O_BQKV + 1536], 0, 512)])
                for g in range(NG):
                    so = nstg()
                    for tt in range(4):
                        pn = self.rot()
                        t0 = g * 512 + tt * 128
                        for k in range(8):
                            c.op("pe", lambda h, k=k, pn=pn, t0=t0: h.matmul(pn[:, :], lhsT=hT[:, k, t0:t0 + 128], rhs=wn[:, k, :], start=(k == 0), stop=(k == 7)),
                                 reads=[wn, hT], writes=[pn], sig=(k == 7))
                        c.op("act", lambda h, pn=pn, tt=tt, so=so: h.activation(out=so[:, tt, :], in_=pn[:, :], func=AF.Copy), reads=[pn], writes=[so])
                    c.dma("sp", self.vB.rearrange("(n p) d -> p n d", p=128)[:, g * 4:(g + 1) * 4, :], so[:, :, :], reads=[so], writes=[self.db("vB", g)])
                with ExitStack() as es3:
                    xcs = [c.sb(es3, "xc", [128, 515], F32) for _ in range(4)]
                    accs = [c.sb(es3, "acc", [128, 512], F32) for _ in range(2)]
                    sls = [c.sb(es3, "sl", [128, 512], F32) for _ in range(2)]
                    sqb = [c.sb(es3, "sqb", [128, 512], BF16) for _ in range(2)]
                    rr = [c.sb(es3, "rr", [128, 512], F32) for _ in range(2)]
                    tok = [c.sb(es3, "tok", [128, 4, 512], BF16) for _ in range(2)]
                    for sec, (dstT, dstTok) in enumerate(((self.qcT, None), (self.kcT, self.kC), (None, self.vC))):
                        c0 = O_CQKV + sec * 512
                        wn = load_w([(w_in[:, c0:c0 + 512], 0, 512)])
                        for j in range(4):
                            c.op("pool", lambda h, j=j: h.memset(xcs[j][:, 0:3], 0.0), writes=[xcs[j]])
                        for g in range(NG):
                            tsl = slice(g * 512, (g + 1) * 512)
                            so = nstg()
                            for j in range(4):
                                ch = sec * 4 + j
                                pn = self.rot()
                                fm(wn, j * 128, 128, g, pn)
                                xc = xcs[j]
                                acc, sl = accs[j % 2], sls[j % 2]
                                wc = cvb + CV_CONV + ch * 4
                                c.op("act", lambda h, pn=pn, xc=xc: h.activation(out=xc[:, 3:515], in_=pn[:, :], func=AF.Copy), reads=[pn], writes=[xc])
                                c.op("dve", lambda h, xc=xc, acc=acc, wc=wc: h.tensor_scalar(out=acc[:, :], in0=xc[:, 3:515], scalar1=self.cvec[:, wc + 3:wc + 4], scalar2=None, op0=ALU.mult),
                                     reads=[xc, self.cvec], writes=[acc])
                                for tap in (2, 1, 0):
                                    c.op("dve", lambda h, xc=xc, acc=acc, wc=wc, tap=tap: h.scalar_tensor_tensor(out=acc[:, :], in0=xc[:, tap:tap + 512], scalar=self.cvec[:, wc + tap:wc + tap + 1],
                                                                                                         in1=acc[:, :], op0=ALU.mult, op1=ALU.add),
                                         reads=[xc, acc, self.cvec], writes=[acc])
                                c.op("pool", lambda h, xc=xc: h.tensor_copy(out=xc[:, 0:3], in_=xc[:, 512:515]), reads=[xc], writes=[xc])
                                if sec == 2:
                                    c.op("act", lambda h, acc=acc, so=so, j=j: h.activation(out=so[:, j, :], in_=acc[:, :], func=AF.Silu), reads=[acc], writes=[so])
                                else:
                                    c.op("act", lambda h, acc=acc, sl=sl: h.activation(out=sl[:, :], in_=acc[:, :], func=AF.Silu), reads=[acc], writes=[sl])
                                    sb_, r_ = sqb[j % 2], rr[j % 2]
                                    c.op("act", lambda h, sl=sl, sb_=sb_: h.activation(out=sb_[:, :], in_=sl[:, :], func=AF.Square), reads=[sl], writes=[sb_])
                                    p2 = self.rot()
                                    c.op("pe", lambda h, p2=p2, sb_=sb_: h.matmul(p2[:, :], lhsT=self.ones_b[:, :], rhs=sb_[:, :], start=True, stop=True), reads=[self.ones_b, sb_], writes=[p2])
                                    c.op("dve", lambda h, p2=p2, r_=r_: h.tensor_scalar(out=r_[:, :], in0=p2[:, :], scalar1=1e-6, scalar2=None, op0=ALU.add), reads=[p2], writes=[r_])
                                    c.op("act", lambda h, r_=r_: h.activation(out=r_[:, :], in_=r_[:, :], func=AF.Ln), reads=[r_], writes=[r_])
                                    c.op("act", lambda h, r_=r_: h.activation(out=r_[:, :], in_=r_[:, :], func=AF.Exp, scale=-0.5), reads=[r_], writes=[r_])
                                    qs = float(128 ** -0.5) if sec == 0 else 1.0
                                    c.op("dve", lambda h, sl=sl, r_=r_, so=so, j=j, qs=qs: h.scalar_tensor_tensor(out=so[:, j, :], in0=sl[:, :], scalar=qs, in1=r_[:, :], op0=ALU.mult, op1=ALU.mult),
                                         reads=[sl, r_], writes=[so])
                            if dstT is not None:
                                c.dma("sp", dstT.rearrange("(c p) t -> p c t", p=128)[:, :, tsl], so[:, :, :], reads=[so], writes=[self.db(dstT.name, g)])
                            if dstTok is not None:
                                tk = tok[g % 2]
                                for tt in range(4):
                                    pt = self.rot()
                                    for j in range(4):
                                        c.op("pe", lambda h, pt=pt, j=j, tt=tt, so=so: h.matmul(pt[:, j * 128:(j + 1) * 128], lhsT=so[:, j, tt * 128:(tt + 1) * 128], rhs=self.ident_b[:, :], start=True, stop=True),
                                             reads=[so, self.ident_b], writes=[pt], sig=(j == 3))
                                    c.op("act", lambda h, pt=pt, tk=tk, tt=tt: h.activation(out=tk[:, tt, :], in_=pt[:, :], func=AF.Copy), reads=[pt], writes=[tk])
                                c.dma("sp", dstTok.rearrange("(n p) d -> p n d", p=128)[:, g * 4:(g + 1) * 4, :], tk[:, :, :], reads=[tk], writes=[self.db(dstTok.name, g)])
                wn = load_w([(w_in[:, O_CZ:O_CZ + 512], 0, 512)])
                for g in range(NG):
                    tsl = slice(g * 512, (g + 1) * 512)
                    so = nstg()
                    for ch in range(4):
                        pn = self.rot()
                        fm(wn, ch * 128, 128, g, pn)
                        c.op("act", lambda h, pn=pn, ch=ch, so=so: h.activation(out=so[:, ch, :], in_=pn[:, :], func=AF.Silu), reads=[pn], writes=[so])
                    c.dma("sp", self.szT.rearrange("(c p) t -> p c t", p=128)[:, :, tsl], so[:, :, :], reads=[so], writes=[self.db("szT", g)])
                for sec in range(6):
                    wn = load_w([(w_in[:, O_G + sec * 512:O_G + (sec + 1) * 512], 0, 512)])
                    for g in range(NG):
                        tsl = slice(g * 512, (g + 1) * 512)
                        so = nstg()
                        for ch in range(4):
                            pn = self.rot()
                            fm(wn, ch * 128, 128, g, pn)
                            bc = cvb + CV_BG + sec * 4 + ch
                            c.op("act", lambda h, pn=pn, ch=ch, so=so, bc=bc: h.activation(out=so[:, ch, :], in_=pn[:, :], func=AF.Sigmoid, bias=self.cvec[:, bc:bc + 1]),
                                 reads=[pn, self.cvec], writes=[so])
                        c.dma("sp", self.gT.rearrange("(c p) t -> p c t", p=128)[:, sec * 4:(sec + 1) * 4, tsl], so[:, :, :], reads=[so], writes=[self.db("gT", sec * 100 + g)])
                with ExitStack() as es3:
                    wsm = c.sb(es3, "wsm", [128, 8, 84], BF16)
                    c.dma("pool", wsm[:, :, :], w_small.rearrange("(k p) n -> p k n", p=128), reads=[wb], writes=[wsm])
                    negA = c.sb(es3, "negA", [128, 4], F32)
                    c.op("act", lambda h: h.activation(out=negA[:, :], in_=self.cvec[:, cvb + CV_ALOG:cvb + CV_ALOG + 4], func=AF.Exp), reads=[self.cvec], writes=[negA])
                    c.op("dve", lambda h: h.tensor_scalar(out=negA[:, :], in0=negA[:, :], scalar1=-1.0, scalar2=None, op0=ALU.mult), reads=[negA], writes=[negA])
                    sms = [c.sb(es3, "sm", [128, 4, 24], F32) for _ in range(2)]
                    vas = [c.sb(es3, "vas", [128, 4, 64], BF16) for _ in range(2)]
                    tmp = [c.sb(es3, "tmps", [128, 16], F32) for _ in range(2)]
                    for g in range(NG):
                        sm, va = sms[g % 2], vas[g % 2]
                        for tt in range(4):
                            pn = self.rot()
                            t0 = g * 512 + tt * 128
                            tp = tmp[tt % 2]
                            for k in range(8):
                                c.op("pe", lambda h, k=k, pn=pn, t0=t0: h.matmul(pn[:, 0:84], lhsT=hT[:, k, t0:t0 + 128], rhs=wsm[:, k, :], start=(k == 0), stop=(k == 7)),
                                     reads=[wsm, hT], writes=[pn], sig=(k == 7))
                            c.op("act", lambda h, pn=pn, va=va, tt=tt: h.activation(out=va[:, tt, :], in_=pn[:, 0:64], func=AF.Copy), reads=[pn], writes=[va])
                            c.op("dve", lambda h, pn=pn, sm=sm, tt=tt: h.tensor_scalar(out=sm[:, tt, 0:4], in0=pn[:, 64:68], scalar1=1.0 / 16.0, scalar2=None, op0=ALU.mult), reads=[pn], writes=[sm])
                            c.op("dve", lambda h, pn=pn, tp=tp: h.tensor_tensor(out=tp[:, 0:8], in0=pn[:, 68:76], in1=self.cvec[:, cvb + CV_BF:cvb + CV_BF + 8], op=ALU.add), reads=[pn, self.cvec], writes=[tp])
                            c.op("dve", lambda h, pn=pn, tp=tp: h.tensor_tensor(out=tp[:, 8:12], in0=pn[:, 80:84], in1=self.cvec[:, cvb + CV_DT:cvb + CV_DT + 4], op=ALU.add), reads=[pn, self.cvec, tp], writes=[tp])
                            c.op("act", lambda h, tp=tp: h.activation(out=tp[:, 0:8], in_=tp[:, 0:8], func=AF.Exp, scale=-1.0), reads=[tp], writes=[tp])
                            c.op("act", lambda h, tp=tp: h.activation(out=tp[:, 8:12], in_=tp[:, 8:12], func=AF.Exp), reads=[tp], writes=[tp])
                            c.op("act", lambda h, tp=tp: h.activation(out=tp[:, 0:12], in_=tp[:, 0:12], func=AF.Ln, bias=1.0), reads=[tp], writes=[tp])
                            c.op("dve", lambda h, tp=tp, sm=sm, tt=tt: h.tensor_scalar(out=sm[:, tt, 4:12], in0=tp[:, 0:8], scalar1=-1.0, scalar2=None, op0=ALU.mult), reads=[tp], writes=[sm])
                            c.op("dve", lambda h, tp=tp, sm=sm, tt=tt: h.tensor_tensor(out=sm[:, tt, 16:20], in0=tp[:, 8:12], in1=negA[:, :], op=ALU.mult), reads=[tp, negA, sm], writes=[sm])
                            c.op("act", lambda h, pn=pn, sm=sm, tt=tt: h.activation(out=sm[:, tt, 12:16], in_=pn[:, 76:80], func=AF.Sigmoid), reads=[pn, sm], writes=[sm])
                        c.dma("sp", self.smallD.rearrange("(n p) c -> p n c", p=128)[:, g * 4:(g + 1) * 4, 0:20], sm[:, :, 0:20], reads=[sm], writes=[self.db("smallD", g)])
                        c.dma("sp", self.vA.rearrange("(n p) d -> p n d", p=128)[:, g * 4:(g + 1) * 4, :], va[:, :, :], reads=[va], writes=[self.db("vA", g)])
                c.barrier()


class ProgAB(ProgM1):
    def rotset(self, key, banks):
        d = self.__dict__.setdefault("_rs", {})
        d[key] = (d.get(key, -1) + 1) % len(banks)
        return self.ps[banks[d[key]]]

    def _b_setup(self, es, lbanks, pbanks):
        c, S, NT, NG = self.c, self.S, self.NT, self.NG
        qbh = [c.sb(es, "qbh", [128, S], BF16) for _ in range(2)]
        kbh = [c.sb(es, "kbh", [128, S], BF16) for _ in range(2)]
        vext = [c.sb(es, "vext", [128, NT, 128], BF16) for _ in range(2)]
        lf = c.sb(es, "lf", [128, NT, 8], F32)
        lfacc = c.sb(es, "lfacc", [128, NT + 1, 8], F32)
        csb = c.sb(es, "csb", [128, NT, 8], F32)
        carry = c.sb(es, "carry", [128, NT, 8], F32)
        npair = NT * (NT + 1) // 2
        bias = c.sb(es, "biasall", [128, npair, 8], F32)
        pts = [c.sb(es, "pt", [128, 512], BF16) for _ in range(4)]
        ptq = [[Buf(name=f"ptq{a}_{b}") for b in range(4)] for a in range(4)]
        rsum = [c.sb(es, "rsum", [64, 512], F32) for _ in range(2)]
        nums = [c.sb(es, "bnum", [64, 512], F32) for _ in range(2)]
        yst = [c.sb(es, "yst", [64, 512], BF16) for _ in range(2)]
        allg = lambda nm: [self.db(nm, g) for g in range(NG)]
        c.dma("sp", lf[:, :, :], self.smallD.rearrange("(n p) c -> p n c", p=128)[:, :, 4:12], reads=allg("smallD"), writes=[lf])
        for v in vext:
            c.op("pool", lambda h, v=v: h.memset(v[:, :, 64:128], 1.0), writes=[v])
        c.op("pool", lambda h: h.memset(lfacc[:, 0, :], 0.0), writes=[lfacc])
        for n in range(NT):
            c.op("dve", lambda h, n=n: h.tensor_tensor(out=lfacc[:, n + 1, :], in0=lfacc[:, n, :], in1=lf[:, n, :], op=ALU.add), reads=[lfacc, lf], writes=[lfacc])
        for n in range(NT):
            pb = self.rotset("bpl", lbanks)
            c.op("pe", lambda h, n=n, pb=pb: h.matmul(pb[:, 0:8], lhsT=self.triu_f[:, :], rhs=lf[:, n, :], start=True, stop=False), reads=[self.triu_f, lf], writes=[pb], sig=False)
            c.op("pe", lambda h, n=n, pb=pb: h.matmul(pb[:, 0:8], lhsT=self.ones_f[:, :], rhs=lfacc[:, n, :], start=False, stop=True), reads=[self.ones_f, lfacc], writes=[pb])
            c.op("act", lambda h, n=n, pb=pb: h.activation(out=csb[:, n, :], in_=pb[:, 0:8], func=AF.Copy), reads=[pb], writes=[csb])
            pb = self.rotset("bpl", lbanks)
            c.op("pe", lambda h, n=n, pb=pb: h.matmul(pb[:, 0:8], lhsT=self.ones_f[:, :], rhs=lfacc[:, n, :], start=True, stop=True), reads=[self.ones_f, lfacc], writes=[pb])
            c.op("act", lambda h, n=n, pb=pb: h.activation(out=carry[:, n, :], in_=pb[:, 0:8], func=AF.Copy), reads=[pb], writes=[carry])
        pidx = {}
        pi = 0
        for i in range(NT):
            for j in range(i + 1):
                pidx[(i, j)] = pi
                c.op("pool", lambda h, i=i, j=j, pi=pi: h.tensor_tensor(out=bias[:, pi, :], in0=carry[:, i, :], in1=csb[:, j, :], op=ALU.subtract), reads=[carry, csb], writes=[bias])
                pi += 1
        vB3 = self.vB.rearrange("(n p) d -> p n d", p=128)
        yb = self.yT[1]
        steps = [(hh, g, j) for hh in range(8) for g in range(NG) for j in range(4 * g + 4)]
        pls = {}
        loaded = set()

        def load_pair(hp):
            if hp in loaded or hp >= 4:
                return
            loaded.add(hp)
            c.dma("sp", qbh[hp % 2][:, :], self.qbT[hp * 128:(hp + 1) * 128, :], reads=allg("qbT"), writes=[qbh[hp % 2]])
            c.dma("sp", kbh[hp % 2][:, :], self.kbT[hp * 128:(hp + 1) * 128, :], reads=allg("kbT"), writes=[kbh[hp % 2]])

        def logits(k):
            hh, g, j = steps[k]
            hb, hp = (hh % 2) * 64, hh // 2
            load_pair(hp)
            qb, kb = qbh[hp % 2], kbh[hp % 2]
            col0 = max(j - 4 * g, 0) * 128
            pl = self.rotset("bpl", lbanks)
            pls[k] = pl
            c.op("pe", lambda h: h.matmul(pl[:, col0:512], lhsT=kb[hb:hb + 64, j * 128:(j + 1) * 128],
                                          rhs=qb[hb:hb + 64, g * 512 + col0:(g + 1) * 512], start=True, stop=True),
                 reads=[kb, qb], writes=[pl])

        def gen():
            po = None
            logits(0)
            for k, (hh, g, j) in enumerate(steps):
                ve = vext[hh % 2]
                if g == 0 and j == 0:
                    c.dma("sp", ve[:, :, 0:64], vB3[:, :, hh * 64:(hh + 1) * 64], reads=allg("vB"), writes=[ve])
                if j == 0:
                    po = self.rotset("bpo", pbanks)
                if k + 1 < len(steps):
                    logits(k + 1)
                nj = 4 * g + 4
                r = j - 4 * g
                col0 = max(r, 0) * 128
                pl = pls.pop(k)
                pt = pts[j % 4]
                for qq in range(max(r, 0), 4):
                    pi = pidx[(4 * g + qq, j)]
                    qs_ = slice(qq * 128, (qq + 1) * 128)
                    c.op("act", lambda h, pl=pl, pt=pt, qs_=qs_, pi=pi, hh=hh: h.activation(out=pt[:, qs_], in_=pl[:, qs_], func=AF.Exp, scale=0.125, bias=bias[:, pi, hh:hh + 1]),
                         reads=[pl, bias], writes=[ptq[j % 4][qq]])
                if r >= 0:
                    c.op("pool", lambda h, pt=pt, col0=col0: h.tensor_tensor(out=pt[:, col0:col0 + 128], in0=pt[:, col0:col0 + 128], in1=self.tri01_b[:, :], op=ALU.mult),
                         reads=[ptq[j % 4][r], self.tri01_b], writes=[ptq[j % 4][r]])
                c.op("pe", lambda h, po=po, pt=pt, j=j, col0=col0, nj=nj, ve=ve: h.matmul(po[:, col0:512], lhsT=ve[:, j, :], rhs=pt[:, col0:512], start=(j == 0), stop=(j == nj - 1)),
                     reads=[ve] + ptq[j % 4][max(r, 0):4], writes=[po], sig=(j == nj - 1))
                if j == nj - 1:
                    rs_, ys_, nm_ = rsum[g % 2], yst[g % 2], nums[g % 2]
                    c.op("act", lambda h, po=po, rs_=rs_: h.activation(out=rs_[:, :], in_=po[64:128, :], func=AF.Copy), reads=[po], writes=[rs_])
                    c.op("act", lambda h, po=po, nm_=nm_: h.activation(out=nm_[:, :], in_=po[0:64, :], func=AF.Copy), reads=[po], writes=[nm_])
                    c.op("act", lambda h, rs_=rs_: h.activation(out=rs_[:, :], in_=rs_[:, :], func=AF.Ln), reads=[rs_], writes=[rs_])
                    c.op("act", lambda h, rs_=rs_: h.activation(out=rs_[:, :], in_=rs_[:, :], func=AF.Exp, scale=-1.0), reads=[rs_], writes=[rs_])
                    c.op("pool", lambda h, nm_=nm_, rs_=rs_, ys_=ys_: h.tensor_tensor(out=ys_[:, :], in0=nm_[:, :], in1=rs_[:, :], op=ALU.mult), reads=[nm_, rs_], writes=[ys_])
                    c.dma("sp", yb[hh * 64:(hh + 1) * 64, g * 512:(g + 1) * 512], ys_[:, :], reads=[ys_], writes=[self.db("yT1", hh * 100 + g)])
                yield k
        return gen(), len(steps)

    def b_phase(self):
        with ExitStack() as es:
            g, n = self._b_setup(es, [0, 1, 2, 3, 4, 5], [6, 7])
            for _ in g:
                pass
            self.c.barrier()

    NITER = 16
    TIE = True

    def _a_setup(self, es, sbanks, lpairs, pvpair):
        c, S, NT, NG = self.c, self.S, self.NT, self.NG
        NIT = self.NITER
        qi = c.sb(es, "qi", [128, 2, S], BF16)
        ki = c.sb(es, "ki", [128, S], BF16)
        ka = c.sb(es, "ka", [64, S], BF16)
        vext = c.sb(es, "vexta", [128, NT, 128], BF16)
        wi = c.sb(es, "wi", [128, NT, 4], F32)
        qat = [c.sb(es, "qat", [64, 1024], BF16) for _ in range(2)]
        score = c.sb(es, "score", [128, S], F32)
        isz = c.sb(es, "isz", [128, S], BF16)
        zrk = c.sb(es, "zrk", [128, S], BF16)
        maskb = [c.sb(es, "maskb", [128, S], BF16) for _ in range(2)]
        irep = c.sb(es, "irep", [128, 512], BF16)
        negc = c.sb(es, "negc", [128, 128], F32)
        pow2 = c.sb(es, "pow2", [128, NIT], F32)
        rts = [c.sb(es, "rt", [128, 512], F32) for _ in range(3)]
        pts = [c.sb(es, "pta", [128, 1024], BF16) for _ in range(3)]
        sm = c.sb(es, "bis", [128, 16], F32)
        steps = c.sb(es, "steps", [128, NIT], F32)
        rsum = c.sb(es, "rsuma", [64, 1024], F32)
        yst = [c.sb(es, "ysta", [64, 1024], BF16) for _ in range(2)]
        cm = self.dram["cmat"]
        cb = self.db("cmat")
        for r in range(4):
            c.dma("pool", irep[:, r * 128:(r + 1) * 128], cm[0], reads=[cb], writes=[irep])
        c.dma("sp", negc[:, :], cm[4], reads=[cb], writes=[negc])
        c.dma("sp", pow2[:, :], self.dram["pow2"][:, 0:NIT], reads=[self.db("pow2")], writes=[pow2])
        allg = lambda nm: [self.db(nm, g) for g in range(NG)]
        c.dma("sp", qi[:, :, :], self.qiT.rearrange("(hp p) t -> p hp t", p=128), reads=allg("qiT"), writes=[qi])
        c.dma("sp", ki[0:64, :], self.kiT[:, :], reads=allg("kiT"), writes=[ki])
        c.dma("sp", ki[64:128, :], self.kiT[:, :], reads=allg("kiT"), writes=[ki])
        c.dma("sp", ka[:, :], self.kaT[:, :], reads=allg("kaT"), writes=[ka])
        c.dma("sp", vext[:, :, 0:64], self.vA.rearrange("(n p) d -> p n d", p=128), reads=allg("vA"), writes=[vext])
        c.op("pool", lambda h: h.memset(vext[:, :, 64:128], 1.0), writes=[vext])
        c.dma("sp", wi[:, :, :], self.smallD.rearrange("(n p) c -> p n c", p=128)[:, :, 0:4], reads=allg("smallD"), writes=[wi])
        qa3 = self.qaT.rearrange("(h d) t -> d h t", d=64)
        ya = self.yT[0].rearrange("(h d) t -> d h t", d=64)
        NEGM = -32768.0

        def stage1(i):
            ncols = (i + 1) * 128
            qt = qat[i % 2]
            mb = maskb[i % 2]
            c.dma("sp", qt[:, :].rearrange("d (h t) -> d h t", h=8), qa3[:, :, i * 128:(i + 1) * 128], reads=allg("qaT"), writes=[qt])
            for s0 in range(0, ncols, 512):
                w = min(512, ncols - s0)
                for hd in range(4):
                    pl = self.rotset("apl", sbanks)
                    hb, hp = (hd % 2) * 64, hd // 2
                    c.op("pe", lambda h, pl=pl, hb=hb, hp=hp, s0=s0, w=w: h.matmul(pl[:, 0:w], lhsT=qi[hb:hb + 64, hp, i * 128:(i + 1) * 128], rhs=ki[hb:hb + 64, s0:s0 + w], start=True, stop=True),
                         reads=[qi, ki], writes=[pl])
                    if hd == 0:
                        c.op("dve", lambda h, pl=pl, s0=s0, w=w: h.tensor_scalar(out=score[:, s0:s0 + w], in0=pl[:, 0:w], scalar1=0.0, scalar2=wi[:, i, 0:1], op0=ALU.max, op1=ALU.mult),
                             reads=[pl, wi], writes=[score])
                    else:
                        rt = rts[hd - 1]
                        c.op("act", lambda h, pl=pl, rt=rt, w=w: h.activation(out=rt[:, 0:w], in_=pl[:, 0:w], func=AF.Relu), reads=[pl], writes=[rt])
                        c.op("dve", lambda h, rt=rt, hd=hd, s0=s0, w=w: h.scalar_tensor_tensor(out=score[:, s0:s0 + w], in0=rt[:, 0:w], scalar=wi[:, i, hd:hd + 1], in1=score[:, s0:s0 + w],
                                                                                            op0=ALU.mult, op1=ALU.add),
                             reads=[rt, wi, score], writes=[score])
            sc = score[:, 0:ncols]
            c.op("dve", lambda h: h.tensor_reduce(out=sm[:, 5:6], in_=sc, axis=AX.X, op=ALU.max), reads=[score], writes=[sm])
            c.op("dve", lambda h: h.tensor_reduce(out=sm[:, 6:7], in_=sc, axis=AX.X, op=ALU.min), reads=[score, sm], writes=[sm])
            c.op("dve", lambda h: h.tensor_tensor(out=score[:, i * 128:ncols], in0=score[:, i * 128:ncols], in1=negc[:, :], op=ALU.add), reads=[score, negc], writes=[score])
            c.op("dve", lambda h: h.tensor_scalar(out=sm[:, 0:1], in0=sm[:, 6:7], scalar1=-1.0, scalar2=None, op0=ALU.add), reads=[sm], writes=[sm])
            c.op("dve", lambda h: h.scalar_tensor_tensor(out=sm[:, 1:2], in0=sm[:, 5:6], scalar=1.0, in1=sm[:, 0:1], op0=ALU.add, op1=ALU.subtract), reads=[sm], writes=[sm])
            c.op("dve", lambda h: h.tensor_scalar(out=steps[:, :], in0=pow2[:, :], scalar1=sm[:, 1:2], scalar2=None, op0=ALU.mult), reads=[sm, pow2], writes=[steps])
            for k in range(NIT):
                c.op("dve", lambda h, k=k: h.tensor_tensor(out=sm[:, 2:3], in0=sm[:, 0:1], in1=steps[:, k:k + 1], op=ALU.add), reads=[sm, steps], writes=[sm])
                c.op("dve", lambda h: h.tensor_scalar(out=isz[:, 0:ncols], in0=sc, scalar1=sm[:, 2:3], scalar2=0.0, op0=ALU.is_ge, op1=ALU.add, accum_out=sm[:, 3:4]),
                     reads=[score, sm], writes=[isz, sm])
                c.op("dve", lambda h, k=k: h.scalar_tensor_tensor(out=sm[:, 4:5], in0=sm[:, 3:4], scalar=255.5, in1=steps[:, k:k + 1], op0=ALU.is_ge, op1=ALU.mult), reads=[sm, steps], writes=[sm])
                c.op("dve", lambda h: h.tensor_tensor(out=sm[:, 0:1], in0=sm[:, 0:1], in1=sm[:, 4:5], op=ALU.add), reads=[sm], writes=[sm])
            c.op("dve", lambda h: h.tensor_scalar(out=isz[:, 0:ncols], in0=sc, scalar1=0.0, scalar2=0.0, op0=ALU.is_gt, op1=ALU.add, accum_out=sm[:, 7:8]), reads=[score, sm], writes=[isz, sm])
            c.op("dve", lambda h: h.tensor_scalar(out=isz[:, 0:ncols], in0=sc, scalar1=0.0, scalar2=0.0, op0=ALU.is_equal, op1=ALU.add, accum_out=sm[:, 8:9]), reads=[score, sm], writes=[isz, sm])
            c.op("dve", lambda h: h.tensor_tensor(out=sm[:, 8:9], in0=sm[:, 8:9], in1=sm[:, 7:8], op=ALU.add), reads=[sm], writes=[sm])
            c.op("dve", lambda h: h.tensor_scalar(out=sm[:, 13:14], in0=sm[:, 7:8], scalar1=255.5, scalar2=None, op0=ALU.is_lt), reads=[sm], writes=[sm])
            c.op("dve", lambda h: h.scalar_tensor_tensor(out=sm[:, 9:10], in0=sm[:, 8:9], scalar=255.5, in1=sm[:, 13:14], op0=ALU.is_ge, op1=ALU.mult), reads=[sm], writes=[sm])
            c.op("dve", lambda h: h.tensor_scalar(out=sm[:, 10:11], in0=sm[:, 7:8], scalar1=-1.0, scalar2=256.5, op0=ALU.mult, op1=ALU.add), reads=[sm], writes=[sm])
            c.op("dve", lambda h: h.tensor_scalar(out=sm[:, 13:14], in0=sm[:, 9:10], scalar1=-1.0, scalar2=1.0, op0=ALU.mult, op1=ALU.add), reads=[sm], writes=[sm])
            c.op("dve", lambda h: h.tensor_tensor(out=sm[:, 13:14], in0=sm[:, 13:14], in1=sm[:, 0:1], op=ALU.mult), reads=[sm], writes=[sm])
            c.op("dve", lambda h: h.scalar_tensor_tensor(out=sm[:, 11:12], in0=sm[:, 9:10], scalar=1e-30, in1=sm[:, 13:14], op0=ALU.mult, op1=ALU.add), reads=[sm], writes=[sm])
            c.op("dve", lambda h: h.tensor_scalar(out=sm[:, 12:13], in0=sm[:, 9:10], scalar1=-NEGM, scalar2=None, op0=ALU.mult), reads=[sm], writes=[sm])
            c.op("dve", lambda h: h.tensor_tensor_scan(out=zrk[:, 0:ncols], data0=isz[:, 0:ncols], data1=isz[:, 0:ncols], initial=0.0, op0=ALU.add, op1=ALU.max), reads=[isz], writes=[zrk])
            c.op("dve", lambda h: h.scalar_tensor_tensor(out=isz[:, 0:ncols], in0=zrk[:, 0:ncols], scalar=sm[:, 10:11], in1=isz[:, 0:ncols], op0=ALU.is_le, op1=ALU.mult), reads=[zrk, sm, isz], writes=[isz])
            c.op("dve", lambda h, mb=mb: h.tensor_scalar(out=mb[:, 0:ncols], in0=sc, scalar1=sm[:, 11:12], scalar2=NEGM, op0=ALU.is_lt, op1=ALU.mult), reads=[score, sm], writes=[mb])
            if self.TIE:
                c.op("dve", lambda h, mb=mb: h.scalar_tensor_tensor(out=mb[:, 0:ncols], in0=isz[:, 0:ncols], scalar=sm[:, 12:13], in1=mb[:, 0:ncols], op0=ALU.mult, op1=ALU.add), reads=[isz, sm, mb], writes=[mb])

        def stage2(i):
            qt = qat[i % 2]
            mb = maskb[i % 2]
            po = (self.ps[pvpair[0]], self.ps[pvpair[1]])
            lb = [b_ for pr in lpairs for b_ in pr]
            nlb = len(lb)
            hsteps = [(j, half) for j in range(i + 1) for half in range(2)]

            def alog(k):
                j, half = hsteps[k]
                pl = self.ps[lb[k % nlb]]
                c.op("pe", lambda h: h.matmul(pl[:, :], lhsT=ka[:, j * 128:(j + 1) * 128], rhs=qt[:, half * 512:(half + 1) * 512], start=True, stop=False),
                     reads=[ka, qt], writes=[pl], sig=False)
                c.op("pe", lambda h: h.matmul(pl[:, :], lhsT=mb[:, j * 128:(j + 1) * 128], rhs=irep[:, :], start=False, stop=True),
                     reads=[mb, irep], writes=[pl])

            ptb = [[Buf(name=f"apt{a_}_{b_}") for b_ in range(2)] for a_ in range(3)]
            la = min(nlb - 1, 3)
            for k in range(min(la, len(hsteps))):
                alog(k)
            for k, (j, half) in enumerate(hsteps):
                if k + la < len(hsteps):
                    alog(k + la)
                pl = self.ps[lb[k % nlb]]
                pt = pts[j % 3]
                c.op("act", lambda h, pl=pl, half=half, pt=pt: h.activation(out=pt[:, half * 512:(half + 1) * 512], in_=pl[:, :], func=AF.Exp, scale=0.125), reads=[pl], writes=[self._aptb(pt, half)])
                c.op("pe", lambda h, half=half, j=j, pt=pt, po=po: h.matmul(po[half][:, :], lhsT=vext[:, j, :], rhs=pt[:, half * 512:(half + 1) * 512], start=(j == 0), stop=(j == i)),
                     reads=[vext, self._aptb(pt, half)], writes=[po[half]], sig=(j == i))
            ys_ = yst[i % 2]
            for half in range(2):
                hs = slice(half * 512, (half + 1) * 512)
                c.op("act", lambda h, half=half, hs=hs: h.activation(out=rsum[:, hs], in_=po[half][64:128, :], func=AF.Copy), reads=[po[half]], writes=[rsum])
                c.op("act", lambda h, hs=hs: h.activation(out=rsum[:, hs], in_=rsum[:, hs], func=AF.Ln), reads=[rsum], writes=[rsum])
                c.op("act", lambda h, hs=hs: h.activation(out=rsum[:, hs], in_=rsum[:, hs], func=AF.Exp, scale=-1.0), reads=[rsum], writes=[rsum])
                c.op("dve", lambda h, half=half, hs=hs, ys_=ys_: h.tensor_tensor(out=ys_[:, hs], in0=po[half][0:64, :], in1=rsum[:, hs], op=ALU.mult), reads=[po[half], rsum], writes=[ys_])
            c.dma("sp", ya[:, :, i * 128:(i + 1) * 128], ys_[:, :].rearrange("d (h t) -> d h t", h=8), reads=[ys_], writes=[self.db("yT0", i)])

        return stage1, stage2

    def _aptb(self, pt, half):
        d = self.__dict__.setdefault("_aptbufs", {})
        k = (id(pt), half)
        if k not in d:
            d[k] = Buf(name=f"aptb{len(d)}")
        return d[k]

    def a_phase(self):
        NT = self.NT
        with ExitStack() as es:
            stage1, stage2 = self._a_setup(es, [0, 1], [(2, 3), (4, 5)], (6, 7))
            stage1(0)
            for i in range(NT):
                if i + 1 < NT:
                    stage1(i + 1)
                stage2(i)
            self.c.barrier()

    def ab_phase(self):
        NT = self.NT
        with ExitStack() as es:
            stage1, stage2 = self._a_setup(es, [0, 1, 2], [(1, 2)], (3, 4))
            bgen, nb = self._b_setup(es, [5, 6], [7])
            done = 0

            def advance(upto):
                nonlocal done
                while done < min(upto, nb):
                    next(bgen)
                    done += 1
            stage1(0)
            for i in range(NT):
                if i + 1 < NT:
                    stage1(i + 1)
                stage2(i)
                advance((nb * (i + 1) + NT - 1) // NT)
            advance(nb)
            self.c.barrier()


def build_gmask():
    i = np.arange(128)
    out = np.zeros((14, 128, 512), np.float32)
    for l in range(7):
        b = 1 << l
        t, tp = i[:, None], i[None, :]
        m = ((t // (2 * b)) == (tp // (2 * b))) & ((t % (2 * b)) >= b) & ((tp % (2 * b)) < b)
        m = m.astype(np.float32)
        out[l] = np.tile(m, (1, 4))
        out[7 + l] = np.tile(m.T, (1, 4))
    return out


class ProgC(ProgAB):
    def c_phase(self, cvb):
        c, S, NT = self.c, self.S, self.NT
        with ExitStack() as es:
            def T(name, shape, dt, n=1):
                return [c.sb(es, name, shape, dt) for _ in range(n)]
            gm = T("gm", [128, 14, 512], BF16)[0]
            identrep = T("identrep", [128, 512], BF16)[0]
            negm4 = T("negm4", [128, 512], F32)[0]
            strict4 = T("strict4", [128, 512], BF16)[0]
            cm = self.dram["cmat"]
            cb = self.db("cmat")
            c.dma("pool", gm[:, :, :], self.dram["gmask"].rearrange("l p n -> p l n"), reads=[self.db("gmask")], writes=[gm])
            for r in range(4):
                c.dma("pool", identrep[:, r * 128:(r + 1) * 128], cm[0], reads=[cb], writes=[identrep])
                c.dma("sp", negm4[:, r * 128:(r + 1) * 128], cm[3], reads=[cb], writes=[negm4])
                c.dma("pool", strict4[:, r * 128:(r + 1) * 128], cm[5], reads=[cb], writes=[strict4])
            kTs = T("kTc", [128, 4, 128], BF16, 2); qTs = T("qTc", [128, 4, 128], BF16, 2)
            ktoks = T("ktok", [128, 512], BF16, 2); vtoks = T("vtok", [128, 512], BF16, 2)
            szs = T("szc", [128, 4, 128], BF16, 2); gbs = T("gb", [128, 8], F32, 2)
            gtri = T("gtri", [128, 4, 128], F32)[0]; ngtri = T("ngtri", [128, 4, 128], F32)[0]; bdiag = T("bdiag", [128, 4, 128], F32)[0]
            dm = T("dm", [128, 512], F32)[0]; decT = T("decT", [128, 512], F32)[0]; egcb = T("egcb", [128, 512], F32)[0]
            bbc = T("bbc", [128, 512], F32)[0]; dsb = T("dsb", [128, 512], F32)[0]
            ngb = T("ngb", [128, 4], F32)[0]
            gcs = T("gcs", [128, 8], F32)[0]; sm = T("csm", [128, 16], F32)[0]
            ATb = T("ATb", [128, 4, 128], BF16)[0]; Ab = T("Ab", [128, 4, 128], BF16)[0]; qkb = T("qkb", [128, 4, 128], BF16)[0]
            Ts = T("Tm", [128, 4, 128], BF16, 2); Us = T("Um", [128, 4, 128], BF16, 2)
            Xb = T("Xb", [128, 4, 128], BF16)[0]; Xpb = T("Xpb", [128, 4, 128], BF16)[0]; tmpb = T("tmpb", [128, 512], BF16, 2)
            kbg = T("kbg", [128, 512], BF16)[0]; vb = T("vb", [128, 512], BF16)[0]; kdec = T("kdec", [128, 512], BF16)[0]
            qg = T("qg", [128, 4, 128], BF16)[0]; nwT = T("nwT", [128, 4, 128], BF16)[0]; vnew = T("vnew", [128, 512], BF16)[0]
            state_f = T("state_f", [128, 4, 128], F32)[0]; state_b = T("state_b", [128, 4, 128], BF16)[0]
            osq = T("osq", [128, 4, 128], BF16)[0]; rstd = T("crstd", [128, 512], F32)[0]; yn = T("yn", [128, 512], F32)[0]
            yst = T("ystc", [128, 4, 128], BF16, 2)
            c.op("pool", lambda h: h.memset(state_f[:, :, :], 0.0), writes=[state_f])
            c.op("pool", lambda h: h.memset(state_b[:, :, :], 0.0), writes=[state_b])
            qc3 = self.qcT.rearrange("(h d) t -> d h t", d=128); kc3 = self.kcT.rearrange("(h d) t -> d h t", d=128)
            sz3 = self.szT.rearrange("(h d) t -> d h t", d=128); yc3 = self.yT[2].rearrange("(h d) t -> d h t", d=128)
            allg = lambda nm: [self.db(nm, g) for g in range(self.NG)]
            f2 = lambda b: b[:, :, :].rearrange("p h t -> p (h t)")
            for n in range(NT):
                cs = slice(n * 128, (n + 1) * 128)
                kT, qT, ktok, vtok, sz, gb = kTs[n % 2], qTs[n % 2], ktoks[n % 2], vtoks[n % 2], szs[n % 2], gbs[n % 2]
                c.dma("sp", kT[:, :, :], kc3[:, :, cs], reads=allg("kcT"), writes=[kT])
                c.dma("sp", qT[:, :, :], qc3[:, :, cs], reads=allg("qcT"), writes=[qT])
                c.dma("sp", sz[:, :, :], sz3[:, :, cs], reads=allg("szT"), writes=[sz])
                c.dma("sp", ktok[:, :], self.kC[cs, :], reads=allg("kC"), writes=[ktok])
                c.dma("sp", vtok[:, :], self.vC[cs, :], reads=allg("vC"), writes=[vtok])
                c.dma("sp", gb[:, :], self.smallD[cs, 12:20], reads=allg("smallD"), writes=[gb])
                c.op("dve", lambda h: h.tensor_scalar(out=ngb[:, :], in0=gb[:, 4:8], scalar1=-1.0, scalar2=None, op0=ALU.mult), reads=[gb], writes=[ngb])
                for hd in range(4):
                    c.op("dve", lambda h, hd=hd: h.tensor_scalar(out=gtri[:, hd, :], in0=self.triu_f[:, :], scalar1=gb[:, 4 + hd:5 + hd], scalar2=None, op0=ALU.mult), reads=[self.triu_f, gb], writes=[gtri])
                    c.op("act", lambda h, hd=hd: h.activation(out=ngtri[:, hd, :], in_=self.triu_f[:, :], func=AF.Copy, scale=ngb[:, hd:hd + 1]), reads=[self.triu_f, ngb], writes=[ngtri])
                    c.op("act", lambda h, hd=hd: h.activation(out=bdiag[:, hd, :], in_=self.ident_f[:, :], func=AF.Copy, scale=gb[:, hd:hd + 1]), reads=[self.ident_f, gb], writes=[bdiag])
                pD, pG, pB, pS = self.rot(), self.rot(), self.rot(), self.rot()
                for hd in range(4):
                    hs = slice(hd * 128, (hd + 1) * 128)
                    c.op("pe", lambda h, hd=hd, hs=hs: h.matmul(pD[:, hs], lhsT=self.ones_f[:, :], rhs=gtri[:, hd, :], start=True, stop=False), reads=[self.ones_f, gtri], writes=[pD], sig=False)
                    c.op("pe", lambda h, hd=hd, hs=hs: h.matmul(pD[:, hs], lhsT=ngtri[:, hd, :], rhs=self.ones_f[:, :], start=False, stop=True), reads=[self.ones_f, ngtri], writes=[pD], sig=(hd == 3))
                for hd in range(4):
                    hs = slice(hd * 128, (hd + 1) * 128)
                    c.op("pe", lambda h, hd=hd, hs=hs: h.matmul(pG[:, hs], lhsT=self.ones_f[:, :], rhs=gtri[:, hd, :], start=True, stop=True), reads=[self.ones_f, gtri], writes=[pG], sig=(hd == 3))
                for hd in range(4):
                    hs = slice(hd * 128, (hd + 1) * 128)
                    c.op("pe", lambda h, hd=hd, hs=hs: h.matmul(pB[:, hs], lhsT=self.ones_f[:, :], rhs=bdiag[:, hd, :], start=True, stop=True), reads=[self.ones_f, bdiag], writes=[pB], sig=(hd == 3))
                c.op("pe", lambda h: h.matmul(pS[:, 0:4], lhsT=self.triu_f[:, :], rhs=gb[:, 4:8], start=True, stop=True), reads=[self.triu_f, gb], writes=[pS], sig=False)
                c.op("pe", lambda h: h.matmul(pS[:, 4:8], lhsT=self.ones_f[:, :], rhs=gb[:, 4:8], start=True, stop=True), reads=[self.ones_f, gb], writes=[pS])
                c.op("dve", lambda h: h.scalar_tensor_tensor(out=dm[:, :], in0=pD[:, :], scalar=0.0, in1=negm4[:, :], op0=ALU.min, op1=ALU.add), reads=[pD, negm4], writes=[dm])
                c.op("act", lambda h: h.activation(out=decT[:, :], in_=dm[:, :], func=AF.Exp), reads=[dm], writes=[decT])
                c.op("act", lambda h: h.activation(out=egcb[:, :], in_=pG[:, :], func=AF.Exp), reads=[pG], writes=[egcb])
                c.op("act", lambda h: h.activation(out=bbc[:, :], in_=pB[:, :], func=AF.Copy), reads=[pB], writes=[bbc])
                c.op("act", lambda h: h.activation(out=gcs[:, :], in_=pS[:, 0:8], func=AF.Copy), reads=[pS], writes=[gcs])
                c.op("act", lambda h: h.activation(out=sm[:, 0:4], in_=gcs[:, 0:4], func=AF.Exp), reads=[gcs], writes=[sm])
                c.op("dve", lambda h: h.tensor_tensor(out=sm[:, 0:4], in0=sm[:, 0:4], in1=gb[:, 0:4], op=ALU.mult), reads=[sm, gb], writes=[sm])
                c.op("dve", lambda h: h.tensor_tensor(out=sm[:, 12:16], in0=gcs[:, 4:8], in1=gcs[:, 0:4], op=ALU.subtract), reads=[gcs, sm], writes=[sm])
                c.op("act", lambda h: h.activation(out=sm[:, 4:8], in_=sm[:, 12:16], func=AF.Exp), reads=[sm], writes=[sm])
                c.op("act", lambda h: h.activation(out=sm[:, 8:12], in_=gcs[:, 4:8], func=AF.Exp), reads=[gcs, sm], writes=[sm])
                pK, pQ = self.rot(), self.rot()
                for hd in range(4):
                    hs = slice(hd * 128, (hd + 1) * 128)
                    c.op("pe", lambda h, hd=hd, hs=hs: h.matmul(pK[:, hs], lhsT=kT[:, hd, :], rhs=kT[:, hd, :], start=True, stop=True), reads=[kT], writes=[pK], sig=(hd == 3))
                for hd in range(4):
                    hs = slice(hd * 128, (hd + 1) * 128)
                    c.op("pe", lambda h, hd=hd, hs=hs: h.matmul(pQ[:, hs], lhsT=kT[:, hd, :], rhs=qT[:, hd, :], start=True, stop=True), reads=[kT, qT], writes=[pQ], sig=(hd == 3))
                c.op("dve", lambda h: h.tensor_tensor(out=dsb[:, :], in0=decT[:, :], in1=bbc[:, :], op=ALU.mult), reads=[decT, bbc], writes=[dsb])
                c.op("dve", lambda h: h.tensor_tensor(out=dsb[:, :], in0=dsb[:, :], in1=strict4[:, :], op=ALU.mult), reads=[dsb, strict4], writes=[dsb])
                c.op("dve", lambda h: h.tensor_tensor(out=f2(ATb), in0=pK[:, :], in1=dsb[:, :], op=ALU.mult), reads=[pK, dsb], writes=[ATb])
                c.op("dve", lambda h: h.tensor_tensor(out=f2(qkb), in0=pQ[:, :], in1=decT[:, :], op=ALU.mult), reads=[pQ, decT], writes=[qkb])
                pA = self.rot()
                for hd in range(4):
                    hs = slice(hd * 128, (hd + 1) * 128)
                    c.op("pe", lambda h, hd=hd, hs=hs: h.matmul(pA[:, hs], lhsT=ATb[:, hd, :], rhs=self.ident_b[:, :], start=True, stop=True), reads=[ATb, self.ident_b], writes=[pA], sig=(hd == 3))
                c.op("act", lambda h: h.activation(out=f2(Ab), in_=pA[:, :], func=AF.Copy), reads=[pA], writes=[Ab])
                Tc, Uc = Ts[0], Us[0]
                c.op("dve", lambda h: h.tensor_tensor(out=tmpb[0][:, :], in0=f2(Ab), in1=gm[:, 0, :], op=ALU.mult), reads=[Ab, gm], writes=[tmpb[0]])
                c.op("dve", lambda h: h.tensor_tensor(out=f2(Tc), in0=identrep[:, :], in1=tmpb[0][:, :], op=ALU.subtract), reads=[tmpb[0], identrep], writes=[Tc])
                c.op("dve", lambda h: h.tensor_tensor(out=tmpb[1][:, :], in0=f2(ATb), in1=gm[:, 7, :], op=ALU.mult), reads=[ATb, gm], writes=[tmpb[1]])
                c.op("dve", lambda h: h.tensor_tensor(out=f2(Uc), in0=identrep[:, :], in1=tmpb[1][:, :], op=ALU.subtract), reads=[tmpb[1], identrep], writes=[Uc])
                for l in range(1, 7):
                    Tn, Un = Ts[l % 2], Us[l % 2]
                    pXp = self.rot()
                    for hd in range(4):
                        hs = slice(hd * 128, (hd + 1) * 128)
                        c.op("pe", lambda h, hd=hd, hs=hs, Uc=Uc: h.matmul(pXp[:, hs], lhsT=Ab[:, hd, :], rhs=Uc[:, hd, :], start=True, stop=True), reads=[Ab, Uc], writes=[pXp], sig=(hd == 3))
                    c.op("dve", lambda h, l=l: h.tensor_tensor(out=f2(Xpb), in0=pXp[:, :], in1=gm[:, 7 + l, :], op=ALU.mult), reads=[pXp, gm], writes=[Xpb])
                    if l < 6:
                        pX = self.rot()
                        for hd in range(4):
                            hs = slice(hd * 128, (hd + 1) * 128)
                            c.op("pe", lambda h, hd=hd, hs=hs, Tc=Tc: h.matmul(pX[:, hs], lhsT=ATb[:, hd, :], rhs=Tc[:, hd, :], start=True, stop=True), reads=[ATb, Tc], writes=[pX], sig=(hd == 3))
                        c.op("dve", lambda h, l=l: h.tensor_tensor(out=f2(Xb), in0=pX[:, :], in1=gm[:, l, :], op=ALU.mult), reads=[pX, gm], writes=[Xb])
                    pYp = self.rot()
                    for hd in range(4):
                        hs = slice(hd * 128, (hd + 1) * 128)
                        c.op("pe", lambda h, hd=hd, hs=hs, Tc=Tc: h.matmul(pYp[:, hs], lhsT=Tc[:, hd, :], rhs=Xpb[:, hd, :], start=True, stop=True), reads=[Tc, Xpb], writes=[pYp], sig=(hd == 3))
                    c.op("dve", lambda h, Uc=Uc, Un=Un: h.tensor_tensor(out=f2(Un), in0=f2(Uc), in1=pYp[:, :], op=ALU.subtract), reads=[Uc, pYp], writes=[Un])
                    if l < 6:
                        pY = self.rot()
                        for hd in range(4):
                            hs = slice(hd * 128, (hd + 1) * 128)
                            c.op("pe", lambda h, hd=hd, hs=hs, Uc=Uc: h.matmul(pY[:, hs], lhsT=Uc[:, hd, :], rhs=Xb[:, hd, :], start=True, stop=True), reads=[Uc, Xb], writes=[pY], sig=(hd == 3))
                        c.op("dve", lambda h, Tc=Tc, Tn=Tn: h.tensor_tensor(out=f2(Tn), in0=f2(Tc), in1=pY[:, :], op=ALU.subtract), reads=[Tc, pY], writes=[Tn])
                    Tc, Uc = Tn, Un
                U = Uc
                for hd in range(4):
                    hs = slice(hd * 128, (hd + 1) * 128)
                    c.op("act", lambda h, hd=hd, hs=hs: h.activation(out=kbg[:, hs], in_=ktok[:, hs], func=AF.Copy, scale=sm[:, hd:hd + 1]), reads=[ktok, sm], writes=[kbg])
                    c.op("act", lambda h, hd=hd, hs=hs: h.activation(out=vb[:, hs], in_=vtok[:, hs], func=AF.Copy, scale=gb[:, hd:hd + 1]), reads=[vtok, gb], writes=[vb])
                    c.op("act", lambda h, hd=hd, hs=hs: h.activation(out=kdec[:, hs], in_=ktok[:, hs], func=AF.Copy, scale=sm[:, 4 + hd:5 + hd]), reads=[ktok, sm], writes=[kdec])
                c.op("dve", lambda h: h.tensor_tensor(out=f2(qg), in0=f2(qT), in1=egcb[:, :], op=ALU.mult), reads=[qT, egcb], writes=[qg])
                pW = self.rot()
                for hd in range(4):
                    hs = slice(hd * 128, (hd + 1) * 128)
                    c.op("pe", lambda h, hd=hd, hs=hs: h.matmul(pW[:, hs], lhsT=kbg[:, hs], rhs=U[:, hd, :], start=True, stop=True), reads=[kbg, U], writes=[pW], sig=(hd == 3))
                c.op("act", lambda h: h.activation(out=f2(nwT), in_=pW[:, :], func=AF.Copy, scale=-1.0), reads=[pW], writes=[nwT])
                pV = self.rot()
                for hd in range(4):
                    hs = slice(hd * 128, (hd + 1) * 128)
                    c.op("pe", lambda h, hd=hd, hs=hs: h.matmul(pV[:, hs], lhsT=U[:, hd, :], rhs=vb[:, hs], start=True, stop=False), reads=[U, vb], writes=[pV], sig=False)
                    c.op("pe", lambda h, hd=hd, hs=hs: h.matmul(pV[:, hs], lhsT=nwT[:, hd, :], rhs=state_b[:, hd, :], start=False, stop=True), reads=[nwT, state_b], writes=[pV], sig=(hd == 3))
                c.op("act", lambda h: h.activation(out=vnew[:, :], in_=pV[:, :], func=AF.Copy), reads=[pV], writes=[vnew])
                pO = self.rot()
                for hd in range(4):
                    hs = slice(hd * 128, (hd + 1) * 128)
                    c.op("pe", lambda h, hd=hd, hs=hs: h.matmul(pO[:, hs], lhsT=state_b[:, hd, :], rhs=qg[:, hd, :], start=True, stop=False), reads=[state_b, qg], writes=[pO], sig=False)
                    c.op("pe", lambda h, hd=hd, hs=hs: h.matmul(pO[:, hs], lhsT=vnew[:, hs], rhs=qkb[:, hd, :], start=False, stop=True), reads=[vnew, qkb], writes=[pO], sig=(hd == 3))
                pS2 = self.rot()
                for hd in range(4):
                    hs = slice(hd * 128, (hd + 1) * 128)
                    c.op("pe", lambda h, hd=hd, hs=hs: h.matmul(pS2[:, hs], lhsT=kdec[:, hs], rhs=vnew[:, hs], start=True, stop=True), reads=[kdec, vnew], writes=[pS2], sig=(hd == 3))
                for hd in range(4):
                    hs = slice(hd * 128, (hd + 1) * 128)
                    c.op("dve", lambda h, hd=hd, hs=hs: h.scalar_tensor_tensor(out=state_f[:, hd, :], in0=state_f[:, hd, :], scalar=sm[:, 8 + hd:9 + hd], in1=pS2[:, hs], op0=ALU.mult, op1=ALU.add),
                         reads=[state_f, sm, pS2], writes=[state_f])
                c.op("act", lambda h: h.activation(out=f2(state_b), in_=f2(state_f), func=AF.Copy), reads=[state_f], writes=[state_b])
                c.op("act", lambda h: h.activation(out=f2(osq), in_=pO[:, :], func=AF.Square), reads=[pO], writes=[osq])
                pN = self.rot()
                for hd in range(4):
                    hs = slice(hd * 128, (hd + 1) * 128)
                    c.op("pe", lambda h, hd=hd, hs=hs: h.matmul(pN[:, hs], lhsT=self.ones_b[:, :], rhs=osq[:, hd, :], start=True, stop=True), reads=[self.ones_b, osq], writes=[pN], sig=(hd == 3))
                c.op("dve", lambda h: h.tensor_scalar(out=rstd[:, :], in0=pN[:, :], scalar1=1.0 / 128, scalar2=1e-6, op0=ALU.mult, op1=ALU.add), reads=[pN], writes=[rstd])
                c.op("act", lambda h: h.activation(out=rstd[:, :], in_=rstd[:, :], func=AF.Ln), reads=[rstd], writes=[rstd])
                c.op("act", lambda h: h.activation(out=rstd[:, :], in_=rstd[:, :], func=AF.Exp, scale=-0.5), reads=[rstd], writes=[rstd])
                c.op("dve", lambda h: h.tensor_tensor(out=yn[:, :], in0=pO[:, :], in1=rstd[:, :], op=ALU.mult), reads=[pO, rstd], writes=[yn])
                ys_ = yst[n % 2]
                c.op("dve", lambda h, ys_=ys_: h.scalar_tensor_tensor(out=f2(ys_), in0=yn[:, :], scalar=self.cvec[:, cvb + CV_DN:cvb + CV_DN + 1], in1=f2(sz), op0=ALU.mult, op1=ALU.mult),
                     reads=[yn, self.cvec, sz], writes=[ys_])
                c.dma("sp", yc3[:, :, cs], ys_[:, :, :], reads=[ys_], writes=[self.db("yT2", n)])
            c.barrier()


class ProgFull(ProgC):
    def merge_phase(self, xsrc, xdst, wbr, w_out):
        c, S, NG = self.c, self.S, self.NG
        with ExitStack() as es:
            wbs = [c.sb(es, "wbr", [128, 4, 1024], BF16) for _ in range(3)]
            wo = c.sb(es, "wo", [128, 8, 1024], BF16)
            wb = self.db("w")
            for i in range(3):
                c.dma("pool", wbs[i][:, :, :], wbr[i].rearrange("(k p) n -> p k n", p=128), reads=[wb], writes=[wbs[i]])
            c.dma("pool", wo[:, :, :], w_out.rearrange("(k p) n -> p k n", p=128), reads=[wb], writes=[wo])
            ys = [c.sb(es, "ymg", [128, 3, 4, 512], BF16) for _ in range(2)]
            gts = [c.sb(es, "gmg", [128, 24, 512], BF16) for _ in range(2)]
            xgs = [c.sb(es, "xmg", [128, 8, 512], F32) for _ in range(2)]
            mg = c.sb(es, "mg", [128, 8, 512], BF16)
            t1 = [c.sb(es, "mt1", [128, 512], F32) for _ in range(2)]
            t2 = [c.sb(es, "mt2", [128, 512], F32) for _ in range(2)]
            xs3 = xsrc.rearrange("(c p) t -> p c t", p=128)
            xd3 = xdst.rearrange("(c p) t -> p c t", p=128)
            for g in range(NG):
                tsl = slice(g * 512, (g + 1) * 512)
                y, gt, xg = ys[g % 2], gts[g % 2], xgs[g % 2]
                deps = [self.db("yT0", i) for i in range(g * 4, g * 4 + 4)] + [self.db("yT1", hh * 100 + g) for hh in range(8)] + [self.db("yT2", n) for n in range(g * 4, g * 4 + 4)]
                for br in range(3):
                    c.dma("sp", y[:, br, :, :], self.yT[br].rearrange("(k p) t -> p k t", p=128)[:, :, tsl], reads=deps, writes=[y])
                c.dma("sp", gt[:, :, :], self.gT.rearrange("(k p) t -> p k t", p=128)[:, :, tsl], reads=[self.db("gT", sec * 100 + g) for sec in range(6)], writes=[gt])
                c.dma("sp", xg[:, :, :], xs3[:, :, tsl], reads=[self.db(xsrc.name, g)], writes=[xg])
                for d in range(8):
                    pbs = []
                    for br in range(3):
                        pb = self.rot()
                        pbs.append(pb)
                        for k in range(4):
                            c.op("pe", lambda h, br=br, k=k, d=d, pb=pb: h.matmul(pb[:, :], lhsT=wbs[br][:, k, d * 128:(d + 1) * 128], rhs=y[:, br, k, :], start=(k == 0), stop=(k == 3)),
                                 reads=[wbs[br], y], writes=[pb], sig=(k == 3))
                    a, b = t1[d % 2], t2[d % 2]
                    c.op("dve", lambda h, d=d, a=a: h.tensor_tensor(out=a[:, :], in0=pbs[0][:, :], in1=gt[:, d, :], op=ALU.mult), reads=[pbs[0], gt], writes=[a])
                    c.op("dve", lambda h, d=d, b=b: h.tensor_tensor(out=b[:, :], in0=pbs[1][:, :], in1=gt[:, 8 + d, :], op=ALU.mult), reads=[pbs[1], gt], writes=[b])
                    c.op("dve", lambda h, a=a, b=b: h.tensor_tensor(out=a[:, :], in0=a[:, :], in1=b[:, :], op=ALU.add), reads=[a, b], writes=[a])
                    c.op("dve", lambda h, d=d, b=b: h.tensor_tensor(out=b[:, :], in0=pbs[2][:, :], in1=gt[:, 16 + d, :], op=ALU.mult), reads=[pbs[2], gt], writes=[b])
                    c.op("dve", lambda h, d=d, a=a, b=b: h.tensor_tensor(out=mg[:, d, :], in0=a[:, :], in1=b[:, :], op=ALU.add), reads=[a, b], writes=[mg])
                for d in range(8):
                    po = self.rot()
                    for k in range(8):
                        c.op("pe", lambda h, d=d, k=k, po=po: h.matmul(po[:, :], lhsT=wo[:, k, d * 128:(d + 1) * 128], rhs=mg[:, k, :], start=(k == 0), stop=(k == 7)),
                             reads=[wo, mg], writes=[po], sig=(k == 7))
                    c.op("dve", lambda h, d=d, po=po, xg=xg: h.tensor_tensor(out=xg[:, d, :], in0=po[:, :], in1=xg[:, d, :], op=ALU.add), reads=[po, xg], writes=[xg])
                c.dma("sp", xd3[:, :, tsl], xg[:, :, :], reads=[xg], writes=[self.db(xdst.name, g)])
            c.barrier()

    def final_phase(self, xsrc, out):
        c, S, NG = self.c, self.S, self.NG
        with ExitStack() as es:
            xgs = [c.sb(es, "xf", [128, 8, 512], F32) for _ in range(2)]
            ogs = [c.sb(es, "of", [128, 8, 512], F32) for _ in range(2)]
            sq = c.sb(es, "sqf", [128, 8, 512], BF16)
            rstd = c.sb(es, "rstdf", [128, 512], F32)
            xs3 = xsrc.rearrange("(c p) t -> p c t", p=128)
            o3 = out.rearrange("(c p) t -> p c t", p=128)
            for g in range(NG):
                tsl = slice(g * 512, (g + 1) * 512)
                xg, og = xgs[g % 2], ogs[g % 2]
                c.dma("sp", xg[:, :, :], xs3[:, :, tsl], reads=[self.db(xsrc.name, g)], writes=[xg])
                self.rmsnorm_group(xg, sq, lambda k, og=og: og[:, k, :], og, rstd, DEPTH * CV_PER_LAYER, self.rot())
                c.dma("sp", o3[:, :, tsl], og[:, :, :], reads=[og], writes=[self.db("out", g)])
            c.barrier()


def build_full(S=4096):
    P = ProgFull(S)
    es = ExitStack()
    P.setup_consts(es)
    P.declare_scratch()
    d = P.dr
    EI = "ExternalInput"
    d("rotC", [128, S], F32, kind=EI); d("rotS", [128, S], F32, kind=EI)
    d("pow2", [128, 26], F32, kind=EI); d("gmask", [14, 128, 512], F32, kind=EI)
    xin = d("xT_in", [1024, S], F32, kind=EI)
    out = d("outT", [1024, S], F32, kind="ExternalOutput")
    xa = d("xTa", [1024, S], F32); xb = d("xTb", [1024, S], F32)
    f1i = d("ffn1_w_in", [DEPTH, 1024, 4096], F32, kind=EI); f1o = d("ffn1_w_out", [DEPTH, 2048, 1024], F32, kind=EI)
    f2i = d("ffn2_w_in", [DEPTH, 1024, 4096], F32, kind=EI); f2o = d("ffn2_w_out", [DEPTH, 2048, 1024], F32, kind=EI)
    w = d("w_in", [DEPTH, 1024, 7636], F32, kind=EI); wsw = d("w_sw", [DEPTH, 1024, 896], F32, kind=EI); wsm = d("w_small", [DEPTH, 1024, 84], F32, kind=EI)
    wba = d("w_branch_a", [DEPTH, 512, 1024], F32, kind=EI); wbb = d("w_branch_b", [DEPTH, 512, 1024], F32, kind=EI); wbc = d("w_branch_c", [DEPTH, 512, 1024], F32, kind=EI)
    wo = d("w_out", [DEPTH, 1024, 1024], F32, kind=EI)
    cur = xin
    for l in range(DEPTH):
        cvb = l * CV_PER_LAYER
        P.ffn_phase(cur, xa, f1i[l], f1o[l], cvb + CV_FFN1)
        P.m1_phase(xa, w[l], wsw[l], wsm[l], cvb)
        P.ab_phase()
        P.c_phase(cvb)
        P.merge_phase(xa, xb, [wba[l], wbb[l], wbc[l]], wo[l])
        P.ffn_phase(xb, xa, f2i[l], f2o[l], cvb + CV_FFN2)
        cur = xa
    P.final_phase(xa, out)
    es.close()
    return P


_CACHE = {}


def kernel(**inputs):
    S = 4096
    inp = {k: np.asarray(v) for k, v in inputs.items()}
    if "prog" not in _CACHE:
        _CACHE["prog"] = build_full(S)
    P = _CACHE["prog"]
    rotC, rotS = build_rot(S)
    shared = {
        "cmat": build_cmat(), "cvec": build_cvec(inp), "rotC": rotC, "rotS": rotS, "pow2": build_pow2(), "gmask": build_gmask(),
        "ffn1_w_in": inp["ffn1_w_in"], "ffn1_w_out": inp["ffn1_w_out"], "ffn2_w_in": inp["ffn2_w_in"], "ffn2_w_out": inp["ffn2_w_out"],
        "w_in": inp["w_in"], "w_sw": np.ascontiguousarray(inp["w_in"][:, :, swap_cols()]), "w_small": np.ascontiguousarray(inp["w_in"][:, :, small_cols()]),
        "w_branch_a": inp["w_branch_a"], "w_branch_b": inp["w_branch_b"], "w_branch_c": inp["w_branch_c"], "w_out": inp["w_out"],
    }
    in_maps = []
    for b in range(NB):
        m = dict(shared)
        m["xT_in"] = np.ascontiguousarray(inp["x"][b].T)
        in_maps.append(m)
    res = run_bass_kernel_spmd(P.nc, in_maps, core_ids=list(range(NB)))
    out = np.stack([np.ascontiguousarray(r["outT"].T) for r in res.results], axis=0)
    return out.astype(np.float32)
```

```python
from contextlib import ExitStack
import numpy as np
import concourse.bass as bass
import concourse.mybir as mybir
from concourse.bass_utils import run_bass_kernel_spmd

F32 = mybir.dt.float32
BF16 = mybir.dt.bfloat16
ALU = mybir.AluOpType
AF = mybir.ActivationFunctionType
AX = mybir.AxisListType

D = 1024
DEPTH = 2
NB = 8
IN_SIZES = (512, 64, 64, 256, 64, 4, 1536, 8, 1536, 512, 4, 4, 3072)
OFF = np.concatenate([[0], np.cumsum(IN_SIZES)]).tolist()
(O_AQ, O_AK, O_AV, O_IQ, O_IK, O_IW, O_BQKV, O_BF, O_CQKV, O_CZ, O_CB, O_CA, O_G) = OFF[:13]
NEG = -32768.0


class Buf:
    __slots__ = ("t", "last_w", "readers", "name")

    def __init__(self, t=None, name=""):
        self.t = t
        self.last_w = None
        self.readers = {}
        self.name = name

    def __getitem__(self, k):
        return self.t[k]


class Eng:
    def __init__(self, name, handle, sem):
        self.name = name
        self.h = handle
        self.sem = sem
        self.count = 0
        self.seen = {}


class Ctx:
    SAME_ENGINE_SYNC = True
    RAW_ONLY_SAME_ENGINE = False

    def __init__(self, nc, n_dma_sems=10):
        self.nc = nc
        self.sems = {}
        self.eng = {}
        for nm, h in (("pe", nc.tensor), ("act", nc.scalar), ("dve", nc.vector),
                      ("pool", nc.gpsimd), ("sp", nc.sync)):
            self.sems["s_" + nm] = nc.alloc_semaphore("s_" + nm)
            self.eng[nm] = Eng(nm, h, "s_" + nm)
        self.dma_pool = {}
        for q in ("sp", "pool", "act"):
            lst = []
            for i in range(n_dma_sems):
                k = f"d_{q}{i}"
                self.sems[k] = nc.alloc_semaphore(k)
                lst.append([k, 0])
            self.dma_pool[q] = [lst, 0]
        self.n_instr = 0
        self.n_wait = 0
        self.uid = 0

    def sb(self, es, name, shape, dt):
        self.uid += 1
        nm = f"{name}_{self.uid}"
        return Buf(es.enter_context(self.nc.sbuf_tensor(nm, list(shape), dt)), nm)

    def _need(self, reads, writes, own=None):
        need = {}

        def add(ev, raw):
            if ev is None:
                return
            k, v = ev
            if k == own and not raw and self.RAW_ONLY_SAME_ENGINE:
                return
            if need.get(k, 0) < v:
                need[k] = v
        for b in reads:
            add(b.last_w, True)
        for b in writes:
            add(b.last_w, False)
            for k, v in b.readers.items():
                add((k, v), False)
        return need

    def _emit_waits(self, e, need):
        for k, v in need.items():
            if k == e.sem and (e.name == "pe" or not self.SAME_ENGINE_SYNC):
                continue
            if e.seen.get(k, 0) >= v:
                continue
            e.h.wait_ge(self.sems[k], v)
            e.seen[k] = v
            self.n_wait += 1

    def _record(self, ev, reads, writes):
        k, v = ev
        for b in writes:
            b.last_w = ev
            b.readers = {}
        for b in reads:
            if b.readers.get(k, 0) < v:
                b.readers[k] = v

    def op(self, en, fn, reads=(), writes=(), sig=True):
        e = self.eng[en]
        self._emit_waits(e, self._need(reads, writes, e.sem))
        ins = fn(e.h)
        self.n_instr += 1
        if sig:
            ins.then_inc(self.sems[e.sem], 1)
            e.count += 1
            ev = (e.sem, e.count)
        else:
            ev = (e.sem, e.count + 1)
        self._record(ev, reads, writes)
        return ins

    def dma(self, q, out, in_, reads=(), writes=(), **kw):
        e = self.eng[q]
        lst, idx = self.dma_pool[q]
        ent = lst[idx % len(lst)]
        self.dma_pool[q][1] = idx + 1
        need = self._need(reads, writes, None)
        if ent[1] > 0 and need.get(ent[0], 0) < ent[1]:
            need[ent[0]] = ent[1]
        self._emit_waits(e, need)
        ins = e.h.dma_start(out=out, in_=in_, **kw)
        ent[1] += 16
        ins.then_inc(self.sems[ent[0]], 16)
        self.n_instr += 1
        self._record((ent[0], ent[1]), reads, writes)
        return ins

    def barrier(self):
        for e in self.eng.values():
            need = {}
            for f in self.eng.values():
                if f is not e and f.count > 0:
                    need[f.sem] = f.count
            for q in self.dma_pool:
                for k, v in self.dma_pool[q][0]:
                    if v > 0:
                        need[k] = v
            self._emit_waits(e, need)


class Prog:
    def __init__(self, S, ext=None):
        self.S = S
        self.NT = S // 128
        self.NG = S // 512
        self.nc = bass.Bass("TRN2", target_bir_lowering=False)
        self.c = Ctx(self.nc)
        self.ext = ext or {}
        self.dram = {}
        self.dbuf = {}
        nc = self.nc
        self.psall = nc.alloc_psum_tensor("psall", [128, 8 * 512], F32)
        self.ps = [Buf(self.psall[:, i * 512:(i + 1) * 512], f"ps{i}") for i in range(8)]

    def dr(self, name, shape, dt, kind=None):
        if kind is None:
            kind = {"in": "ExternalInput", "out": "ExternalOutput"}.get(self.ext.get(name), "Internal")
        t = self.nc.dram_tensor(name, list(shape), dt, kind=kind)
        self.dram[name] = t.ap()
        return self.dram[name]

    def db(self, name, idx=0):
        k = (name, idx)
        if k not in self.dbuf:
            self.dbuf[k] = Buf(name=f"{name}{idx}")
        return self.dbuf[k]

    def setup_consts(self, es):
        c, nc = self.c, self.nc
        cm = self.dr("cmat", [7, 128, 128], F32, kind="ExternalInput")
        self.ident_b = c.sb(es, "identb", [128, 128], BF16)
        self.ones_b = c.sb(es, "onesb", [128, 128], BF16)
        self.ones_f = c.sb(es, "onesf", [128, 128], F32)
        self.triu_f = c.sb(es, "triuf", [128, 128], F32)
        self.ident_f = c.sb(es, "identf", [128, 128], F32)
        self.negm_f = c.sb(es, "negmf", [128, 128], F32)
        self.tri01_b = c.sb(es, "tri01b", [128, 128], BF16)
        cb = self.db("cmat")
        c.dma("pool", self.ident_b[:, :], cm[0], reads=[cb], writes=[self.ident_b])
        c.dma("pool", self.ones_b[:, :], cm[1], reads=[cb], writes=[self.ones_b])
        c.dma("sp", self.ones_f[:, :], cm[1], reads=[cb], writes=[self.ones_f])
        c.dma("sp", self.triu_f[:, :], cm[2], reads=[cb], writes=[self.triu_f])
        c.dma("sp", self.ident_f[:, :], cm[0], reads=[cb], writes=[self.ident_f])
        c.dma("sp", self.negm_f[:, :], cm[3], reads=[cb], writes=[self.negm_f])
        c.dma("pool", self.tri01_b[:, :], cm[2], reads=[cb], writes=[self.tri01_b])
        self.NCV = DEPTH * CV_PER_LAYER + 8
        cv = self.dr("cvec", [128, self.NCV], F32, kind="ExternalInput")
        self.cvec = c.sb(es, "cvec", [128, self.NCV], F32)
        c.dma("sp", self.cvec[:, :], cv[:, :], reads=[self.db("cvec")], writes=[self.cvec])

    def rmsnorm_group(self, xg, sq, hT_ap_fn, hT_buf, rstd, gcol, psb, nch=8, ncols=512):
        c = self.c
        c.op("act", lambda h: h.activation(out=sq[:, :, :], in_=xg[:, :, :], func=AF.Square), reads=[xg], writes=[sq])
        for k in range(nch):
            c.op("pe", lambda h, k=k: h.matmul(psb[:, :ncols], lhsT=self.ones_b[:, :], rhs=sq[:, k, :], start=(k == 0), stop=(k == nch - 1)),
                 reads=[self.ones_b, sq], writes=[psb], sig=(k == nch - 1))
        c.op("dve", lambda h: h.tensor_scalar(out=rstd[:, :], in0=psb[:, :ncols], scalar1=1.0 / (nch * 128), scalar2=1e-6, op0=ALU.mult, op1=ALU.add),
             reads=[psb], writes=[rstd])
        c.op("act", lambda h: h.activation(out=rstd[:, :], in_=rstd[:, :], func=AF.Ln), reads=[rstd], writes=[rstd])
        c.op("act", lambda h: h.activation(out=rstd[:, :], in_=rstd[:, :], func=AF.Exp, scale=-0.5), reads=[rstd], writes=[rstd])
        for k in range(nch):
            c.op("dve", lambda h, k=k: h.scalar_tensor_tensor(out=hT_ap_fn(k), in0=xg[:, k, :], scalar=self.cvec[:, gcol + k:gcol + k + 1],
                                                            in1=rstd[:, :], op0=ALU.mult, op1=ALU.mult),
                 reads=[xg, rstd, self.cvec], writes=[hT_buf])

    def ffn_phase(self, xsrc, xdst, w_in, w_out, gcol):
        c, S = self.c, self.S
        with ExitStack() as es:
            win = c.sb(es, "win", [128, 8, 4096], BF16)
            wout = c.sb(es, "wout", [128, 16, 1024], BF16)
            xgs = [c.sb(es, "xg", [128, 8, 512], F32) for _ in range(2)]
            sq = c.sb(es, "sq", [128, 8, 512], BF16)
            hT = c.sb(es, "hT", [128, 8, 512], BF16)
            act = c.sb(es, "actT", [128, 16, 512], BF16)
            rstd = c.sb(es, "rstd", [128, 512], F32)
            sgs = [c.sb(es, "sg", [128, 512], F32) for _ in range(2)]
            wb = self.db("w")
            for k in range(8):
                c.dma("pool", win[:, k, :], w_in[k * 128:(k + 1) * 128, :], reads=[wb], writes=[win])
            for k in range(16):
                c.dma("pool", wout[:, k, :], w_out[k * 128:(k + 1) * 128, :], reads=[wb], writes=[wout])
            xs3 = xsrc.rearrange("(c p) t -> p c t", p=128)
            xd3 = xdst.rearrange("(c p) t -> p c t", p=128)
            ps = self.ps
            for g in range(self.NG):
                xg = xgs[g % 2]
                tsl = slice(g * 512, (g + 1) * 512)
                c.dma("sp", xg[:, :, :], xs3[:, :, tsl], reads=[self.db(xsrc.name, g)], writes=[xg])
                self.rmsnorm_group(xg, sq, lambda k: hT[:, k, :], hT, rstd, gcol, ps[0])
                for j in range(16):
                    pg, pu = ps[1 + 2 * (j % 2)], ps[2 + 2 * (j % 2)]
                    for k in range(8):
                        c.op("pe", lambda h, k=k, j=j, pg=pg: h.matmul(pg[:, :], lhsT=win[:, k, j * 128:(j + 1) * 128], rhs=hT[:, k, :], start=(k == 0), stop=(k == 7)),
                             reads=[win, hT], writes=[pg], sig=(k == 7))
                    for k in range(8):
                        c.op("pe", lambda h, k=k, j=j, pu=pu: h.matmul(pu[:, :], lhsT=win[:, k, 2048 + j * 128:2048 + (j + 1) * 128], rhs=hT[:, k, :], start=(k == 0), stop=(k == 7)),
                             reads=[win, hT], writes=[pu], sig=(k == 7))
                    sg = sgs[j % 2]
                    c.op("act", lambda h, pg=pg, sg=sg: h.activation(out=sg[:, :], in_=pg[:, :], func=AF.Silu), reads=[pg], writes=[sg])
                    c.op("dve", lambda h, pu=pu, sg=sg, j=j: h.tensor_tensor(out=act[:, j, :], in0=sg[:, :], in1=pu[:, :], op=ALU.mult), reads=[sg, pu], writes=[act])
                for d in range(8):
                    po = ps[5 + d % 2]
                    for j in range(16):
                        c.op("pe", lambda h, d=d, j=j, po=po: h.matmul(po[:, :], lhsT=wout[:, j, d * 128:(d + 1) * 128], rhs=act[:, j, :], start=(j == 0), stop=(j == 15)),
                             reads=[wout, act], writes=[po], sig=(j == 15))
                    c.op("dve", lambda h, d=d, po=po, xg=xg: h.scalar_tensor_tensor(out=xg[:, d, :], in0=po[:, :], scalar=0.5, in1=xg[:, d, :], op0=ALU.mult, op1=ALU.add),
                         reads=[po, xg], writes=[xg])
                c.dma("sp", xd3[:, :, tsl], xg[:, :, :], reads=[xg], writes=[self.db(xdst.name, g)])
            c.barrier()


CV_FFN1, CV_MIX, CV_FFN2, CV_BG, CV_CONV, CV_DN, CV_BF, CV_ALOG, CV_DT = 0, 8, 16, 24, 48, 96, 97, 105, 109
CV_PER_LAYER = 113


def build_cvec(inp):
    cv = np.zeros((128, DEPTH * CV_PER_LAYER + 8), np.float32)
    for l in range(DEPTH):
        b = l * CV_PER_LAYER
        cv[:, b + CV_FFN1:b + CV_FFN1 + 8] = inp["ffn1_norm"][l].reshape(8, 128).T
        cv[:, b + CV_MIX:b + CV_MIX + 8] = inp["mix_norm"][l].reshape(8, 128).T
        cv[:, b + CV_FFN2:b + CV_FFN2 + 8] = inp["ffn2_norm"][l].reshape(8, 128).T
        cv[:, b + CV_BG:b + CV_BG + 24] = inp["b_gate"][l].reshape(24, 128).T
        cv[:, b + CV_CONV:b + CV_CONV + 48] = inp["conv_w"][l].reshape(4, 12, 128).transpose(2, 1, 0).reshape(128, 48)
        cv[:, b + CV_DN] = inp["delta_norm"][l]
        cv[:, b + CV_BF:b + CV_BF + 8] = inp["b_forget"][l][None, :]
        cv[:, b + CV_ALOG:b + CV_ALOG + 4] = inp["a_log"][l][None, :]
        cv[:, b + CV_DT:b + CV_DT + 4] = inp["dt_bias"][l][None, :]
    cv[:, DEPTH * CV_PER_LAYER:] = inp["final_norm"].reshape(8, 128).T
    return cv


def build_cmat():
    i = np.arange(128)
    ident = np.eye(128, dtype=np.float32)
    ones = np.ones((128, 128), np.float32)
    triu = (i[:, None] <= i[None, :]).astype(np.float32)
    negm = np.where(i[None, :] >= i[:, None], 0.0, -1e4).astype(np.float32)
    negc = np.where(i[None, :] <= i[:, None], 0.0, -1e30).astype(np.float32)
    z = np.zeros((128, 128), np.float32)
    strictu = (i[:, None] < i[None, :]).astype(np.float32)
    return np.stack([ident, ones, triu, negm, negc, strictu, z])


def build_pow2(nit=26):
    return np.tile((0.5 ** np.arange(1, nit + 1)).astype(np.float32)[None, :], (128, 1))


def swap_cols():
    idx = []
    for base, n in ((O_AQ, 512), (O_AK, 64), (O_IQ, 256), (O_IK, 64)):
        for j in range(n):
            d = j % 64
            hb = base + (j // 64) * 64
            if d < 8:
                idx.append(hb + d + 8)
            elif d < 16:
                idx.append(hb + d - 8)
            else:
                idx.append(hb + d)
    return np.array(idx)


def small_cols():
    return np.concatenate([np.arange(O_AV, O_AV + 64), np.arange(O_IW, O_IW + 4), np.arange(O_BF, O_BF + 8),
                           np.arange(O_CB, O_CB + 4), np.arange(O_CA, O_CA + 4)])


def build_rot(S):
    pos = np.arange(S, dtype=np.float32)
    inv = np.power(np.float32(500000.0), -np.arange(0, 16, 2, dtype=np.float32) / np.float32(16)).astype(np.float32)
    ang = (pos[:, None] * inv[None, :]).astype(np.float32)
    cos, sin = np.cos(ang).astype(np.float32), np.sin(ang).astype(np.float32)
    C = np.ones((128, S), np.float32)
    Sg = np.zeros((128, S), np.float32)
    for p in range(128):
        d = p % 64
        if d < 8:
            C[p] = cos[:, d]
            Sg[p] = -sin[:, d]
        elif d < 16:
            C[p] = cos[:, d - 8]
            Sg[p] = sin[:, d - 8]
    return C, Sg


class ProgM1(Prog):
    def declare_scratch(self):
        S = self.S
        d = self.dr
        self.qaT = d("qaT", [512, S], BF16); self.kaT = d("kaT", [64, S], BF16)
        self.qiT = d("qiT", [256, S], BF16); self.kiT = d("kiT", [64, S], BF16)
        self.vA = d("vA", [S, 64], BF16)
        self.qbT = d("qbT", [512, S], BF16); self.kbT = d("kbT", [512, S], BF16); self.vB = d("vB", [S, 512], BF16)
        self.qcT = d("qcT", [512, S], BF16); self.kcT = d("kcT", [512, S], BF16)
        self.kC = d("kC", [S, 512], BF16); self.vC = d("vC", [S, 512], BF16)
        self.szT = d("szT", [512, S], BF16)
        self.smallD = d("smallD", [S, 24], F32)
        self.gT = d("gT", [3072, S], BF16)
        self.yT = d("yT", [3, 512, S], BF16)

    def rot(self):
        self._rot = (getattr(self, "_rot", -1) + 1) % 8
        return self.ps[self._rot]

    def m1_phase(self, xT, w_in, w_sw, w_small, cvb):
        c, S, NG = self.c, self.S, self.NG
        rotC = self.dram["rotC"]; rotS = self.dram["rotS"]
        with ExitStack() as es:
            hT = c.sb(es, "hTall", [128, 8, S], BF16)
            wts = [c.sb(es, "wt", [128, 8, 512], BF16) for _ in range(3)]
            wti = [0]
            wb = self.db("w")

            def load_w(src_list):
                wt = wts[wti[0] % 3]
                wti[0] += 1
                for (ap, c0, n) in src_list:
                    c.dma("pool", wt[:, :, c0:c0 + n], ap.rearrange("(k p) n -> p k n", p=128), reads=[wb], writes=[wt])
                return wt

            def fm(wt, c0, M, g, psb, rows0=0):
                tsl = slice(g * 512, (g + 1) * 512)
                for k in range(8):
                    c.op("pe", lambda h, k=k: h.matmul(psb[rows0:rows0 + M, :], lhsT=wt[:, k, c0:c0 + M], rhs=hT[:, k, tsl], start=(k == 0), stop=(k == 7)),
                         reads=[wt, hT], writes=[psb], sig=(k == 7))

            with ExitStack() as es2:
                xgs = [c.sb(es2, "xg", [128, 8, 512], F32) for _ in range(2)]
                sq = c.sb(es2, "sq", [128, 8, 512], BF16)
                rstd = c.sb(es2, "rstd", [128, 512], F32)
                x3 = xT.rearrange("(c p) t -> p c t", p=128)
                for g in range(NG):
                    xg = xgs[g % 2]
                    tsl = slice(g * 512, (g + 1) * 512)
                    c.dma("sp", xg[:, :, :], x3[:, :, tsl], reads=[self.db(xT.name, g)], writes=[xg])
                    self.rmsnorm_group(xg, sq, lambda k, tsl=tsl: hT[:, k, tsl], hT, rstd, cvb + CV_MIX, self.rot())
                c.barrier()

            with ExitStack() as es2:
                stg = [c.sb(es2, "stg", [128, 4, 512], BF16) for _ in range(2)]
                stgi = [0]
                t1s = [c.sb(es2, "t1", [128, 512], F32) for _ in range(2)]
                t2s = [c.sb(es2, "t2", [128, 512], F32) for _ in range(2)]
                rc = [c.sb(es2, "rc", [128, 512], F32) for _ in range(2)]
                rs = [c.sb(es2, "rs", [128, 512], F32) for _ in range(2)]

                def nstg():
                    stgi[0] += 1
                    return stg[stgi[0] % 2]

                def load_rot(g):
                    tsl = slice(g * 512, (g + 1) * 512)
                    c.dma("sp", rc[g % 2][:, :], rotC[:, tsl], reads=[self.db("rot")], writes=[rc[g % 2]])
                    c.dma("sp", rs[g % 2][:, :], rotS[:, tsl], reads=[self.db("rot")], writes=[rs[g % 2]])

                def rotary(pn, psw, g, out_ap, out_buf, i):
                    t1, t2 = t1s[i % 2], t2s[i % 2]
                    c.op("dve", lambda h: h.tensor_tensor(out=t1[:, :], in0=pn[:, :], in1=rc[g % 2][:, :], op=ALU.mult), reads=[pn, rc[g % 2]], writes=[t1])
                    c.op("dve", lambda h: h.tensor_tensor(out=t2[:, :], in0=psw[:, :], in1=rs[g % 2][:, :], op=ALU.mult), reads=[psw, rs[g % 2]], writes=[t2])
                    c.op("pool", lambda h: h.tensor_tensor(out=out_ap, in0=t1[:, :], in1=t2[:, :], op=ALU.add), reads=[t1, t2], writes=[out_buf])

                wn = load_w([(w_in[:, O_AQ:O_AQ + 512], 0, 512)])
                ws = load_w([(w_sw[:, 0:512], 0, 512)])
                for g in range(NG):
                    tsl = slice(g * 512, (g + 1) * 512)
                    load_rot(g)
                    so = nstg()
                    for ch in range(4):
                        pn, psw = self.rot(), self.rot()
                        fm(wn, ch * 128, 128, g, pn)
                        fm(ws, ch * 128, 128, g, psw)
                        rotary(pn, psw, g, so[:, ch, :], so, ch)
                    c.dma("sp", self.qaT.rearrange("(c p) t -> p c t", p=128)[:, :, tsl], so[:, :, :], reads=[so], writes=[self.db("qaT", g)])
                wn = load_w([(w_in[:, O_IQ:O_IQ + 256], 0, 256), (w_in[:, O_AK:O_AK + 64], 256, 64), (w_in[:, O_IK:O_IK + 64], 320, 64)])
                ws = load_w([(w_sw[:, 576:832], 0, 256), (w_sw[:, 512:576], 256, 64), (w_sw[:, 832:896], 320, 64)])
                for g in range(NG):
                    tsl = slice(g * 512, (g + 1) * 512)
                    load_rot(g)
                    so = nstg()
                    for ch in range(3):
                        pn, psw = self.rot(), self.rot()
                        fm(wn, ch * 128, 128, g, pn)
                        fm(ws, ch * 128, 128, g, psw)
                        rotary(pn, psw, g, so[:, ch, :], so, ch)
                    c.dma("sp", self.qiT.rearrange("(c p) t -> p c t", p=128)[:, :, tsl], so[:, 0:2, :], reads=[so], writes=[self.db("qiT", g)])
                    c.dma("sp", self.kaT[:, tsl], so[0:64, 2, :], reads=[so], writes=[self.db("kaT", g)])
                    c.dma("sp", self.kiT[:, tsl], so[64:128, 2, :], reads=[so], writes=[self.db("kiT", g)])
                for (c0, dst) in ((O_BQKV, self.qbT), (O_BQKV + 512, self.kbT)):
                    wn = load_w([(w_in[:, c0:c0 + 512], 0, 512)])
                    for g in range(NG):
                        tsl = slice(g * 512, (g + 1) * 512)
                        so = nstg()
                        for ch in range(4):
                            pn = self.rot()
                            fm(wn, ch * 128, 128, g, pn)
                            c.op("act", lambda h, pn=pn, ch=ch, so=so: h.activation(out=so[:, ch, :], in_=pn[:, :], func=AF.Copy), reads=[pn], writes=[so])
                        c.dma("sp", dst.rearrange("(c p) t -> p c t", p=128)[:, :, tsl], so[:, :, :], reads=[so], writes=[self.db(dst.name, g)])
                wn = load_w([(w_in[:, O_BQKV + 1024:O_BQKV + 1536], 0, 512)])
                for g in range(NG):
                    so = nstg()
                    for tt in range(4):
                        pn = self.rot()
                        t0 = g * 512 + tt * 128
                        for k in range(8):
                            c.op("pe", lambda h, k=k, pn=pn, t0=t0: h.matmul(pn[:, :], lhsT=hT[:, k, t0:t0 + 128], rhs=wn[:, k, :], start=(k == 0), stop=(k == 7)),
                                 reads=[wn, hT], writes=[pn], sig=(k == 7))
                        c.op("act", lambda h, pn=pn, tt=tt, so=so: h.activation(out=so[:, tt, :], in_=pn[:, :], func=AF.Copy), reads=[pn], writes=[so])
                    c.dma("sp", self.vB.rearrange("(n p) d -> p n d", p=128)[:, g * 4:(g + 1) * 4, :], so[:, :, :], reads=[so], writes=[self.db("vB", g)])
                with ExitStack() as es3:
                    xcs = [c.sb(es3, "xc", [128, 515], F32) for _ in range(4)]
                    accs = [c.sb(es3, "acc", [128, 512], F32) for _ in range(2)]
                    sls = [c.sb(es3, "sl", [128, 512], F32) for _ in range(2)]
                    sqb = [c.sb(es3, "sqb", [128, 512], BF16) for _ in range(2)]
                    rr = [c.sb(es3, "rr", [128, 512], F32) for _ in range(2)]
                    tok = [c.sb(es3, "tok", [128, 4, 512], BF16) for _ in range(2)]
                    for sec, (dstT, dstTok) in enumerate(((self.qcT, None), (self.kcT, self.kC), (None, self.vC))):
                        c0 = O_CQKV + sec * 512
                        wn = load_w([(w_in[:, c0:c0 + 512], 0, 512)])
                        for j in range(4):
                            c.op("pool", lambda h, j=j: h.memset(xcs[j][:, 0:3], 0.0), writes=[xcs[j]])
                        for g in range(NG):
                            tsl = slice(g * 512, (g + 1) * 512)
                            so = nstg()
                            for j in range(4):
                                ch = sec * 4 + j
                                pn = self.rot()
                                fm(wn, j * 128, 128, g, pn)
                                xc = xcs[j]
                                acc, sl = accs[j % 2], sls[j % 2]
                                wc = cvb + CV_CONV + ch * 4
                                c.op("act", lambda h, pn=pn, xc=xc: h.activation(out=xc[:, 3:515], in_=pn[:, :], func=AF.Copy), reads=[pn], writes=[xc])
                                c.op("dve", lambda h, xc=xc, acc=acc, wc=wc: h.tensor_scalar(out=acc[:, :], in0=xc[:, 3:515], scalar1=self.cvec[:, wc + 3:wc + 4], scalar2=None, op0=ALU.mult),
                                     reads=[xc, self.cvec], writes=[acc])
                                for tap in (2, 1, 0):
                                    c.op("dve", lambda h, xc=xc, acc=acc, wc=wc, tap=tap: h.scalar_tensor_tensor(out=acc[:, :], in0=xc[:, tap:tap + 512], scalar=self.cvec[:, wc + tap:wc + tap + 1],
                                                                                                         in1=acc[:, :], op0=ALU.mult, op1=ALU.add),
                                         reads=[xc, acc, self.cvec], writes=[acc])
                                c.op("pool", lambda h, xc=xc: h.tensor_copy(out=xc[:, 0:3], in_=xc[:, 512:515]), reads=[xc], writes=[xc])
                                if sec == 2:
                                    c.op("act", lambda h, acc=acc, so=so, j=j: h.activation(out=so[:, j, :], in_=acc[:, :], func=AF.Silu), reads=[acc], writes=[so])
                                else:
                                    c.op("act", lambda h, acc=acc, sl=sl: h.activation(out=sl[:, :], in_=acc[:, :], func=AF.Silu), reads=[acc], writes=[sl])
                                    sb_, r_ = sqb[j % 2], rr[j % 2]
                                    c.op("act", lambda h, sl=sl, sb_=sb_: h.activation(out=sb_[:, :], in_=sl[:, :], func=AF.Square), reads=[sl], writes=[sb_])
                                    p2 = self.rot()
                                    c.op("pe", lambda h, p2=p2, sb_=sb_: h.matmul(p2[:, :], lhsT=self.ones_b[:, :], rhs=sb_[:, :], start=True, stop=True), reads=[self.ones_b, sb_], writes=[p2])
                                    c.op("dve", lambda h, p2=p2, r_=r_: h.tensor_scalar(out=r_[:, :], in0=p2[:, :], scalar1=1e-6, scalar2=None, op0=ALU.add), reads=[p2], writes=[r_])
                                    c.op("act", lambda h, r_=r_: h.activation(out=r_[:, :], in_=r_[:, :], func=AF.Ln), reads=[r_], writes=[r_])
                                    c.op("act", lambda h, r_=r_: h.activation(out=r_[:, :], in_=r_[:, :], func=AF.Exp, scale=-0.5), reads=[r_], writes=[r_])
                                    qs = float(128 ** -0.5) if sec == 0 else 1.0
                                    c.op("dve", lambda h, sl=sl, r_=r_, so=so, j=j, qs=qs: h.scalar_tensor_tensor(out=so[:, j, :], in0=sl[:, :], scalar=qs, in1=r_[:, :], op0=ALU.mult, op1=ALU.mult),
                                         reads=[sl, r_], writes=[so])
                            if dstT is not None:
                                c.dma("sp", dstT.rearrange("(c p) t -> p c t", p=128)[:, :, tsl], so[:, :, :], reads=[so], writes=[self.db(dstT.name, g)])
                            if dstTok is not None:
                                tk = tok[g % 2]
                                for tt in range(4):
                                    pt = self.rot()
                                    for j in range(4):
                                        c.op("pe", lambda h, pt=pt, j=j, tt=tt, so=so: h.matmul(pt[:, j * 128:(j + 1) * 128], lhsT=so[:, j, tt * 128:(tt + 1) * 128], rhs=self.ident_b[:, :], start=True, stop=True),
                                             reads=[so, self.ident_b], writes=[pt], sig=(j == 3))
                                    c.op("act", lambda h, pt=pt, tk=tk, tt=tt: h.activation(out=tk[:, tt, :], in_=pt[:, :], func=AF.Copy), reads=[pt], writes=[tk])
                                c.dma("sp", dstTok.rearrange("(n p) d -> p n d", p=128)[:, g * 4:(g + 1) * 4, :], tk[:, :, :], reads=[tk], writes=[self.db(dstTok.name, g)])
                wn = load_w([(w_in[:, O_CZ:O_CZ + 512], 0, 512)])
                for g in range(NG):
                    tsl = slice(g * 512, (g + 1) * 512)
                    so = nstg()
                    for ch in range(4):
                        pn = self.rot()
                        fm(wn, ch * 128, 128, g, pn)
                        c.op("act", lambda h, pn=pn, ch=ch, so=so: h.activation(out=so[:, ch, :], in_=pn[:, :], func=AF.Silu), reads=[pn], writes=[so])
                    c.dma("sp", self.szT.rearrange("(c p) t -> p c t", p=128)[:, :, tsl], so[:, :, :], reads=[so], writes=[self.db("szT", g)])
                for sec in range(6):
                    wn = load_w([(w_in[:, O_G + sec * 512:O_G + (sec + 1) * 512], 0, 512)])
                    for g in range(NG):
                        tsl = slice(g * 512, (g + 1) * 512)
                        so = nstg()
                        for ch in range(4):
                            pn = self.rot()
                            fm(wn, ch * 128, 128, g, pn)
                            bc = cvb + CV_BG + sec * 4 + ch
                            c.op("act", lambda h, pn=pn, ch=ch, so=so, bc=bc: h.activation(out=so[:, ch, :], in_=pn[:, :], func=AF.Sigmoid, bias=self.cvec[:, bc:bc + 1]),
                                 reads=[pn, self.cvec], writes=[so])
                        c.dma("sp", self.gT.rearrange("(c p) t -> p c t", p=128)[:, sec * 4:(sec + 1) * 4, tsl], so[:, :, :], reads=[so], writes=[self.db("gT", sec * 100 + g)])
                with ExitStack() as es3:
                    wsm = c.sb(es3, "wsm", [128, 8, 84], BF16)
                    c.dma("pool", wsm[:, :, :], w_small.rearrange("(k p) n -> p k n", p=128), reads=[wb], writes=[wsm])
                    negA = c.sb(es3, "negA", [128, 4], F32)
                    c.op("act", lambda h: h.activation(out=negA[:, :], in_=self.cvec[:, cvb + CV_ALOG:cvb + CV_ALOG + 4], func=AF.Exp), reads=[self.cvec], writes=[negA])
                    c.op("dve", lambda h: h.tensor_scalar(out=negA[:, :], in0=negA[:, :], scalar1=-1.0, scalar2=None, op0=ALU.mult), reads=[negA], writes=[negA])
                    sms = [c.sb(es3, "sm", [128, 4, 24], F32) for _ in range(2)]
                    vas = [c.sb(es3, "vas", [128, 4, 64], BF16) for _ in range(2)]
                    tmp = [c.sb(es3, "tmps", [128, 16], F32) for _ in range(2)]
                    for g in range(NG):
                        sm, va = sms[g % 2], vas[g % 2]
                        for tt in range(4):
                            pn = self.rot()
                            t0 = g * 512 + tt * 128
                            tp = tmp[tt % 2]
                            for k in range(8):
                                c.op("pe", lambda h, k=k, pn=pn, t0=t0: h.matmul(pn[:, 0:84], lhsT=hT[:, k, t0:t0 + 128], rhs=wsm[:, k, :], start=(k == 0), stop=(k == 7)),
                                     reads=[wsm, hT], writes=[pn], sig=(k == 7))
                            c.op("act", lambda h, pn=pn, va=va, tt=tt: h.activation(out=va[:, tt, :], in_=pn[:, 0:64], func=AF.Copy), reads=[pn], writes=[va])
                            c.op("dve", lambda h, pn=pn, sm=sm, tt=tt: h.tensor_scalar(out=sm[:, tt, 0:4], in0=pn[:, 64:68], scalar1=1.0 / 16.0, scalar2=None, op0=ALU.mult), reads=[pn], writes=[sm])
                            c.op("dve", lambda h, pn=pn, tp=tp: h.tensor_tensor(out=tp[:, 0:8], in0=pn[:, 68:76], in1=self.cvec[:, cvb + CV_BF:cvb + CV_BF + 8], op=ALU.add), reads=[pn, self.cvec], writes=[tp])
                            c.op("dve", lambda h, pn=pn, tp=tp: h.tensor_tensor(out=tp[:, 8:12], in0=pn[:, 80:84], in1=self.cvec[:, cvb + CV_DT:cvb + CV_DT + 4], op=ALU.add), reads=[pn, self.cvec, tp], writes=[tp])
                            c.op("act", lambda h, tp=tp: h.activation(out=tp[:, 0:8], in_=tp[:, 0:8], func=AF.Exp, scale=-1.0), reads=[tp], writes=[tp])
                            c.op("act", lambda h, tp=tp: h.activation(out=tp[:, 8:12], in_=tp[:, 8:12], func=AF.Exp), reads=[tp], writes=[tp])
                            c.op("act", lambda h, tp=tp: h.activation(out=tp[:, 0:12], in_=tp[:, 0:12], func=AF.Ln, bias=1.0), reads=[tp], writes=[tp])
                            c.op("dve", lambda h, tp=tp, sm=sm, tt=tt: h.tensor_scalar(out=sm[:, tt, 4:12], in0=tp[:, 0:8], scalar1=-1.0, scalar2=None, op0=ALU.mult), reads=[tp], writes=[sm])
                            c.op("dve", lambda h, tp=tp, sm=sm, tt=tt: h.tensor_tensor(out=sm[:, tt, 16:20], in0=tp[:, 8:12], in1=negA[:, :], op=ALU.mult), reads=[tp, negA, sm], writes=[sm])
                            c.op("act", lambda h, pn=pn, sm=sm, tt=tt: h.activation(out=sm[:, tt, 12:16], in_=pn[:, 76:80], func=AF.Sigmoid), reads=[pn, sm], writes=[sm])
                        c.dma("sp", self.smallD.rearrange("(n p) c -> p n c", p=128)[:, g * 4:(g + 1) * 4, 0:20], sm[:, :, 0:20], reads=[sm], writes=[self.db("smallD", g)])
                        c.dma("sp", self.vA.rearrange("(n p) d -> p n d", p=128)[:, g * 4:(g + 1) * 4, :], va[:, :, :], reads=[va], writes=[self.db("vA", g)])
                c.barrier()


class ProgAB(ProgM1):
    def rotset(self, key, banks):
        d = self.__dict__.setdefault("_rs", {})
        d[key] = (d.get(key, -1) + 1) % len(banks)
        return self.ps[banks[d[key]]]

    def _b_setup(self, es, lbanks, pbanks):
        c, S, NT, NG = self.c, self.S, self.NT, self.NG
        qbh = [c.sb(es, "qbh", [128, S], BF16) for _ in range(2)]
        kbh = [c.sb(es, "kbh", [128, S], BF16) for _ in range(2)]
        vext = [c.sb(es, "vext", [128, NT, 128], BF16) for _ in range(2)]
        lf = c.sb(es, "lf", [128, NT, 8], F32)
        lfacc = c.sb(es, "lfacc", [128, NT + 1, 8], F32)
        csb = c.sb(es, "csb", [128, NT, 8], F32)
        carry = c.sb(es, "carry", [128, NT, 8], F32)
        npair = NT * (NT + 1) // 2
        bias = c.sb(es, "biasall", [128, npair, 8], F32)
        pts = [c.sb(es, "pt", [128, 512], BF16) for _ in range(4)]
        ptq = [[Buf(name=f"ptq{a}_{b}") for b in range(4)] for a in range(4)]
        rsum = [c.sb(es, "rsum", [64, 512], F32) for _ in range(2)]
        nums = [c.sb(es, "bnum", [64, 512], F32) for _ in range(2)]
        yst = [c.sb(es, "yst", [64, 512], BF16) for _ in range(2)]
        allg = lambda nm: [self.db(nm, g) for g in range(NG)]
        c.dma("sp", lf[:, :, :], self.smallD.rearrange("(n p) c -> p n c", p=128)[:, :, 4:12], reads=allg("smallD"), writes=[lf])
        for v in vext:
            c.op("pool", lambda h, v=v: h.memset(v[:, :, 64:128], 1.0), writes=[v])
        c.op("pool", lambda h: h.memset(lfacc[:, 0, :], 0.0), writes=[lfacc])
        for n in range(NT):
            c.op("dve", lambda h, n=n: h.tensor_tensor(out=lfacc[:, n + 1, :], in0=lfacc[:, n, :], in1=lf[:, n, :], op=ALU.add), reads=[lfacc, lf], writes=[lfacc])
        for n in range(NT):
            pb = self.rotset("bpl", lbanks)
            c.op("pe", lambda h, n=n, pb=pb: h.matmul(pb[:, 0:8], lhsT=self.triu_f[:, :], rhs=lf[:, n, :], start=True, stop=False), reads=[self.triu_f, lf], writes=[pb], sig=False)
            c.op("pe", lambda h, n=n, pb=pb: h.matmul(pb[:, 0:8], lhsT=self.ones_f[:, :], rhs=lfacc[:, n, :], start=False, stop=True), reads=[self.ones_f, lfacc], writes=[pb])
            c.op("act", lambda h, n=n, pb=pb: h.activation(out=csb[:, n, :], in_=pb[:, 0:8], func=AF.Copy), reads=[pb], writes=[csb])
            pb = self.rotset("bpl", lbanks)
            c.op("pe", lambda h, n=n, pb=pb: h.matmul(pb[:, 0:8], lhsT=self.ones_f[:, :], rhs=lfacc[:, n, :], start=True, stop=True), reads=[self.ones_f, lfacc], writes=[pb])
            c.op("act", lambda h, n=n, pb=pb: h.activation(out=carry[:, n, :], in_=pb[:, 0:8], func=AF.Copy), reads=[pb], writes=[carry])
        pidx = {}
        pi = 0
        for i in range(NT):
            for j in range(i + 1):
                pidx[(i, j)] = pi
                c.op("pool", lambda h, i=i, j=j, pi=pi: h.tensor_tensor(out=bias[:, pi, :], in0=carry[:, i, :], in1=csb[:, j, :], op=ALU.subtract), reads=[carry, csb], writes=[bias])
                pi += 1
        vB3 = self.vB.rearrange("(n p) d -> p n d", p=128)
        yb = self.yT[1]
        steps = [(hh, g, j) for hh in range(8) for g in range(NG) for j in range(4 * g + 4)]
        pls = {}
        loaded = set()

        def load_pair(hp):
            if hp in loaded or hp >= 4:
                return
            loaded.add(hp)
            c.dma("pool", qbh[hp % 2][:, :], self.qbT[hp * 128:(hp + 1) * 128, :], reads=allg("qbT"), writes=[qbh[hp % 2]])
            c.dma("pool", kbh[hp % 2][:, :], self.kbT[hp * 128:(hp + 1) * 128, :], reads=allg("kbT"), writes=[kbh[hp % 2]])

        def logits(k):
            hh, g, j = steps[k]
            hb, hp = (hh % 2) * 64, hh // 2
            load_pair(hp)
            qb, kb = qbh[hp % 2], kbh[hp % 2]
            col0 = max(j - 4 * g, 0) * 128
            pl = self.rotset("bpl", lbanks)
            pls[k] = pl
            c.op("pe", lambda h: h.matmul(pl[:, col0:512], lhsT=kb[hb:hb + 64, j * 128:(j + 1) * 128],
                                          rhs=qb[hb:hb + 64, g * 512 + col0:(g + 1) * 512], start=True, stop=True),
                 reads=[kb, qb], writes=[pl])

        def gen():
            po = None
            logits(0)
            for k, (hh, g, j) in enumerate(steps):
                ve = vext[hh % 2]
                if g == 0 and j == 0:
                    c.dma("pool", ve[:, :, 0:64], vB3[:, :, hh * 64:(hh + 1) * 64], reads=allg("vB"), writes=[ve])
                if j == 0:
                    po = self.rotset("bpo", pbanks)
                if k + 1 < len(steps):
                    logits(k + 1)
                nj = 4 * g + 4
                r = j - 4 * g
                col0 = max(r, 0) * 128
                pl = pls.pop(k)
                pt = pts[j % 4]
                for qq in range(max(r, 0), 4):
                    pi = pidx[(4 * g + qq, j)]
                    qs_ = slice(qq * 128, (qq + 1) * 128)
                    c.op("act", lambda h, pl=pl, pt=pt, qs_=qs_, pi=pi, hh=hh: h.activation(out=pt[:, qs_], in_=pl[:, qs_], func=AF.Exp, scale=0.125, bias=bias[:, pi, hh:hh + 1]),
                         reads=[pl, bias], writes=[ptq[j % 4][qq]])
                if r >= 0:
                    c.op("pool", lambda h, pt=pt, col0=col0: h.tensor_tensor(out=pt[:, col0:col0 + 128], in0=pt[:, col0:col0 + 128], in1=self.tri01_b[:, :], op=ALU.mult),
                         reads=[ptq[j % 4][r], self.tri01_b], writes=[ptq[j % 4][r]])
                c.op("pe", lambda h, po=po, pt=pt, j=j, col0=col0, nj=nj, ve=ve: h.matmul(po[:, col0:512], lhsT=ve[:, j, :], rhs=pt[:, col0:512], start=(j == 0), stop=(j == nj - 1)),
                     reads=[ve] + ptq[j % 4][max(r, 0):4], writes=[po], sig=(j == nj - 1))
                if j == nj - 1:
                    rs_, ys_, nm_ = rsum[g % 2], yst[g % 2], nums[g % 2]
                    c.op("act", lambda h, po=po, rs_=rs_: h.activation(out=rs_[:, :], in_=po[64:128, :], func=AF.Copy), reads=[po], writes=[rs_])
                    c.op("act", lambda h, po=po, nm_=nm_: h.activation(out=nm_[:, :], in_=po[0:64, :], func=AF.Copy), reads=[po], writes=[nm_])
                    c.op("act", lambda h, rs_=rs_: h.activation(out=rs_[:, :], in_=rs_[:, :], func=AF.Ln), reads=[rs_], writes=[rs_])
                    c.op("act", lambda h, rs_=rs_: h.activation(out=rs_[:, :], in_=rs_[:, :], func=AF.Exp, scale=-1.0), reads=[rs_], writes=[rs_])
                    c.op("pool", lambda h, nm_=nm_, rs_=rs_, ys_=ys_: h.tensor_tensor(out=ys_[:, :], in0=nm_[:, :], in1=rs_[:, :], op=ALU.mult), reads=[nm_, rs_], writes=[ys_])
                    c.dma("pool", yb[hh * 64:(hh + 1) * 64, g * 512:(g + 1) * 512], ys_[:, :], reads=[ys_], writes=[self.db("yT1", hh * 100 + g)])
                yield k
        return gen(), len(steps)

    def b_phase(self):
        with ExitStack() as es:
            g, n = self._b_setup(es, [0, 1, 2, 3, 4, 5], [6, 7])
            for _ in g:
                pass
            self.c.barrier()

    NITER = 16
    TIE = True

    def _a_setup(self, es, sbanks, lpairs, pvpair):
        c, S, NT, NG = self.c, self.S, self.NT, self.NG
        NIT = self.NITER
        qi = c.sb(es, "qi", [128, 2, S], BF16)
        ki = c.sb(es, "ki", [128, S], BF16)
        ka = c.sb(es, "ka", [64, S], BF16)
        vext = c.sb(es, "vexta", [128, NT, 128], BF16)
        wi = c.sb(es, "wi", [128, NT, 4], F32)
        qat = [c.sb(es, "qat", [64, 1024], BF16) for _ in range(2)]
        score = c.sb(es, "score", [128, S], F32)
        isz = c.sb(es, "isz", [128, S], BF16)
        zrk = c.sb(es, "zrk", [128, S], BF16)
        maskb = [c.sb(es, "maskb", [128, S], BF16) for _ in range(2)]
        irep = c.sb(es, "irep", [128, 512], BF16)
        negc = c.sb(es, "negc", [128, 128], F32)
        pow2 = c.sb(es, "pow2", [128, NIT], F32)
        rts = [c.sb(es, "rt", [128, 512], F32) for _ in range(3)]
        pts = [c.sb(es, "pta", [128, 1024], BF16) for _ in range(3)]
        sm = c.sb(es, "bis", [128, 16], F32)
        steps = c.sb(es, "steps", [128, NIT], F32)
        rsum = c.sb(es, "rsuma", [64, 1024], F32)
        yst = [c.sb(es, "ysta", [64, 1024], BF16) for _ in range(2)]
        cm = self.dram["cmat"]
        cb = self.db("cmat")
        for r in range(4):
            c.dma("pool", irep[:, r * 128:(r + 1) * 128], cm[0], reads=[cb], writes=[irep])
        c.dma("sp", negc[:, :], cm[4], reads=[cb], writes=[negc])
        c.dma("sp", pow2[:, :], self.dram["pow2"][:, 0:NIT], reads=[self.db("pow2")], writes=[pow2])
        allg = lambda nm: [self.db(nm, g) for g in range(NG)]
        c.dma("sp", qi[:, :, :], self.qiT.rearrange("(hp p) t -> p hp t", p=128), reads=allg("qiT"), writes=[qi])
        c.dma("sp", ki[0:64, :], self.kiT[:, :], reads=allg("kiT"), writes=[ki])
        c.dma("sp", ki[64:128, :], self.kiT[:, :], reads=allg("kiT"), writes=[ki])
        c.dma("sp", ka[:, :], self.kaT[:, :], reads=allg("kaT"), writes=[ka])
        c.dma("sp", vext[:, :, 0:64], self.vA.rearrange("(n p) d -> p n d", p=128), reads=allg("vA"), writes=[vext])
        c.op("pool", lambda h: h.memset(vext[:, :, 64:128], 1.0), writes=[vext])
        c.dma("sp", wi[:, :, :], self.smallD.rearrange("(n p) c -> p n c", p=128)[:, :, 0:4], reads=allg("smallD"), writes=[wi])
        qa3 = self.qaT.rearrange("(h d) t -> d h t", d=64)
        ya = self.yT[0].rearrange("(h d) t -> d h t", d=64)
        NEGM = -32768.0

        def stage1(i):
            ncols = (i + 1) * 128
            qt = qat[i % 2]
            mb = maskb[i % 2]
            c.dma("sp", qt[:, :].rearrange("d (h t) -> d h t", h=8), qa3[:, :, i * 128:(i + 1) * 128], reads=allg("qaT"), writes=[qt])
            for s0 in range(0, ncols, 512):
                w = min(512, ncols - s0)
                for hd in range(4):
                    pl = self.rotset("apl", sbanks)
                    hb, hp = (hd % 2) * 64, hd // 2
                    c.op("pe", lambda h, pl=pl, hb=hb, hp=hp, s0=s0, w=w: h.matmul(pl[:, 0:w], lhsT=qi[hb:hb + 64, hp, i * 128:(i + 1) * 128], rhs=ki[hb:hb + 64, s0:s0 + w], start=True, stop=True),
                         reads=[qi, ki], writes=[pl])
                    if hd == 0:
                        c.op("dve", lambda h, pl=pl, s0=s0, w=w: h.tensor_scalar(out=score[:, s0:s0 + w], in0=pl[:, 0:w], scalar1=0.0, scalar2=wi[:, i, 0:1], op0=ALU.max, op1=ALU.mult),
                             reads=[pl, wi], writes=[score])
                    else:
                        rt = rts[hd - 1]
                        c.op("act", lambda h, pl=pl, rt=rt, w=w: h.activation(out=rt[:, 0:w], in_=pl[:, 0:w], func=AF.Relu), reads=[pl], writes=[rt])
                        c.op("dve", lambda h, rt=rt, hd=hd, s0=s0, w=w: h.scalar_tensor_tensor(out=score[:, s0:s0 + w], in0=rt[:, 0:w], scalar=wi[:, i, hd:hd + 1], in1=score[:, s0:s0 + w],
                                                                                            op0=ALU.mult, op1=ALU.add),
                             reads=[rt, wi, score], writes=[score])
            sc = score[:, 0:ncols]
            c.op("dve", lambda h: h.tensor_reduce(out=sm[:, 5:6], in_=sc, axis=AX.X, op=ALU.max), reads=[score], writes=[sm])
            c.op("dve", lambda h: h.tensor_reduce(out=sm[:, 6:7], in_=sc, axis=AX.X, op=ALU.min), reads=[score, sm], writes=[sm])
            c.op("dve", lambda h: h.tensor_tensor(out=score[:, i * 128:ncols], in0=score[:, i * 128:ncols], in1=negc[:, :], op=ALU.add), reads=[score, negc], writes=[score])
            c.op("dve", lambda h: h.tensor_scalar(out=sm[:, 0:1], in0=sm[:, 6:7], scalar1=-1.0, scalar2=None, op0=ALU.add), reads=[sm], writes=[sm])
            c.op("dve", lambda h: h.scalar_tensor_tensor(out=sm[:, 1:2], in0=sm[:, 5:6], scalar=1.0, in1=sm[:, 0:1], op0=ALU.add, op1=ALU.subtract), reads=[sm], writes=[sm])
            c.op("dve", lambda h: h.tensor_scalar(out=steps[:, :], in0=pow2[:, :], scalar1=sm[:, 1:2], scalar2=None, op0=ALU.mult), reads=[sm, pow2], writes=[steps])
            for k in range(NIT):
                c.op("dve", lambda h, k=k: h.tensor_tensor(out=sm[:, 2:3], in0=sm[:, 0:1], in1=steps[:, k:k + 1], op=ALU.add), reads=[sm, steps], writes=[sm])
                c.op("dve", lambda h: h.tensor_scalar(out=isz[:, 0:ncols], in0=sc, scalar1=sm[:, 2:3], scalar2=0.0, op0=ALU.is_ge, op1=ALU.add, accum_out=sm[:, 3:4]),
                     reads=[score, sm], writes=[isz, sm])
                c.op("dve", lambda h, k=k: h.scalar_tensor_tensor(out=sm[:, 4:5], in0=sm[:, 3:4], scalar=255.5, in1=steps[:, k:k + 1], op0=ALU.is_ge, op1=ALU.mult), reads=[sm, steps], writes=[sm])
                c.op("dve", lambda h: h.tensor_tensor(out=sm[:, 0:1], in0=sm[:, 0:1], in1=sm[:, 4:5], op=ALU.add), reads=[sm], writes=[sm])
            c.op("dve", lambda h: h.tensor_scalar(out=isz[:, 0:ncols], in0=sc, scalar1=0.0, scalar2=0.0, op0=ALU.is_gt, op1=ALU.add, accum_out=sm[:, 7:8]), reads=[score, sm], writes=[isz, sm])
            c.op("dve", lambda h: h.tensor_scalar(out=isz[:, 0:ncols], in0=sc, scalar1=0.0, scalar2=0.0, op0=ALU.is_equal, op1=ALU.add, accum_out=sm[:, 8:9]), reads=[score, sm], writes=[isz, sm])
            c.op("dve", lambda h: h.tensor_tensor(out=sm[:, 8:9], in0=sm[:, 8:9], in1=sm[:, 7:8], op=ALU.add), reads=[sm], writes=[sm])
            c.op("dve", lambda h: h.tensor_scalar(out=sm[:, 13:14], in0=sm[:, 7:8], scalar1=255.5, scalar2=None, op0=ALU.is_lt), reads=[sm], writes=[sm])
            c.op("dve", lambda h: h.scalar_tensor_tensor(out=sm[:, 9:10], in0=sm[:, 8:9], scalar=255.5, in1=sm[:, 13:14], op0=ALU.is_ge, op1=ALU.mult), reads=[sm], writes=[sm])
            c.op("dve", lambda h: h.tensor_scalar(out=sm[:, 10:11], in0=sm[:, 7:8], scalar1=-1.0, scalar2=256.5, op0=ALU.mult, op1=ALU.add), reads=[sm], writes=[sm])
            c.op("dve", lambda h: h.tensor_scalar(out=sm[:, 13:14], in0=sm[:, 9:10], scalar1=-1.0, scalar2=1.0, op0=ALU.mult, op1=ALU.add), reads=[sm], writes=[sm])
            c.op("dve", lambda h: h.tensor_tensor(out=sm[:, 13:14], in0=sm[:, 13:14], in1=sm[:, 0:1], op=ALU.mult), reads=[sm], writes=[sm])
            c.op("dve", lambda h: h.scalar_tensor_tensor(out=sm[:, 11:12], in0=sm[:, 9:10], scalar=1e-30, in1=sm[:, 13:14], op0=ALU.mult, op1=ALU.add), reads=[sm], writes=[sm])
            c.op("dve", lambda h: h.tensor_scalar(out=sm[:, 12:13], in0=sm[:, 9:10], scalar1=-NEGM, scalar2=None, op0=ALU.mult), reads=[sm], writes=[sm])
            c.op("dve", lambda h: h.tensor_tensor_scan(out=zrk[:, 0:ncols], data0=isz[:, 0:ncols], data1=isz[:, 0:ncols], initial=0.0, op0=ALU.add, op1=ALU.max), reads=[isz], writes=[zrk])
            c.op("dve", lambda h: h.scalar_tensor_tensor(out=isz[:, 0:ncols], in0=zrk[:, 0:ncols], scalar=sm[:, 10:11], in1=isz[:, 0:ncols], op0=ALU.is_le, op1=ALU.mult), reads=[zrk, sm, isz], writes=[isz])
            c.op("dve", lambda h, mb=mb: h.tensor_scalar(out=mb[:, 0:ncols], in0=sc, scalar1=sm[:, 11:12], scalar2=NEGM, op0=ALU.is_lt, op1=ALU.mult), reads=[score, sm], writes=[mb])
            if self.TIE:
                c.op("dve", lambda h, mb=mb: h.scalar_tensor_tensor(out=mb[:, 0:ncols], in0=isz[:, 0:ncols], scalar=sm[:, 12:13], in1=mb[:, 0:ncols], op0=ALU.mult, op1=ALU.add), reads=[isz, sm, mb], writes=[mb])

        def stage2(i):
            qt = qat[i % 2]
            mb = maskb[i % 2]
            po = (self.ps[pvpair[0]], self.ps[pvpair[1]])
            lb = [b_ for pr in lpairs for b_ in pr]
            nlb = len(lb)
            hsteps = [(j, half) for j in range(i + 1) for half in range(2)]

            def alog(k):
                j, half = hsteps[k]
                pl = self.ps[lb[k % nlb]]
                c.op("pe", lambda h: h.matmul(pl[:, :], lhsT=ka[:, j * 128:(j + 1) * 128], rhs=qt[:, half * 512:(half + 1) * 512], start=True, stop=False),
                     reads=[ka, qt], writes=[pl], sig=False)
                c.op("pe", lambda h: h.matmul(pl[:, :], lhsT=mb[:, j * 128:(j + 1) * 128], rhs=irep[:, :], start=False, stop=True),
                     reads=[mb, irep], writes=[pl])

            ptb = [[Buf(name=f"apt{a_}_{b_}") for b_ in range(2)] for a_ in range(3)]
            la = min(nlb - 1, 3)
            for k in range(min(la, len(hsteps))):
                alog(k)
            for k, (j, half) in enumerate(hsteps):
                if k + la < len(hsteps):
                    alog(k + la)
                pl = self.ps[lb[k % nlb]]
                pt = pts[j % 3]
                c.op("act", lambda h, pl=pl, half=half, pt=pt: h.activation(out=pt[:, half * 512:(half + 1) * 512], in_=pl[:, :], func=AF.Exp, scale=0.125), reads=[pl], writes=[self._aptb(pt, half)])
                c.op("pe", lambda h, half=half, j=j, pt=pt, po=po: h.matmul(po[half][:, :], lhsT=vext[:, j, :], rhs=pt[:, half * 512:(half + 1) * 512], start=(j == 0), stop=(j == i)),
                     reads=[vext, self._aptb(pt, half)], writes=[po[half]], sig=(j == i))
            ys_ = yst[i % 2]
            for half in range(2):
                hs = slice(half * 512, (half + 1) * 512)
                c.op("act", lambda h, half=half, hs=hs: h.activation(out=rsum[:, hs], in_=po[half][64:128, :], func=AF.Copy), reads=[po[half]], writes=[rsum])
                c.op("act", lambda h, hs=hs: h.activation(out=rsum[:, hs], in_=rsum[:, hs], func=AF.Ln), reads=[rsum], writes=[rsum])
                c.op("act", lambda h, hs=hs: h.activation(out=rsum[:, hs], in_=rsum[:, hs], func=AF.Exp, scale=-1.0), reads=[rsum], writes=[rsum])
                c.op("dve", lambda h, half=half, hs=hs, ys_=ys_: h.tensor_tensor(out=ys_[:, hs], in0=po[half][0:64, :], in1=rsum[:, hs], op=ALU.mult), reads=[po[half], rsum], writes=[ys_])
            c.dma("sp", ya[:, :, i * 128:(i + 1) * 128], ys_[:, :].rearrange("d (h t) -> d h t", h=8), reads=[ys_], writes=[self.db("yT0", i)])

        return stage1, stage2

    def _aptb(self, pt, half):
        d = self.__dict__.setdefault("_aptbufs", {})
        k = (id(pt), half)
        if k not in d:
            d[k] = Buf(name=f"aptb{len(d)}")
        return d[k]

    def a_phase(self):
        NT = self.NT
        with ExitStack() as es:
            stage1, stage2 = self._a_setup(es, [0, 1], [(2, 3), (4, 5)], (6, 7))
            stage1(0)
            for i in range(NT):
                if i + 1 < NT:
                    stage1(i + 1)
                stage2(i)
            self.c.barrier()

    def ab_phase(self):
        NT = self.NT
        with ExitStack() as es:
            stage1, stage2 = self._a_setup(es, [0, 1, 2], [(1, 2)], (3, 4))
            bgen, nb = self._b_setup(es, [5, 6], [7])
            done = 0

            def advance(upto):
                nonlocal done
                while done < min(upto, nb):
                    next(bgen)
                    done += 1
            stage1(0)
            for i in range(NT):
                if i + 1 < NT:
                    stage1(i + 1)
                stage2(i)
                advance((nb * (i + 1) + NT - 1) // NT)
            advance(nb)
            self.c.barrier()


def build_gmask():
    i = np.arange(128)
    out = np.zeros((14, 128, 512), np.float32)
    for l in range(7):
        b = 1 << l
        t, tp = i[:, None], i[None, :]
        m = ((t // (2 * b)) == (tp // (2 * b))) & ((t % (2 * b)) >= b) & ((tp % (2 * b)) < b)
        m = m.astype(np.float32)
        out[l] = np.tile(m, (1, 4))
        out[7 + l] = np.tile(m.T, (1, 4))
    return out


class ProgC(ProgAB):
    def c_phase(self, cvb):
        c, S, NT = self.c, self.S, self.NT
        with ExitStack() as es:
            def T(name, shape, dt, n=1):
                return [c.sb(es, name, shape, dt) for _ in range(n)]
            gm = T("gm", [128, 14, 512], BF16)[0]
            identrep = T("identrep", [128, 512], BF16)[0]
            negm4 = T("negm4", [128, 512], F32)[0]
            strict4 = T("strict4", [128, 512], BF16)[0]
            cm = self.dram["cmat"]
            cb = self.db("cmat")
            c.dma("pool", gm[:, :, :], self.dram["gmask"].rearrange("l p n -> p l n"), reads=[self.db("gmask")], writes=[gm])
            for r in range(4):
                c.dma("pool", identrep[:, r * 128:(r + 1) * 128], cm[0], reads=[cb], writes=[identrep])
                c.dma("sp", negm4[:, r * 128:(r + 1) * 128], cm[3], reads=[cb], writes=[negm4])
                c.dma("pool", strict4[:, r * 128:(r + 1) * 128], cm[5], reads=[cb], writes=[strict4])
            kTs = T("kTc", [128, 4, 128], BF16, 2); qTs = T("qTc", [128, 4, 128], BF16, 2)
            ktoks = T("ktok", [128, 512], BF16, 2); vtoks = T("vtok", [128, 512], BF16, 2)
            szs = T("szc", [128, 4, 128], BF16, 2); gbs = T("gb", [128, 8], F32, 2)
            gtri = T("gtri", [128, 4, 128], F32)[0]; ngtri = T("ngtri", [128, 4, 128], F32)[0]; bdiag = T("bdiag", [128, 4, 128], F32)[0]
            dm = T("dm", [128, 512], F32)[0]; decT = T("decT", [128, 512], F32)[0]; egcb = T("egcb", [128, 512], F32)[0]
            bbc = T("bbc", [128, 512], F32)[0]; dsb = T("dsb", [128, 512], F32)[0]
            ngb = T("ngb", [128, 4], F32)[0]
            gcs = T("gcs", [128, 8], F32)[0]; sm = T("csm", [128, 16], F32)[0]
            ATb = T("ATb", [128, 4, 128], BF16)[0]; Ab = T("Ab", [128, 4, 128], BF16)[0]; qkb = T("qkb", [128, 4, 128], BF16)[0]
            Ts = T("Tm", [128, 4, 128], BF16, 2); Us = T("Um", [128, 4, 128], BF16, 2)
            Xb = T("Xb", [128, 4, 128], BF16)[0]; Xpb = T("Xpb", [128, 4, 128], BF16)[0]; tmpb = T("tmpb", [128, 512], BF16, 2)
            kbg = T("kbg", [128, 512], BF16)[0]; vb = T("vb", [128, 512], BF16)[0]; kdec = T("kdec", [128, 512], BF16)[0]
            qg = T("qg", [128, 4, 128], BF16)[0]; nwT = T("nwT", [128, 4, 128], BF16)[0]; vnew = T("vnew", [128, 512], BF16)[0]
            state_f = T("state_f", [128, 4, 128], F32)[0]; state_b = T("state_b", [128, 4, 128], BF16)[0]
            osq = T("osq", [128, 4, 128], BF16)[0]; rstd = T("crstd", [128, 512], F32)[0]; yn = T("yn", [128, 512], F32)[0]
            yst = T("ystc", [128, 4, 128], BF16, 2)
            c.op("pool", lambda h: h.memset(state_f[:, :, :], 0.0), writes=[state_f])
            c.op("pool", lambda h: h.memset(state_b[:, :, :], 0.0), writes=[state_b])
            qc3 = self.qcT.rearrange("(h d) t -> d h t", d=128); kc3 = self.kcT.rearrange("(h d) t -> d h t", d=128)
            sz3 = self.szT.rearrange("(h d) t -> d h t", d=128); yc3 = self.yT[2].rearrange("(h d) t -> d h t", d=128)
            allg = lambda nm: [self.db(nm, g) for g in range(self.NG)]
            f2 = lambda b: b[:, :, :].rearrange("p h t -> p (h t)")
            for n in range(NT):
                cs = slice(n * 128, (n + 1) * 128)
                kT, qT, ktok, vtok, sz, gb = kTs[n % 2], qTs[n % 2], ktoks[n % 2], vtoks[n % 2], szs[n % 2], gbs[n % 2]
                c.dma("sp", kT[:, :, :], kc3[:, :, cs], reads=allg("kcT"), writes=[kT])
                c.dma("sp", qT[:, :, :], qc3[:, :, cs], reads=allg("qcT"), writes=[qT])
                c.dma("sp", sz[:, :, :], sz3[:, :, cs], reads=allg("szT"), writes=[sz])
                c.dma("sp", ktok[:, :], self.kC[cs, :], reads=allg("kC"), writes=[ktok])
                c.dma("sp", vtok[:, :], self.vC[cs, :], reads=allg("vC"), writes=[vtok])
                c.dma("sp", gb[:, :], self.smallD[cs, 12:20], reads=allg("smallD"), writes=[gb])
                c.op("dve", lambda h: h.tensor_scalar(out=ngb[:, :], in0=gb[:, 4:8], scalar1=-1.0, scalar2=None, op0=ALU.mult), reads=[gb], writes=[ngb])
                for hd in range(4):
                    c.op("dve", lambda h, hd=hd: h.tensor_scalar(out=gtri[:, hd, :], in0=self.triu_f[:, :], scalar1=gb[:, 4 + hd:5 + hd], scalar2=None, op0=ALU.mult), reads=[self.triu_f, gb], writes=[gtri])
                    c.op("act", lambda h, hd=hd: h.activation(out=ngtri[:, hd, :], in_=self.triu_f[:, :], func=AF.Copy, scale=ngb[:, hd:hd + 1]), reads=[self.triu_f, ngb], writes=[ngtri])
                    c.op("act", lambda h, hd=hd: h.activation(out=bdiag[:, hd, :], in_=self.ident_f[:, :], func=AF.Copy, scale=gb[:, hd:hd + 1]), reads=[self.ident_f, gb], writes=[bdiag])
                pD, pG, pB, pS = self.rot(), self.rot(), self.rot(), self.rot()
                for hd in range(4):
                    hs = slice(hd * 128, (hd + 1) * 128)
                    c.op("pe", lambda h, hd=hd, hs=hs: h.matmul(pD[:, hs], lhsT=self.ones_f[:, :], rhs=gtri[:, hd, :], start=True, stop=False), reads=[self.ones_f, gtri], writes=[pD], sig=False)
                    c.op("pe", lambda h, hd=hd, hs=hs: h.matmul(pD[:, hs], lhsT=ngtri[:, hd, :], rhs=self.ones_f[:, :], start=False, stop=True), reads=[self.ones_f, ngtri], writes=[pD], sig=(hd == 3))
                for hd in range(4):
                    hs = slice(hd * 128, (hd + 1) * 128)
                    c.op("pe", lambda h, hd=hd, hs=hs: h.matmul(pG[:, hs], lhsT=self.ones_f[:, :], rhs=gtri[:, hd, :], start=True, stop=True), reads=[self.ones_f, gtri], writes=[pG], sig=(hd == 3))
                for hd in range(4):
                    hs = slice(hd * 128, (hd + 1) * 128)
                    c.op("pe", lambda h, hd=hd, hs=hs: h.matmul(pB[:, hs], lhsT=self.ones_f[:, :], rhs=bdiag[:, hd, :], start=True, stop=True), reads=[self.ones_f, bdiag], writes=[pB], sig=(hd == 3))
                c.op("pe", lambda h: h.matmul(pS[:, 0:4], lhsT=self.triu_f[:, :], rhs=gb[:, 4:8], start=True, stop=True), reads=[self.triu_f, gb], writes=[pS], sig=False)
                c.op("pe", lambda h: h.matmul(pS[:, 4:8], lhsT=self.ones_f[:, :], rhs=gb[:, 4:8], start=True, stop=True), reads=[self.ones_f, gb], writes=[pS])
                c.op("dve", lambda h: h.scalar_tensor_tensor(out=dm[:, :], in0=pD[:, :], scalar=0.0, in1=negm4[:, :], op0=ALU.min, op1=ALU.add), reads=[pD, negm4], writes=[dm])
                c.op("act", lambda h: h.activation(out=decT[:, :], in_=dm[:, :], func=AF.Exp), reads=[dm], writes=[decT])
                c.op("act", lambda h: h.activation(out=egcb[:, :], in_=pG[:, :], func=AF.Exp), reads=[pG], writes=[egcb])
                c.op("act", lambda h: h.activation(out=bbc[:, :], in_=pB[:, :], func=AF.Copy), reads=[pB], writes=[bbc])
                c.op("act", lambda h: h.activation(out=gcs[:, :], in_=pS[:, 0:8], func=AF.Copy), reads=[pS], writes=[gcs])
                c.op("act", lambda h: h.activation(out=sm[:, 0:4], in_=gcs[:, 0:4], func=AF.Exp), reads=[gcs], writes=[sm])
                c.op("dve", lambda h: h.tensor_tensor(out=sm[:, 0:4], in0=sm[:, 0:4], in1=gb[:, 0:4], op=ALU.mult), reads=[sm, gb], writes=[sm])
                c.op("dve", lambda h: h.tensor_tensor(out=sm[:, 12:16], in0=gcs[:, 4:8], in1=gcs[:, 0:4], op=ALU.subtract), reads=[gcs, sm], writes=[sm])
                c.op("act", lambda h: h.activation(out=sm[:, 4:8], in_=sm[:, 12:16], func=AF.Exp), reads=[sm], writes=[sm])
                c.op("act", lambda h: h.activation(out=sm[:, 8:12], in_=gcs[:, 4:8], func=AF.Exp), reads=[gcs, sm], writes=[sm])
                pK, pQ = self.rot(), self.rot()
                for hd in range(4):
                    hs = slice(hd * 128, (hd + 1) * 128)
                    c.op("pe", lambda h, hd=hd, hs=hs: h.matmul(pK[:, hs], lhsT=kT[:, hd, :], rhs=kT[:, hd, :], start=True, stop=True), reads=[kT], writes=[pK], sig=(hd == 3))
                for hd in range(4):
                    hs = slice(hd * 128, (hd + 1) * 128)
                    c.op("pe", lambda h, hd=hd, hs=hs: h.matmul(pQ[:, hs], lhsT=kT[:, hd, :], rhs=qT[:, hd, :], start=True, stop=True), reads=[kT, qT], writes=[pQ], sig=(hd == 3))
                c.op("dve", lambda h: h.tensor_tensor(out=dsb[:, :], in0=decT[:, :], in1=bbc[:, :], op=ALU.mult), reads=[decT, bbc], writes=[dsb])
                c.op("dve", lambda h: h.tensor_tensor(out=dsb[:, :], in0=dsb[:, :], in1=strict4[:, :], op=ALU.mult), reads=[dsb, strict4], writes=[dsb])
                c.op("dve", lambda h: h.tensor_tensor(out=f2(ATb), in0=pK[:, :], in1=dsb[:, :], op=ALU.mult), reads=[pK, dsb], writes=[ATb])
                c.op("dve", lambda h: h.tensor_tensor(out=f2(qkb), in0=pQ[:, :], in1=decT[:, :], op=ALU.mult), reads=[pQ, decT], writes=[qkb])
                pA = self.rot()
                for hd in range(4):
                    hs = slice(hd * 128, (hd + 1) * 128)
                    c.op("pe", lambda h, hd=hd, hs=hs: h.matmul(pA[:, hs], lhsT=ATb[:, hd, :], rhs=self.ident_b[:, :], start=True, stop=True), reads=[ATb, self.ident_b], writes=[pA], sig=(hd == 3))
                c.op("act", lambda h: h.activation(out=f2(Ab), in_=pA[:, :], func=AF.Copy), reads=[pA], writes=[Ab])
                Tc, Uc = Ts[0], Us[0]
                c.op("dve", lambda h: h.tensor_tensor(out=tmpb[0][:, :], in0=f2(Ab), in1=gm[:, 0, :], op=ALU.mult), reads=[Ab, gm], writes=[tmpb[0]])
                c.op("dve", lambda h: h.tensor_tensor(out=f2(Tc), in0=identrep[:, :], in1=tmpb[0][:, :], op=ALU.subtract), reads=[tmpb[0], identrep], writes=[Tc])
                c.op("dve", lambda h: h.tensor_tensor(out=tmpb[1][:, :], in0=f2(ATb), in1=gm[:, 7, :], op=ALU.mult), reads=[ATb, gm], writes=[tmpb[1]])
                c.op("dve", lambda h: h.tensor_tensor(out=f2(Uc), in0=identrep[:, :], in1=tmpb[1][:, :], op=ALU.subtract), reads=[tmpb[1], identrep], writes=[Uc])
                for l in range(1, 7):
                    Tn, Un = Ts[l % 2], Us[l % 2]
                    pXp = self.rot()
                    for hd in range(4):
                        hs = slice(hd * 128, (hd + 1) * 128)
                        c.op("pe", lambda h, hd=hd, hs=hs, Uc=Uc: h.matmul(pXp[:, hs], lhsT=Ab[:, hd, :], rhs=Uc[:, hd, :], start=True, stop=True), reads=[Ab, Uc], writes=[pXp], sig=(hd == 3))
                    c.op("dve", lambda h, l=l: h.tensor_tensor(out=f2(Xpb), in0=pXp[:, :], in1=gm[:, 7 + l, :], op=ALU.mult), reads=[pXp, gm], writes=[Xpb])
                    if l < 6:
                        pX = self.rot()
                        for hd in range(4):
                            hs = slice(hd * 128, (hd + 1) * 128)
                            c.op("pe", lambda h, hd=hd, hs=hs, Tc=Tc: h.matmul(pX[:, hs], lhsT=ATb[:, hd, :], rhs=Tc[:, hd, :], start=True, stop=True), reads=[ATb, Tc], writes=[pX], sig=(hd == 3))
                        c.op("dve", lambda h, l=l: h.tensor_tensor(out=f2(Xb), in0=pX[:, :], in1=gm[:, l, :], op=ALU.mult), reads=[pX, gm], writes=[Xb])
                    pYp = self.rot()
                    for hd in range(4):
                        hs = slice(hd * 128, (hd + 1) * 128)
                        c.op("pe", lambda h, hd=hd, hs=hs, Tc=Tc: h.matmul(pYp[:, hs], lhsT=Tc[:, hd, :], rhs=Xpb[:, hd, :], start=True, stop=True), reads=[Tc, Xpb], writes=[pYp], sig=(hd == 3))
                    c.op("dve", lambda h, Uc=Uc, Un=Un: h.tensor_tensor(out=f2(Un), in0=f2(Uc), in1=pYp[:, :], op=ALU.subtract), reads=[Uc, pYp], writes=[Un])
                    if l < 6:
                        pY = self.rot()
                        for hd in range(4):
                            hs = slice(hd * 128, (hd + 1) * 128)
                            c.op("pe", lambda h, hd=hd, hs=hs, Uc=Uc: h.matmul(pY[:, hs], lhsT=Uc[:, hd, :], rhs=Xb[:, hd, :], start=True, stop=True), reads=[Uc, Xb], writes=[pY], sig=(hd == 3))
                        c.op("dve", lambda h, Tc=Tc, Tn=Tn: h.tensor_tensor(out=f2(Tn), in0=f2(Tc), in1=pY[:, :], op=ALU.subtract), reads=[Tc, pY], writes=[Tn])
                    Tc, Uc = Tn, Un
                U = Uc
                for hd in range(4):
                    hs = slice(hd * 128, (hd + 1) * 128)
                    c.op("act", lambda h, hd=hd, hs=hs: h.activation(out=kbg[:, hs], in_=ktok[:, hs], func=AF.Copy, scale=sm[:, hd:hd + 1]), reads=[ktok, sm], writes=[kbg])
                    c.op("act", lambda h, hd=hd, hs=hs: h.activation(out=vb[:, hs], in_=vtok[:, hs], func=AF.Copy, scale=gb[:, hd:hd + 1]), reads=[vtok, gb], writes=[vb])
                    c.op("act", lambda h, hd=hd, hs=hs: h.activation(out=kdec[:, hs], in_=ktok[:, hs], func=AF.Copy, scale=sm[:, 4 + hd:5 + hd]), reads=[ktok, sm], writes=[kdec])
                c.op("dve", lambda h: h.tensor_tensor(out=f2(qg), in0=f2(qT), in1=egcb[:, :], op=ALU.mult), reads=[qT, egcb], writes=[qg])
                pW = self.rot()
                for hd in range(4):
                    hs = slice(hd * 128, (hd + 1) * 128)
                    c.op("pe", lambda h, hd=hd, hs=hs: h.matmul(pW[:, hs], lhsT=kbg[:, hs], rhs=U[:, hd, :], start=True, stop=True), reads=[kbg, U], writes=[pW], sig=(hd == 3))
                c.op("act", lambda h: h.activation(out=f2(nwT), in_=pW[:, :], func=AF.Copy, scale=-1.0), reads=[pW], writes=[nwT])
                pV = self.rot()
                for hd in range(4):
                    hs = slice(hd * 128, (hd + 1) * 128)
                    c.op("pe", lambda h, hd=hd, hs=hs: h.matmul(pV[:, hs], lhsT=U[:, hd, :], rhs=vb[:, hs], start=True, stop=False), reads=[U, vb], writes=[pV], sig=False)
                    c.op("pe", lambda h, hd=hd, hs=hs: h.matmul(pV[:, hs], lhsT=nwT[:, hd, :], rhs=state_b[:, hd, :], start=False, stop=True), reads=[nwT, state_b], writes=[pV], sig=(hd == 3))
                c.op("act", lambda h: h.activation(out=vnew[:, :], in_=pV[:, :], func=AF.Copy), reads=[pV], writes=[vnew])
                pO = self.rot()
                for hd in range(4):
                    hs = slice(hd * 128, (hd + 1) * 128)
                    c.op("pe", lambda h, hd=hd, hs=hs: h.matmul(pO[:, hs], lhsT=state_b[:, hd, :], rhs=qg[:, hd, :], start=True, stop=False), reads=[state_b, qg], writes=[pO], sig=False)
                    c.op("pe", lambda h, hd=hd, hs=hs: h.matmul(pO[:, hs], lhsT=vnew[:, hs], rhs=qkb[:, hd, :], start=False, stop=True), reads=[vnew, qkb], writes=[pO], sig=(hd == 3))
                pS2 = self.rot()
                for hd in range(4):
                    hs = slice(hd * 128, (hd + 1) * 128)
                    c.op("pe", lambda h, hd=hd, hs=hs: h.matmul(pS2[:, hs], lhsT=kdec[:, hs], rhs=vnew[:, hs], start=True, stop=True), reads=[kdec, vnew], writes=[pS2], sig=(hd == 3))
                for hd in range(4):
                    hs = slice(hd * 128, (hd + 1) * 128)
                    c.op("dve", lambda h, hd=hd, hs=hs: h.scalar_tensor_tensor(out=state_f[:, hd, :], in0=state_f[:, hd, :], scalar=sm[:, 8 + hd:9 + hd], in1=pS2[:, hs], op0=ALU.mult, op1=ALU.add),
                         reads=[state_f, sm, pS2], writes=[state_f])
                c.op("act", lambda h: h.activation(out=f2(state_b), in_=f2(state_f), func=AF.Copy), reads=[state_f], writes=[state_b])
                c.op("act", lambda h: h.activation(out=f2(osq), in_=pO[:, :], func=AF.Square), reads=[pO], writes=[osq])
                pN = self.rot()
                for hd in range(4):
                    hs = slice(hd * 128, (hd + 1) * 128)
                    c.op("pe", lambda h, hd=hd, hs=hs: h.matmul(pN[:, hs], lhsT=self.ones_b[:, :], rhs=osq[:, hd, :], start=True, stop=True), reads=[self.ones_b, osq], writes=[pN], sig=(hd == 3))
                c.op("dve", lambda h: h.tensor_scalar(out=rstd[:, :], in0=pN[:, :], scalar1=1.0 / 128, scalar2=1e-6, op0=ALU.mult, op1=ALU.add), reads=[pN], writes=[rstd])
                c.op("act", lambda h: h.activation(out=rstd[:, :], in_=rstd[:, :], func=AF.Ln), reads=[rstd], writes=[rstd])
                c.op("act", lambda h: h.activation(out=rstd[:, :], in_=rstd[:, :], func=AF.Exp, scale=-0.5), reads=[rstd], writes=[rstd])
                c.op("dve", lambda h: h.tensor_tensor(out=yn[:, :], in0=pO[:, :], in1=rstd[:, :], op=ALU.mult), reads=[pO, rstd], writes=[yn])
                ys_ = yst[n % 2]
                c.op("dve", lambda h, ys_=ys_: h.scalar_tensor_tensor(out=f2(ys_), in0=yn[:, :], scalar=self.cvec[:, cvb + CV_DN:cvb + CV_DN + 1], in1=f2(sz), op0=ALU.mult, op1=ALU.mult),
                     reads=[yn, self.cvec, sz], writes=[ys_])
                c.dma("sp", yc3[:, :, cs], ys_[:, :, :], reads=[ys_], writes=[self.db("yT2", n)])
            c.barrier()


class ProgFull(ProgC):
    def merge_phase(self, xsrc, xdst, wbr, w_out):
        c, S, NG = self.c, self.S, self.NG
        with ExitStack() as es:
            wbs = [c.sb(es, "wbr", [128, 4, 1024], BF16) for _ in range(3)]
            wo = c.sb(es, "wo", [128, 8, 1024], BF16)
            wb = self.db("w")
            for i in range(3):
                c.dma("pool", wbs[i][:, :, :], wbr[i].rearrange("(k p) n -> p k n", p=128), reads=[wb], writes=[wbs[i]])
            c.dma("pool", wo[:, :, :], w_out.rearrange("(k p) n -> p k n", p=128), reads=[wb], writes=[wo])
            ys = [c.sb(es, "ymg", [128, 3, 4, 512], BF16) for _ in range(2)]
            gts = [c.sb(es, "gmg", [128, 24, 512], BF16) for _ in range(2)]
            xgs = [c.sb(es, "xmg", [128, 8, 512], F32) for _ in range(2)]
            mg = c.sb(es, "mg", [128, 8, 512], BF16)
            t1 = [c.sb(es, "mt1", [128, 512], F32) for _ in range(2)]
            t2 = [c.sb(es, "mt2", [128, 512], F32) for _ in range(2)]
            xs3 = xsrc.rearrange("(c p) t -> p c t", p=128)
            xd3 = xdst.rearrange("(c p) t -> p c t", p=128)
            for g in range(NG):
                tsl = slice(g * 512, (g + 1) * 512)
                y, gt, xg = ys[g % 2], gts[g % 2], xgs[g % 2]
                deps = [self.db("yT0", i) for i in range(g * 4, g * 4 + 4)] + [self.db("yT1", hh * 100 + g) for hh in range(8)] + [self.db("yT2", n) for n in range(g * 4, g * 4 + 4)]
                for br in range(3):
                    c.dma("sp", y[:, br, :, :], self.yT[br].rearrange("(k p) t -> p k t", p=128)[:, :, tsl], reads=deps, writes=[y])
                c.dma("sp", gt[:, :, :], self.gT.rearrange("(k p) t -> p k t", p=128)[:, :, tsl], reads=[self.db("gT", sec * 100 + g) for sec in range(6)], writes=[gt])
                c.dma("sp", xg[:, :, :], xs3[:, :, tsl], reads=[self.db(xsrc.name, g)], writes=[xg])
                for d in range(8):
                    pbs = []
                    for br in range(3):
                        pb = self.rot()
                        pbs.append(pb)
                        for k in range(4):
                            c.op("pe", lambda h, br=br, k=k, d=d, pb=pb: h.matmul(pb[:, :], lhsT=wbs[br][:, k, d * 128:(d + 1) * 128], rhs=y[:, br, k, :], start=(k == 0), stop=(k == 3)),
                                 reads=[wbs[br], y], writes=[pb], sig=(k == 3))
                    a, b = t1[d % 2], t2[d % 2]
                    c.op("dve", lambda h, d=d, a=a: h.tensor_tensor(out=a[:, :], in0=pbs[0][:, :], in1=gt[:, d, :], op=ALU.mult), reads=[pbs[0], gt], writes=[a])
                    c.op("dve", lambda h, d=d, b=b: h.tensor_tensor(out=b[:, :], in0=pbs[1][:, :], in1=gt[:, 8 + d, :], op=ALU.mult), reads=[pbs[1], gt], writes=[b])
                    c.op("dve", lambda h, a=a, b=b: h.tensor_tensor(out=a[:, :], in0=a[:, :], in1=b[:, :], op=ALU.add), reads=[a, b], writes=[a])
                    c.op("dve", lambda h, d=d, b=b: h.tensor_tensor(out=b[:, :], in0=pbs[2][:, :], in1=gt[:, 16 + d, :], op=ALU.mult), reads=[pbs[2], gt], writes=[b])
                    c.op("dve", lambda h, d=d, a=a, b=b: h.tensor_tensor(out=mg[:, d, :], in0=a[:, :], in1=b[:, :], op=ALU.add), reads=[a, b], writes=[mg])
                for d in range(8):
                    po = self.rot()
                    for k in range(8):
                        c.op("pe", lambda h, d=d, k=k, po=po: h.matmul(po[:, :], lhsT=wo[:, k, d * 128:(d + 1) * 128], rhs=mg[:, k, :], start=(k == 0), stop=(k == 7)),
                             reads=[wo, mg], writes=[po], sig=(k == 7))
                    c.op("dve", lambda h, d=d, po=po, xg=xg: h.tensor_tensor(out=xg[:, d, :], in0=po[:, :], in1=xg[:, d, :], op=ALU.add), reads=[po, xg], writes=[xg])
                c.dma("sp", xd3[:, :, tsl], xg[:, :, :], reads=[xg], writes=[self.db(xdst.name, g)])
            c.barrier()

    def final_phase(self, xsrc, out):
        c, S, NG = self.c, self.S, self.NG
        with ExitStack() as es:
            xgs = [c.sb(es, "xf", [128, 8, 512], F32) for _ in range(2)]
            ogs = [c.sb(es, "of", [128, 8, 512], F32) for _ in range(2)]
            sq = c.sb(es, "sqf", [128, 8, 512], BF16)
            rstd = c.sb(es, "rstdf", [128, 512], F32)
            xs3 = xsrc.rearrange("(c p) t -> p c t", p=128)
            o3 = out.rearrange("(c p) t -> p c t", p=128)
            for g in range(NG):
                tsl = slice(g * 512, (g + 1) * 512)
                xg, og = xgs[g % 2], ogs[g % 2]
                c.dma("sp", xg[:, :, :], xs3[:, :, tsl], reads=[self.db(xsrc.name, g)], writes=[xg])
                self.rmsnorm_group(xg, sq, lambda k, og=og: og[:, k, :], og, rstd, DEPTH * CV_PER_LAYER, self.rot())
                c.dma("sp", o3[:, :, tsl], og[:, :, :], reads=[og], writes=[self.db("out", g)])
            c.barrier()


def build_full(S=4096):
    P = ProgFull(S)
    es = ExitStack()
    P.setup_consts(es)
    P.declare_scratch()
    d = P.dr
    EI = "ExternalInput"
    d("rotC", [128, S], F32, kind=EI); d("rotS", [128, S], F32, kind=EI)
    d("pow2", [128, 26], F32, kind=EI); d("gmask", [14, 128, 512], F32, kind=EI)
    xin = d("xT_in", [1024, S], F32, kind=EI)
    out = d("outT", [1024, S], F32, kind="ExternalOutput")
    xa = d("xTa", [1024, S], F32); xb = d("xTb", [1024, S], F32)
    f1i = d("ffn1_w_in", [DEPTH, 1024, 4096], F32, kind=EI); f1o = d("ffn1_w_out", [DEPTH, 2048, 1024], F32, kind=EI)
    f2i = d("ffn2_w_in", [DEPTH, 1024, 4096], F32, kind=EI); f2o = d("ffn2_w_out", [DEPTH, 2048, 1024], F32, kind=EI)
    w = d("w_in", [DEPTH, 1024, 7636], F32, kind=EI); wsw = d("w_sw", [DEPTH, 1024, 896], F32, kind=EI); wsm = d("w_small", [DEPTH, 1024, 84], F32, kind=EI)
    wba = d("w_branch_a", [DEPTH, 512, 1024], F32, kind=EI); wbb = d("w_branch_b", [DEPTH, 512, 1024], F32, kind=EI); wbc = d("w_branch_c", [DEPTH, 512, 1024], F32, kind=EI)
    wo = d("w_out", [DEPTH, 1024, 1024], F32, kind=EI)
    cur = xin
    for l in range(DEPTH):
        cvb = l * CV_PER_LAYER
        P.ffn_phase(cur, xa, f1i[l], f1o[l], cvb + CV_FFN1)
        P.m1_phase(xa, w[l], wsw[l], wsm[l], cvb)
        P.ab_phase()
        P.c_phase(cvb)
        P.merge_phase(xa, xb, [wba[l], wbb[l], wbc[l]], wo[l])
        P.ffn_phase(xb, xa, f2i[l], f2o[l], cvb + CV_FFN2)
        cur = xa
    P.final_phase(xa, out)
    es.close()
    return P


_CACHE = {}


def kernel(**inputs):
    S = 4096
    inp = {k: np.asarray(v) for k, v in inputs.items()}
    if "prog" not in _CACHE:
        _CACHE["prog"] = build_full(S)
    P = _CACHE["prog"]
    rotC, rotS = build_rot(S)
    shared = {
        "cmat": build_cmat(), "cvec": build_cvec(inp), "rotC": rotC, "rotS": rotS, "pow2": build_pow2(), "gmask": build_gmask(),
        "ffn1_w_in": inp["ffn1_w_in"], "ffn1_w_out": inp["ffn1_w_out"], "ffn2_w_in": inp["ffn2_w_in"], "ffn2_w_out": inp["ffn2_w_out"],
        "w_in": inp["w_in"], "w_sw": np.ascontiguousarray(inp["w_in"][:, :, swap_cols()]), "w_small": np.ascontiguousarray(inp["w_in"][:, :, small_cols()]),
        "w_branch_a": inp["w_branch_a"], "w_branch_b": inp["w_branch_b"], "w_branch_c": inp["w_branch_c"], "w_out": inp["w_out"],
    }
    in_maps = []
    for b in range(NB):
        m = dict(shared)
        m["xT_in"] = np.ascontiguousarray(inp["x"][b].T)
        in_maps.append(m)
    res = run_bass_kernel_spmd(P.nc, in_maps, core_ids=list(range(NB)))
    out = np.stack([np.ascontiguousarray(r["outT"].T) for r in res.results], axis=0)
    return out.astype(np.float32)
```

```python
from contextlib import ExitStack
import numpy as np
import concourse.bass as bass
import concourse.mybir as mybir
from concourse.bass_utils import run_bass_kernel_spmd

F32 = mybir.dt.float32
BF16 = mybir.dt.bfloat16
ALU = mybir.AluOpType
AF = mybir.ActivationFunctionType
AX = mybir.AxisListType

D = 1024
DEPTH = 2
NB = 8
IN_SIZES = (512, 64, 64, 256, 64, 4, 1536, 8, 1536, 512, 4, 4, 3072)
OFF = np.concatenate([[0], np.cumsum(IN_SIZES)]).tolist()
(O_AQ, O_AK, O_AV, O_IQ, O_IK, O_IW, O_BQKV, O_BF, O_CQKV, O_CZ, O_CB, O_CA, O_G) = OFF[:13]
NEG = -32768.0


class Buf:
    __slots__ = ("t", "last_w", "readers", "name")

    def __init__(self, t=None, name=""):
        self.t = t
        self.last_w = None
        self.readers = {}
        self.name = name

    def __getitem__(self, k):
        return self.t[k]


class Eng:
    def __init__(self, name, handle, sem):
        self.name = name
        self.h = handle
        self.sem = sem
        self.count = 0
        self.seen = {}


class Ctx:
    SAME_ENGINE_SYNC = True
    RAW_ONLY_SAME_ENGINE = False

    def __init__(self, nc, n_dma_sems=10):
        self.nc = nc
        self.sems = {}
        self.eng = {}
        for nm, h in (("pe", nc.tensor), ("act", nc.scalar), ("dve", nc.vector),
                      ("pool", nc.gpsimd), ("sp", nc.sync)):
            self.sems["s_" + nm] = nc.alloc_semaphore("s_" + nm)
            self.eng[nm] = Eng(nm, h, "s_" + nm)
        self.dma_pool = {}
        for q in ("sp", "pool", "act"):
            lst = []
            for i in range(n_dma_sems):
                k = f"d_{q}{i}"
                self.sems[k] = nc.alloc_semaphore(k)
                lst.append([k, 0])
            self.dma_pool[q] = [lst, 0]
        self.n_instr = 0
        self.n_wait = 0
        self.uid = 0

    def sb(self, es, name, shape, dt):
        self.uid += 1
        nm = f"{name}_{self.uid}"
        return Buf(es.enter_context(self.nc.sbuf_tensor(nm, list(shape), dt)), nm)

    def _need(self, reads, writes, own=None):
        need = {}

        def add(ev, raw):
            if ev is None:
                return
            k, v = ev
            if k == own and not raw and self.RAW_ONLY_SAME_ENGINE:
                return
            if need.get(k, 0) < v:
                need[k] = v
        for b in reads:
            add(b.last_w, True)
        for b in writes:
            add(b.last_w, False)
            for k, v in b.readers.items():
                add((k, v), False)
        return need

    def _emit_waits(self, e, need):
        for k, v in need.items():
            if k == e.sem and (e.name == "pe" or not self.SAME_ENGINE_SYNC):
                continue
            if e.seen.get(k, 0) >= v:
                continue
            e.h.wait_ge(self.sems[k], v)
            e.seen[k] = v
            self.n_wait += 1

    def _record(self, ev, reads, writes):
        k, v = ev
        for b in writes:
            b.last_w = ev
            b.readers = {}
        for b in reads:
            if b.readers.get(k, 0) < v:
                b.readers[k] = v

    def op(self, en, fn, reads=(), writes=(), sig=True):
        e = self.eng[en]
        self._emit_waits(e, self._need(reads, writes, e.sem))
        ins = fn(e.h)
        self.n_instr += 1
        if sig:
            ins.then_inc(self.sems[e.sem], 1)
            e.count += 1
            ev = (e.sem, e.count)
        else:
            ev = (e.sem, e.count + 1)
        self._record(ev, reads, writes)
        return ins

    def dma(self, q, out, in_, reads=(), writes=(), **kw):
        e = self.eng[q]
        lst, idx = self.dma_pool[q]
        ent = lst[idx % len(lst)]
        self.dma_pool[q][1] = idx + 1
        need = self._need(reads, writes, None)
        if ent[1] > 0 and need.get(ent[0], 0) < ent[1]:
            need[ent[0]] = ent[1]
        self._emit_waits(e, need)
        ins = e.h.dma_start(out=out, in_=in_, **kw)
        ent[1] += 16
        ins.then_inc(self.sems[ent[0]], 16)
        self.n_instr += 1
        self._record((ent[0], ent[1]), reads, writes)
        return ins

    def barrier(self):
        for e in self.eng.values():
            need = {}
            for f in self.eng.values():
                if f is not e and f.count > 0:
                    need[f.sem] = f.count
            for q in self.dma_pool:
                for k, v in self.dma_pool[q][0]:
                    if v > 0:
                        need[k] = v
            self._emit_waits(e, need)


class Prog:
    def __init__(self, S, ext=None):
        self.S = S
        self.NT = S // 128
        self.NG = S // 512
        self.nc = bass.Bass("TRN2", target_bir_lowering=False)
        self.c = Ctx(self.nc)
        self.ext = ext or {}
        self.dram = {}
        self.dbuf = {}
        nc = self.nc
        self.psall = nc.alloc_psum_tensor("psall", [128, 8 * 512], F32)
        self.ps = [Buf(self.psall[:, i * 512:(i + 1) * 512], f"ps{i}") for i in range(8)]

    def dr(self, name, shape, dt, kind=None):
        if kind is None:
            kind = {"in": "ExternalInput", "out": "ExternalOutput"}.get(self.ext.get(name), "Internal")
        t = self.nc.dram_tensor(name, list(shape), dt, kind=kind)
        self.dram[name] = t.ap()
        return self.dram[name]

    def db(self, name, idx=0):
        k = (name, idx)
        if k not in self.dbuf:
            self.dbuf[k] = Buf(name=f"{name}{idx}")
        return self.dbuf[k]

    def setup_consts(self, es):
        c, nc = self.c, self.nc
        cm = self.dr("cmat", [7, 128, 128], F32, kind="ExternalInput")
        self.ident_b = c.sb(es, "identb", [128, 128], BF16)
        self.ones_b = c.sb(es, "onesb", [128, 128], BF16)
        self.ones_f = c.sb(es, "onesf", [128, 128], F32)
        self.triu_f = c.sb(es, "triuf", [128, 128], F32)
        self.ident_f = c.sb(es, "identf", [128, 128], F32)
        self.negm_f = c.sb(es, "negmf", [128, 128], F32)
        self.tri01_b = c.sb(es, "tri01b", [128, 128], BF16)
        cb = self.db("cmat")
        c.dma("pool", self.ident_b[:, :], cm[0], reads=[cb], writes=[self.ident_b])
        c.dma("pool", self.ones_b[:, :], cm[1], reads=[cb], writes=[self.ones_b])
        c.dma("sp", self.ones_f[:, :], cm[1], reads=[cb], writes=[self.ones_f])
        c.dma("sp", self.triu_f[:, :], cm[2], reads=[cb], writes=[self.triu_f])
        c.dma("sp", self.ident_f[:, :], cm[0], reads=[cb], writes=[self.ident_f])
        c.dma("sp", self.negm_f[:, :], cm[3], reads=[cb], writes=[self.negm_f])
        c.dma("pool", self.tri01_b[:, :], cm[2], reads=[cb], writes=[self.tri01_b])
        self.NCV = DEPTH * CV_PER_LAYER + 8
        cv = self.dr("cvec", [128, self.NCV], F32, kind="ExternalInput")
        self.cvec = c.sb(es, "cvec", [128, self.NCV], F32)
        c.dma("sp", self.cvec[:, :], cv[:, :], reads=[self.db("cvec")], writes=[self.cvec])

    def rmsnorm_group(self, xg, sq, hT_ap_fn, hT_buf, rstd, gcol, psb, nch=8, ncols=512):
        c = self.c
        c.op("act", lambda h: h.activation(out=sq[:, :, :], in_=xg[:, :, :], func=AF.Square), reads=[xg], writes=[sq])
        for k in range(nch):
            c.op("pe", lambda h, k=k: h.matmul(psb[:, :ncols], lhsT=self.ones_b[:, :], rhs=sq[:, k, :], start=(k == 0), stop=(k == nch - 1)),
                 reads=[self.ones_b, sq], writes=[psb], sig=(k == nch - 1))
        c.op("dve", lambda h: h.tensor_scalar(out=rstd[:, :], in0=psb[:, :ncols], scalar1=1.0 / (nch * 128), scalar2=1e-6, op0=ALU.mult, op1=ALU.add),
             reads=[psb], writes=[rstd])
        c.op("act", lambda h: h.activation(out=rstd[:, :], in_=rstd[:, :], func=AF.Ln), reads=[rstd], writes=[rstd])
        c.op("act", lambda h: h.activation(out=rstd[:, :], in_=rstd[:, :], func=AF.Exp, scale=-0.5), reads=[rstd], writes=[rstd])
        for k in range(nch):
            c.op("dve", lambda h, k=k: h.scalar_tensor_tensor(out=hT_ap_fn(k), in0=xg[:, k, :], scalar=self.cvec[:, gcol + k:gcol + k + 1],
                                                            in1=rstd[:, :], op0=ALU.mult, op1=ALU.mult),
                 reads=[xg, rstd, self.cvec], writes=[hT_buf])

    def ffn_phase(self, xsrc, xdst, w_in, w_out, gcol):
        c, S = self.c, self.S
        with ExitStack() as es:
            win = c.sb(es, "win", [128, 8, 4096], BF16)
            wout = c.sb(es, "wout", [128, 16, 1024], BF16)
            xgs = [c.sb(es, "xg", [128, 8, 512], F32) for _ in range(2)]
            sq = c.sb(es, "sq", [128, 8, 512], BF16)
            hT = c.sb(es, "hT", [128, 8, 512], BF16)
            act = c.sb(es, "actT", [128, 16, 512], BF16)
            rstd = c.sb(es, "rstd", [128, 512], F32)
            sgs = [c.sb(es, "sg", [128, 512], F32) for _ in range(2)]
            wb = self.db("w")
            for k in range(8):
                c.dma("pool", win[:, k, :], w_in[k * 128:(k + 1) * 128, :], reads=[wb], writes=[win])
            for k in range(16):
                c.dma("pool", wout[:, k, :], w_out[k * 128:(k + 1) * 128, :], reads=[wb], writes=[wout])
            xs3 = xsrc.rearrange("(c p) t -> p c t", p=128)
            xd3 = xdst.rearrange("(c p) t -> p c t", p=128)
            ps = self.ps
            for g in range(self.NG):
                xg = xgs[g % 2]
                tsl = slice(g * 512, (g + 1) * 512)
                c.dma("sp", xg[:, :, :], xs3[:, :, tsl], reads=[self.db(xsrc.name, g)], writes=[xg])
                self.rmsnorm_group(xg, sq, lambda k: hT[:, k, :], hT, rstd, gcol, ps[0])
                for j in range(16):
                    pg, pu = ps[1 + 2 * (j % 2)], ps[2 + 2 * (j % 2)]
                    for k in range(8):
                        c.op("pe", lambda h, k=k, j=j, pg=pg: h.matmul(pg[:, :], lhsT=win[:, k, j * 128:(j + 1) * 128], rhs=hT[:, k, :], start=(k == 0), stop=(k == 7)),
                             reads=[win, hT], writes=[pg], sig=(k == 7))
                    for k in range(8):
                        c.op("pe", lambda h, k=k, j=j, pu=pu: h.matmul(pu[:, :], lhsT=win[:, k, 2048 + j * 128:2048 + (j + 1) * 128], rhs=hT[:, k, :], start=(k == 0), stop=(k == 7)),
                             reads=[win, hT], writes=[pu], sig=(k == 7))
                    sg = sgs[j % 2]
                    c.op("act", lambda h, pg=pg, sg=sg: h.activation(out=sg[:, :], in_=pg[:, :], func=AF.Silu), reads=[pg], writes=[sg])
                    c.op("dve", lambda h, pu=pu, sg=sg, j=j: h.tensor_tensor(out=act[:, j, :], in0=sg[:, :], in1=pu[:, :], op=ALU.mult), reads=[sg, pu], writes=[act])
                for d in range(8):
                    po = ps[5 + d % 2]
                    for j in range(16):
                        c.op("pe", lambda h, d=d, j=j, po=po: h.matmul(po[:, :], lhsT=wout[:, j, d * 128:(d + 1) * 128], rhs=act[:, j, :], start=(j == 0), stop=(j == 15)),
                             reads=[wout, act], writes=[po], sig=(j == 15))
                    c.op("dve", lambda h, d=d, po=po, xg=xg: h.scalar_tensor_tensor(out=xg[:, d, :], in0=po[:, :], scalar=0.5, in1=xg[:, d, :], op0=ALU.mult, op1=ALU.add),
                         reads=[po, xg], writes=[xg])
                c.dma("sp", xd3[:, :, tsl], xg[:, :, :], reads=[xg], writes=[self.db(xdst.name, g)])
            c.barrier()


CV_FFN1, CV_MIX, CV_FFN2, CV_BG, CV_CONV, CV_DN, CV_BF, CV_ALOG, CV_DT = 0, 8, 16, 24, 48, 96, 97, 105, 109
CV_PER_LAYER = 113


def build_cvec(inp):
    cv = np.zeros((128, DEPTH * CV_PER_LAYER + 8), np.float32)
    for l in range(DEPTH):
        b = l * CV_PER_LAYER
        cv[:, b + CV_FFN1:b + CV_FFN1 + 8] = inp["ffn1_norm"][l].reshape(8, 128).T
        cv[:, b + CV_MIX:b + CV_MIX + 8] = inp["mix_norm"][l].reshape(8, 128).T
        cv[:, b + CV_FFN2:b + CV_FFN2 + 8] = inp["ffn2_norm"][l].reshape(8, 128).T
        cv[:, b + CV_BG:b + CV_BG + 24] = inp["b_gate"][l].reshape(24, 128).T
        cv[:, b + CV_CONV:b + CV_CONV + 48] = inp["conv_w"][l].reshape(4, 12, 128).transpose(2, 1, 0).reshape(128, 48)
        cv[:, b + CV_DN] = inp["delta_norm"][l]
        cv[:, b + CV_BF:b + CV_BF + 8] = inp["b_forget"][l][None, :]
        cv[:, b + CV_ALOG:b + CV_ALOG + 4] = inp["a_log"][l][None, :]
        cv[:, b + CV_DT:b + CV_DT + 4] = inp["dt_bias"][l][None, :]
    cv[:, DEPTH * CV_PER_LAYER:] = inp["final_norm"].reshape(8, 128).T
    return cv


def build_cmat():
    i = np.arange(128)
    ident = np.eye(128, dtype=np.float32)
    ones = np.ones((128, 128), np.float32)
    triu = (i[:, None] <= i[None, :]).astype(np.float32)
    negm = np.where(i[None, :] >= i[:, None], 0.0, -1e4).astype(np.float32)
    negc = np.where(i[None, :] <= i[:, None], 0.0, -1e30).astype(np.float32)
    z = np.zeros((128, 128), np.float32)
    strictu = (i[:, None] < i[None, :]).astype(np.float32)
    return np.stack([ident, ones, triu, negm, negc, strictu, z])


def build_pow2(nit=26):
    return np.tile((0.5 ** np.arange(1, nit + 1)).astype(np.float32)[None, :], (128, 1))


def swap_cols():
    idx = []
    for base, n in ((O_AQ, 512), (O_AK, 64), (O_IQ, 256), (O_IK, 64)):
        for j in range(n):
            d = j % 64
            hb = base + (j // 64) * 64
            if d < 8:
                idx.append(hb + d + 8)
            elif d < 16:
                idx.append(hb + d - 8)
            else:
                idx.append(hb + d)
    return np.array(idx)


def small_cols():
    return np.concatenate([np.arange(O_AV, O_AV + 64), np.arange(O_IW, O_IW + 4), np.arange(O_BF, O_BF + 8),
                           np.arange(O_CB, O_CB + 4), np.arange(O_CA, O_CA + 4)])


def build_rot(S):
    pos = np.arange(S, dtype=np.float32)
    inv = np.power(np.float32(500000.0), -np.arange(0, 16, 2, dtype=np.float32) / np.float32(16)).astype(np.float32)
    ang = (pos[:, None] * inv[None, :]).astype(np.float32)
    cos, sin = np.cos(ang).astype(np.float32), np.sin(ang).astype(np.float32)
    C = np.ones((128, S), np.float32)
    Sg = np.zeros((128, S), np.float32)
    for p in range(128):
        d = p % 64
        if d < 8:
            C[p] = cos[:, d]
            Sg[p] = -sin[:, d]
        elif d < 16:
            C[p] = cos[:, d - 8]
            Sg[p] = sin[:, d - 8]
    return C, Sg


class ProgM1(Prog):
    def declare_scratch(self):
        S = self.S
        d = self.dr
        self.qaT = d("qaT", [512, S], BF16); self.kaT = d("kaT", [64, S], BF16)
        self.qiT = d("qiT", [256, S], BF16); self.kiT = d("kiT", [64, S], BF16)
        self.vA = d("vA", [S, 64], BF16)
        self.qbT = d("qbT", [512, S], BF16); self.kbT = d("kbT", [512, S], BF16); self.vB = d("vB", [S, 512], BF16)
        self.qcT = d("qcT", [512, S], BF16); self.kcT = d("kcT", [512, S], BF16)
        self.kC = d("kC", [S, 512], BF16); self.vC = d("vC", [S, 512], BF16)
        self.szT = d("szT", [512, S], BF16)
        self.smallD = d("smallD", [S, 24], F32)
        self.gT = d("gT", [3072, S], BF16)
        self.yT = d("yT", [3, 512, S], BF16)

    def rot(self):
        self._rot = (getattr(self, "_rot", -1) + 1) % 8
        return self.ps[self._rot]

    def m1_phase(self, xT, w_in, w_sw, w_small, cvb):
        c, S, NG = self.c, self.S, self.NG
        rotC = self.dram["rotC"]; rotS = self.dram["rotS"]
        with ExitStack() as es:
            hT = c.sb(es, "hTall", [128, 8, S], BF16)
            wts = [c.sb(es, "wt", [128, 8, 512], BF16) for _ in range(3)]
            wti = [0]
            wb = self.db("w")

            def load_w(src_list):
                wt = wts[wti[0] % 3]
                wti[0] += 1
                for (ap, c0, n) in src_list:
                    c.dma("pool", wt[:, :, c0:c0 + n], ap.rearrange("(k p) n -> p k n", p=128), reads=[wb], writes=[wt])
                return wt

            def fm(wt, c0, M, g, psb, rows0=0):
                tsl = slice(g * 512, (g + 1) * 512)
                for k in range(8):
                    c.op("pe", lambda h, k=k: h.matmul(psb[rows0:rows0 + M, :], lhsT=wt[:, k, c0:c0 + M], rhs=hT[:, k, tsl], start=(k == 0), stop=(k == 7)),
                         reads=[wt, hT], writes=[psb], sig=(k == 7))

            with ExitStack() as es2:
                xgs = [c.sb(es2, "xg", [128, 8, 512], F32) for _ in range(2)]
                sq = c.sb(es2, "sq", [128, 8, 512], BF16)
                rstd = c.sb(es2, "rstd", [128, 512], F32)
                x3 = xT.rearrange("(c p) t -> p c t", p=128)
                for g in range(NG):
                    xg = xgs[g % 2]
                    tsl = slice(g * 512, (g + 1) * 512)
                    c.dma("sp", xg[:, :, :], x3[:, :, tsl], reads=[self.db(xT.name, g)], writes=[xg])
                    self.rmsnorm_group(xg, sq, lambda k, tsl=tsl: hT[:, k, tsl], hT, rstd, cvb + CV_MIX, self.rot())
                c.barrier()

            with ExitStack() as es2:
                stg = [c.sb(es2, "stg", [128, 4, 512], BF16) for _ in range(2)]
                stgi = [0]
                t1s = [c.sb(es2, "t1", [128, 512], F32) for _ in range(2)]
                t2s = [c.sb(es2, "t2", [128, 512], F32) for _ in range(2)]
                rc = [c.sb(es2, "rc", [128, 512], F32) for _ in range(2)]
                rs = [c.sb(es2, "rs", [128, 512], F32) for _ in range(2)]

                def nstg():
                    stgi[0] += 1
                    return stg[stgi[0] % 2]

                def load_rot(g):
                    tsl = slice(g * 512, (g + 1) * 512)
                    c.dma("sp", rc[g % 2][:, :], rotC[:, tsl], reads=[self.db("rot")], writes=[rc[g % 2]])
                    c.dma("sp", rs[g % 2][:, :], rotS[:, tsl], reads=[self.db("rot")], writes=[rs[g % 2]])

                def rotary(pn, psw, g, out_ap, out_buf, i):
                    t1, t2 = t1s[i % 2], t2s[i % 2]
                    c.op("dve", lambda h: h.tensor_tensor(out=t1[:, :], in0=pn[:, :], in1=rc[g % 2][:, :], op=ALU.mult), reads=[pn, rc[g % 2]], writes=[t1])
                    c.op("dve", lambda h: h.tensor_tensor(out=t2[:, :], in0=psw[:, :], in1=rs[g % 2][:, :], op=ALU.mult), reads=[psw, rs[g % 2]], writes=[t2])
                    c.op("pool", lambda h: h.tensor_tensor(out=out_ap, in0=t1[:, :], in1=t2[:, :], op=ALU.add), reads=[t1, t2], writes=[out_buf])

                wn = load_w([(w_in[:, O_AQ:O_AQ + 512], 0, 512)])
                ws = load_w([(w_sw[:, 0:512], 0, 512)])
                for g in range(NG):
                    tsl = slice(g * 512, (g + 1) * 512)
                    load_rot(g)
                    so = nstg()
                    for ch in range(4):
                        pn, psw = self.rot(), self.rot()
                        fm(wn, ch * 128, 128, g, pn)
                        fm(ws, ch * 128, 128, g, psw)
                        rotary(pn, psw, g, so[:, ch, :], so, ch)
                    c.dma("sp", self.qaT.rearrange("(c p) t -> p c t", p=128)[:, :, tsl], so[:, :, :], reads=[so], writes=[self.db("qaT", g)])
                wn = load_w([(w_in[:, O_IQ:O_IQ + 256], 0, 256), (w_in[:, O_AK:O_AK + 64], 256, 64), (w_in[:, O_IK:O_IK + 64], 320, 64)])
                ws = load_w([(w_sw[:, 576:832], 0, 256), (w_sw[:, 512:576], 256, 64), (w_sw[:, 832:896], 320, 64)])
                for g in range(NG):
                    tsl = slice(g * 512, (g + 1) * 512)
                    load_rot(g)
                    so = nstg()
                    for ch in range(3):
                        pn, psw = self.rot(), self.rot()
                        fm(wn, ch * 128, 128, g, pn)
                        fm(ws, ch * 128, 128, g, psw)
                        rotary(pn, psw, g, so[:, ch, :], so, ch)
                    c.dma("sp", self.qiT.rearrange("(c p) t -> p c t", p=128)[:, :, tsl], so[:, 0:2, :], reads=[so], writes=[self.db("qiT", g)])
                    c.dma("sp", self.kaT[:, tsl], so[0:64, 2, :], reads=[so], writes=[self.db("kaT", g)])
                    c.dma("sp", self.kiT[:, tsl], so[64:128, 2, :], reads=[so], writes=[self.db("kiT", g)])
                for (c0, dst) in ((O_BQKV, self.qbT), (O_BQKV + 512, self.kbT)):
                    wn = load_w([(w_in[:, c0:c0 + 512], 0, 512)])
                    for g in range(NG):
                        tsl = slice(g * 512, (g + 1) * 512)
                        so = nstg()
                        for ch in range(4):
                            pn = self.rot()
                            fm(wn, ch * 128, 128, g, pn)
                            c.op("act", lambda h, pn=pn, ch=ch, so=so: h.activation(out=so[:, ch, :], in_=pn[:, :], func=AF.Copy), reads=[pn], writes=[so])
                        c.dma("sp", dst.rearrange("(c p) t -> p c t", p=128)[:, :, tsl], so[:, :, :], reads=[so], writes=[self.db(dst.name, g)])
                wn = load_w([(w_in[:, O_BQKV + 1024:O_BQKV + 1536], 0, 512)])
                for g in range(NG):
                    so = nstg()
                    for tt in range(4):
                        pn = self.rot()
                        t0 = g * 512 + tt * 128
                        for k in range(8):
                            c.op("pe", lambda h, k=k, pn=pn, t0=t0: h.matmul(pn[:, :], lhsT=hT[:, k, t0:t0 + 128], rhs=wn[:, k, :], start=(k == 0), stop=(k == 7)),
                                 reads=[wn, hT], writes=[pn], sig=(k == 7))
                        c.op("act", lambda h, pn=pn, tt=tt, so=so: h.activation(out=so[:, tt, :], in_=pn[:, :], func=AF.Copy), reads=[pn], writes=[so])
                    c.dma("sp", self.vB.rearrange("(n p) d -> p n d", p=128)[:, g * 4:(g + 1) * 4, :], so[:, :, :], reads=[so], writes=[self.db("vB", g)])
                with ExitStack() as es3:
                    xcs = [c.sb(es3, "xc", [128, 515], F32) for _ in range(4)]
                    accs = [c.sb(es3, "acc", [128, 512], F32) for _ in range(2)]
                    sls = [c.sb(es3, "sl", [128, 512], F32) for _ in range(2)]
                    sqb = [c.sb(es3, "sqb", [128, 512], BF16) for _ in range(2)]
                    rr = [c.sb(es3, "rr", [128, 512], F32) for _ in range(2)]
                    tok = [c.sb(es3, "tok", [128, 4, 512], BF16) for _ in range(2)]
                    for sec, (dstT, dstTok) in enumerate(((self.qcT, None), (self.kcT, self.kC), (None, self.vC))):
                        c0 = O_CQKV + sec * 512
                        wn = load_w([(w_in[:, c0:c0 + 512], 0, 512)])
                        for j in range(4):
                            c.op("pool", lambda h, j=j: h.memset(xcs[j][:, 0:3], 0.0), writes=[xcs[j]])
                        for g in range(NG):
                            tsl = slice(g * 512, (g + 1) * 512)
                            so = nstg()
                            for j in range(4):
                                ch = sec * 4 + j
                                pn = self.rot()
                                fm(wn, j * 128, 128, g, pn)
                                xc = xcs[j]
                                acc, sl = accs[j % 2], sls[j % 2]
                                wc = cvb + CV_CONV + ch * 4
                                c.op("act", lambda h, pn=pn, xc=xc: h.activation(out=xc[:, 3:515], in_=pn[:, :], func=AF.Copy), reads=[pn], writes=[xc])
                                c.op("dve", lambda h, xc=xc, acc=acc, wc=wc: h.tensor_scalar(out=acc[:, :], in0=xc[:, 3:515], scalar1=self.cvec[:, wc + 3:wc + 4], scalar2=None, op0=ALU.mult),
                                     reads=[xc, self.cvec], writes=[acc])
                                for tap in (2, 1, 0):
                                    c.op("dve", lambda h, xc=xc, acc=acc, wc=wc, tap=tap: h.scalar_tensor_tensor(out=acc[:, :], in0=xc[:, tap:tap + 512], scalar=self.cvec[:, wc + tap:wc + tap + 1],
                                                                                                         in1=acc[:, :], op0=ALU.mult, op1=ALU.add),
                                         reads=[xc, acc, self.cvec], writes=[acc])
                                c.op("pool", lambda h, xc=xc: h.tensor_copy(out=xc[:, 0:3], in_=xc[:, 512:515]), reads=[xc], writes=[xc])
                                if sec == 2:
                                    c.op("act", lambda h, acc=acc, so=so, j=j: h.activation(out=so[:, j, :], in_=acc[:, :], func=AF.Silu), reads=[acc], writes=[so])
                                else:
                                    c.op("act", lambda h, acc=acc, sl=sl: h.activation(out=sl[:, :], in_=acc[:, :], func=AF.Silu), reads=[acc], writes=[sl])
                                    sb_, r_ = sqb[j % 2], rr[j % 2]
                                    c.op("act", lambda h, sl=sl, sb_=sb_: h.activation(out=sb_[:, :], in_=sl[:, :], func=AF.Square), reads=[sl], writes=[sb_])
                                    p2 = self.rot()
                                    c.op("pe", lambda h, p2=p2, sb_=sb_: h.matmul(p2[:, :], lhsT=self.ones_b[:, :], rhs=sb_[:, :], start=True, stop=True), reads=[self.ones_b, sb_], writes=[p2])
                                    c.op("dve", lambda h, p2=p2, r_=r_: h.tensor_scalar(out=r_[:, :], in0=p2[:, :], scalar1=1e-6, scalar2=None, op0=ALU.add), reads=[p2], writes=[r_])
                                    c.op("act", lambda h, r_=r_: h.activation(out=r_[:, :], in_=r_[:, :], func=AF.Ln), reads=[r_], writes=[r_])
                                    c.op("act", lambda h, r_=r_: h.activation(out=r_[:, :], in_=r_[:, :], func=AF.Exp, scale=-0.5), reads=[r_], writes=[r_])
                                    qs = float(128 ** -0.5) if sec == 0 else 1.0
                                    c.op("dve", lambda h, sl=sl, r_=r_, so=so, j=j, qs=qs: h.scalar_tensor_tensor(out=so[:, j, :], in0=sl[:, :], scalar=qs, in1=r_[:, :], op0=ALU.mult, op1=ALU.mult),
                                         reads=[sl, r_], writes=[so])
                            if dstT is not None:
                                c.dma("sp", dstT.rearrange("(c p) t -> p c t", p=128)[:, :, tsl], so[:, :, :], reads=[so], writes=[self.db(dstT.name, g)])
                            if dstTok is not None:
                                tk = tok[g % 2]
                                for tt in range(4):
                                    pt = self.rot()
                                    for j in range(4):
                                        c.op("pe", lambda h, pt=pt, j=j, tt=tt, so=so: h.matmul(pt[:, j * 128:(j + 1) * 128], lhsT=so[:, j, tt * 128:(tt + 1) * 128], rhs=self.ident_b[:, :], start=True, stop=True),
                                             reads=[so, self.ident_b], writes=[pt], sig=(j == 3))
                                    c.op("act", lambda h, pt=pt, tk=tk, tt=tt: h.activation(out=tk[:, tt, :], in_=pt[:, :], func=AF.Copy), reads=[pt], writes=[tk])
                                c.dma("sp", dstTok.rearrange("(n p) d -> p n d", p=128)[:, g * 4:(g + 1) * 4, :], tk[:, :, :], reads=[tk], writes=[self.db(dstTok.name, g)])
                wn = load_w([(w_in[:, O_CZ:O_CZ + 512], 0, 512)])
                for g in range(NG):
                    tsl = slice(g * 512, (g + 1) * 512)
                    so = nstg()
                    for ch in range(4):
                        pn = self.rot()
                        fm(wn, ch * 128, 128, g, pn)
                        c.op("act", lambda h, pn=pn, ch=ch, so=so: h.activation(out=so[:, ch, :], in_=pn[:, :], func=AF.Silu), reads=[pn], writes=[so])
                    c.dma("sp", self.szT.rearrange("(c p) t -> p c t", p=128)[:, :, tsl], so[:, :, :], reads=[so], writes=[self.db("szT", g)])
                for sec in range(6):
                    wn = load_w([(w_in[:, O_G + sec * 512:O_G + (sec + 1) * 512], 0, 512)])
                    for g in range(NG):
                        tsl = slice(g * 512, (g + 1) * 512)
                        so = nstg()
                        for ch in range(4):
                            pn = self.rot()
                            fm(wn, ch * 128, 128, g, pn)
                            bc = cvb + CV_BG + sec * 4 + ch
                            c.op("act", lambda h, pn=pn, ch=ch, so=so, bc=bc: h.activation(out=so[:, ch, :], in_=pn[:, :], func=AF.Sigmoid, bias=self.cvec[:, bc:bc + 1]),
                                 reads=[pn, self.cvec], writes=[so])
                        c.dma("sp", self.gT.rearrange("(c p) t -> p c t", p=128)[:, sec * 4:(sec + 1) * 4, tsl], so[:, :, :], reads=[so], writes=[self.db("gT", sec * 100 + g)])
                with ExitStack() as es3:
                    wsm = c.sb(es3, "wsm", [128, 8, 84], BF16)
                    c.dma("pool", wsm[:, :, :], w_small.rearrange("(k p) n -> p k n", p=128), reads=[wb], writes=[wsm])
                    negA = c.sb(es3, "negA", [128, 4], F32)
                    c.op("act", lambda h: h.activation(out=negA[:, :], in_=self.cvec[:, cvb + CV_ALOG:cvb + CV_ALOG + 4], func=AF.Exp), reads=[self.cvec], writes=[negA])
                    c.op("dve", lambda h: h.tensor_scalar(out=negA[:, :], in0=negA[:, :], scalar1=-1.0, scalar2=None, op0=ALU.mult), reads=[negA], writes=[negA])
                    sms = [c.sb(es3, "sm", [128, 4, 24], F32) for _ in range(2)]
                    vas = [c.sb(es3, "vas", [128, 4, 64], BF16) for _ in range(2)]
                    tmp = [c.sb(es3, "tmps", [128, 16], F32) for _ in range(2)]
                    for g in range(NG):
                        sm, va = sms[g % 2], vas[g % 2]
                        for tt in range(4):
                            pn = self.rot()
                            t0 = g * 512 + tt * 128
                            tp = tmp[tt % 2]
                            for k in range(8):
                                c.op("pe", lambda h, k=k, pn=pn, t0=t0: h.matmul(pn[:, 0:84], lhsT=hT[:, k, t0:t0 + 128], rhs=wsm[:, k, :], start=(k == 0), stop=(k == 7)),
                                     reads=[wsm, hT], writes=[pn], sig=(k == 7))
                            c.op("act", lambda h, pn=pn, va=va, tt=tt: h.activation(out=va[:, tt, :], in_=pn[:, 0:64], func=AF.Copy), reads=[pn], writes=[va])
                            c.op("dve", lambda h, pn=pn, sm=sm, tt=tt: h.tensor_scalar(out=sm[:, tt, 0:4], in0=pn[:, 64:68], scalar1=1.0 / 16.0, scalar2=None, op0=ALU.mult), reads=[pn], writes=[sm])
                            c.op("dve", lambda h, pn=pn, tp=tp: h.tensor_tensor(out=tp[:, 0:8], in0=pn[:, 68:76], in1=self.cvec[:, cvb + CV_BF:cvb + CV_BF + 8], op=ALU.add), reads=[pn, self.cvec], writes=[tp])
                            c.op("dve", lambda h, pn=pn, tp=tp: h.tensor_tensor(out=tp[:, 8:12], in0=pn[:, 80:84], in1=self.cvec[:, cvb + CV_DT:cvb + CV_DT + 4], op=ALU.add), reads=[pn, self.cvec, tp], writes=[tp])
                            c.op("act", lambda h, tp=tp: h.activation(out=tp[:, 0:8], in_=tp[:, 0:8], func=AF.Exp, scale=-1.0), reads=[tp], writes=[tp])
                            c.op("act", lambda h, tp=tp: h.activation(out=tp[:, 8:12], in_=tp[:, 8:12], func=AF.Exp), reads=[tp], writes=[tp])
                            c.op("act", lambda h, tp=tp: h.activation(out=tp[:, 0:12], in_=tp[:, 0:12], func=AF.Ln, bias=1.0), reads=[tp], writes=[tp])
                            c.op("dve", lambda h, tp=tp, sm=sm, tt=tt: h.tensor_scalar(out=sm[:, tt, 4:12], in0=tp[:, 0:8], scalar1=-1.0, scalar2=None, op0=ALU.mult), reads=[tp], writes=[sm])
                            c.op("dve", lambda h, tp=tp, sm=sm, tt=tt: h.tensor_tensor(out=sm[:, tt, 16:20], in0=tp[:, 8:12], in1=negA[:, :], op=ALU.mult), reads=[tp, negA, sm], writes=[sm])
                            c.op("act", lambda h, pn=pn, sm=sm, tt=tt: h.activation(out=sm[:, tt, 12:16], in_=pn[:, 76:80], func=AF.Sigmoid), reads=[pn, sm], writes=[sm])
                        c.dma("sp", self.smallD.rearrange("(n p) c -> p n c", p=128)[:, g * 4:(g + 1) * 4, 0:20], sm[:, :, 0:20], reads=[sm], writes=[self.db("smallD", g)])
                        c.dma("sp", self.vA.rearrange("(n p) d -> p n d", p=128)[:, g * 4:(g + 1) * 4, :], va[:, :, :], reads=[va], writes=[self.db("vA", g)])
                c.barrier()


class ProgAB(ProgM1):
    def rotset(self, key, banks):
        d = self.__dict__.setdefault("_rs", {})
        d[key] = (d.get(key, -1) + 1) % len(banks)
        return self.ps[banks[d[key]]]

    def _b_setup(self, es, lbanks, pbanks):
        c, S, NT, NG = self.c, self.S, self.NT, self.NG
        qbh = [c.sb(es, "qbh", [128, S], BF16) for _ in range(2)]
        kbh = [c.sb(es, "kbh", [128, S], BF16) for _ in range(2)]
        vext = [c.sb(es, "vext", [128, NT, 128], BF16) for _ in range(2)]
        lf = c.sb(es, "lf", [128, NT, 8], F32)
        lfacc = c.sb(es, "lfacc", [128, NT + 1, 8], F32)
        csb = c.sb(es, "csb", [128, NT, 8], F32)
        carry = c.sb(es, "carry", [128, NT, 8], F32)
        npair = NT * (NT + 1) // 2
        bias = c.sb(es, "biasall", [128, npair, 8], F32)
        pts = [c.sb(es, "pt", [128, 512], BF16) for _ in range(4)]
        ptq = [[Buf(name=f"ptq{a}_{b}") for b in range(4)] for a in range(4)]
        rsum = [c.sb(es, "rsum", [64, 512], F32) for _ in range(2)]
        nums = [c.sb(es, "bnum", [64, 512], F32) for _ in range(2)]
        yst = [c.sb(es, "yst", [64, 512], BF16) for _ in range(2)]
        allg = lambda nm: [self.db(nm, g) for g in range(NG)]
        c.dma("sp", lf[:, :, :], self.smallD.rearrange("(n p) c -> p n c", p=128)[:, :, 4:12], reads=allg("smallD"), writes=[lf])
        for v in vext:
            c.op("pool", lambda h, v=v: h.memset(v[:, :, 64:128], 1.0), writes=[v])
        c.op("pool", lambda h: h.memset(lfacc[:, 0, :], 0.0), writes=[lfacc])
        for n in range(NT):
            c.op("dve", lambda h, n=n: h.tensor_tensor(out=lfacc[:, n + 1, :], in0=lfacc[:, n, :], in1=lf[:, n, :], op=ALU.add), reads=[lfacc, lf], writes=[lfacc])
        for n in range(NT):
            pb = self.rotset("bpl", lbanks)
            c.op("pe", lambda h, n=n, pb=pb: h.matmul(pb[:, 0:8], lhsT=self.triu_f[:, :], rhs=lf[:, n, :], start=True, stop=False), reads=[self.triu_f, lf], writes=[pb], sig=False)
            c.op("pe", lambda h, n=n, pb=pb: h.matmul(pb[:, 0:8], lhsT=self.ones_f[:, :], rhs=lfacc[:, n, :], start=False, stop=True), reads=[self.ones_f, lfacc], writes=[pb])
            c.op("act", lambda h, n=n, pb=pb: h.activation(out=csb[:, n, :], in_=pb[:, 0:8], func=AF.Copy), reads=[pb], writes=[csb])
            pb = self.rotset("bpl", lbanks)
            c.op("pe", lambda h, n=n, pb=pb: h.matmul(pb[:, 0:8], lhsT=self.ones_f[:, :], rhs=lfacc[:, n, :], start=True, stop=True), reads=[self.ones_f, lfacc], writes=[pb])
            c.op("act", lambda h, n=n, pb=pb: h.activation(out=carry[:, n, :], in_=pb[:, 0:8], func=AF.Copy), reads=[pb], writes=[carry])
        pidx = {}
        pi = 0
        for i in range(NT):
            for j in range(i + 1):
                pidx[(i, j)] = pi
                c.op("pool", lambda h, i=i, j=j, pi=pi: h.tensor_tensor(out=bias[:, pi, :], in0=carry[:, i, :], in1=csb[:, j, :], op=ALU.subtract), reads=[carry, csb], writes=[bias])
                pi += 1
        vB3 = self.vB.rearrange("(n p) d -> p n d", p=128)
        yb = self.yT[1]
        steps = [(hh, g, j) for hh in range(8) for g in range(NG) for j in range(4 * g + 4)]
        pls = {}
        loaded = set()

        def load_pair(hp):
            if hp in loaded or hp >= 4:
                return
            loaded.add(hp)
            c.dma("pool", qbh[hp % 2][:, :], self.qbT[hp * 128:(hp + 1) * 128, :], reads=allg("qbT"), writes=[qbh[hp % 2]])
            c.dma("pool", kbh[hp % 2][:, :], self.kbT[hp * 128:(hp + 1) * 128, :], reads=allg("kbT"), writes=[kbh[hp % 2]])

        def logits(k):
            hh, g, j = steps[k]
            hb, hp = (hh % 2) * 64, hh // 2
            load_pair(hp)
            qb, kb = qbh[hp % 2], kbh[hp % 2]
            col0 = max(j - 4 * g, 0) * 128
            pl = self.rotset("bpl", lbanks)
            pls[k] = pl
            c.op("pe", lambda h: h.matmul(pl[:, col0:512], lhsT=kb[hb:hb + 64, j * 128:(j + 1) * 128],
                                          rhs=qb[hb:hb + 64, g * 512 + col0:(g + 1) * 512], start=True, stop=True),
                 reads=[kb, qb], writes=[pl])

        def gen():
            po = None
            logits(0)
            for k, (hh, g, j) in enumerate(steps):
                ve = vext[hh % 2]
                if g == 0 and j == 0:
                    c.dma("pool", ve[:, :, 0:64], vB3[:, :, hh * 64:(hh + 1) * 64], reads=allg("vB"), writes=[ve])
                if j == 0:
                    po = self.rotset("bpo", pbanks)
                if k + 1 < len(steps):
                    logits(k + 1)
                nj = 4 * g + 4
                r = j - 4 * g
                col0 = max(r, 0) * 128
                pl = pls.pop(k)
                pt = pts[j % 4]
                for qq in range(max(r, 0), 4):
                    pi = pidx[(4 * g + qq, j)]
                    qs_ = slice(qq * 128, (qq + 1) * 128)
                    c.op("act", lambda h, pl=pl, pt=pt, qs_=qs_, pi=pi, hh=hh: h.activation(out=pt[:, qs_], in_=pl[:, qs_], func=AF.Exp, scale=0.125, bias=bias[:, pi, hh:hh + 1]),
                         reads=[pl, bias], writes=[ptq[j % 4][qq]])
                if r >= 0:
                    c.op("pool", lambda h, pt=pt, col0=col0: h.tensor_tensor(out=pt[:, col0:col0 + 128], in0=pt[:, col0:col0 + 128], in1=self.tri01_b[:, :], op=ALU.mult),
                         reads=[ptq[j % 4][r], self.tri01_b], writes=[ptq[j % 4][r]])
                c.op("pe", lambda h, po=po, pt=pt, j=j, col0=col0, nj=nj, ve=ve: h.matmul(po[:, col0:512], lhsT=ve[:, j, :], rhs=pt[:, col0:512], start=(j == 0), stop=(j == nj - 1)),
                     reads=[ve] + ptq[j % 4][max(r, 0):4], writes=[po], sig=(j == nj - 1))
                if j == nj - 1:
                    rs_, ys_, nm_ = rsum[g % 2], yst[g % 2], nums[g % 2]
                    c.op("act", lambda h, po=po, rs_=rs_: h.activation(out=rs_[:, :], in_=po[64:128, :], func=AF.Copy), reads=[po], writes=[rs_])
                    c.op("act", lambda h, po=po, nm_=nm_: h.activation(out=nm_[:, :], in_=po[0:64, :], func=AF.Copy), reads=[po], writes=[nm_])
                    c.op("act", lambda h, rs_=rs_: h.activation(out=rs_[:, :], in_=rs_[:, :], func=AF.Ln), reads=[rs_], writes=[rs_])
                    c.op("act", lambda h, rs_=rs_: h.activation(out=rs_[:, :], in_=rs_[:, :], func=AF.Exp, scale=-1.0), reads=[rs_], writes=[rs_])
                    c.op("pool", lambda h, nm_=nm_, rs_=rs_, ys_=ys_: h.tensor_tensor(out=ys_[:, :], in0=nm_[:, :], in1=rs_[:, :], op=ALU.mult), reads=[nm_, rs_], writes=[ys_])
                    c.dma("pool", yb[hh * 64:(hh + 1) * 64, g * 512:(g + 1) * 512], ys_[:, :], reads=[ys_], writes=[self.db("yT1", hh * 100 + g)])
                yield k
        return gen(), len(steps)

    def b_phase(self):
        with ExitStack() as es:
            g, n = self._b_setup(es, [0, 1, 2, 3, 4, 5], [6, 7])
            for _ in g:
                pass
            self.c.barrier()

    NITER = 16
    TIE = True

    def _a_setup(self, es, sbanks, lpairs, pvpair):
        c, S, NT, NG = self.c, self.S, self.NT, self.NG
        NIT = self.NITER
        qi = c.sb(es, "qi", [128, 2, S], BF16)
        ki = c.sb(es, "ki", [128, S], BF16)
        ka = c.sb(es, "ka", [64, S], BF16)
        vext = c.sb(es, "vexta", [128, NT, 128], BF16)
        wi = c.sb(es, "wi", [128, NT, 4], F32)
        qat = [c.sb(es, "qat", [64, 1024], BF16) for _ in range(2)]
        score = c.sb(es, "score", [128, S], F32)
        isz = c.sb(es, "isz", [128, S], BF16)
        zrk = c.sb(es, "zrk", [128, S], BF16)
        maskb = [c.sb(es, "maskb", [128, S], BF16) for _ in range(2)]
        irep = c.sb(es, "irep", [128, 512], BF16)
        negc = c.sb(es, "negc", [128, 128], F32)
        pow2 = c.sb(es, "pow2", [128, NIT], F32)
        rts = [c.sb(es, "rt", [128, 512], F32) for _ in range(3)]
        pts = [c.sb(es, "pta", [128, 1024], BF16) for _ in range(3)]
        sm = c.sb(es, "bis", [128, 16], F32)
        steps = c.sb(es, "steps", [128, NIT], F32)
        rsum = c.sb(es, "rsuma", [64, 1024], F32)
        rsb = [Buf(name="rsa0"), Buf(name="rsa1")]
        yst = [c.sb(es, "ysta", [64, 1024], BF16) for _ in range(2)]
        cm = self.dram["cmat"]
        cb = self.db("cmat")
        for r in range(4):
            c.dma("pool", irep[:, r * 128:(r + 1) * 128], cm[0], reads=[cb], writes=[irep])
        c.dma("sp", negc[:, :], cm[4], reads=[cb], writes=[negc])
        c.dma("sp", pow2[:, :], self.dram["pow2"][:, 0:NIT], reads=[self.db("pow2")], writes=[pow2])
        allg = lambda nm: [self.db(nm, g) for g in range(NG)]
        c.dma("sp", qi[:, :, :], self.qiT.rearrange("(hp p) t -> p hp t", p=128), reads=allg("qiT"), writes=[qi])
        c.dma("sp", ki[0:64, :], self.kiT[:, :], reads=allg("kiT"), writes=[ki])
        c.dma("sp", ki[64:128, :], self.kiT[:, :], reads=allg("kiT"), writes=[ki])
        c.dma("sp", ka[:, :], self.kaT[:, :], reads=allg("kaT"), writes=[ka])
        c.dma("sp", vext[:, :, 0:64], self.vA.rearrange("(n p) d -> p n d", p=128), reads=allg("vA"), writes=[vext])
        c.op("pool", lambda h: h.memset(vext[:, :, 64:128], 1.0), writes=[vext])
        c.dma("sp", wi[:, :, :], self.smallD.rearrange("(n p) c -> p n c", p=128)[:, :, 0:4], reads=allg("smallD"), writes=[wi])
        qa3 = self.qaT.rearrange("(h d) t -> d h t", d=64)
        ya = self.yT[0].rearrange("(h d) t -> d h t", d=64)
        NEGM = -32768.0

        def stage1(i):
            ncols = (i + 1) * 128
            qt = qat[i % 2]
            mb = maskb[i % 2]
            c.dma("sp", qt[:, :].rearrange("d (h t) -> d h t", h=8), qa3[:, :, i * 128:(i + 1) * 128], reads=allg("qaT"), writes=[qt])
            for s0 in range(0, ncols, 512):
                w = min(512, ncols - s0)
                for hd in range(4):
                    pl = self.rotset("apl", sbanks)
                    hb, hp = (hd % 2) * 64, hd // 2
                    c.op("pe", lambda h, pl=pl, hb=hb, hp=hp, s0=s0, w=w: h.matmul(pl[:, 0:w], lhsT=qi[hb:hb + 64, hp, i * 128:(i + 1) * 128], rhs=ki[hb:hb + 64, s0:s0 + w], start=True, stop=True),
                         reads=[qi, ki], writes=[pl])
                    if hd == 0:
                        c.op("dve", lambda h, pl=pl, s0=s0, w=w: h.tensor_scalar(out=score[:, s0:s0 + w], in0=pl[:, 0:w], scalar1=0.0, scalar2=wi[:, i, 0:1], op0=ALU.max, op1=ALU.mult),
                             reads=[pl, wi], writes=[score])
                    else:
                        rt = rts[hd - 1]
                        c.op("act", lambda h, pl=pl, rt=rt, w=w: h.activation(out=rt[:, 0:w], in_=pl[:, 0:w], func=AF.Relu), reads=[pl], writes=[rt])
                        c.op("dve", lambda h, rt=rt, hd=hd, s0=s0, w=w: h.scalar_tensor_tensor(out=score[:, s0:s0 + w], in0=rt[:, 0:w], scalar=wi[:, i, hd:hd + 1], in1=score[:, s0:s0 + w],
                                                                                            op0=ALU.mult, op1=ALU.add),
                             reads=[rt, wi, score], writes=[score])
            sc = score[:, 0:ncols]
            c.op("dve", lambda h: h.tensor_reduce(out=sm[:, 5:6], in_=sc, axis=AX.X, op=ALU.max), reads=[score], writes=[sm])
            c.op("dve", lambda h: h.tensor_reduce(out=sm[:, 6:7], in_=sc, axis=AX.X, op=ALU.min), reads=[score, sm], writes=[sm])
            c.op("dve", lambda h: h.tensor_tensor(out=score[:, i * 128:ncols], in0=score[:, i * 128:ncols], in1=negc[:, :], op=ALU.add), reads=[score, negc], writes=[score])
            c.op("dve", lambda h: h.tensor_scalar(out=sm[:, 0:1], in0=sm[:, 6:7], scalar1=-1.0, scalar2=None, op0=ALU.add), reads=[sm], writes=[sm])
            c.op("dve", lambda h: h.scalar_tensor_tensor(out=sm[:, 1:2], in0=sm[:, 5:6], scalar=1.0, in1=sm[:, 0:1], op0=ALU.add, op1=ALU.subtract), reads=[sm], writes=[sm])
            c.op("dve", lambda h: h.tensor_scalar(out=steps[:, :], in0=pow2[:, :], scalar1=sm[:, 1:2], scalar2=None, op0=ALU.mult), reads=[sm, pow2], writes=[steps])
            for k in range(NIT):
                c.op("dve", lambda h, k=k: h.tensor_tensor(out=sm[:, 2:3], in0=sm[:, 0:1], in1=steps[:, k:k + 1], op=ALU.add), reads=[sm, steps], writes=[sm])
                c.op("dve", lambda h: h.tensor_scalar(out=isz[:, 0:ncols], in0=sc, scalar1=sm[:, 2:3], scalar2=0.0, op0=ALU.is_ge, op1=ALU.add, accum_out=sm[:, 3:4]),
                     reads=[score, sm], writes=[isz, sm])
                c.op("dve", lambda h, k=k: h.scalar_tensor_tensor(out=sm[:, 4:5], in0=sm[:, 3:4], scalar=255.5, in1=steps[:, k:k + 1], op0=ALU.is_ge, op1=ALU.mult), reads=[sm, steps], writes=[sm])
                c.op("dve", lambda h: h.tensor_tensor(out=sm[:, 0:1], in0=sm[:, 0:1], in1=sm[:, 4:5], op=ALU.add), reads=[sm], writes=[sm])
            c.op("dve", lambda h: h.tensor_scalar(out=isz[:, 0:ncols], in0=sc, scalar1=0.0, scalar2=0.0, op0=ALU.is_gt, op1=ALU.add, accum_out=sm[:, 7:8]), reads=[score, sm], writes=[isz, sm])
            c.op("dve", lambda h: h.tensor_scalar(out=isz[:, 0:ncols], in0=sc, scalar1=0.0, scalar2=0.0, op0=ALU.is_equal, op1=ALU.add, accum_out=sm[:, 8:9]), reads=[score, sm], writes=[isz, sm])
            c.op("dve", lambda h: h.tensor_tensor(out=sm[:, 8:9], in0=sm[:, 8:9], in1=sm[:, 7:8], op=ALU.add), reads=[sm], writes=[sm])
            c.op("dve", lambda h: h.tensor_scalar(out=sm[:, 13:14], in0=sm[:, 7:8], scalar1=255.5, scalar2=None, op0=ALU.is_lt), reads=[sm], writes=[sm])
            c.op("dve", lambda h: h.scalar_tensor_tensor(out=sm[:, 9:10], in0=sm[:, 8:9], scalar=255.5, in1=sm[:, 13:14], op0=ALU.is_ge, op1=ALU.mult), reads=[sm], writes=[sm])
            c.op("dve", lambda h: h.tensor_scalar(out=sm[:, 10:11], in0=sm[:, 7:8], scalar1=-1.0, scalar2=256.5, op0=ALU.mult, op1=ALU.add), reads=[sm], writes=[sm])
            c.op("dve", lambda h: h.tensor_scalar(out=sm[:, 13:14], in0=sm[:, 9:10], scalar1=-1.0, scalar2=1.0, op0=ALU.mult, op1=ALU.add), reads=[sm], writes=[sm])
            c.op("dve", lambda h: h.tensor_tensor(out=sm[:, 13:14], in0=sm[:, 13:14], in1=sm[:, 0:1], op=ALU.mult), reads=[sm], writes=[sm])
            c.op("dve", lambda h: h.scalar_tensor_tensor(out=sm[:, 11:12], in0=sm[:, 9:10], scalar=1e-30, in1=sm[:, 13:14], op0=ALU.mult, op1=ALU.add), reads=[sm], writes=[sm])
            c.op("dve", lambda h: h.tensor_scalar(out=sm[:, 12:13], in0=sm[:, 9:10], scalar1=-NEGM, scalar2=None, op0=ALU.mult), reads=[sm], writes=[sm])
            c.op("dve", lambda h: h.tensor_tensor_scan(out=zrk[:, 0:ncols], data0=isz[:, 0:ncols], data1=isz[:, 0:ncols], initial=0.0, op0=ALU.add, op1=ALU.max), reads=[isz], writes=[zrk])
            c.op("dve", lambda h: h.scalar_tensor_tensor(out=isz[:, 0:ncols], in0=zrk[:, 0:ncols], scalar=sm[:, 10:11], in1=isz[:, 0:ncols], op0=ALU.is_le, op1=ALU.mult), reads=[zrk, sm, isz], writes=[isz])
            c.op("dve", lambda h, mb=mb: h.tensor_scalar(out=mb[:, 0:ncols], in0=sc, scalar1=sm[:, 11:12], scalar2=NEGM, op0=ALU.is_lt, op1=ALU.mult), reads=[score, sm], writes=[mb])
            if self.TIE:
                c.op("dve", lambda h, mb=mb: h.scalar_tensor_tensor(out=mb[:, 0:ncols], in0=isz[:, 0:ncols], scalar=sm[:, 12:13], in1=mb[:, 0:ncols], op0=ALU.mult, op1=ALU.add), reads=[isz, sm, mb], writes=[mb])

        def stage2(i):
            qt = qat[i % 2]
            mb = maskb[i % 2]
            po = (self.ps[pvpair[0]], self.ps[pvpair[1]])
            lb = [b_ for pr in lpairs for b_ in pr]
            nlb = len(lb)
            hsteps = [(j, half) for j in range(i + 1) for half in range(2)]

            def alog(k):
                j, half = hsteps[k]
                pl = self.ps[lb[k % nlb]]
                c.op("pe", lambda h: h.matmul(pl[:, :], lhsT=ka[:, j * 128:(j + 1) * 128], rhs=qt[:, half * 512:(half + 1) * 512], start=True, stop=False),
                     reads=[ka, qt], writes=[pl], sig=False)
                c.op("pe", lambda h: h.matmul(pl[:, :], lhsT=mb[:, j * 128:(j + 1) * 128], rhs=irep[:, :], start=False, stop=True),
                     reads=[mb, irep], writes=[pl])

            ptb = [[Buf(name=f"apt{a_}_{b_}") for b_ in range(2)] for a_ in range(3)]
            la = min(nlb - 1, 3)
            for k in range(min(la, len(hsteps))):
                alog(k)
            for k, (j, half) in enumerate(hsteps):
                if k + la < len(hsteps):
                    alog(k + la)
                pl = self.ps[lb[k % nlb]]
                pt = pts[j % 3]
                c.op("act", lambda h, pl=pl, half=half, pt=pt: h.activation(out=pt[:, half * 512:(half + 1) * 512], in_=pl[:, :], func=AF.Exp, scale=0.125), reads=[pl], writes=[self._aptb(pt, half)])
                c.op("pe", lambda h, half=half, j=j, pt=pt, po=po: h.matmul(po[half][:, :], lhsT=vext[:, j, :], rhs=pt[:, half * 512:(half + 1) * 512], start=(j == 0), stop=(j == i)),
                     reads=[vext, self._aptb(pt, half)], writes=[po[half]], sig=(j == i))
            ys_ = yst[i % 2]
            for half in range(2):
                hs = slice(half * 512, (half + 1) * 512)
                rb = rsb[half]
                c.op("act", lambda h, half=half, hs=hs: h.activation(out=rsum[:, hs], in_=po[half][64:128, :], func=AF.Copy), reads=[po[half]], writes=[rb])
                c.op("act", lambda h, hs=hs: h.activation(out=rsum[:, hs], in_=rsum[:, hs], func=AF.Ln), reads=[rb], writes=[rb])
                c.op("act", lambda h, hs=hs: h.activation(out=rsum[:, hs], in_=rsum[:, hs], func=AF.Exp, scale=-1.0), reads=[rb], writes=[rb])
            for half in range(2):
                hs = slice(half * 512, (half + 1) * 512)
                c.op("dve", lambda h, half=half, hs=hs, ys_=ys_: h.tensor_tensor(out=ys_[:, hs], in0=po[half][0:64, :], in1=rsum[:, hs], op=ALU.mult), reads=[po[half], rsb[half]], writes=[ys_])
            c.dma("sp", ya[:, :, i * 128:(i + 1) * 128], ys_[:, :].rearrange("d (h t) -> d h t", h=8), reads=[ys_], writes=[self.db("yT0", i)])

        return stage1, stage2

    def _aptb(self, pt, half):
        d = self.__dict__.setdefault("_aptbufs", {})
        k = (id(pt), half)
        if k not in d:
            d[k] = Buf(name=f"aptb{len(d)}")
        return d[k]

    def a_phase(self):
        NT = self.NT
        with ExitStack() as es:
            stage1, stage2 = self._a_setup(es, [0, 1], [(2, 3), (4, 5)], (6, 7))
            stage1(0)
            for i in range(NT):
                if i + 1 < NT:
                    stage1(i + 1)
                stage2(i)
            self.c.barrier()

    def ab_phase(self):
        NT = self.NT
        with ExitStack() as es:
            stage1, stage2 = self._a_setup(es, [0, 1, 2], [(1, 2)], (3, 4))
            bgen, nb = self._b_setup(es, [5, 6], [7])
            done = 0

            def advance(upto):
                nonlocal done
                while done < min(upto, nb):
                    next(bgen)
                    done += 1
            wts = [15.5 + 1.24 * (u + 1) for u in range(NT)]
            tot = sum(wts)
            cum, acc = [], 0.0
            for w_ in wts:
                acc += w_
                cum.append(int(round(nb * acc / tot)))
            stage1(0)
            for i in range(NT):
                if i + 1 < NT:
                    stage1(i + 1)
                stage2(i)
                advance(cum[i])
            advance(nb)
            self.c.barrier()


def build_gmask():
    i = np.arange(128)
    out = np.zeros((14, 128, 512), np.float32)
    for l in range(7):
        b = 1 << l
        t, tp = i[:, None], i[None, :]
        m = ((t // (2 * b)) == (tp // (2 * b))) & ((t % (2 * b)) >= b) & ((tp % (2 * b)) < b)
        m = m.astype(np.float32)
        out[l] = np.tile(m, (1, 4))
        out[7 + l] = np.tile(m.T, (1, 4))
    return out


class ProgC(ProgAB):
    def c_phase(self, cvb):
        c, S, NT = self.c, self.S, self.NT
        with ExitStack() as es:
            def T(name, shape, dt, n=1):
                return [c.sb(es, name, shape, dt) for _ in range(n)]
            gm = T("gm", [128, 14, 512], BF16)[0]
            identrep = T("identrep", [128, 512], BF16)[0]
            negm4 = T("negm4", [128, 512], F32)[0]
            strict4 = T("strict4", [128, 512], BF16)[0]
            cm = self.dram["cmat"]
            cb = self.db("cmat")
            c.dma("pool", gm[:, :, :], self.dram["gmask"].rearrange("l p n -> p l n"), reads=[self.db("gmask")], writes=[gm])
            for r in range(4):
                c.dma("pool", identrep[:, r * 128:(r + 1) * 128], cm[0], reads=[cb], writes=[identrep])
                c.dma("sp", negm4[:, r * 128:(r + 1) * 128], cm[3], reads=[cb], writes=[negm4])
                c.dma("pool", strict4[:, r * 128:(r + 1) * 128], cm[5], reads=[cb], writes=[strict4])
            kTs = T("kTc", [128, 4, 128], BF16, 2); qTs = T("qTc", [128, 4, 128], BF16, 2)
            ktoks = T("ktok", [128, 512], BF16, 2); vtoks = T("vtok", [128, 512], BF16, 2)
            szs = T("szc", [128, 4, 128], BF16, 2); gbs = T("gb", [128, 8], F32, 2)
            gtri = T("gtri", [128, 4, 128], F32)[0]; ngtri = T("ngtri", [128, 4, 128], F32)[0]; bdiag = T("bdiag", [128, 4, 128], F32)[0]
            dm = T("dm", [128, 512], F32)[0]; decT = T("decT", [128, 512], F32)[0]; egcb = T("egcb", [128, 512], F32)[0]
            bbc = T("bbc", [128, 512], F32)[0]; dsb = T("dsb", [128, 512], F32)[0]
            ngb = T("ngb", [128, 4], F32)[0]
            gcs = T("gcs", [128, 8], F32)[0]; sm = T("csm", [128, 16], F32)[0]
            ATb = T("ATb", [128, 4, 128], BF16)[0]; Ab = T("Ab", [128, 4, 128], BF16)[0]; qkb = T("qkb", [128, 4, 128], BF16)[0]
            Ts = T("Tm", [128, 4, 128], BF16, 2); Us = T("Um", [128, 4, 128], BF16, 2)
            Xb = T("Xb", [128, 4, 128], BF16)[0]; Xpb = T("Xpb", [128, 4, 128], BF16)[0]; tmpb = T("tmpb", [128, 512], BF16, 2)
            kbg = T("kbg", [128, 512], BF16)[0]; vb = T("vb", [128, 512], BF16)[0]; kdec = T("kdec", [128, 512], BF16)[0]
            qg = T("qg", [128, 4, 128], BF16)[0]; nwT = T("nwT", [128, 4, 128], BF16)[0]; vnew = T("vnew", [128, 512], BF16)[0]
            state_f = T("state_f", [128, 4, 128], F32)[0]; state_b = T("state_b", [128, 4, 128], BF16)[0]
            osq = T("osq", [128, 4, 128], BF16)[0]; rstd = T("crstd", [128, 512], F32)[0]; yn = T("yn", [128, 512], F32)[0]
            yst = T("ystc", [128, 4, 128], BF16, 2)
            c.op("pool", lambda h: h.memset(state_f[:, :, :], 0.0), writes=[state_f])
            c.op("pool", lambda h: h.memset(state_b[:, :, :], 0.0), writes=[state_b])
            qc3 = self.qcT.rearrange("(h d) t -> d h t", d=128); kc3 = self.kcT.rearrange("(h d) t -> d h t", d=128)
            sz3 = self.szT.rearrange("(h d) t -> d h t", d=128); yc3 = self.yT[2].rearrange("(h d) t -> d h t", d=128)
            allg = lambda nm: [self.db(nm, g) for g in range(self.NG)]
            f2 = lambda b: b[:, :, :].rearrange("p h t -> p (h t)")
            for n in range(NT):
                cs = slice(n * 128, (n + 1) * 128)
                kT, qT, ktok, vtok, sz, gb = kTs[n % 2], qTs[n % 2], ktoks[n % 2], vtoks[n % 2], szs[n % 2], gbs[n % 2]
                c.dma("sp", kT[:, :, :], kc3[:, :, cs], reads=allg("kcT"), writes=[kT])
                c.dma("sp", qT[:, :, :], qc3[:, :, cs], reads=allg("qcT"), writes=[qT])
                c.dma("sp", sz[:, :, :], sz3[:, :, cs], reads=allg("szT"), writes=[sz])
                c.dma("sp", ktok[:, :], self.kC[cs, :], reads=allg("kC"), writes=[ktok])
                c.dma("sp", vtok[:, :], self.vC[cs, :], reads=allg("vC"), writes=[vtok])
                c.dma("sp", gb[:, :], self.smallD[cs, 12:20], reads=allg("smallD"), writes=[gb])
                c.op("dve", lambda h: h.tensor_scalar(out=ngb[:, :], in0=gb[:, 4:8], scalar1=-1.0, scalar2=None, op0=ALU.mult), reads=[gb], writes=[ngb])
                for hd in range(4):
                    c.op("dve", lambda h, hd=hd: h.tensor_scalar(out=gtri[:, hd, :], in0=self.triu_f[:, :], scalar1=gb[:, 4 + hd:5 + hd], scalar2=None, op0=ALU.mult), reads=[self.triu_f, gb], writes=[gtri])
                    c.op("act", lambda h, hd=hd: h.activation(out=ngtri[:, hd, :], in_=self.triu_f[:, :], func=AF.Copy, scale=ngb[:, hd:hd + 1]), reads=[self.triu_f, ngb], writes=[ngtri])
                    c.op("act", lambda h, hd=hd: h.activation(out=bdiag[:, hd, :], in_=self.ident_f[:, :], func=AF.Copy, scale=gb[:, hd:hd + 1]), reads=[self.ident_f, gb], writes=[bdiag])
                pD, pG, pB, pS = self.rot(), self.rot(), self.rot(), self.rot()
                for hd in range(4):
                    hs = slice(hd * 128, (hd + 1) * 128)
                    c.op("pe", lambda h, hd=hd, hs=hs: h.matmul(pD[:, hs], lhsT=self.ones_f[:, :], rhs=gtri[:, hd, :], start=True, stop=False), reads=[self.ones_f, gtri], writes=[pD], sig=False)
                    c.op("pe", lambda h, hd=hd, hs=hs: h.matmul(pD[:, hs], lhsT=ngtri[:, hd, :], rhs=self.ones_f[:, :], start=False, stop=True), reads=[self.ones_f, ngtri], writes=[pD], sig=(hd == 3))
                for hd in range(4):
                    hs = slice(hd * 128, (hd + 1) * 128)
                    c.op("pe", lambda h, hd=hd, hs=hs: h.matmul(pG[:, hs], lhsT=self.ones_f[:, :], rhs=gtri[:, hd, :], start=True, stop=True), reads=[self.ones_f, gtri], writes=[pG], sig=(hd == 3))
                for hd in range(4):
                    hs = slice(hd * 128, (hd + 1) * 128)
                    c.op("pe", lambda h, hd=hd, hs=hs: h.matmul(pB[:, hs], lhsT=self.ones_f[:, :], rhs=bdiag[:, hd, :], start=True, stop=True), reads=[self.ones_f, bdiag], writes=[pB], sig=(hd == 3))
                c.op("pe", lambda h: h.matmul(pS[:, 0:4], lhsT=self.triu_f[:, :], rhs=gb[:, 4:8], start=True, stop=True), reads=[self.triu_f, gb], writes=[pS], sig=False)
                c.op("pe", lambda h: h.matmul(pS[:, 4:8], lhsT=self.ones_f[:, :], rhs=gb[:, 4:8], start=True, stop=True), reads=[self.ones_f, gb], writes=[pS])
                c.op("dve", lambda h: h.scalar_tensor_tensor(out=dm[:, :], in0=pD[:, :], scalar=0.0, in1=negm4[:, :], op0=ALU.min, op1=ALU.add), reads=[pD, negm4], writes=[dm])
                c.op("act", lambda h: h.activation(out=decT[:, :], in_=dm[:, :], func=AF.Exp), reads=[dm], writes=[decT])
                c.op("act", lambda h: h.activation(out=egcb[:, :], in_=pG[:, :], func=AF.Exp), reads=[pG], writes=[egcb])
                c.op("act", lambda h: h.activation(out=bbc[:, :], in_=pB[:, :], func=AF.Copy), reads=[pB], writes=[bbc])
                c.op("act", lambda h: h.activation(out=gcs[:, :], in_=pS[:, 0:8], func=AF.Copy), reads=[pS], writes=[gcs])
                c.op("act", lambda h: h.activation(out=sm[:, 0:4], in_=gcs[:, 0:4], func=AF.Exp), reads=[gcs], writes=[sm])
                c.op("dve", lambda h: h.tensor_tensor(out=sm[:, 0:4], in0=sm[:, 0:4], in1=gb[:, 0:4], op=ALU.mult), reads=[sm, gb], writes=[sm])
                c.op("dve", lambda h: h.tensor_tensor(out=sm[:, 12:16], in0=gcs[:, 4:8], in1=gcs[:, 0:4], op=ALU.subtract), reads=[gcs, sm], writes=[sm])
                c.op("act", lambda h: h.activation(out=sm[:, 4:8], in_=sm[:, 12:16], func=AF.Exp), reads=[sm], writes=[sm])
                c.op("act", lambda h: h.activation(out=sm[:, 8:12], in_=gcs[:, 4:8], func=AF.Exp), reads=[gcs, sm], writes=[sm])
                pK, pQ = self.rot(), self.rot()
                for hd in range(4):
                    hs = slice(hd * 128, (hd + 1) * 128)
                    c.op("pe", lambda h, hd=hd, hs=hs: h.matmul(pK[:, hs], lhsT=kT[:, hd, :], rhs=kT[:, hd, :], start=True, stop=True), reads=[kT], writes=[pK], sig=(hd == 3))
                for hd in range(4):
                    hs = slice(hd * 128, (hd + 1) * 128)
                    c.op("pe", lambda h, hd=hd, hs=hs: h.matmul(pQ[:, hs], lhsT=kT[:, hd, :], rhs=qT[:, hd, :], start=True, stop=True), reads=[kT, qT], writes=[pQ], sig=(hd == 3))
                c.op("dve", lambda h: h.tensor_tensor(out=dsb[:, :], in0=decT[:, :], in1=bbc[:, :], op=ALU.mult), reads=[decT, bbc], writes=[dsb])
                c.op("dve", lambda h: h.tensor_tensor(out=dsb[:, :], in0=dsb[:, :], in1=strict4[:, :], op=ALU.mult), reads=[dsb, strict4], writes=[dsb])
                c.op("dve", lambda h: h.tensor_tensor(out=f2(ATb), in0=pK[:, :], in1=dsb[:, :], op=ALU.mult), reads=[pK, dsb], writes=[ATb])
                c.op("dve", lambda h: h.tensor_tensor(out=f2(qkb), in0=pQ[:, :], in1=decT[:, :], op=ALU.mult), reads=[pQ, decT], writes=[qkb])
                pA = self.rot()
                for hd in range(4):
                    hs = slice(hd * 128, (hd + 1) * 128)
                    c.op("pe", lambda h, hd=hd, hs=hs: h.matmul(pA[:, hs], lhsT=ATb[:, hd, :], rhs=self.ident_b[:, :], start=True, stop=True), reads=[ATb, self.ident_b], writes=[pA], sig=(hd == 3))
                c.op("act", lambda h: h.activation(out=f2(Ab), in_=pA[:, :], func=AF.Copy), reads=[pA], writes=[Ab])
                Tc, Uc = Ts[0], Us[0]
                c.op("dve", lambda h: h.tensor_tensor(out=tmpb[0][:, :], in0=f2(Ab), in1=gm[:, 0, :], op=ALU.mult), reads=[Ab, gm], writes=[tmpb[0]])
                c.op("dve", lambda h: h.tensor_tensor(out=f2(Tc), in0=identrep[:, :], in1=tmpb[0][:, :], op=ALU.subtract), reads=[tmpb[0], identrep], writes=[Tc])
                c.op("dve", lambda h: h.tensor_tensor(out=tmpb[1][:, :], in0=f2(ATb), in1=gm[:, 7, :], op=ALU.mult), reads=[ATb, gm], writes=[tmpb[1]])
                c.op("dve", lambda h: h.tensor_tensor(out=f2(Uc), in0=identrep[:, :], in1=tmpb[1][:, :], op=ALU.subtract), reads=[tmpb[1], identrep], writes=[Uc])
                for l in range(1, 7):
                    Tn, Un = Ts[l % 2], Us[l % 2]
                    pXp = self.rot()
                    for hd in range(4):
                        hs = slice(hd * 128, (hd + 1) * 128)
                        c.op("pe", lambda h, hd=hd, hs=hs, Uc=Uc: h.matmul(pXp[:, hs], lhsT=Ab[:, hd, :], rhs=Uc[:, hd, :], start=True, stop=True), reads=[Ab, Uc], writes=[pXp], sig=(hd == 3))
                    c.op("dve", lambda h, l=l: h.tensor_tensor(out=f2(Xpb), in0=pXp[:, :], in1=gm[:, 7 + l, :], op=ALU.mult), reads=[pXp, gm], writes=[Xpb])
                    if l < 6:
                        pX = self.rot()
                        for hd in range(4):
                            hs = slice(hd * 128, (hd + 1) * 128)
                            c.op("pe", lambda h, hd=hd, hs=hs, Tc=Tc: h.matmul(pX[:, hs], lhsT=ATb[:, hd, :], rhs=Tc[:, hd, :], start=True, stop=True), reads=[ATb, Tc], writes=[pX], sig=(hd == 3))
                        c.op("dve", lambda h, l=l: h.tensor_tensor(out=f2(Xb), in0=pX[:, :], in1=gm[:, l, :], op=ALU.mult), reads=[pX, gm], writes=[Xb])
                    pYp = self.rot()
                    for hd in range(4):
                        hs = slice(hd * 128, (hd + 1) * 128)
                        c.op("pe", lambda h, hd=hd, hs=hs, Tc=Tc: h.matmul(pYp[:, hs], lhsT=Tc[:, hd, :], rhs=Xpb[:, hd, :], start=True, stop=True), reads=[Tc, Xpb], writes=[pYp], sig=(hd == 3))
                    c.op("dve", lambda h, Uc=Uc, Un=Un: h.tensor_tensor(out=f2(Un), in0=f2(Uc), in1=pYp[:, :], op=ALU.subtract), reads=[Uc, pYp], writes=[Un])
                    if l < 6:
                        pY = self.rot()
                        for hd in range(4):
                            hs = slice(hd * 128, (hd + 1) * 128)
                            c.op("pe", lambda h, hd=hd, hs=hs, Uc=Uc: h.matmul(pY[:, hs], lhsT=Uc[:, hd, :], rhs=Xb[:, hd, :], start=True, stop=True), reads=[Uc, Xb], writes=[pY], sig=(hd == 3))
                        c.op("dve", lambda h, Tc=Tc, Tn=Tn: h.tensor_tensor(out=f2(Tn), in0=f2(Tc), in1=pY[:, :], op=ALU.subtract), reads=[Tc, pY], writes=[Tn])
                    Tc, Uc = Tn, Un
                U = Uc
                for hd in range(4):
                    hs = slice(hd * 128, (hd + 1) * 128)
                    c.op("act", lambda h, hd=hd, hs=hs: h.activation(out=kbg[:, hs], in_=ktok[:, hs], func=AF.Copy, scale=sm[:, hd:hd + 1]), reads=[ktok, sm], writes=[kbg])
                    c.op("act", lambda h, hd=hd, hs=hs: h.activation(out=vb[:, hs], in_=vtok[:, hs], func=AF.Copy, scale=gb[:, hd:hd + 1]), reads=[vtok, gb], writes=[vb])
                    c.op("act", lambda h, hd=hd, hs=hs: h.activation(out=kdec[:, hs], in_=ktok[:, hs], func=AF.Copy, scale=sm[:, 4 + hd:5 + hd]), reads=[ktok, sm], writes=[kdec])
                c.op("dve", lambda h: h.tensor_tensor(out=f2(qg), in0=f2(qT), in1=egcb[:, :], op=ALU.mult), reads=[qT, egcb], writes=[qg])
                pW = self.rot()
                for hd in range(4):
                    hs = slice(hd * 128, (hd + 1) * 128)
                    c.op("pe", lambda h, hd=hd, hs=hs: h.matmul(pW[:, hs], lhsT=kbg[:, hs], rhs=U[:, hd, :], start=True, stop=True), reads=[kbg, U], writes=[pW], sig=(hd == 3))
                c.op("act", lambda h: h.activation(out=f2(nwT), in_=pW[:, :], func=AF.Copy, scale=-1.0), reads=[pW], writes=[nwT])
                pV = self.rot()
                for hd in range(4):
                    hs = slice(hd * 128, (hd + 1) * 128)
                    c.op("pe", lambda h, hd=hd, hs=hs: h.matmul(pV[:, hs], lhsT=U[:, hd, :], rhs=vb[:, hs], start=True, stop=False), reads=[U, vb], writes=[pV], sig=False)
                    c.op("pe", lambda h, hd=hd, hs=hs: h.matmul(pV[:, hs], lhsT=nwT[:, hd, :], rhs=state_b[:, hd, :], start=False, stop=True), reads=[nwT, state_b], writes=[pV], sig=(hd == 3))
                c.op("act", lambda h: h.activation(out=vnew[:, :], in_=pV[:, :], func=AF.Copy), reads=[pV], writes=[vnew])
                pO = self.rot()
                for hd in range(4):
                    hs = slice(hd * 128, (hd + 1) * 128)
                    c.op("pe", lambda h, hd=hd, hs=hs: h.matmul(pO[:, hs], lhsT=state_b[:, hd, :], rhs=qg[:, hd, :], start=True, stop=False), reads=[state_b, qg], writes=[pO], sig=False)
                    c.op("pe", lambda h, hd=hd, hs=hs: h.matmul(pO[:, hs], lhsT=vnew[:, hs], rhs=qkb[:, hd, :], start=False, stop=True), reads=[vnew, qkb], writes=[pO], sig=(hd == 3))
                pS2 = self.rot()
                for hd in range(4):
                    hs = slice(hd * 128, (hd + 1) * 128)
                    c.op("pe", lambda h, hd=hd, hs=hs: h.matmul(pS2[:, hs], lhsT=kdec[:, hs], rhs=vnew[:, hs], start=True, stop=True), reads=[kdec, vnew], writes=[pS2], sig=(hd == 3))
                for hd in range(4):
                    hs = slice(hd * 128, (hd + 1) * 128)
                    c.op("dve", lambda h, hd=hd, hs=hs: h.scalar_tensor_tensor(out=state_f[:, hd, :], in0=state_f[:, hd, :], scalar=sm[:, 8 + hd:9 + hd], in1=pS2[:, hs], op0=ALU.mult, op1=ALU.add),
                         reads=[state_f, sm, pS2], writes=[state_f])
                c.op("act", lambda h: h.activation(out=f2(state_b), in_=f2(state_f), func=AF.Copy), reads=[state_f], writes=[state_b])
                c.op("act", lambda h: h.activation(out=f2(osq), in_=pO[:, :], func=AF.Square), reads=[pO], writes=[osq])
                pN = self.rot()
                for hd in range(4):
                    hs = slice(hd * 128, (hd + 1) * 128)
                    c.op("pe", lambda h, hd=hd, hs=hs: h.matmul(pN[:, hs], lhsT=self.ones_b[:, :], rhs=osq[:, hd, :], start=True, stop=True), reads=[self.ones_b, osq], writes=[pN], sig=(hd == 3))
                c.op("dve", lambda h: h.tensor_scalar(out=rstd[:, :], in0=pN[:, :], scalar1=1.0 / 128, scalar2=1e-6, op0=ALU.mult, op1=ALU.add), reads=[pN], writes=[rstd])
                c.op("act", lambda h: h.activation(out=rstd[:, :], in_=rstd[:, :], func=AF.Ln), reads=[rstd], writes=[rstd])
                c.op("act", lambda h: h.activation(out=rstd[:, :], in_=rstd[:, :], func=AF.Exp, scale=-0.5), reads=[rstd], writes=[rstd])
                c.op("dve", lambda h: h.tensor_tensor(out=yn[:, :], in0=pO[:, :], in1=rstd[:, :], op=ALU.mult), reads=[pO, rstd], writes=[yn])
                ys_ = yst[n % 2]
                c.op("dve", lambda h, ys_=ys_: h.scalar_tensor_tensor(out=f2(ys_), in0=yn[:, :], scalar=self.cvec[:, cvb + CV_DN:cvb + CV_DN + 1], in1=f2(sz), op0=ALU.mult, op1=ALU.mult),
                     reads=[yn, self.cvec, sz], writes=[ys_])
                c.dma("sp", yc3[:, :, cs], ys_[:, :, :], reads=[ys_], writes=[self.db("yT2", n)])
            c.barrier()


class ProgFull(ProgC):
    def merge_phase(self, xsrc, xdst, wbr, w_out):
        c, S, NG = self.c, self.S, self.NG
        with ExitStack() as es:
            wbs = [c.sb(es, "wbr", [128, 4, 1024], BF16) for _ in range(3)]
            wo = c.sb(es, "wo", [128, 8, 1024], BF16)
            wb = self.db("w")
            for i in range(3):
                c.dma("pool", wbs[i][:, :, :], wbr[i].rearrange("(k p) n -> p k n", p=128), reads=[wb], writes=[wbs[i]])
            c.dma("pool", wo[:, :, :], w_out.rearrange("(k p) n -> p k n", p=128), reads=[wb], writes=[wo])
            ys = [c.sb(es, "ymg", [128, 3, 4, 512], BF16) for _ in range(2)]
            gts = [c.sb(es, "gmg", [128, 24, 512], BF16) for _ in range(2)]
            xgs = [c.sb(es, "xmg", [128, 8, 512], F32) for _ in range(2)]
            mg = c.sb(es, "mg", [128, 8, 512], BF16)
            t1 = [c.sb(es, "mt1", [128, 512], F32) for _ in range(2)]
            t2 = [c.sb(es, "mt2", [128, 512], F32) for _ in range(2)]
            xs3 = xsrc.rearrange("(c p) t -> p c t", p=128)
            xd3 = xdst.rearrange("(c p) t -> p c t", p=128)
            for g in range(NG):
                tsl = slice(g * 512, (g + 1) * 512)
                y, gt, xg = ys[g % 2], gts[g % 2], xgs[g % 2]
                deps = [self.db("yT0", i) for i in range(g * 4, g * 4 + 4)] + [self.db("yT1", hh * 100 + g) for hh in range(8)] + [self.db("yT2", n) for n in range(g * 4, g * 4 + 4)]
                for br in range(3):
                    c.dma("sp", y[:, br, :, :], self.yT[br].rearrange("(k p) t -> p k t", p=128)[:, :, tsl], reads=deps, writes=[y])
                c.dma("sp", gt[:, :, :], self.gT.rearrange("(k p) t -> p k t", p=128)[:, :, tsl], reads=[self.db("gT", sec * 100 + g) for sec in range(6)], writes=[gt])
                c.dma("sp", xg[:, :, :], xs3[:, :, tsl], reads=[self.db(xsrc.name, g)], writes=[xg])
                for d in range(8):
                    pbs = []
                    for br in range(3):
                        pb = self.rot()
                        pbs.append(pb)
                        for k in range(4):
                            c.op("pe", lambda h, br=br, k=k, d=d, pb=pb: h.matmul(pb[:, :], lhsT=wbs[br][:, k, d * 128:(d + 1) * 128], rhs=y[:, br, k, :], start=(k == 0), stop=(k == 3)),
                                 reads=[wbs[br], y], writes=[pb], sig=(k == 3))
                    a, b = t1[d % 2], t2[d % 2]
                    c.op("dve", lambda h, d=d, a=a: h.tensor_tensor(out=a[:, :], in0=pbs[0][:, :], in1=gt[:, d, :], op=ALU.mult), reads=[pbs[0], gt], writes=[a])
                    c.op("dve", lambda h, d=d, b=b: h.tensor_tensor(out=b[:, :], in0=pbs[1][:, :], in1=gt[:, 8 + d, :], op=ALU.mult), reads=[pbs[1], gt], writes=[b])
                    c.op("dve", lambda h, a=a, b=b: h.tensor_tensor(out=a[:, :], in0=a[:, :], in1=b[:, :], op=ALU.add), reads=[a, b], writes=[a])
                    c.op("dve", lambda h, d=d, b=b: h.tensor_tensor(out=b[:, :], in0=pbs[2][:, :], in1=gt[:, 16 + d, :], op=ALU.mult), reads=[pbs[2], gt], writes=[b])
                    c.op("dve", lambda h, d=d, a=a, b=b: h.tensor_tensor(out=mg[:, d, :], in0=a[:, :], in1=b[:, :], op=ALU.add), reads=[a, b], writes=[mg])
                for d in range(8):
                    po = self.rot()
                    for k in range(8):
                        c.op("pe", lambda h, d=d, k=k, po=po: h.matmul(po[:, :], lhsT=wo[:, k, d * 128:(d + 1) * 128], rhs=mg[:, k, :], start=(k == 0), stop=(k == 7)),
                             reads=[wo, mg], writes=[po], sig=(k == 7))
                    c.op("dve", lambda h, d=d, po=po, xg=xg: h.tensor_tensor(out=xg[:, d, :], in0=po[:, :], in1=xg[:, d, :], op=ALU.add), reads=[po, xg], writes=[xg])
                c.dma("sp", xd3[:, :, tsl], xg[:, :, :], reads=[xg], writes=[self.db(xdst.name, g)])
            c.barrier()

    def final_phase(self, xsrc, out):
        c, S, NG = self.c, self.S, self.NG
        with ExitStack() as es:
            xgs = [c.sb(es, "xf", [128, 8, 512], F32) for _ in range(2)]
            ogs = [c.sb(es, "of", [128, 8, 512], F32) for _ in range(2)]
            sq = c.sb(es, "sqf", [128, 8, 512], BF16)
            rstd = c.sb(es, "rstdf", [128, 512], F32)
            xs3 = xsrc.rearrange("(c p) t -> p c t", p=128)
            o3 = out.rearrange("(c p) t -> p c t", p=128)
            for g in range(NG):
                tsl = slice(g * 512, (g + 1) * 512)
                xg, og = xgs[g % 2], ogs[g % 2]
                c.dma("sp", xg[:, :, :], xs3[:, :, tsl], reads=[self.db(xsrc.name, g)], writes=[xg])
                self.rmsnorm_group(xg, sq, lambda k, og=og: og[:, k, :], og, rstd, DEPTH * CV_PER_LAYER, self.rot())
                c.dma("sp", o3[:, :, tsl], og[:, :, :], reads=[og], writes=[self.db("out", g)])
            c.barrier()


def build_full(S=4096):
    P = ProgFull(S)
    es = ExitStack()
    P.setup_consts(es)
    P.declare_scratch()
    d = P.dr
    EI = "ExternalInput"
    d("rotC", [128, S], F32, kind=EI); d("rotS", [128, S], F32, kind=EI)
    d("pow2", [128, 26], F32, kind=EI); d("gmask", [14, 128, 512], F32, kind=EI)
    xin = d("xT_in", [1024, S], F32, kind=EI)
    out = d("outT", [1024, S], F32, kind="ExternalOutput")
    xa = d("xTa", [1024, S], F32); xb = d("xTb", [1024, S], F32)
    f1i = d("ffn1_w_in", [DEPTH, 1024, 4096], F32, kind=EI); f1o = d("ffn1_w_out", [DEPTH, 2048, 1024], F32, kind=EI)
    f2i = d("ffn2_w_in", [DEPTH, 1024, 4096], F32, kind=EI); f2o = d("ffn2_w_out", [DEPTH, 2048, 1024], F32, kind=EI)
    w = d("w_in", [DEPTH, 1024, 7636], F32, kind=EI); wsw = d("w_sw", [DEPTH, 1024, 896], F32, kind=EI); wsm = d("w_small", [DEPTH, 1024, 84], F32, kind=EI)
    wba = d("w_branch_a", [DEPTH, 512, 1024], F32, kind=EI); wbb = d("w_branch_b", [DEPTH, 512, 1024], F32, kind=EI); wbc = d("w_branch_c", [DEPTH, 512, 1024], F32, kind=EI)
    wo = d("w_out", [DEPTH, 1024, 1024], F32, kind=EI)
    cur = xin
    for l in range(DEPTH):
        cvb = l * CV_PER_LAYER
        P.ffn_phase(cur, xa, f1i[l], f1o[l], cvb + CV_FFN1)
        P.m1_phase(xa, w[l], wsw[l], wsm[l], cvb)
        P.ab_phase()
        P.c_phase(cvb)
        P.merge_phase(xa, xb, [wba[l], wbb[l], wbc[l]], wo[l])
        P.ffn_phase(xb, xa, f2i[l], f2o[l], cvb + CV_FFN2)
        cur = xa
    P.final_phase(xa, out)
    es.close()
    return P


_CACHE = {}


def kernel(**inputs):
    S = 4096
    inp = {k: np.asarray(v) for k, v in inputs.items()}
    if "prog" not in _CACHE:
        _CACHE["prog"] = build_full(S)
    P = _CACHE["prog"]
    rotC, rotS = build_rot(S)
    shared = {
        "cmat": build_cmat(), "cvec": build_cvec(inp), "rotC": rotC, "rotS": rotS, "pow2": build_pow2(), "gmask": build_gmask(),
        "ffn1_w_in": inp["ffn1_w_in"], "ffn1_w_out": inp["ffn1_w_out"], "ffn2_w_in": inp["ffn2_w_in"], "ffn2_w_out": inp["ffn2_w_out"],
        "w_in": inp["w_in"], "w_sw": np.ascontiguousarray(inp["w_in"][:, :, swap_cols()]), "w_small": np.ascontiguousarray(inp["w_in"][:, :, small_cols()]),
        "w_branch_a": inp["w_branch_a"], "w_branch_b": inp["w_branch_b"], "w_branch_c": inp["w_branch_c"], "w_out": inp["w_out"],
    }
    in_maps = []
    for b in range(NB):
        m = dict(shared)
        m["xT_in"] = np.ascontiguousarray(inp["x"][b].T)
        in_maps.append(m)
    res = run_bass_kernel_spmd(P.nc, in_maps, core_ids=list(range(NB)))
    out = np.stack([np.ascontiguousarray(r["outT"].T) for r in res.results], axis=0)
    return out.astype(np.float32)
```

```python
from contextlib import ExitStack
import numpy as np
import concourse.bass as bass
import concourse.mybir as mybir
from concourse.bass_utils import run_bass_kernel_spmd

F32 = mybir.dt.float32
BF16 = mybir.dt.bfloat16
ALU = mybir.AluOpType
AF = mybir.ActivationFunctionType
AX = mybir.AxisListType

D = 1024
DEPTH = 2
NB = 8
IN_SIZES = (512, 64, 64, 256, 64, 4, 1536, 8, 1536, 512, 4, 4, 3072)
OFF = np.concatenate([[0], np.cumsum(IN_SIZES)]).tolist()
(O_AQ, O_AK, O_AV, O_IQ, O_IK, O_IW, O_BQKV, O_BF, O_CQKV, O_CZ, O_CB, O_CA, O_G) = OFF[:13]
NEG = -32768.0


class Buf:
    __slots__ = ("t", "last_w", "readers", "name")

    def __init__(self, t=None, name=""):
        self.t = t
        self.last_w = None
        self.readers = {}
        self.name = name

    def __getitem__(self, k):
        return self.t[k]


class Eng:
    def __init__(self, name, handle, sem):
        self.name = name
        self.h = handle
        self.sem = sem
        self.count = 0
        self.seen = {}


class Ctx:
    SAME_ENGINE_SYNC = True
    RAW_ONLY_SAME_ENGINE = False

    def __init__(self, nc, n_dma_sems=10):
        self.nc = nc
        self.sems = {}
        self.eng = {}
        for nm, h in (("pe", nc.tensor), ("act", nc.scalar), ("dve", nc.vector),
                      ("pool", nc.gpsimd), ("sp", nc.sync)):
            self.sems["s_" + nm] = nc.alloc_semaphore("s_" + nm)
            self.eng[nm] = Eng(nm, h, "s_" + nm)
        self.dma_pool = {}
        for q in ("sp", "pool", "act"):
            lst = []
            for i in range(n_dma_sems):
                k = f"d_{q}{i}"
                self.sems[k] = nc.alloc_semaphore(k)
                lst.append([k, 0])
            self.dma_pool[q] = [lst, 0]
        self.n_instr = 0
        self.n_wait = 0
        self.uid = 0

    def sb(self, es, name, shape, dt):
        self.uid += 1
        nm = f"{name}_{self.uid}"
        return Buf(es.enter_context(self.nc.sbuf_tensor(nm, list(shape), dt)), nm)

    def _need(self, reads, writes, own=None):
        need = {}

        def add(ev, raw):
            if ev is None:
                return
            k, v = ev
            if k == own and not raw and self.RAW_ONLY_SAME_ENGINE:
                return
            if need.get(k, 0) < v:
                need[k] = v
        for b in reads:
            add(b.last_w, True)
        for b in writes:
            add(b.last_w, False)
            for k, v in b.readers.items():
                add((k, v), False)
        return need

    def _emit_waits(self, e, need):
        for k, v in need.items():
            if k == e.sem and (e.name == "pe" or not self.SAME_ENGINE_SYNC):
                continue
            if e.seen.get(k, 0) >= v:
                continue
            e.h.wait_ge(self.sems[k], v)
            e.seen[k] = v
            self.n_wait += 1

    def _record(self, ev, reads, writes):
        k, v = ev
        for b in writes:
            b.last_w = ev
            b.readers = {}
        for b in reads:
            if b.readers.get(k, 0) < v:
                b.readers[k] = v

    def op(self, en, fn, reads=(), writes=(), sig=True):
        e = self.eng[en]
        self._emit_waits(e, self._need(reads, writes, e.sem))
        ins = fn(e.h)
        self.n_instr += 1
        if sig:
            ins.then_inc(self.sems[e.sem], 1)
            e.count += 1
            ev = (e.sem, e.count)
        else:
            ev = (e.sem, e.count + 1)
        self._record(ev, reads, writes)
        return ins

    def dma(self, q, out, in_, reads=(), writes=(), **kw):
        e = self.eng[q]
        lst, idx = self.dma_pool[q]
        ent = lst[idx % len(lst)]
        self.dma_pool[q][1] = idx + 1
        need = self._need(reads, writes, None)
        if ent[1] > 0 and need.get(ent[0], 0) < ent[1]:
            need[ent[0]] = ent[1]
        self._emit_waits(e, need)
        ins = e.h.dma_start(out=out, in_=in_, **kw)
        ent[1] += 16
        ins.then_inc(self.sems[ent[0]], 16)
        self.n_instr += 1
        self._record((ent[0], ent[1]), reads, writes)
        return ins

    def barrier(self):
        for e in self.eng.values():
            need = {}
            for f in self.eng.values():
                if f is not e and f.count > 0:
                    need[f.sem] = f.count
            for q in self.dma_pool:
                for k, v in self.dma_pool[q][0]:
                    if v > 0:
                        need[k] = v
            self._emit_waits(e, need)


class Prog:
    def __init__(self, S, ext=None):
        self.S = S
        self.NT = S // 128
        self.NG = S // 512
        self.nc = bass.Bass("TRN2", target_bir_lowering=False)
        self.c = Ctx(self.nc)
        self.ext = ext or {}
        self.dram = {}
        self.dbuf = {}
        nc = self.nc
        self.psall = nc.alloc_psum_tensor("psall", [128, 8 * 512], F32)
        self.ps = [Buf(self.psall[:, i * 512:(i + 1) * 512], f"ps{i}") for i in range(8)]

    def dr(self, name, shape, dt, kind=None):
        if kind is None:
            kind = {"in": "ExternalInput", "out": "ExternalOutput"}.get(self.ext.get(name), "Internal")
        t = self.nc.dram_tensor(name, list(shape), dt, kind=kind)
        self.dram[name] = t.ap()
        return self.dram[name]

    def db(self, name, idx=0):
        k = (name, idx)
        if k not in self.dbuf:
            self.dbuf[k] = Buf(name=f"{name}{idx}")
        return self.dbuf[k]

    def setup_consts(self, es):
        c, nc = self.c, self.nc
        cm = self.dr("cmat", [7, 128, 128], F32, kind="ExternalInput")
        self.ident_b = c.sb(es, "identb", [128, 128], BF16)
        self.ones_b = c.sb(es, "onesb", [128, 128], BF16)
        self.ones_f = c.sb(es, "onesf", [128, 128], F32)
        self.triu_f = c.sb(es, "triuf", [128, 128], F32)
        self.ident_f = c.sb(es, "identf", [128, 128], F32)
        self.negm_f = c.sb(es, "negmf", [128, 128], F32)
        self.tri01_b = c.sb(es, "tri01b", [128, 128], BF16)
        cb = self.db("cmat")
        c.dma("pool", self.ident_b[:, :], cm[0], reads=[cb], writes=[self.ident_b])
        c.dma("pool", self.ones_b[:, :], cm[1], reads=[cb], writes=[self.ones_b])
        c.dma("sp", self.ones_f[:, :], cm[1], reads=[cb], writes=[self.ones_f])
        c.dma("sp", self.triu_f[:, :], cm[2], reads=[cb], writes=[self.triu_f])
        c.dma("sp", self.ident_f[:, :], cm[0], reads=[cb], writes=[self.ident_f])
        c.dma("sp", self.negm_f[:, :], cm[3], reads=[cb], writes=[self.negm_f])
        c.dma("pool", self.tri01_b[:, :], cm[2], reads=[cb], writes=[self.tri01_b])
        self.NCV = DEPTH * CV_PER_LAYER + 8
        cv = self.dr("cvec", [128, self.NCV], F32, kind="ExternalInput")
        self.cvec = c.sb(es, "cvec", [128, self.NCV], F32)
        c.dma("sp", self.cvec[:, :], cv[:, :], reads=[self.db("cvec")], writes=[self.cvec])

    def rmsnorm_group(self, xg, sq, hT_ap_fn, hT_buf, rstd, gcol, psb, nch=8, ncols=512):
        c = self.c
        c.op("act", lambda h: h.activation(out=sq[:, :, :], in_=xg[:, :, :], func=AF.Square), reads=[xg], writes=[sq])
        for k in range(nch):
            c.op("pe", lambda h, k=k: h.matmul(psb[:, :ncols], lhsT=self.ones_b[:, :], rhs=sq[:, k, :], start=(k == 0), stop=(k == nch - 1)),
                 reads=[self.ones_b, sq], writes=[psb], sig=(k == nch - 1))
        c.op("dve", lambda h: h.tensor_scalar(out=rstd[:, :], in0=psb[:, :ncols], scalar1=1.0 / (nch * 128), scalar2=1e-6, op0=ALU.mult, op1=ALU.add),
             reads=[psb], writes=[rstd])
        c.op("act", lambda h: h.activation(out=rstd[:, :], in_=rstd[:, :], func=AF.Ln), reads=[rstd], writes=[rstd])
        c.op("act", lambda h: h.activation(out=rstd[:, :], in_=rstd[:, :], func=AF.Exp, scale=-0.5), reads=[rstd], writes=[rstd])
        for k in range(nch):
            c.op("dve", lambda h, k=k: h.scalar_tensor_tensor(out=hT_ap_fn(k), in0=xg[:, k, :], scalar=self.cvec[:, gcol + k:gcol + k + 1],
                                                            in1=rstd[:, :], op0=ALU.mult, op1=ALU.mult),
                 reads=[xg, rstd, self.cvec], writes=[hT_buf])

    def ffn_phase(self, xsrc, xdst, w_in, w_out, gcol):
        c, S = self.c, self.S
        with ExitStack() as es:
            win = c.sb(es, "win", [128, 8, 4096], BF16)
            wout = c.sb(es, "wout", [128, 16, 1024], BF16)
            xgs = [c.sb(es, "xg", [128, 8, 512], F32) for _ in range(2)]
            sq = c.sb(es, "sq", [128, 8, 512], BF16)
            hT = c.sb(es, "hT", [128, 8, 512], BF16)
            act = c.sb(es, "actT", [128, 16, 512], BF16)
            rstd = c.sb(es, "rstd", [128, 512], F32)
            sgs = [c.sb(es, "sg", [128, 512], F32) for _ in range(2)]
            wb = self.db("w")
            for k in range(8):
                c.dma("pool", win[:, k, :], w_in[k * 128:(k + 1) * 128, :], reads=[wb], writes=[win])
            for k in range(16):
                c.dma("pool", wout[:, k, :], w_out[k * 128:(k + 1) * 128, :], reads=[wb], writes=[wout])
            xs3 = xsrc.rearrange("(c p) t -> p c t", p=128)
            xd3 = xdst.rearrange("(c p) t -> p c t", p=128)
            ps = self.ps
            for g in range(self.NG):
                xg = xgs[g % 2]
                tsl = slice(g * 512, (g + 1) * 512)
                c.dma("sp", xg[:, :, :], xs3[:, :, tsl], reads=[self.db(xsrc.name, g)], writes=[xg])
                self.rmsnorm_group(xg, sq, lambda k: hT[:, k, :], hT, rstd, gcol, ps[0])
                for j in range(16):
                    pg, pu = ps[1 + 2 * (j % 2)], ps[2 + 2 * (j % 2)]
                    for k in range(8):
                        c.op("pe", lambda h, k=k, j=j, pg=pg: h.matmul(pg[:, :], lhsT=win[:, k, j * 128:(j + 1) * 128], rhs=hT[:, k, :], start=(k == 0), stop=(k == 7)),
                             reads=[win, hT], writes=[pg], sig=(k == 7))
                    for k in range(8):
                        c.op("pe", lambda h, k=k, j=j, pu=pu: h.matmul(pu[:, :], lhsT=win[:, k, 2048 + j * 128:2048 + (j + 1) * 128], rhs=hT[:, k, :], start=(k == 0), stop=(k == 7)),
                             reads=[win, hT], writes=[pu], sig=(k == 7))
                    sg = sgs[j % 2]
                    c.op("act", lambda h, pg=pg, sg=sg: h.activation(out=sg[:, :], in_=pg[:, :], func=AF.Silu), reads=[pg], writes=[sg])
                    c.op("dve", lambda h, pu=pu, sg=sg, j=j: h.tensor_tensor(out=act[:, j, :], in0=sg[:, :], in1=pu[:, :], op=ALU.mult), reads=[sg, pu], writes=[act])
                for d in range(8):
                    po = ps[5 + d % 2]
                    for j in range(16):
                        c.op("pe", lambda h, d=d, j=j, po=po: h.matmul(po[:, :], lhsT=wout[:, j, d * 128:(d + 1) * 128], rhs=act[:, j, :], start=(j == 0), stop=(j == 15)),
                             reads=[wout, act], writes=[po], sig=(j == 15))
                    c.op("dve", lambda h, d=d, po=po, xg=xg: h.scalar_tensor_tensor(out=xg[:, d, :], in0=po[:, :], scalar=0.5, in1=xg[:, d, :], op0=ALU.mult, op1=ALU.add),
                         reads=[po, xg], writes=[xg])
                c.dma("sp", xd3[:, :, tsl], xg[:, :, :], reads=[xg], writes=[self.db(xdst.name, g)])
            c.barrier()


CV_FFN1, CV_MIX, CV_FFN2, CV_BG, CV_CONV, CV_DN, CV_BF, CV_ALOG, CV_DT = 0, 8, 16, 24, 48, 96, 97, 105, 109
CV_PER_LAYER = 113


def build_cvec(inp):
    cv = np.zeros((128, DEPTH * CV_PER_LAYER + 8), np.float32)
    for l in range(DEPTH):
        b = l * CV_PER_LAYER
        cv[:, b + CV_FFN1:b + CV_FFN1 + 8] = inp["ffn1_norm"][l].reshape(8, 128).T
        cv[:, b + CV_MIX:b + CV_MIX + 8] = inp["mix_norm"][l].reshape(8, 128).T
        cv[:, b + CV_FFN2:b + CV_FFN2 + 8] = inp["ffn2_norm"][l].reshape(8, 128).T
        cv[:, b + CV_BG:b + CV_BG + 24] = inp["b_gate"][l].reshape(24, 128).T
        cv[:, b + CV_CONV:b + CV_CONV + 48] = inp["conv_w"][l].reshape(4, 12, 128).transpose(2, 1, 0).reshape(128, 48)
        cv[:, b + CV_DN] = inp["delta_norm"][l]
        cv[:, b + CV_BF:b + CV_BF + 8] = inp["b_forget"][l][None, :]
        cv[:, b + CV_ALOG:b + CV_ALOG + 4] = inp["a_log"][l][None, :]
        cv[:, b + CV_DT:b + CV_DT + 4] = inp["dt_bias"][l][None, :]
    cv[:, DEPTH * CV_PER_LAYER:] = inp["final_norm"].reshape(8, 128).T
    return cv


def build_cmat():
    i = np.arange(128)
    ident = np.eye(128, dtype=np.float32)
    ones = np.ones((128, 128), np.float32)
    triu = (i[:, None] <= i[None, :]).astype(np.float32)
    negm = np.where(i[None, :] >= i[:, None], 0.0, -1e4).astype(np.float32)
    negc = np.where(i[None, :] <= i[:, None], 0.0, -1e30).astype(np.float32)
    z = np.zeros((128, 128), np.float32)
    strictu = (i[:, None] < i[None, :]).astype(np.float32)
    return np.stack([ident, ones, triu, negm, negc, strictu, z])


def build_pow2(nit=26):
    return np.tile((0.5 ** np.arange(1, nit + 1)).astype(np.float32)[None, :], (128, 1))


def swap_cols():
    idx = []
    for base, n in ((O_AQ, 512), (O_AK, 64), (O_IQ, 256), (O_IK, 64)):
        for j in range(n):
            d = j % 64
            hb = base + (j // 64) * 64
            if d < 8:
                idx.append(hb + d + 8)
            elif d < 16:
                idx.append(hb + d - 8)
            else:
                idx.append(hb + d)
    return np.array(idx)


def small_cols():
    return np.concatenate([np.arange(O_AV, O_AV + 64), np.arange(O_IW, O_IW + 4), np.arange(O_BF, O_BF + 8),
                           np.arange(O_CB, O_CB + 4), np.arange(O_CA, O_CA + 4)])


def build_rot(S):
    pos = np.arange(S, dtype=np.float32)
    inv = np.power(np.float32(500000.0), -np.arange(0, 16, 2, dtype=np.float32) / np.float32(16)).astype(np.float32)
    ang = (pos[:, None] * inv[None, :]).astype(np.float32)
    cos, sin = np.cos(ang).astype(np.float32), np.sin(ang).astype(np.float32)
    C = np.ones((128, S), np.float32)
    Sg = np.zeros((128, S), np.float32)
    for p in range(128):
        d = p % 64
        if d < 8:
            C[p] = cos[:, d]
            Sg[p] = -sin[:, d]
        elif d < 16:
            C[p] = cos[:, d - 8]
            Sg[p] = sin[:, d - 8]
    return C, Sg


class ProgM1(Prog):
    def declare_scratch(self):
        S = self.S
        d = self.dr
        self.qaT = d("qaT", [512, S], BF16); self.kaT = d("kaT", [64, S], BF16)
        self.qiT = d("qiT", [256, S], BF16); self.kiT = d("kiT", [64, S], BF16)
        self.vA = d("vA", [S, 64], BF16)
        self.qbT = d("qbT", [512, S], BF16); self.kbT = d("kbT", [512, S], BF16); self.vB = d("vB", [S, 512], BF16)
        self.qcT = d("qcT", [512, S], BF16); self.kcT = d("kcT", [512, S], BF16)
        self.kC = d("kC", [S, 512], BF16); self.vC = d("vC", [S, 512], BF16)
        self.szT = d("szT", [512, S], BF16)
        self.smallD = d("smallD", [S, 24], F32)
        self.gT = d("gT", [3072, S], BF16)
        self.yT = d("yT", [3, 512, S], BF16)

    def rot(self):
        self._rot = (getattr(self, "_rot", -1) + 1) % 8
        return self.ps[self._rot]

    def m1_phase(self, xT, w_in, w_sw, w_small, cvb):
        c, S, NG = self.c, self.S, self.NG
        rotC = self.dram["rotC"]; rotS = self.dram["rotS"]
        with ExitStack() as es:
            hT = c.sb(es, "hTall", [128, 8, S], BF16)
            wts = [c.sb(es, "wt", [128, 8, 512], BF16) for _ in range(3)]
            wti = [0]
            wb = self.db("w")

            def load_w(src_list):
                wt = wts[wti[0] % 3]
                wti[0] += 1
                for (ap, c0, n) in src_list:
                    c.dma("pool", wt[:, :, c0:c0 + n], ap.rearrange("(k p) n -> p k n", p=128), reads=[wb], writes=[wt])
                return wt

            def fm(wt, c0, M, g, psb, rows0=0):
                tsl = slice(g * 512, (g + 1) * 512)
                for k in range(8):
                    c.op("pe", lambda h, k=k: h.matmul(psb[rows0:rows0 + M, :], lhsT=wt[:, k, c0:c0 + M], rhs=hT[:, k, tsl], start=(k == 0), stop=(k == 7)),
                         reads=[wt, hT], writes=[psb], sig=(k == 7))

            with ExitStack() as es2:
                xgs = [c.sb(es2, "xg", [128, 8, 512], F32) for _ in range(2)]
                sq = c.sb(es2, "sq", [128, 8, 512], BF16)
                rstd = c.sb(es2, "rstd", [128, 512], F32)
                x3 = xT.rearrange("(c p) t -> p c t", p=128)
                for g in range(NG):
                    xg = xgs[g % 2]
                    tsl = slice(g * 512, (g + 1) * 512)
                    c.dma("sp", xg[:, :, :], x3[:, :, tsl], reads=[self.db(xT.name, g)], writes=[xg])
                    self.rmsnorm_group(xg, sq, lambda k, tsl=tsl: hT[:, k, tsl], hT, rstd, cvb + CV_MIX, self.rot())
                c.barrier()

            with ExitStack() as es2:
                stg = [c.sb(es2, "stg", [128, 4, 512], BF16) for _ in range(2)]
                stgi = [0]
                t1s = [c.sb(es2, "t1", [128, 512], F32) for _ in range(2)]
                t2s = [c.sb(es2, "t2", [128, 512], F32) for _ in range(2)]
                rc = [c.sb(es2, "rc", [128, 512], F32) for _ in range(2)]
                rs = [c.sb(es2, "rs", [128, 512], F32) for _ in range(2)]

                def nstg():
                    stgi[0] += 1
                    return stg[stgi[0] % 2]

                def load_rot(g):
                    tsl = slice(g * 512, (g + 1) * 512)
                    c.dma("sp", rc[g % 2][:, :], rotC[:, tsl], reads=[self.db("rot")], writes=[rc[g % 2]])
                    c.dma("sp", rs[g % 2][:, :], rotS[:, tsl], reads=[self.db("rot")], writes=[rs[g % 2]])

                def rotary(pn, psw, g, out_ap, out_buf, i):
                    t1, t2 = t1s[i % 2], t2s[i % 2]
                    c.op("dve", lambda h: h.tensor_tensor(out=t1[:, :], in0=pn[:, :], in1=rc[g % 2][:, :], op=ALU.mult), reads=[pn, rc[g % 2]], writes=[t1])
                    c.op("dve", lambda h: h.tensor_tensor(out=t2[:, :], in0=psw[:, :], in1=rs[g % 2][:, :], op=ALU.mult), reads=[psw, rs[g % 2]], writes=[t2])
                    c.op("pool", lambda h: h.tensor_tensor(out=out_ap, in0=t1[:, :], in1=t2[:, :], op=ALU.add), reads=[t1, t2], writes=[out_buf])

                wn = load_w([(w_in[:, O_AQ:O_AQ + 512], 0, 512)])
                ws = load_w([(w_sw[:, 0:512], 0, 512)])
                for g in range(NG):
                    tsl = slice(g * 512, (g + 1) * 512)
                    load_rot(g)
                    so = nstg()
                    for ch in range(4):
                        pn, psw = self.rot(), self.rot()
                        fm(wn, ch * 128, 128, g, pn)
                        fm(ws, ch * 128, 128, g, psw)
                        rotary(pn, psw, g, so[:, ch, :], so, ch)
                    c.dma("sp", self.qaT.rearrange("(c p) t -> p c t", p=128)[:, :, tsl], so[:, :, :], reads=[so], writes=[self.db("qaT", g)])
                wn = load_w([(w_in[:, O_IQ:O_IQ + 256], 0, 256), (w_in[:, O_AK:O_AK + 64], 256, 64), (w_in[:, O_IK:O_IK + 64], 320, 64)])
                ws = load_w([(w_sw[:, 576:832], 0, 256), (w_sw[:, 512:576], 256, 64), (w_sw[:, 832:896], 320, 64)])
                for g in range(NG):
                    tsl = slice(g * 512, (g + 1) * 512)
                    load_rot(g)
                    so = nstg()
                    for ch in range(3):
                        pn, psw = self.rot(), self.rot()
                        fm(wn, ch * 128, 128, g, pn)
                        fm(ws, ch * 128, 128, g, psw)
                        rotary(pn, psw, g, so[:, ch, :], so, ch)
                    c.dma("sp", self.qiT.rearrange("(c p) t -> p c t", p=128)[:, :, tsl], so[:, 0:2, :], reads=[so], writes=[self.db("qiT", g)])
                    c.dma("sp", self.kaT[:, tsl], so[0:64, 2, :], reads=[so], writes=[self.db("kaT", g)])
                    c.dma("sp", self.kiT[:, tsl], so[64:128, 2, :], reads=[so], writes=[self.db("kiT", g)])
                for (c0, dst) in ((O_BQKV, self.qbT), (O_BQKV + 512, self.kbT)):
                    wn = load_w([(w_in[:, c0:c0 + 512], 0, 512)])
                    for g in range(NG):
                        tsl = slice(g * 512, (g + 1) * 512)
                        so = nstg()
                        for ch in range(4):
                            pn = self.rot()
                            fm(wn, ch * 128, 128, g, pn)
                            c.op("act", lambda h, pn=pn, ch=ch, so=so: h.activation(out=so[:, ch, :], in_=pn[:, :], func=AF.Copy), reads=[pn], writes=[so])
                        c.dma("sp", dst.rearrange("(c p) t -> p c t", p=128)[:, :, tsl], so[:, :, :], reads=[so], writes=[self.db(dst.name, g)])
                wn = load_w([(w_in[:, O_BQKV + 1024:O_BQKV + 1536], 0, 512)])
                for g in range(NG):
                    so = nstg()
                    for tt in range(4):
                        pn = self.rot()
                        t0 = g * 512 + tt * 128
                        for k in range(8):
                            c.op("pe", lambda h, k=k, pn=pn, t0=t0: h.matmul(pn[:, :], lhsT=hT[:, k, t0:t0 + 128], rhs=wn[:, k, :], start=(k == 0), stop=(k == 7)),
                                 reads=[wn, hT], writes=[pn], sig=(k == 7))
                        c.op("act", lambda h, pn=pn, tt=tt, so=so: h.activation(out=so[:, tt, :], in_=pn[:, :], func=AF.Copy), reads=[pn], writes=[so])
                    c.dma("sp", self.vB.rearrange("(n p) d -> p n d", p=128)[:, g * 4:(g + 1) * 4, :], so[:, :, :], reads=[so], writes=[self.db("vB", g)])
                with ExitStack() as es3:
                    xcs = [c.sb(es3, "xc", [128, 515], F32) for _ in range(4)]
                    accs = [c.sb(es3, "acc", [128, 512], F32) for _ in range(4)]
                    sls = [c.sb(es3, "sl", [128, 512], F32) for _ in range(4)]
                    sqb = [c.sb(es3, "sqb", [128, 512], BF16) for _ in range(4)]
                    rr = [c.sb(es3, "rr", [128, 512], F32) for _ in range(4)]
                    tok = [c.sb(es3, "tok", [128, 4, 512], BF16) for _ in range(2)]
                    for sec, (dstT, dstTok) in enumerate(((self.qcT, None), (self.kcT, self.kC), (None, self.vC))):
                        c0 = O_CQKV + sec * 512
                        wn = load_w([(w_in[:, c0:c0 + 512], 0, 512)])
                        for j in range(4):
                            c.op("pool", lambda h, j=j: h.memset(xcs[j][:, 0:3], 0.0), writes=[xcs[j]])
                        for g in range(NG):
                            tsl = slice(g * 512, (g + 1) * 512)
                            so = nstg()
                            for j in range(4):
                                ch = sec * 4 + j
                                pn = self.rot()
                                fm(wn, j * 128, 128, g, pn)
                                xc = xcs[j]
                                acc, sl = accs[j % 4], sls[j % 4]
                                wc = cvb + CV_CONV + ch * 4
                                c.op("act", lambda h, pn=pn, xc=xc: h.activation(out=xc[:, 3:515], in_=pn[:, :], func=AF.Copy), reads=[pn], writes=[xc])
                                c.op("dve", lambda h, xc=xc, acc=acc, wc=wc: h.tensor_scalar(out=acc[:, :], in0=xc[:, 3:515], scalar1=self.cvec[:, wc + 3:wc + 4], scalar2=None, op0=ALU.mult),
                                     reads=[xc, self.cvec], writes=[acc])
                                for tap in (2, 1, 0):
                                    c.op("dve", lambda h, xc=xc, acc=acc, wc=wc, tap=tap: h.scalar_tensor_tensor(out=acc[:, :], in0=xc[:, tap:tap + 512], scalar=self.cvec[:, wc + tap:wc + tap + 1],
                                                                                                         in1=acc[:, :], op0=ALU.mult, op1=ALU.add),
                                         reads=[xc, acc, self.cvec], writes=[acc])
                                c.op("act", lambda h, xc=xc: h.activation(out=xc[:, 0:3], in_=xc[:, 512:515], func=AF.Copy), reads=[xc], writes=[xc])
                                if sec == 2:
                                    c.op("act", lambda h, acc=acc, so=so, j=j: h.activation(out=so[:, j, :], in_=acc[:, :], func=AF.Silu), reads=[acc], writes=[so])
                                else:
                                    c.op("act", lambda h, acc=acc, sl=sl: h.activation(out=sl[:, :], in_=acc[:, :], func=AF.Silu), reads=[acc], writes=[sl])
                                    sb_, r_ = sqb[j % 4], rr[j % 4]
                                    c.op("act", lambda h, sl=sl, sb_=sb_: h.activation(out=sb_[:, :], in_=sl[:, :], func=AF.Square), reads=[sl], writes=[sb_])
                                    p2 = self.rot()
                                    c.op("pe", lambda h, p2=p2, sb_=sb_: h.matmul(p2[:, :], lhsT=self.ones_b[:, :], rhs=sb_[:, :], start=True, stop=True), reads=[self.ones_b, sb_], writes=[p2])
                                    c.op("dve", lambda h, p2=p2, r_=r_: h.tensor_scalar(out=r_[:, :], in0=p2[:, :], scalar1=1e-6, scalar2=None, op0=ALU.add), reads=[p2], writes=[r_])
                                    c.op("act", lambda h, r_=r_: h.activation(out=r_[:, :], in_=r_[:, :], func=AF.Ln), reads=[r_], writes=[r_])
                                    c.op("act", lambda h, r_=r_: h.activation(out=r_[:, :], in_=r_[:, :], func=AF.Exp, scale=-0.5), reads=[r_], writes=[r_])
                                    qs = float(128 ** -0.5) if sec == 0 else 1.0
                                    c.op("dve", lambda h, sl=sl, r_=r_, so=so, j=j, qs=qs: h.scalar_tensor_tensor(out=so[:, j, :], in0=sl[:, :], scalar=qs, in1=r_[:, :], op0=ALU.mult, op1=ALU.mult),
                                         reads=[sl, r_], writes=[so])
                            if dstT is not None:
                                c.dma("sp", dstT.rearrange("(c p) t -> p c t", p=128)[:, :, tsl], so[:, :, :], reads=[so], writes=[self.db(dstT.name, g)])
                            if dstTok is not None:
                                tk = tok[g % 2]
                                for tt in range(4):
                                    pt = self.rot()
                                    for j in range(4):
                                        c.op("pe", lambda h, pt=pt, j=j, tt=tt, so=so: h.matmul(pt[:, j * 128:(j + 1) * 128], lhsT=so[:, j, tt * 128:(tt + 1) * 128], rhs=self.ident_b[:, :], start=True, stop=True),
                                             reads=[so, self.ident_b], writes=[pt], sig=(j == 3))
                                    c.op("act", lambda h, pt=pt, tk=tk, tt=tt: h.activation(out=tk[:, tt, :], in_=pt[:, :], func=AF.Copy), reads=[pt], writes=[tk])
                                c.dma("sp", dstTok.rearrange("(n p) d -> p n d", p=128)[:, g * 4:(g + 1) * 4, :], tk[:, :, :], reads=[tk], writes=[self.db(dstTok.name, g)])
                wn = load_w([(w_in[:, O_CZ:O_CZ + 512], 0, 512)])
                for g in range(NG):
                    tsl = slice(g * 512, (g + 1) * 512)
                    so = nstg()
                    for ch in range(4):
                        pn = self.rot()
                        fm(wn, ch * 128, 128, g, pn)
                        c.op("act", lambda h, pn=pn, ch=ch, so=so: h.activation(out=so[:, ch, :], in_=pn[:, :], func=AF.Silu), reads=[pn], writes=[so])
                    c.dma("sp", self.szT.rearrange("(c p) t -> p c t", p=128)[:, :, tsl], so[:, :, :], reads=[so], writes=[self.db("szT", g)])
                for sec in range(6):
                    wn = load_w([(w_in[:, O_G + sec * 512:O_G + (sec + 1) * 512], 0, 512)])
                    for g in range(NG):
                        tsl = slice(g * 512, (g + 1) * 512)
                        so = nstg()
                        for ch in range(4):
                            pn = self.rot()
                            fm(wn, ch * 128, 128, g, pn)
                            bc = cvb + CV_BG + sec * 4 + ch
                            c.op("act", lambda h, pn=pn, ch=ch, so=so, bc=bc: h.activation(out=so[:, ch, :], in_=pn[:, :], func=AF.Sigmoid, bias=self.cvec[:, bc:bc + 1]),
                                 reads=[pn, self.cvec], writes=[so])
                        c.dma("sp", self.gT.rearrange("(c p) t -> p c t", p=128)[:, sec * 4:(sec + 1) * 4, tsl], so[:, :, :], reads=[so], writes=[self.db("gT", sec * 100 + g)])
                with ExitStack() as es3:
                    wsm = c.sb(es3, "wsm", [128, 8, 84], BF16)
                    c.dma("pool", wsm[:, :, :], w_small.rearrange("(k p) n -> p k n", p=128), reads=[wb], writes=[wsm])
                    negA = c.sb(es3, "negA", [128, 4], F32)
                    c.op("act", lambda h: h.activation(out=negA[:, :], in_=self.cvec[:, cvb + CV_ALOG:cvb + CV_ALOG + 4], func=AF.Exp), reads=[self.cvec], writes=[negA])
                    c.op("dve", lambda h: h.tensor_scalar(out=negA[:, :], in0=negA[:, :], scalar1=-1.0, scalar2=None, op0=ALU.mult), reads=[negA], writes=[negA])
                    sms = [c.sb(es3, "sm", [128, 4, 24], F32) for _ in range(2)]
                    vas = [c.sb(es3, "vas", [128, 4, 64], BF16) for _ in range(2)]
                    tmp = [c.sb(es3, "tmps", [128, 16], F32) for _ in range(2)]
                    for g in range(NG):
                        sm, va = sms[g % 2], vas[g % 2]
                        for tt in range(4):
                            pn = self.rot()
                            t0 = g * 512 + tt * 128
                            tp = tmp[tt % 2]
                            for k in range(8):
                                c.op("pe", lambda h, k=k, pn=pn, t0=t0: h.matmul(pn[:, 0:84], lhsT=hT[:, k, t0:t0 + 128], rhs=wsm[:, k, :], start=(k == 0), stop=(k == 7)),
                                     reads=[wsm, hT], writes=[pn], sig=(k == 7))
                            c.op("act", lambda h, pn=pn, va=va, tt=tt: h.activation(out=va[:, tt, :], in_=pn[:, 0:64], func=AF.Copy), reads=[pn], writes=[va])
                            c.op("dve", lambda h, pn=pn, sm=sm, tt=tt: h.tensor_scalar(out=sm[:, tt, 0:4], in0=pn[:, 64:68], scalar1=1.0 / 16.0, scalar2=None, op0=ALU.mult), reads=[pn], writes=[sm])
                            c.op("dve", lambda h, pn=pn, tp=tp: h.tensor_tensor(out=tp[:, 0:8], in0=pn[:, 68:76], in1=self.cvec[:, cvb + CV_BF:cvb + CV_BF + 8], op=ALU.add), reads=[pn, self.cvec], writes=[tp])
                            c.op("dve", lambda h, pn=pn, tp=tp: h.tensor_tensor(out=tp[:, 8:12], in0=pn[:, 80:84], in1=self.cvec[:, cvb + CV_DT:cvb + CV_DT + 4], op=ALU.add), reads=[pn, self.cvec, tp], writes=[tp])
                            c.op("act", lambda h, tp=tp: h.activation(out=tp[:, 0:8], in_=tp[:, 0:8], func=AF.Exp, scale=-1.0), reads=[tp], writes=[tp])
                            c.op("act", lambda h, tp=tp: h.activation(out=tp[:, 8:12], in_=tp[:, 8:12], func=AF.Exp), reads=[tp], writes=[tp])
                            c.op("act", lambda h, tp=tp: h.activation(out=tp[:, 0:12], in_=tp[:, 0:12], func=AF.Ln, bias=1.0), reads=[tp], writes=[tp])
                            c.op("dve", lambda h, tp=tp, sm=sm, tt=tt: h.tensor_scalar(out=sm[:, tt, 4:12], in0=tp[:, 0:8], scalar1=-1.0, scalar2=None, op0=ALU.mult), reads=[tp], writes=[sm])
                            c.op("dve", lambda h, tp=tp, sm=sm, tt=tt: h.tensor_tensor(out=sm[:, tt, 16:20], in0=tp[:, 8:12], in1=negA[:, :], op=ALU.mult), reads=[tp, negA, sm], writes=[sm])
                            c.op("act", lambda h, pn=pn, sm=sm, tt=tt: h.activation(out=sm[:, tt, 12:16], in_=pn[:, 76:80], func=AF.Sigmoid), reads=[pn, sm], writes=[sm])
                        c.dma("sp", self.smallD.rearrange("(n p) c -> p n c", p=128)[:, g * 4:(g + 1) * 4, 0:20], sm[:, :, 0:20], reads=[sm], writes=[self.db("smallD", g)])
                        c.dma("sp", self.vA.rearrange("(n p) d -> p n d", p=128)[:, g * 4:(g + 1) * 4, :], va[:, :, :], reads=[va], writes=[self.db("vA", g)])
                c.barrier()


class ProgAB(ProgM1):
    def rotset(self, key, banks):
        d = self.__dict__.setdefault("_rs", {})
        d[key] = (d.get(key, -1) + 1) % len(banks)
        return self.ps[banks[d[key]]]

    def _b_setup(self, es, lbanks, pbanks):
        c, S, NT, NG = self.c, self.S, self.NT, self.NG
        qbh = [c.sb(es, "qbh", [128, S], BF16) for _ in range(2)]
        kbh = [c.sb(es, "kbh", [128, S], BF16) for _ in range(2)]
        vext = [c.sb(es, "vext", [128, NT, 128], BF16) for _ in range(2)]
        lf = c.sb(es, "lf", [128, NT, 8], F32)
        lfacc = c.sb(es, "lfacc", [128, NT + 1, 8], F32)
        csb = c.sb(es, "csb", [128, NT, 8], F32)
        carry = c.sb(es, "carry", [128, NT, 8], F32)
        npair = NT * (NT + 1) // 2
        bias = c.sb(es, "biasall", [128, npair, 8], F32)
        pts = [c.sb(es, "pt", [128, 512], BF16) for _ in range(4)]
        ptq = [[Buf(name=f"ptq{a}_{b}") for b in range(4)] for a in range(4)]
        rsum = [c.sb(es, "rsum", [64, 512], F32) for _ in range(2)]
        nums = [c.sb(es, "bnum", [64, 512], F32) for _ in range(2)]
        yst = [c.sb(es, "yst", [64, 512], BF16) for _ in range(2)]
        allg = lambda nm: [self.db(nm, g) for g in range(NG)]
        c.dma("sp", lf[:, :, :], self.smallD.rearrange("(n p) c -> p n c", p=128)[:, :, 4:12], reads=allg("smallD"), writes=[lf])
        for v in vext:
            c.op("pool", lambda h, v=v: h.memset(v[:, :, 64:128], 1.0), writes=[v])
        c.op("pool", lambda h: h.memset(lfacc[:, 0, :], 0.0), writes=[lfacc])
        for n in range(NT):
            c.op("dve", lambda h, n=n: h.tensor_tensor(out=lfacc[:, n + 1, :], in0=lfacc[:, n, :], in1=lf[:, n, :], op=ALU.add), reads=[lfacc, lf], writes=[lfacc])
        for n in range(NT):
            pb = self.rotset("bpl", lbanks)
            c.op("pe", lambda h, n=n, pb=pb: h.matmul(pb[:, 0:8], lhsT=self.triu_f[:, :], rhs=lf[:, n, :], start=True, stop=False), reads=[self.triu_f, lf], writes=[pb], sig=False)
            c.op("pe", lambda h, n=n, pb=pb: h.matmul(pb[:, 0:8], lhsT=self.ones_f[:, :], rhs=lfacc[:, n, :], start=False, stop=True), reads=[self.ones_f, lfacc], writes=[pb])
            c.op("act", lambda h, n=n, pb=pb: h.activation(out=csb[:, n, :], in_=pb[:, 0:8], func=AF.Copy), reads=[pb], writes=[csb])
            pb = self.rotset("bpl", lbanks)
            c.op("pe", lambda h, n=n, pb=pb: h.matmul(pb[:, 0:8], lhsT=self.ones_f[:, :], rhs=lfacc[:, n, :], start=True, stop=True), reads=[self.ones_f, lfacc], writes=[pb])
            c.op("act", lambda h, n=n, pb=pb: h.activation(out=carry[:, n, :], in_=pb[:, 0:8], func=AF.Copy), reads=[pb], writes=[carry])
        pidx = {}
        pi = 0
        for i in range(NT):
            for j in range(i + 1):
                pidx[(i, j)] = pi
                c.op("pool", lambda h, i=i, j=j, pi=pi: h.tensor_tensor(out=bias[:, pi, :], in0=carry[:, i, :], in1=csb[:, j, :], op=ALU.subtract), reads=[carry, csb], writes=[bias])
                pi += 1
        vB3 = self.vB.rearrange("(n p) d -> p n d", p=128)
        yb = self.yT[1]
        steps = [(hh, g, j) for hh in range(8) for g in range(NG) for j in range(4 * g + 4)]
        pls = {}
        loaded = set()

        def load_pair(hp):
            if hp in loaded or hp >= 4:
                return
            loaded.add(hp)
            c.dma("pool", qbh[hp % 2][:, :], self.qbT[hp * 128:(hp + 1) * 128, :], reads=allg("qbT"), writes=[qbh[hp % 2]])
            c.dma("pool", kbh[hp % 2][:, :], self.kbT[hp * 128:(hp + 1) * 128, :], reads=allg("kbT"), writes=[kbh[hp % 2]])

        def logits(k):
            hh, g, j = steps[k]
            hb, hp = (hh % 2) * 64, hh // 2
            load_pair(hp)
            qb, kb = qbh[hp % 2], kbh[hp % 2]
            col0 = max(j - 4 * g, 0) * 128
            pl = self.rotset("bpl", lbanks)
            pls[k] = pl
            c.op("pe", lambda h: h.matmul(pl[:, col0:512], lhsT=kb[hb:hb + 64, j * 128:(j + 1) * 128],
                                          rhs=qb[hb:hb + 64, g * 512 + col0:(g + 1) * 512], start=True, stop=True),
                 reads=[kb, qb], writes=[pl])

        def gen():
            po = None
            logits(0)
            for k, (hh, g, j) in enumerate(steps):
                ve = vext[hh % 2]
                if g == 0 and j == 0:
                    c.dma("pool", ve[:, :, 0:64], vB3[:, :, hh * 64:(hh + 1) * 64], reads=allg("vB"), writes=[ve])
                if j == 0:
                    po = self.rotset("bpo", pbanks)
                if k + 1 < len(steps):
                    logits(k + 1)
                nj = 4 * g + 4
                r = j - 4 * g
                col0 = max(r, 0) * 128
                pl = pls.pop(k)
                pt = pts[j % 4]
                for qq in range(max(r, 0), 4):
                    pi = pidx[(4 * g + qq, j)]
                    qs_ = slice(qq * 128, (qq + 1) * 128)
                    c.op("act", lambda h, pl=pl, pt=pt, qs_=qs_, pi=pi, hh=hh: h.activation(out=pt[:, qs_], in_=pl[:, qs_], func=AF.Exp, scale=0.125, bias=bias[:, pi, hh:hh + 1]),
                         reads=[pl, bias], writes=[ptq[j % 4][qq]])
                if r >= 0:
                    c.op("pool", lambda h, pt=pt, col0=col0: h.tensor_tensor(out=pt[:, col0:col0 + 128], in0=pt[:, col0:col0 + 128], in1=self.tri01_b[:, :], op=ALU.mult),
                         reads=[ptq[j % 4][r], self.tri01_b], writes=[ptq[j % 4][r]])
                c.op("pe", lambda h, po=po, pt=pt, j=j, col0=col0, nj=nj, ve=ve: h.matmul(po[:, col0:512], lhsT=ve[:, j, :], rhs=pt[:, col0:512], start=(j == 0), stop=(j == nj - 1)),
                     reads=[ve] + ptq[j % 4][max(r, 0):4], writes=[po], sig=(j == nj - 1))
                if j == nj - 1:
                    rs_, ys_, nm_ = rsum[g % 2], yst[g % 2], nums[g % 2]
                    c.op("act", lambda h, po=po, rs_=rs_: h.activation(out=rs_[:, :], in_=po[64:128, :], func=AF.Copy), reads=[po], writes=[rs_])
                    c.op("act", lambda h, po=po, nm_=nm_: h.activation(out=nm_[:, :], in_=po[0:64, :], func=AF.Copy), reads=[po], writes=[nm_])
                    c.op("act", lambda h, rs_=rs_: h.activation(out=rs_[:, :], in_=rs_[:, :], func=AF.Ln), reads=[rs_], writes=[rs_])
                    c.op("act", lambda h, rs_=rs_: h.activation(out=rs_[:, :], in_=rs_[:, :], func=AF.Exp, scale=-1.0), reads=[rs_], writes=[rs_])
                    c.op("pool", lambda h, nm_=nm_, rs_=rs_, ys_=ys_: h.tensor_tensor(out=ys_[:, :], in0=nm_[:, :], in1=rs_[:, :], op=ALU.mult), reads=[nm_, rs_], writes=[ys_])
                    c.dma("pool", yb[hh * 64:(hh + 1) * 64, g * 512:(g + 1) * 512], ys_[:, :], reads=[ys_], writes=[self.db("yT1", hh * 100 + g)])
                yield k
        return gen(), len(steps)

    def b_phase(self):
        with ExitStack() as es:
            g, n = self._b_setup(es, [0, 1, 2, 3, 4, 5], [6, 7])
            for _ in g:
                pass
            self.c.barrier()

    NITER = 16
    TIE = True

    def _a_setup(self, es, sbanks, lpairs, pvpair):
        c, S, NT, NG = self.c, self.S, self.NT, self.NG
        NIT = self.NITER
        qi = c.sb(es, "qi", [128, 2, S], BF16)
        ki = c.sb(es, "ki", [128, S], BF16)
        ka = c.sb(es, "ka", [64, S], BF16)
        vext = c.sb(es, "vexta", [128, NT, 128], BF16)
        wi = c.sb(es, "wi", [128, NT, 4], F32)
        qat = [c.sb(es, "qat", [64, 1024], BF16) for _ in range(2)]
        score = c.sb(es, "score", [128, S], F32)
        isz = c.sb(es, "isz", [128, S], BF16)
        zrk = c.sb(es, "zrk", [128, S], BF16)
        maskb = [c.sb(es, "maskb", [128, S], BF16) for _ in range(2)]
        irep = c.sb(es, "irep", [128, 512], BF16)
        negc = c.sb(es, "negc", [128, 128], F32)
        pow2 = c.sb(es, "pow2", [128, NIT], F32)
        rts = [c.sb(es, "rt", [128, 512], F32) for _ in range(3)]
        pts = [c.sb(es, "pta", [128, 1024], BF16) for _ in range(3)]
        sm = c.sb(es, "bis", [128, 16], F32)
        steps = c.sb(es, "steps", [128, NIT], F32)
        rsum = c.sb(es, "rsuma", [64, 1024], F32)
        rsb = [Buf(name="rsa0"), Buf(name="rsa1")]
        yst = [c.sb(es, "ysta", [64, 1024], BF16) for _ in range(2)]
        cm = self.dram["cmat"]
        cb = self.db("cmat")
        for r in range(4):
            c.dma("pool", irep[:, r * 128:(r + 1) * 128], cm[0], reads=[cb], writes=[irep])
        c.dma("sp", negc[:, :], cm[4], reads=[cb], writes=[negc])
        c.dma("sp", pow2[:, :], self.dram["pow2"][:, 0:NIT], reads=[self.db("pow2")], writes=[pow2])
        allg = lambda nm: [self.db(nm, g) for g in range(NG)]
        c.dma("sp", qi[:, :, :], self.qiT.rearrange("(hp p) t -> p hp t", p=128), reads=allg("qiT"), writes=[qi])
        c.dma("sp", ki[0:64, :], self.kiT[:, :], reads=allg("kiT"), writes=[ki])
        c.dma("sp", ki[64:128, :], self.kiT[:, :], reads=allg("kiT"), writes=[ki])
        c.dma("sp", ka[:, :], self.kaT[:, :], reads=allg("kaT"), writes=[ka])
        c.dma("sp", vext[:, :, 0:64], self.vA.rearrange("(n p) d -> p n d", p=128), reads=allg("vA"), writes=[vext])
        c.op("pool", lambda h: h.memset(vext[:, :, 64:128], 1.0), writes=[vext])
        c.dma("sp", wi[:, :, :], self.smallD.rearrange("(n p) c -> p n c", p=128)[:, :, 0:4], reads=allg("smallD"), writes=[wi])
        qa3 = self.qaT.rearrange("(h d) t -> d h t", d=64)
        ya = self.yT[0].rearrange("(h d) t -> d h t", d=64)
        NEGM = -32768.0

        def stage1(i):
            ncols = (i + 1) * 128
            qt = qat[i % 2]
            mb = maskb[i % 2]
            c.dma("sp", qt[:, :].rearrange("d (h t) -> d h t", h=8), qa3[:, :, i * 128:(i + 1) * 128], reads=allg("qaT"), writes=[qt])
            for s0 in range(0, ncols, 512):
                w = min(512, ncols - s0)
                for hd in range(4):
                    pl = self.rotset("apl", sbanks)
                    hb, hp = (hd % 2) * 64, hd // 2
                    c.op("pe", lambda h, pl=pl, hb=hb, hp=hp, s0=s0, w=w: h.matmul(pl[:, 0:w], lhsT=qi[hb:hb + 64, hp, i * 128:(i + 1) * 128], rhs=ki[hb:hb + 64, s0:s0 + w], start=True, stop=True),
                         reads=[qi, ki], writes=[pl])
                    if hd == 0:
                        c.op("dve", lambda h, pl=pl, s0=s0, w=w: h.tensor_scalar(out=score[:, s0:s0 + w], in0=pl[:, 0:w], scalar1=0.0, scalar2=wi[:, i, 0:1], op0=ALU.max, op1=ALU.mult),
                             reads=[pl, wi], writes=[score])
                    else:
                        rt = rts[hd - 1]
                        c.op("act", lambda h, pl=pl, rt=rt, w=w: h.activation(out=rt[:, 0:w], in_=pl[:, 0:w], func=AF.Relu), reads=[pl], writes=[rt])
                        c.op("dve", lambda h, rt=rt, hd=hd, s0=s0, w=w: h.scalar_tensor_tensor(out=score[:, s0:s0 + w], in0=rt[:, 0:w], scalar=wi[:, i, hd:hd + 1], in1=score[:, s0:s0 + w],
                                                                                            op0=ALU.mult, op1=ALU.add),
                             reads=[rt, wi, score], writes=[score])
            sc = score[:, 0:ncols]
            c.op("dve", lambda h: h.tensor_reduce(out=sm[:, 5:6], in_=sc, axis=AX.X, op=ALU.max), reads=[score], writes=[sm])
            c.op("dve", lambda h: h.tensor_reduce(out=sm[:, 6:7], in_=sc, axis=AX.X, op=ALU.min), reads=[score, sm], writes=[sm])
            c.op("dve", lambda h: h.tensor_tensor(out=score[:, i * 128:ncols], in0=score[:, i * 128:ncols], in1=negc[:, :], op=ALU.add), reads=[score, negc], writes=[score])
            c.op("dve", lambda h: h.tensor_scalar(out=sm[:, 0:1], in0=sm[:, 6:7], scalar1=-1.0, scalar2=None, op0=ALU.add), reads=[sm], writes=[sm])
            c.op("dve", lambda h: h.scalar_tensor_tensor(out=sm[:, 1:2], in0=sm[:, 5:6], scalar=1.0, in1=sm[:, 0:1], op0=ALU.add, op1=ALU.subtract), reads=[sm], writes=[sm])
            c.op("dve", lambda h: h.tensor_scalar(out=steps[:, :], in0=pow2[:, :], scalar1=sm[:, 1:2], scalar2=None, op0=ALU.mult), reads=[sm, pow2], writes=[steps])
            for k in range(NIT):
                c.op("dve", lambda h, k=k: h.tensor_tensor(out=sm[:, 2:3], in0=sm[:, 0:1], in1=steps[:, k:k + 1], op=ALU.add), reads=[sm, steps], writes=[sm])
                c.op("dve", lambda h: h.tensor_scalar(out=isz[:, 0:ncols], in0=sc, scalar1=sm[:, 2:3], scalar2=0.0, op0=ALU.is_ge, op1=ALU.add, accum_out=sm[:, 3:4]),
                     reads=[score, sm], writes=[isz, sm])
                c.op("dve", lambda h, k=k: h.scalar_tensor_tensor(out=sm[:, 4:5], in0=sm[:, 3:4], scalar=255.5, in1=steps[:, k:k + 1], op0=ALU.is_ge, op1=ALU.mult), reads=[sm, steps], writes=[sm])
                c.op("dve", lambda h: h.tensor_tensor(out=sm[:, 0:1], in0=sm[:, 0:1], in1=sm[:, 4:5], op=ALU.add), reads=[sm], writes=[sm])
            c.op("dve", lambda h: h.tensor_scalar(out=isz[:, 0:ncols], in0=sc, scalar1=0.0, scalar2=0.0, op0=ALU.is_gt, op1=ALU.add, accum_out=sm[:, 7:8]), reads=[score, sm], writes=[isz, sm])
            c.op("dve", lambda h: h.tensor_scalar(out=isz[:, 0:ncols], in0=sc, scalar1=0.0, scalar2=0.0, op0=ALU.is_equal, op1=ALU.add, accum_out=sm[:, 8:9]), reads=[score, sm], writes=[isz, sm])
            c.op("dve", lambda h: h.tensor_tensor(out=sm[:, 8:9], in0=sm[:, 8:9], in1=sm[:, 7:8], op=ALU.add), reads=[sm], writes=[sm])
            c.op("dve", lambda h: h.tensor_scalar(out=sm[:, 13:14], in0=sm[:, 7:8], scalar1=255.5, scalar2=None, op0=ALU.is_lt), reads=[sm], writes=[sm])
            c.op("dve", lambda h: h.scalar_tensor_tensor(out=sm[:, 9:10], in0=sm[:, 8:9], scalar=255.5, in1=sm[:, 13:14], op0=ALU.is_ge, op1=ALU.mult), reads=[sm], writes=[sm])
            c.op("dve", lambda h: h.tensor_scalar(out=sm[:, 10:11], in0=sm[:, 7:8], scalar1=-1.0, scalar2=256.5, op0=ALU.mult, op1=ALU.add), reads=[sm], writes=[sm])
            c.op("dve", lambda h: h.tensor_scalar(out=sm[:, 13:14], in0=sm[:, 9:10], scalar1=-1.0, scalar2=1.0, op0=ALU.mult, op1=ALU.add), reads=[sm], writes=[sm])
            c.op("dve", lambda h: h.tensor_tensor(out=sm[:, 13:14], in0=sm[:, 13:14], in1=sm[:, 0:1], op=ALU.mult), reads=[sm], writes=[sm])
            c.op("dve", lambda h: h.scalar_tensor_tensor(out=sm[:, 11:12], in0=sm[:, 9:10], scalar=1e-30, in1=sm[:, 13:14], op0=ALU.mult, op1=ALU.add), reads=[sm], writes=[sm])
            c.op("dve", lambda h: h.tensor_scalar(out=sm[:, 12:13], in0=sm[:, 9:10], scalar1=-NEGM, scalar2=None, op0=ALU.mult), reads=[sm], writes=[sm])
            c.op("dve", lambda h: h.tensor_tensor_scan(out=zrk[:, 0:ncols], data0=isz[:, 0:ncols], data1=isz[:, 0:ncols], initial=0.0, op0=ALU.add, op1=ALU.max), reads=[isz], writes=[zrk])
            c.op("dve", lambda h: h.scalar_tensor_tensor(out=isz[:, 0:ncols], in0=zrk[:, 0:ncols], scalar=sm[:, 10:11], in1=isz[:, 0:ncols], op0=ALU.is_le, op1=ALU.mult), reads=[zrk, sm, isz], writes=[isz])
            c.op("dve", lambda h, mb=mb: h.tensor_scalar(out=mb[:, 0:ncols], in0=sc, scalar1=sm[:, 11:12], scalar2=NEGM, op0=ALU.is_lt, op1=ALU.mult), reads=[score, sm], writes=[mb])
            if self.TIE:
                c.op("dve", lambda h, mb=mb: h.scalar_tensor_tensor(out=mb[:, 0:ncols], in0=isz[:, 0:ncols], scalar=sm[:, 12:13], in1=mb[:, 0:ncols], op0=ALU.mult, op1=ALU.add), reads=[isz, sm, mb], writes=[mb])

        def stage2(i):
            qt = qat[i % 2]
            mb = maskb[i % 2]
            po = (self.ps[pvpair[0]], self.ps[pvpair[1]])
            lb = [b_ for pr in lpairs for b_ in pr]
            nlb = len(lb)
            hsteps = [(j, half) for j in range(i + 1) for half in range(2)]

            def alog(k):
                j, half = hsteps[k]
                pl = self.ps[lb[k % nlb]]
                c.op("pe", lambda h: h.matmul(pl[:, :], lhsT=ka[:, j * 128:(j + 1) * 128], rhs=qt[:, half * 512:(half + 1) * 512], start=True, stop=False),
                     reads=[ka, qt], writes=[pl], sig=False)
                c.op("pe", lambda h: h.matmul(pl[:, :], lhsT=mb[:, j * 128:(j + 1) * 128], rhs=irep[:, :], start=False, stop=True),
                     reads=[mb, irep], writes=[pl])

            ptb = [[Buf(name=f"apt{a_}_{b_}") for b_ in range(2)] for a_ in range(3)]
            la = min(nlb - 1, 3)
            for k in range(min(la, len(hsteps))):
                alog(k)
            for k, (j, half) in enumerate(hsteps):
                if k + la < len(hsteps):
                    alog(k + la)
                pl = self.ps[lb[k % nlb]]
                pt = pts[j % 3]
                c.op("act", lambda h, pl=pl, half=half, pt=pt: h.activation(out=pt[:, half * 512:(half + 1) * 512], in_=pl[:, :], func=AF.Exp, scale=0.125), reads=[pl], writes=[self._aptb(pt, half)])
                c.op("pe", lambda h, half=half, j=j, pt=pt, po=po: h.matmul(po[half][:, :], lhsT=vext[:, j, :], rhs=pt[:, half * 512:(half + 1) * 512], start=(j == 0), stop=(j == i)),
                     reads=[vext, self._aptb(pt, half)], writes=[po[half]], sig=(j == i))
            ys_ = yst[i % 2]
            for half in range(2):
                hs = slice(half * 512, (half + 1) * 512)
                rb = rsb[half]
                c.op("act", lambda h, half=half, hs=hs: h.activation(out=rsum[:, hs], in_=po[half][64:128, :], func=AF.Copy), reads=[po[half]], writes=[rb])
                c.op("act", lambda h, hs=hs: h.activation(out=rsum[:, hs], in_=rsum[:, hs], func=AF.Ln), reads=[rb], writes=[rb])
                c.op("act", lambda h, hs=hs: h.activation(out=rsum[:, hs], in_=rsum[:, hs], func=AF.Exp, scale=-1.0), reads=[rb], writes=[rb])
            for half in range(2):
                hs = slice(half * 512, (half + 1) * 512)
                c.op("dve", lambda h, half=half, hs=hs, ys_=ys_: h.tensor_tensor(out=ys_[:, hs], in0=po[half][0:64, :], in1=rsum[:, hs], op=ALU.mult), reads=[po[half], rsb[half]], writes=[ys_])
            c.dma("sp", ya[:, :, i * 128:(i + 1) * 128], ys_[:, :].rearrange("d (h t) -> d h t", h=8), reads=[ys_], writes=[self.db("yT0", i)])

        return stage1, stage2

    def _aptb(self, pt, half):
        d = self.__dict__.setdefault("_aptbufs", {})
        k = (id(pt), half)
        if k not in d:
            d[k] = Buf(name=f"aptb{len(d)}")
        return d[k]

    def a_phase(self):
        NT = self.NT
        with ExitStack() as es:
            stage1, stage2 = self._a_setup(es, [0, 1], [(2, 3), (4, 5)], (6, 7))
            stage1(0)
            for i in range(NT):
                if i + 1 < NT:
                    stage1(i + 1)
                stage2(i)
            self.c.barrier()

    def ab_phase(self):
        NT = self.NT
        with ExitStack() as es:
            stage1, stage2 = self._a_setup(es, [0, 1, 2], [(1, 2)], (3, 4))
            bgen, nb = self._b_setup(es, [5, 6], [7])
            done = 0

            def advance(upto):
                nonlocal done
                while done < min(upto, nb):
                    next(bgen)
                    done += 1
            stage1(0)
            for i in range(NT):
                if i + 1 < NT:
                    stage1(i + 1)
                stage2(i)
                advance((nb * (i + 1) + NT - 1) // NT)
            advance(nb)
            self.c.barrier()


def build_gmask():
    i = np.arange(128)
    out = np.zeros((14, 128, 512), np.float32)
    for l in range(7):
        b = 1 << l
        t, tp = i[:, None], i[None, :]
        m = ((t // (2 * b)) == (tp // (2 * b))) & ((t % (2 * b)) >= b) & ((tp % (2 * b)) < b)
        m = m.astype(np.float32)
        out[l] = np.tile(m, (1, 4))
        out[7 + l] = np.tile(m.T, (1, 4))
    return out


class ProgC(ProgAB):
    def c_phase(self, cvb):
        c, S, NT = self.c, self.S, self.NT
        with ExitStack() as es:
            def T(name, shape, dt, n=1):
                return [c.sb(es, name, shape, dt) for _ in range(n)]
            gm = T("gm", [128, 14, 512], BF16)[0]
            identrep = T("identrep", [128, 512], BF16)[0]
            negm4 = T("negm4", [128, 512], F32)[0]
            strict4 = T("strict4", [128, 512], BF16)[0]
            cm = self.dram["cmat"]
            cb = self.db("cmat")
            c.dma("pool", gm[:, :, :], self.dram["gmask"].rearrange("l p n -> p l n"), reads=[self.db("gmask")], writes=[gm])
            for r in range(4):
                c.dma("pool", identrep[:, r * 128:(r + 1) * 128], cm[0], reads=[cb], writes=[identrep])
                c.dma("sp", negm4[:, r * 128:(r + 1) * 128], cm[3], reads=[cb], writes=[negm4])
                c.dma("pool", strict4[:, r * 128:(r + 1) * 128], cm[5], reads=[cb], writes=[strict4])
            kTs = T("kTc", [128, 4, 128], BF16, 2); qTs = T("qTc", [128, 4, 128], BF16, 2)
            ktoks = T("ktok", [128, 512], BF16, 2); vtoks = T("vtok", [128, 512], BF16, 2)
            szs = T("szc", [128, 4, 128], BF16, 2); gbs = T("gb", [128, 8], F32, 2)
            gtri = T("gtri", [128, 4, 128], F32)[0]; ngtri = T("ngtri", [128, 4, 128], F32)[0]; bdiag = T("bdiag", [128, 4, 128], F32)[0]
            dm = T("dm", [128, 512], F32)[0]; decT = T("decT", [128, 512], F32)[0]; egcb = T("egcb", [128, 512], F32)[0]
            bbc = T("bbc", [128, 512], F32)[0]; dsb = T("dsb", [128, 512], F32)[0]
            ngb = T("ngb", [128, 4], F32)[0]
            gcs = T("gcs", [128, 8], F32)[0]; sm = T("csm", [128, 16], F32)[0]
            ATb = T("ATb", [128, 4, 128], BF16)[0]; Ab = T("Ab", [128, 4, 128], BF16)[0]; qkb = T("qkb", [128, 4, 128], BF16)[0]
            Ts = T("Tm", [128, 4, 128], BF16, 2); Us = T("Um", [128, 4, 128], BF16, 2)
            Xb = T("Xb", [128, 4, 128], BF16)[0]; Xpb = T("Xpb", [128, 4, 128], BF16)[0]; tmpb = T("tmpb", [128, 512], BF16, 2)
            kbg = T("kbg", [128, 512], BF16)[0]; vb = T("vb", [128, 512], BF16)[0]; kdec = T("kdec", [128, 512], BF16)[0]
            qg = T("qg", [128, 4, 128], BF16)[0]; nwT = T("nwT", [128, 4, 128], BF16)[0]; vnew = T("vnew", [128, 512], BF16)[0]
            state_f = T("state_f", [128, 4, 128], F32)[0]; state_b = T("state_b", [128, 4, 128], BF16)[0]
            osq = T("osq", [128, 4, 128], BF16)[0]; rstd = T("crstd", [128, 512], F32)[0]; yn = T("yn", [128, 512], F32)[0]
            yst = T("ystc", [128, 4, 128], BF16, 2)
            c.op("pool", lambda h: h.memset(state_f[:, :, :], 0.0), writes=[state_f])
            c.op("pool", lambda h: h.memset(state_b[:, :, :], 0.0), writes=[state_b])
            qc3 = self.qcT.rearrange("(h d) t -> d h t", d=128); kc3 = self.kcT.rearrange("(h d) t -> d h t", d=128)
            sz3 = self.szT.rearrange("(h d) t -> d h t", d=128); yc3 = self.yT[2].rearrange("(h d) t -> d h t", d=128)
            allg = lambda nm: [self.db(nm, g) for g in range(self.NG)]
            f2 = lambda b: b[:, :, :].rearrange("p h t -> p (h t)")
            for n in range(NT):
                cs = slice(n * 128, (n + 1) * 128)
                kT, qT, ktok, vtok, sz, gb = kTs[n % 2], qTs[n % 2], ktoks[n % 2], vtoks[n % 2], szs[n % 2], gbs[n % 2]
                c.dma("sp", kT[:, :, :], kc3[:, :, cs], reads=allg("kcT"), writes=[kT])
                c.dma("sp", qT[:, :, :], qc3[:, :, cs], reads=allg("qcT"), writes=[qT])
                c.dma("sp", sz[:, :, :], sz3[:, :, cs], reads=allg("szT"), writes=[sz])
                c.dma("sp", ktok[:, :], self.kC[cs, :], reads=allg("kC"), writes=[ktok])
                c.dma("sp", vtok[:, :], self.vC[cs, :], reads=allg("vC"), writes=[vtok])
                c.dma("sp", gb[:, :], self.smallD[cs, 12:20], reads=allg("smallD"), writes=[gb])
                c.op("dve", lambda h: h.tensor_scalar(out=ngb[:, :], in0=gb[:, 4:8], scalar1=-1.0, scalar2=None, op0=ALU.mult), reads=[gb], writes=[ngb])
                for hd in range(4):
                    c.op("dve", lambda h, hd=hd: h.tensor_scalar(out=gtri[:, hd, :], in0=self.triu_f[:, :], scalar1=gb[:, 4 + hd:5 + hd], scalar2=None, op0=ALU.mult), reads=[self.triu_f, gb], writes=[gtri])
                    c.op("act", lambda h, hd=hd: h.activation(out=ngtri[:, hd, :], in_=self.triu_f[:, :], func=AF.Copy, scale=ngb[:, hd:hd + 1]), reads=[self.triu_f, ngb], writes=[ngtri])
                    c.op("act", lambda h, hd=hd: h.activation(out=bdiag[:, hd, :], in_=self.ident_f[:, :], func=AF.Copy, scale=gb[:, hd:hd + 1]), reads=[self.ident_f, gb], writes=[bdiag])
                pD, pG, pB, pS = self.rot(), self.rot(), self.rot(), self.rot()
                for hd in range(4):
                    hs = slice(hd * 128, (hd + 1) * 128)
                    c.op("pe", lambda h, hd=hd, hs=hs: h.matmul(pD[:, hs], lhsT=self.ones_f[:, :], rhs=gtri[:, hd, :], start=True, stop=False), reads=[self.ones_f, gtri], writes=[pD], sig=False)
                    c.op("pe", lambda h, hd=hd, hs=hs: h.matmul(pD[:, hs], lhsT=ngtri[:, hd, :], rhs=self.ones_f[:, :], start=False, stop=True), reads=[self.ones_f, ngtri], writes=[pD], sig=(hd == 3))
                for hd in range(4):
                    hs = slice(hd * 128, (hd + 1) * 128)
                    c.op("pe", lambda h, hd=hd, hs=hs: h.matmul(pG[:, hs], lhsT=self.ones_f[:, :], rhs=gtri[:, hd, :], start=True, stop=True), reads=[self.ones_f, gtri], writes=[pG], sig=(hd == 3))
                for hd in range(4):
                    hs = slice(hd * 128, (hd + 1) * 128)
                    c.op("pe", lambda h, hd=hd, hs=hs: h.matmul(pB[:, hs], lhsT=self.ones_f[:, :], rhs=bdiag[:, hd, :], start=True, stop=True), reads=[self.ones_f, bdiag], writes=[pB], sig=(hd == 3))
                c.op("pe", lambda h: h.matmul(pS[:, 0:4], lhsT=self.triu_f[:, :], rhs=gb[:, 4:8], start=True, stop=True), reads=[self.triu_f, gb], writes=[pS], sig=False)
                c.op("pe", lambda h: h.matmul(pS[:, 4:8], lhsT=self.ones_f[:, :], rhs=gb[:, 4:8], start=True, stop=True), reads=[self.ones_f, gb], writes=[pS])
                c.op("dve", lambda h: h.scalar_tensor_tensor(out=dm[:, :], in0=pD[:, :], scalar=0.0, in1=negm4[:, :], op0=ALU.min, op1=ALU.add), reads=[pD, negm4], writes=[dm])
                c.op("act", lambda h: h.activation(out=decT[:, :], in_=dm[:, :], func=AF.Exp), reads=[dm], writes=[decT])
                c.op("act", lambda h: h.activation(out=egcb[:, :], in_=pG[:, :], func=AF.Exp), reads=[pG], writes=[egcb])
                c.op("act", lambda h: h.activation(out=bbc[:, :], in_=pB[:, :], func=AF.Copy), reads=[pB], writes=[bbc])
                c.op("act", lambda h: h.activation(out=gcs[:, :], in_=pS[:, 0:8], func=AF.Copy), reads=[pS], writes=[gcs])
                c.op("act", lambda h: h.activation(out=sm[:, 0:4], in_=gcs[:, 0:4], func=AF.Exp), reads=[gcs], writes=[sm])
                c.op("dve", lambda h: h.tensor_tensor(out=sm[:, 0:4], in0=sm[:, 0:4], in1=gb[:, 0:4], op=ALU.mult), reads=[sm, gb], writes=[sm])
                c.op("dve", lambda h: h.tensor_tensor(out=sm[:, 12:16], in0=gcs[:, 4:8], in1=gcs[:, 0:4], op=ALU.subtract), reads=[gcs, sm], writes=[sm])
                c.op("act", lambda h: h.activation(out=sm[:, 4:8], in_=sm[:, 12:16], func=AF.Exp), reads=[sm], writes=[sm])
                c.op("act", lambda h: h.activation(out=sm[:, 8:12], in_=gcs[:, 4:8], func=AF.Exp), reads=[gcs, sm], writes=[sm])
                pK, pQ = self.rot(), self.rot()
                for hd in range(4):
                    hs = slice(hd * 128, (hd + 1) * 128)
                    c.op("pe", lambda h, hd=hd, hs=hs: h.matmul(pK[:, hs], lhsT=kT[:, hd, :], rhs=kT[:, hd, :], start=True, stop=True), reads=[kT], writes=[pK], sig=(hd == 3))
                for hd in range(4):
                    hs = slice(hd * 128, (hd + 1) * 128)
                    c.op("pe", lambda h, hd=hd, hs=hs: h.matmul(pQ[:, hs], lhsT=kT[:, hd, :], rhs=qT[:, hd, :], start=True, stop=True), reads=[kT, qT], writes=[pQ], sig=(hd == 3))
                c.op("dve", lambda h: h.tensor_tensor(out=dsb[:, :], in0=decT[:, :], in1=bbc[:, :], op=ALU.mult), reads=[decT, bbc], writes=[dsb])
                c.op("dve", lambda h: h.tensor_tensor(out=dsb[:, :], in0=dsb[:, :], in1=strict4[:, :], op=ALU.mult), reads=[dsb, strict4], writes=[dsb])
                c.op("dve", lambda h: h.tensor_tensor(out=f2(ATb), in0=pK[:, :], in1=dsb[:, :], op=ALU.mult), reads=[pK, dsb], writes=[ATb])
                c.op("dve", lambda h: h.tensor_tensor(out=f2(qkb), in0=pQ[:, :], in1=decT[:, :], op=ALU.mult), reads=[pQ, decT], writes=[qkb])
                pA = self.rot()
                for hd in range(4):
                    hs = slice(hd * 128, (hd + 1) * 128)
                    c.op("pe", lambda h, hd=hd, hs=hs: h.matmul(pA[:, hs], lhsT=ATb[:, hd, :], rhs=self.ident_b[:, :], start=True, stop=True), reads=[ATb, self.ident_b], writes=[pA], sig=(hd == 3))
                c.op("act", lambda h: h.activation(out=f2(Ab), in_=pA[:, :], func=AF.Copy), reads=[pA], writes=[Ab])
                Tc, Uc = Ts[0], Us[0]
                c.op("dve", lambda h: h.tensor_tensor(out=tmpb[0][:, :], in0=f2(Ab), in1=gm[:, 0, :], op=ALU.mult), reads=[Ab, gm], writes=[tmpb[0]])
                c.op("dve", lambda h: h.tensor_tensor(out=f2(Tc), in0=identrep[:, :], in1=tmpb[0][:, :], op=ALU.subtract), reads=[tmpb[0], identrep], writes=[Tc])
                c.op("dve", lambda h: h.tensor_tensor(out=tmpb[1][:, :], in0=f2(ATb), in1=gm[:, 7, :], op=ALU.mult), reads=[ATb, gm], writes=[tmpb[1]])
                c.op("dve", lambda h: h.tensor_tensor(out=f2(Uc), in0=identrep[:, :], in1=tmpb[1][:, :], op=ALU.subtract), reads=[tmpb[1], identrep], writes=[Uc])
                for l in range(1, 7):
                    Tn, Un = Ts[l % 2], Us[l % 2]
                    pXp = self.rot()
                    for hd in range(4):
                        hs = slice(hd * 128, (hd + 1) * 128)
                        c.op("pe", lambda h, hd=hd, hs=hs, Uc=Uc: h.matmul(pXp[:, hs], lhsT=Ab[:, hd, :], rhs=Uc[:, hd, :], start=True, stop=True), reads=[Ab, Uc], writes=[pXp], sig=(hd == 3))
                    c.op("dve", lambda h, l=l: h.tensor_tensor(out=f2(Xpb), in0=pXp[:, :], in1=gm[:, 7 + l, :], op=ALU.mult), reads=[pXp, gm], writes=[Xpb])
                    if l < 6:
                        pX = self.rot()
                        for hd in range(4):
                            hs = slice(hd * 128, (hd + 1) * 128)
                            c.op("pe", lambda h, hd=hd, hs=hs, Tc=Tc: h.matmul(pX[:, hs], lhsT=ATb[:, hd, :], rhs=Tc[:, hd, :], start=True, stop=True), reads=[ATb, Tc], writes=[pX], sig=(hd == 3))
                        c.op("dve", lambda h, l=l: h.tensor_tensor(out=f2(Xb), in0=pX[:, :], in1=gm[:, l, :], op=ALU.mult), reads=[pX, gm], writes=[Xb])
                    pYp = self.rot()
                    for hd in range(4):
                        hs = slice(hd * 128, (hd + 1) * 128)
                        c.op("pe", lambda h, hd=hd, hs=hs, Tc=Tc: h.matmul(pYp[:, hs], lhsT=Tc[:, hd, :], rhs=Xpb[:, hd, :], start=True, stop=True), reads=[Tc, Xpb], writes=[pYp], sig=(hd == 3))
                    c.op("dve", lambda h, Uc=Uc, Un=Un: h.tensor_tensor(out=f2(Un), in0=f2(Uc), in1=pYp[:, :], op=ALU.subtract), reads=[Uc, pYp], writes=[Un])
                    if l < 6:
                        pY = self.rot()
                        for hd in range(4):
                            hs = slice(hd * 128, (hd + 1) * 128)
                            c.op("pe", lambda h, hd=hd, hs=hs, Uc=Uc: h.matmul(pY[:, hs], lhsT=Uc[:, hd, :], rhs=Xb[:, hd, :], start=True, stop=True), reads=[Uc, Xb], writes=[pY], sig=(hd == 3))
                        c.op("dve", lambda h, Tc=Tc, Tn=Tn: h.tensor_tensor(out=f2(Tn), in0=f2(Tc), in1=pY[:, :], op=ALU.subtract), reads=[Tc, pY], writes=[Tn])
                    Tc, Uc = Tn, Un
                U = Uc
                for hd in range(4):
                    hs = slice(hd * 128, (hd + 1) * 128)
                    c.op("act", lambda h, hd=hd, hs=hs: h.activation(out=kbg[:, hs], in_=ktok[:, hs], func=AF.Copy, scale=sm[:, hd:hd + 1]), reads=[ktok, sm], writes=[kbg])
                    c.op("act", lambda h, hd=hd, hs=hs: h.activation(out=vb[:, hs], in_=vtok[:, hs], func=AF.Copy, scale=gb[:, hd:hd + 1]), reads=[vtok, gb], writes=[vb])
                    c.op("act", lambda h, hd=hd, hs=hs: h.activation(out=kdec[:, hs], in_=ktok[:, hs], func=AF.Copy, scale=sm[:, 4 + hd:5 + hd]), reads=[ktok, sm], writes=[kdec])
                c.op("dve", lambda h: h.tensor_tensor(out=f2(qg), in0=f2(qT), in1=egcb[:, :], op=ALU.mult), reads=[qT, egcb], writes=[qg])
                pW = self.rot()
                for hd in range(4):
                    hs = slice(hd * 128, (hd + 1) * 128)
                    c.op("pe", lambda h, hd=hd, hs=hs: h.matmul(pW[:, hs], lhsT=kbg[:, hs], rhs=U[:, hd, :], start=True, stop=True), reads=[kbg, U], writes=[pW], sig=(hd == 3))
                c.op("act", lambda h: h.activation(out=f2(nwT), in_=pW[:, :], func=AF.Copy, scale=-1.0), reads=[pW], writes=[nwT])
                pV = self.rot()
                for hd in range(4):
                    hs = slice(hd * 128, (hd + 1) * 128)
                    c.op("pe", lambda h, hd=hd, hs=hs: h.matmul(pV[:, hs], lhsT=U[:, hd, :], rhs=vb[:, hs], start=True, stop=False), reads=[U, vb], writes=[pV], sig=False)
                    c.op("pe", lambda h, hd=hd, hs=hs: h.matmul(pV[:, hs], lhsT=nwT[:, hd, :], rhs=state_b[:, hd, :], start=False, stop=True), reads=[nwT, state_b], writes=[pV], sig=(hd == 3))
                c.op("act", lambda h: h.activation(out=vnew[:, :], in_=pV[:, :], func=AF.Copy), reads=[pV], writes=[vnew])
                pO = self.rot()
                for hd in range(4):
                    hs = slice(hd * 128, (hd + 1) * 128)
                    c.op("pe", lambda h, hd=hd, hs=hs: h.matmul(pO[:, hs], lhsT=state_b[:, hd, :], rhs=qg[:, hd, :], start=True, stop=False), reads=[state_b, qg], writes=[pO], sig=False)
                    c.op("pe", lambda h, hd=hd, hs=hs: h.matmul(pO[:, hs], lhsT=vnew[:, hs], rhs=qkb[:, hd, :], start=False, stop=True), reads=[vnew, qkb], writes=[pO], sig=(hd == 3))
                pS2 = self.rot()
                for hd in range(4):
                    hs = slice(hd * 128, (hd + 1) * 128)
                    c.op("pe", lambda h, hd=hd, hs=hs: h.matmul(pS2[:, hs], lhsT=kdec[:, hs], rhs=vnew[:, hs], start=True, stop=True), reads=[kdec, vnew], writes=[pS2], sig=(hd == 3))
                for hd in range(4):
                    hs = slice(hd * 128, (hd + 1) * 128)
                    c.op("dve", lambda h, hd=hd, hs=hs: h.scalar_tensor_tensor(out=state_f[:, hd, :], in0=state_f[:, hd, :], scalar=sm[:, 8 + hd:9 + hd], in1=pS2[:, hs], op0=ALU.mult, op1=ALU.add),
                         reads=[state_f, sm, pS2], writes=[state_f])
                c.op("act", lambda h: h.activation(out=f2(state_b), in_=f2(state_f), func=AF.Copy), reads=[state_f], writes=[state_b])
                c.op("act", lambda h: h.activation(out=f2(osq), in_=pO[:, :], func=AF.Square), reads=[pO], writes=[osq])
                pN = self.rot()
                for hd in range(4):
                    hs = slice(hd * 128, (hd + 1) * 128)
                    c.op("pe", lambda h, hd=hd, hs=hs: h.matmul(pN[:, hs], lhsT=self.ones_b[:, :], rhs=osq[:, hd, :], start=True, stop=True), reads=[self.ones_b, osq], writes=[pN], sig=(hd == 3))
                c.op("dve", lambda h: h.tensor_scalar(out=rstd[:, :], in0=pN[:, :], scalar1=1.0 / 128, scalar2=1e-6, op0=ALU.mult, op1=ALU.add), reads=[pN], writes=[rstd])
                c.op("act", lambda h: h.activation(out=rstd[:, :], in_=rstd[:, :], func=AF.Ln), reads=[rstd], writes=[rstd])
                c.op("act", lambda h: h.activation(out=rstd[:, :], in_=rstd[:, :], func=AF.Exp, scale=-0.5), reads=[rstd], writes=[rstd])
                c.op("dve", lambda h: h.tensor_tensor(out=yn[:, :], in0=pO[:, :], in1=rstd[:, :], op=ALU.mult), reads=[pO, rstd], writes=[yn])
                ys_ = yst[n % 2]
                c.op("dve", lambda h, ys_=ys_: h.scalar_tensor_tensor(out=f2(ys_), in0=yn[:, :], scalar=self.cvec[:, cvb + CV_DN:cvb + CV_DN + 1], in1=f2(sz), op0=ALU.mult, op1=ALU.mult),
                     reads=[yn, self.cvec, sz], writes=[ys_])
                c.dma("sp", yc3[:, :, cs], ys_[:, :, :], reads=[ys_], writes=[self.db("yT2", n)])
            c.barrier()


class ProgFull(ProgC):
    def merge_phase(self, xsrc, xdst, wbr, w_out):
        c, S, NG = self.c, self.S, self.NG
        with ExitStack() as es:
            wbs = [c.sb(es, "wbr", [128, 4, 1024], BF16) for _ in range(3)]
            wo = c.sb(es, "wo", [128, 8, 1024], BF16)
            wb = self.db("w")
            for i in range(3):
                c.dma("pool", wbs[i][:, :, :], wbr[i].rearrange("(k p) n -> p k n", p=128), reads=[wb], writes=[wbs[i]])
            c.dma("pool", wo[:, :, :], w_out.rearrange("(k p) n -> p k n", p=128), reads=[wb], writes=[wo])
            ys = [c.sb(es, "ymg", [128, 3, 4, 512], BF16) for _ in range(2)]
            gts = [c.sb(es, "gmg", [128, 24, 512], BF16) for _ in range(2)]
            xgs = [c.sb(es, "xmg", [128, 8, 512], F32) for _ in range(2)]
            mg = c.sb(es, "mg", [128, 8, 512], BF16)
            t1 = [c.sb(es, "mt1", [128, 512], F32) for _ in range(2)]
            t2 = [c.sb(es, "mt2", [128, 512], F32) for _ in range(2)]
            xs3 = xsrc.rearrange("(c p) t -> p c t", p=128)
            xd3 = xdst.rearrange("(c p) t -> p c t", p=128)
            for g in range(NG):
                tsl = slice(g * 512, (g + 1) * 512)
                y, gt, xg = ys[g % 2], gts[g % 2], xgs[g % 2]
                deps = [self.db("yT0", i) for i in range(g * 4, g * 4 + 4)] + [self.db("yT1", hh * 100 + g) for hh in range(8)] + [self.db("yT2", n) for n in range(g * 4, g * 4 + 4)]
                for br in range(3):
                    c.dma("sp", y[:, br, :, :], self.yT[br].rearrange("(k p) t -> p k t", p=128)[:, :, tsl], reads=deps, writes=[y])
                c.dma("sp", gt[:, :, :], self.gT.rearrange("(k p) t -> p k t", p=128)[:, :, tsl], reads=[self.db("gT", sec * 100 + g) for sec in range(6)], writes=[gt])
                c.dma("sp", xg[:, :, :], xs3[:, :, tsl], reads=[self.db(xsrc.name, g)], writes=[xg])
                for d in range(8):
                    pbs = []
                    for br in range(3):
                        pb = self.rot()
                        pbs.append(pb)
                        for k in range(4):
                            c.op("pe", lambda h, br=br, k=k, d=d, pb=pb: h.matmul(pb[:, :], lhsT=wbs[br][:, k, d * 128:(d + 1) * 128], rhs=y[:, br, k, :], start=(k == 0), stop=(k == 3)),
                                 reads=[wbs[br], y], writes=[pb], sig=(k == 3))
                    a, b = t1[d % 2], t2[d % 2]
                    c.op("dve", lambda h, d=d, a=a: h.tensor_tensor(out=a[:, :], in0=pbs[0][:, :], in1=gt[:, d, :], op=ALU.mult), reads=[pbs[0], gt], writes=[a])
                    c.op("dve", lambda h, d=d, b=b: h.tensor_tensor(out=b[:, :], in0=pbs[1][:, :], in1=gt[:, 8 + d, :], op=ALU.mult), reads=[pbs[1], gt], writes=[b])
                    c.op("dve", lambda h, a=a, b=b: h.tensor_tensor(out=a[:, :], in0=a[:, :], in1=b[:, :], op=ALU.add), reads=[a, b], writes=[a])
                    c.op("dve", lambda h, d=d, b=b: h.tensor_tensor(out=b[:, :], in0=pbs[2][:, :], in1=gt[:, 16 + d, :], op=ALU.mult), reads=[pbs[2], gt], writes=[b])
                    c.op("dve", lambda h, d=d, a=a, b=b: h.tensor_tensor(out=mg[:, d, :], in0=a[:, :], in1=b[:, :], op=ALU.add), reads=[a, b], writes=[mg])
                for d in range(8):
                    po = self.rot()
                    for k in range(8):
                        c.op("pe", lambda h, d=d, k=k, po=po: h.matmul(po[:, :], lhsT=wo[:, k, d * 128:(d + 1) * 128], rhs=mg[:, k, :], start=(k == 0), stop=(k == 7)),
                             reads=[wo, mg], writes=[po], sig=(k == 7))
                    c.op("dve", lambda h, d=d, po=po, xg=xg: h.tensor_tensor(out=xg[:, d, :], in0=po[:, :], in1=xg[:, d, :], op=ALU.add), reads=[po, xg], writes=[xg])
                c.dma("sp", xd3[:, :, tsl], xg[:, :, :], reads=[xg], writes=[self.db(xdst.name, g)])
            c.barrier()

    def final_phase(self, xsrc, out):
        c, S, NG = self.c, self.S, self.NG
        with ExitStack() as es:
            xgs = [c.sb(es, "xf", [128, 8, 512], F32) for _ in range(2)]
            ogs = [c.sb(es, "of", [128, 8, 512], F32) for _ in range(2)]
            sq = c.sb(es, "sqf", [128, 8, 512], BF16)
            rstd = c.sb(es, "rstdf", [128, 512], F32)
            xs3 = xsrc.rearrange("(c p) t -> p c t", p=128)
            o3 = out.rearrange("(c p) t -> p c t", p=128)
            for g in range(NG):
                tsl = slice(g * 512, (g + 1) * 512)
                xg, og = xgs[g % 2], ogs[g % 2]
                c.dma("sp", xg[:, :, :], xs3[:, :, tsl], reads=[self.db(xsrc.name, g)], writes=[xg])
                self.rmsnorm_group(xg, sq, lambda k, og=og: og[:, k, :], og, rstd, DEPTH * CV_PER_LAYER, self.rot())
                c.dma("sp", o3[:, :, tsl], og[:, :, :], reads=[og], writes=[self.db("out", g)])
            c.barrier()


def build_full(S=4096):
    P = ProgFull(S)
    es = ExitStack()
    P.setup_consts(es)
    P.declare_scratch()
    d = P.dr
    EI = "ExternalInput"
    d("rotC", [128, S], F32, kind=EI); d("rotS", [128, S], F32, kind=EI)
    d("pow2", [128, 26], F32, kind=EI); d("gmask", [14, 128, 512], F32, kind=EI)
    xin = d("xT_in", [1024, S], F32, kind=EI)
    out = d("outT", [1024, S], F32, kind="ExternalOutput")
    xa = d("xTa", [1024, S], F32); xb = d("xTb", [1024, S], F32)
    f1i = d("ffn1_w_in", [DEPTH, 1024, 4096], F32, kind=EI); f1o = d("ffn1_w_out", [DEPTH, 2048, 1024], F32, kind=EI)
    f2i = d("ffn2_w_in", [DEPTH, 1024, 4096], F32, kind=EI); f2o = d("ffn2_w_out", [DEPTH, 2048, 1024], F32, kind=EI)
    w = d("w_in", [DEPTH, 1024, 7636], F32, kind=EI); wsw = d("w_sw", [DEPTH, 1024, 896], F32, kind=EI); wsm = d("w_small", [DEPTH, 1024, 84], F32, kind=EI)
    wba = d("w_branch_a", [DEPTH, 512, 1024], F32, kind=EI); wbb = d("w_branch_b", [DEPTH, 512, 1024], F32, kind=EI); wbc = d("w_branch_c", [DEPTH, 512, 1024], F32, kind=EI)
    wo = d("w_out", [DEPTH, 1024, 1024], F32, kind=EI)
    cur = xin
    for l in range(DEPTH):
        cvb = l * CV_PER_LAYER
        P.ffn_phase(cur, xa, f1i[l], f1o[l], cvb + CV_FFN1)
        P.m1_phase(xa, w[l], wsw[l], wsm[l], cvb)
        P.ab_phase()
        P.c_phase(cvb)
        P.merge_phase(xa, xb, [wba[l], wbb[l], wbc[l]], wo[l])
        P.ffn_phase(xb, xa, f2i[l], f2o[l], cvb + CV_FFN2)
        cur = xa
    P.final_phase(xa, out)
    es.close()
    return P


_CACHE = {}


def kernel(**inputs):
    S = 4096
    inp = {k: np.asarray(v) for k, v in inputs.items()}
    if "prog" not in _CACHE:
        _CACHE["prog"] = build_full(S)
    P = _CACHE["prog"]
    rotC, rotS = build_rot(S)
    shared = {
        "cmat": build_cmat(), "cvec": build_cvec(inp), "rotC": rotC, "rotS": rotS, "pow2": build_pow2(), "gmask": build_gmask(),
        "ffn1_w_in": inp["ffn1_w_in"], "ffn1_w_out": inp["ffn1_w_out"], "ffn2_w_in": inp["ffn2_w_in"], "ffn2_w_out": inp["ffn2_w_out"],
        "w_in": inp["w_in"], "w_sw": np.ascontiguousarray(inp["w_in"][:, :, swap_cols()]), "w_small": np.ascontiguousarray(inp["w_in"][:, :, small_cols()]),
        "w_branch_a": inp["w_branch_a"], "w_branch_b": inp["w_branch_b"], "w_branch_c": inp["w_branch_c"], "w_out": inp["w_out"],
    }
    in_maps = []
    for b in range(NB):
        m = dict(shared)
        m["xT_in"] = np.ascontiguousarray(inp["x"][b].T)
        in_maps.append(m)
    res = run_bass_kernel_spmd(P.nc, in_maps, core_ids=list(range(NB)))
    out = np.stack([np.ascontiguousarray(r["outT"].T) for r in res.results], axis=0)
    return out.astype(np.float32)
```

```python
from contextlib import ExitStack
import numpy as np
import concourse.bass as bass
import concourse.mybir as mybir
from concourse.bass_utils import run_bass_kernel_spmd

F32 = mybir.dt.float32
BF16 = mybir.dt.bfloat16
ALU = mybir.AluOpType
AF = mybir.ActivationFunctionType
AX = mybir.AxisListType

D = 1024
DEPTH = 2
NB = 8
IN_SIZES = (512, 64, 64, 256, 64, 4, 1536, 8, 1536, 512, 4, 4, 3072)
OFF = np.concatenate([[0], np.cumsum(IN_SIZES)]).tolist()
(O_AQ, O_AK, O_AV, O_IQ, O_IK, O_IW, O_BQKV, O_BF, O_CQKV, O_CZ, O_CB, O_CA, O_G) = OFF[:13]
NEG = -32768.0


class Buf:
    __slots__ = ("t", "last_w", "readers", "name")

    def __init__(self, t=None, name=""):
        self.t = t
        self.last_w = None
        self.readers = {}
        self.name = name

    def __getitem__(self, k):
        return self.t[k]


class Eng:
    def __init__(self, name, handle, sem):
        self.name = name
        self.h = handle
        self.sem = sem
        self.count = 0
        self.seen = {}


class Ctx:
    SAME_ENGINE_SYNC = True
    RAW_ONLY_SAME_ENGINE = False

    def __init__(self, nc, n_dma_sems=10):
        self.nc = nc
        self.sems = {}
        self.eng = {}
        for nm, h in (("pe", nc.tensor), ("act", nc.scalar), ("dve", nc.vector),
                      ("pool", nc.gpsimd), ("sp", nc.sync)):
            self.sems["s_" + nm] = nc.alloc_semaphore("s_" + nm)
            self.eng[nm] = Eng(nm, h, "s_" + nm)
        self.dma_pool = {}
        for q in ("sp", "pool", "act"):
            lst = []
            for i in range(n_dma_sems):
                k = f"d_{q}{i}"
                self.sems[k] = nc.alloc_semaphore(k)
                lst.append([k, 0])
            self.dma_pool[q] = [lst, 0]
        self.n_instr = 0
        self.n_wait = 0
        self.uid = 0

    def sb(self, es, name, shape, dt):
        self.uid += 1
        nm = f"{name}_{self.uid}"
        return Buf(es.enter_context(self.nc.sbuf_tensor(nm, list(shape), dt)), nm)

    def _need(self, reads, writes, own=None):
        need = {}

        def add(ev, raw):
            if ev is None:
                return
            k, v = ev
            if k == own and not raw and self.RAW_ONLY_SAME_ENGINE:
                return
            if need.get(k, 0) < v:
                need[k] = v
        for b in reads:
            add(b.last_w, True)
        for b in writes:
            add(b.last_w, False)
            for k, v in b.readers.items():
                add((k, v), False)
        return need

    def _emit_waits(self, e, need):
        for k, v in need.items():
            if k == e.sem and (e.name == "pe" or not self.SAME_ENGINE_SYNC):
                continue
            if e.seen.get(k, 0) >= v:
                continue
            e.h.wait_ge(self.sems[k], v)
            e.seen[k] = v
            self.n_wait += 1

    def _record(self, ev, reads, writes):
        k, v = ev
        for b in writes:
            b.last_w = ev
            b.readers = {}
        for b in reads:
            if b.readers.get(k, 0) < v:
                b.readers[k] = v

    def op(self, en, fn, reads=(), writes=(), sig=True):
        e = self.eng[en]
        self._emit_waits(e, self._need(reads, writes, e.sem))
        ins = fn(e.h)
        self.n_instr += 1
        if sig:
            ins.then_inc(self.sems[e.sem], 1)
            e.count += 1
            ev = (e.sem, e.count)
        else:
            ev = (e.sem, e.count + 1)
        self._record(ev, reads, writes)
        return ins

    def dma(self, q, out, in_, reads=(), writes=(), **kw):
        e = self.eng[q]
        lst, idx = self.dma_pool[q]
        ent = lst[idx % len(lst)]
        self.dma_pool[q][1] = idx + 1
        need = self._need(reads, writes, None)
        if ent[1] > 0 and need.get(ent[0], 0) < ent[1]:
            need[ent[0]] = ent[1]
        self._emit_waits(e, need)
        ins = e.h.dma_start(out=out, in_=in_, **kw)
        ent[1] += 16
        ins.then_inc(self.sems[ent[0]], 16)
        self.n_instr += 1
        self._record((ent[0], ent[1]), reads, writes)
        return ins

    def barrier(self):
        for e in self.eng.values():
            need = {}
            for f in self.eng.values():
                if f is not e and f.count > 0:
                    need[f.sem] = f.count
            for q in self.dma_pool:
                for k, v in self.dma_pool[q][0]:
                    if v > 0:
                        need[k] = v
            self._emit_waits(e, need)


class Prog:
    def __init__(self, S, ext=None):
        self.S = S
        self.NT = S // 128
        self.NG = S // 512
        self.nc = bass.Bass("TRN2", target_bir_lowering=False)
        self.c = Ctx(self.nc)
        self.ext = ext or {}
        self.dram = {}
        self.dbuf = {}
        nc = self.nc
        self.psall = nc.alloc_psum_tensor("psall", [128, 8 * 512], F32)
        self.ps = [Buf(self.psall[:, i * 512:(i + 1) * 512], f"ps{i}") for i in range(8)]

    def dr(self, name, shape, dt, kind=None):
        if kind is None:
            kind = {"in": "ExternalInput", "out": "ExternalOutput"}.get(self.ext.get(name), "Internal")
        t = self.nc.dram_tensor(name, list(shape), dt, kind=kind)
        self.dram[name] = t.ap()
        return self.dram[name]

    def db(self, name, idx=0):
        k = (name, idx)
        if k not in self.dbuf:
            self.dbuf[k] = Buf(name=f"{name}{idx}")
        return self.dbuf[k]

    def setup_consts(self, es):
        c, nc = self.c, self.nc
        cm = self.dr("cmat", [7, 128, 128], F32, kind="ExternalInput")
        self.ident_b = c.sb(es, "identb", [128, 128], BF16)
        self.ones_b = c.sb(es, "onesb", [128, 128], BF16)
        self.ones_f = c.sb(es, "onesf", [128, 128], F32)
        self.triu_f = c.sb(es, "triuf", [128, 128], F32)
        self.ident_f = c.sb(es, "identf", [128, 128], F32)
        self.negm_f = c.sb(es, "negmf", [128, 128], F32)
        self.tri01_b = c.sb(es, "tri01b", [128, 128], BF16)
        cb = self.db("cmat")
        c.dma("pool", self.ident_b[:, :], cm[0], reads=[cb], writes=[self.ident_b])
        c.dma("pool", self.ones_b[:, :], cm[1], reads=[cb], writes=[self.ones_b])
        c.dma("sp", self.ones_f[:, :], cm[1], reads=[cb], writes=[self.ones_f])
        c.dma("sp", self.triu_f[:, :], cm[2], reads=[cb], writes=[self.triu_f])
        c.dma("sp", self.ident_f[:, :], cm[0], reads=[cb], writes=[self.ident_f])
        c.dma("sp", self.negm_f[:, :], cm[3], reads=[cb], writes=[self.negm_f])
        c.dma("pool", self.tri01_b[:, :], cm[2], reads=[cb], writes=[self.tri01_b])
        self.NCV = DEPTH * CV_PER_LAYER + 8
        cv = self.dr("cvec", [128, self.NCV], F32, kind="ExternalInput")
        self.cvec = c.sb(es, "cvec", [128, self.NCV], F32)
        c.dma("sp", self.cvec[:, :], cv[:, :], reads=[self.db("cvec")], writes=[self.cvec])

    def rmsnorm_group(self, xg, sq, hT_ap_fn, hT_buf, rstd, gcol, psb, nch=8, ncols=512):
        c = self.c
        c.op("act", lambda h: h.activation(out=sq[:, :, :], in_=xg[:, :, :], func=AF.Square), reads=[xg], writes=[sq])
        for k in range(nch):
            c.op("pe", lambda h, k=k: h.matmul(psb[:, :ncols], lhsT=self.ones_b[:, :], rhs=sq[:, k, :], start=(k == 0), stop=(k == nch - 1)),
                 reads=[self.ones_b, sq], writes=[psb], sig=(k == nch - 1))
        c.op("dve", lambda h: h.tensor_scalar(out=rstd[:, :], in0=psb[:, :ncols], scalar1=1.0 / (nch * 128), scalar2=1e-6, op0=ALU.mult, op1=ALU.add),
             reads=[psb], writes=[rstd])
        c.op("act", lambda h: h.activation(out=rstd[:, :], in_=rstd[:, :], func=AF.Ln), reads=[rstd], writes=[rstd])
        c.op("act", lambda h: h.activation(out=rstd[:, :], in_=rstd[:, :], func=AF.Exp, scale=-0.5), reads=[rstd], writes=[rstd])
        for k in range(nch):
            c.op("dve", lambda h, k=k: h.scalar_tensor_tensor(out=hT_ap_fn(k), in0=xg[:, k, :], scalar=self.cvec[:, gcol + k:gcol + k + 1],
                                                            in1=rstd[:, :], op0=ALU.mult, op1=ALU.mult),
                 reads=[xg, rstd, self.cvec], writes=[hT_buf])

    def ffn_phase(self, xsrc, xdst, w_in, w_out, gcol):
        c, S = self.c, self.S
        with ExitStack() as es:
            win = c.sb(es, "win", [128, 8, 4096], BF16)
            wout = c.sb(es, "wout", [128, 16, 1024], BF16)
            xgs = [c.sb(es, "xg", [128, 8, 512], F32) for _ in range(2)]
            sq = c.sb(es, "sq", [128, 8, 512], BF16)
            hT = c.sb(es, "hT", [128, 8, 512], BF16)
            act = c.sb(es, "actT", [128, 16, 512], BF16)
            rstd = c.sb(es, "rstd", [128, 512], F32)
            sgs = [c.sb(es, "sg", [128, 512], F32) for _ in range(2)]
            wb = self.db("w")
            for k in range(8):
                c.dma("pool", win[:, k, :], w_in[k * 128:(k + 1) * 128, :], reads=[wb], writes=[win])
            for k in range(16):
                c.dma("pool", wout[:, k, :], w_out[k * 128:(k + 1) * 128, :], reads=[wb], writes=[wout])
            xs3 = xsrc.rearrange("(c p) t -> p c t", p=128)
            xd3 = xdst.rearrange("(c p) t -> p c t", p=128)
            ps = self.ps
            def ldx(g):
                c.dma("sp", xgs[g % 2][:, :, :], xs3[:, :, g * 512:(g + 1) * 512], reads=[self.db(xsrc.name, g)], writes=[xgs[g % 2]])
            ldx(0)
            for g in range(self.NG):
                xg = xgs[g % 2]
                tsl = slice(g * 512, (g + 1) * 512)
                if g + 1 < self.NG:
                    ldx(g + 1)
                self.rmsnorm_group(xg, sq, lambda k: hT[:, k, :], hT, rstd, gcol, ps[0])
                for j in range(16):
                    pg, pu = ps[1 + 2 * (j % 2)], ps[2 + 2 * (j % 2)]
                    for k in range(8):
                        c.op("pe", lambda h, k=k, j=j, pg=pg: h.matmul(pg[:, :], lhsT=win[:, k, j * 128:(j + 1) * 128], rhs=hT[:, k, :], start=(k == 0), stop=(k == 7)),
                             reads=[win, hT], writes=[pg], sig=(k == 7))
                    for k in range(8):
                        c.op("pe", lambda h, k=k, j=j, pu=pu: h.matmul(pu[:, :], lhsT=win[:, k, 2048 + j * 128:2048 + (j + 1) * 128], rhs=hT[:, k, :], start=(k == 0), stop=(k == 7)),
                             reads=[win, hT], writes=[pu], sig=(k == 7))
                    sg = sgs[j % 2]
                    c.op("act", lambda h, pg=pg, sg=sg: h.activation(out=sg[:, :], in_=pg[:, :], func=AF.Silu), reads=[pg], writes=[sg])
                    c.op("dve", lambda h, pu=pu, sg=sg, j=j: h.tensor_tensor(out=act[:, j, :], in0=sg[:, :], in1=pu[:, :], op=ALU.mult), reads=[sg, pu], writes=[act])
                for d in range(8):
                    po = ps[5 + d % 2]
                    for j in range(16):
                        c.op("pe", lambda h, d=d, j=j, po=po: h.matmul(po[:, :], lhsT=wout[:, j, d * 128:(d + 1) * 128], rhs=act[:, j, :], start=(j == 0), stop=(j == 15)),
                             reads=[wout, act], writes=[po], sig=(j == 15))
                    c.op("dve", lambda h, d=d, po=po, xg=xg: h.scalar_tensor_tensor(out=xg[:, d, :], in0=po[:, :], scalar=0.5, in1=xg[:, d, :], op0=ALU.mult, op1=ALU.add),
                         reads=[po, xg], writes=[xg])
                c.dma("sp", xd3[:, :, tsl], xg[:, :, :], reads=[xg], writes=[self.db(xdst.name, g)])
            c.barrier()


CV_FFN1, CV_MIX, CV_FFN2, CV_BG, CV_CONV, CV_DN, CV_BF, CV_ALOG, CV_DT = 0, 8, 16, 24, 48, 96, 97, 105, 109
CV_PER_LAYER = 113


def build_cvec(inp):
    cv = np.zeros((128, DEPTH * CV_PER_LAYER + 8), np.float32)
    for l in range(DEPTH):
        b = l * CV_PER_LAYER
        cv[:, b + CV_FFN1:b + CV_FFN1 + 8] = inp["ffn1_norm"][l].reshape(8, 128).T
        cv[:, b + CV_MIX:b + CV_MIX + 8] = inp["mix_norm"][l].reshape(8, 128).T
        cv[:, b + CV_FFN2:b + CV_FFN2 + 8] = inp["ffn2_norm"][l].reshape(8, 128).T
        cv[:, b + CV_BG:b + CV_BG + 24] = inp["b_gate"][l].reshape(24, 128).T
        cv[:, b + CV_CONV:b + CV_CONV + 48] = inp["conv_w"][l].reshape(4, 12, 128).transpose(2, 1, 0).reshape(128, 48)
        cv[:, b + CV_DN] = inp["delta_norm"][l]
        cv[:, b + CV_BF:b + CV_BF + 8] = inp["b_forget"][l][None, :]
        cv[:, b + CV_ALOG:b + CV_ALOG + 4] = inp["a_log"][l][None, :]
        cv[:, b + CV_DT:b + CV_DT + 4] = inp["dt_bias"][l][None, :]
    cv[:, DEPTH * CV_PER_LAYER:] = inp["final_norm"].reshape(8, 128).T
    return cv


def build_cmat():
    i = np.arange(128)
    ident = np.eye(128, dtype=np.float32)
    ones = np.ones((128, 128), np.float32)
    triu = (i[:, None] <= i[None, :]).astype(np.float32)
    negm = np.where(i[None, :] >= i[:, None], 0.0, -1e4).astype(np.float32)
    negc = np.where(i[None, :] <= i[:, None], 0.0, -1e30).astype(np.float32)
    z = np.zeros((128, 128), np.float32)
    strictu = (i[:, None] < i[None, :]).astype(np.float32)
    return np.stack([ident, ones, triu, negm, negc, strictu, z])


def build_pow2(nit=26):
    return np.tile((0.5 ** np.arange(1, nit + 1)).astype(np.float32)[None, :], (128, 1))


def swap_cols():
    idx = []
    for base, n in ((O_AQ, 512), (O_AK, 64), (O_IQ, 256), (O_IK, 64)):
        for j in range(n):
            d = j % 64
            hb = base + (j // 64) * 64
            if d < 8:
                idx.append(hb + d + 8)
            elif d < 16:
                idx.append(hb + d - 8)
            else:
                idx.append(hb + d)
    return np.array(idx)


def small_cols():
    return np.concatenate([np.arange(O_AV, O_AV + 64), np.arange(O_IW, O_IW + 4), np.arange(O_BF, O_BF + 8),
                           np.arange(O_CB, O_CB + 4), np.arange(O_CA, O_CA + 4)])


def build_rot(S):
    pos = np.arange(S, dtype=np.float32)
    inv = np.power(np.float32(500000.0), -np.arange(0, 16, 2, dtype=np.float32) / np.float32(16)).astype(np.float32)
    ang = (pos[:, None] * inv[None, :]).astype(np.float32)
    cos, sin = np.cos(ang).astype(np.float32), np.sin(ang).astype(np.float32)
    C = np.ones((128, S), np.float32)
    Sg = np.zeros((128, S), np.float32)
    for p in range(128):
        d = p % 64
        if d < 8:
            C[p] = cos[:, d]
            Sg[p] = -sin[:, d]
        elif d < 16:
            C[p] = cos[:, d - 8]
            Sg[p] = sin[:, d - 8]
    return C, Sg


class ProgM1(Prog):
    def declare_scratch(self):
        S = self.S
        d = self.dr
        self.qaT = d("qaT", [512, S], BF16); self.kaT = d("kaT", [64, S], BF16)
        self.qiT = d("qiT", [256, S], BF16); self.kiT = d("kiT", [64, S], BF16)
        self.vA = d("vA", [S, 64], BF16)
        self.qbT = d("qbT", [512, S], BF16); self.kbT = d("kbT", [512, S], BF16); self.vB = d("vB", [S, 512], BF16)
        self.qcT = d("qcT", [512, S], BF16); self.kcT = d("kcT", [512, S], BF16)
        self.kC = d("kC", [S, 512], BF16); self.vC = d("vC", [S, 512], BF16)
        self.szT = d("szT", [512, S], BF16)
        self.smallD = d("smallD", [S, 24], F32)
        self.gT = d("gT", [3072, S], BF16)
        self.yT = d("yT", [3, 512, S], BF16)

    def rot(self):
        self._rot = (getattr(self, "_rot", -1) + 1) % 8
        return self.ps[self._rot]

    def m1_phase(self, xT, w_in, w_sw, w_small, cvb):
        c, S, NG = self.c, self.S, self.NG
        rotC = self.dram["rotC"]; rotS = self.dram["rotS"]
        with ExitStack() as es:
            hT = c.sb(es, "hTall", [128, 8, S], BF16)
            wts = [c.sb(es, "wt", [128, 8, 512], BF16) for _ in range(3)]
            wti = [0]
            wb = self.db("w")

            def load_w(src_list):
                wt = wts[wti[0] % 3]
                wti[0] += 1
                for (ap, c0, n) in src_list:
                    c.dma("pool", wt[:, :, c0:c0 + n], ap.rearrange("(k p) n -> p k n", p=128), reads=[wb], writes=[wt])
                return wt

            def fm(wt, c0, M, g, psb, rows0=0):
                tsl = slice(g * 512, (g + 1) * 512)
                for k in range(8):
                    c.op("pe", lambda h, k=k: h.matmul(psb[rows0:rows0 + M, :], lhsT=wt[:, k, c0:c0 + M], rhs=hT[:, k, tsl], start=(k == 0), stop=(k == 7)),
                         reads=[wt, hT], writes=[psb], sig=(k == 7))

            with ExitStack() as es2:
                xgs = [c.sb(es2, "xg", [128, 8, 512], F32) for _ in range(2)]
                sq = c.sb(es2, "sq", [128, 8, 512], BF16)
                rstd = c.sb(es2, "rstd", [128, 512], F32)
                x3 = xT.rearrange("(c p) t -> p c t", p=128)
                for g in range(NG):
                    xg = xgs[g % 2]
                    tsl = slice(g * 512, (g + 1) * 512)
                    c.dma("sp", xg[:, :, :], x3[:, :, tsl], reads=[self.db(xT.name, g)], writes=[xg])
                    self.rmsnorm_group(xg, sq, lambda k, tsl=tsl: hT[:, k, tsl], hT, rstd, cvb + CV_MIX, self.rot())
                c.barrier()

            with ExitStack() as es2:
                stg = [c.sb(es2, "stg", [128, 4, 512], BF16) for _ in range(2)]
                stgi = [0]
                t1s = [c.sb(es2, "t1", [128, 512], F32) for _ in range(2)]
                t2s = [c.sb(es2, "t2", [128, 512], F32) for _ in range(2)]
                rc = [c.sb(es2, "rc", [128, 512], F32) for _ in range(2)]
                rs = [c.sb(es2, "rs", [128, 512], F32) for _ in range(2)]

                def nstg():
                    stgi[0] += 1
                    return stg[stgi[0] % 2]

                def load_rot(g):
                    tsl = slice(g * 512, (g + 1) * 512)
                    c.dma("sp", rc[g % 2][:, :], rotC[:, tsl], reads=[self.db("rot")], writes=[rc[g % 2]])
                    c.dma("sp", rs[g % 2][:, :], rotS[:, tsl], reads=[self.db("rot")], writes=[rs[g % 2]])

                def rotary(pn, psw, g, out_ap, out_buf, i):
                    t1, t2 = t1s[i % 2], t2s[i % 2]
                    c.op("dve", lambda h: h.tensor_tensor(out=t1[:, :], in0=pn[:, :], in1=rc[g % 2][:, :], op=ALU.mult), reads=[pn, rc[g % 2]], writes=[t1])
                    c.op("dve", lambda h: h.tensor_tensor(out=t2[:, :], in0=psw[:, :], in1=rs[g % 2][:, :], op=ALU.mult), reads=[psw, rs[g % 2]], writes=[t2])
                    c.op("pool", lambda h: h.tensor_tensor(out=out_ap, in0=t1[:, :], in1=t2[:, :], op=ALU.add), reads=[t1, t2], writes=[out_buf])

                wn = load_w([(w_in[:, O_AQ:O_AQ + 512], 0, 512)])
                ws = load_w([(w_sw[:, 0:512], 0, 512)])
                for g in range(NG):
                    tsl = slice(g * 512, (g + 1) * 512)
                    load_rot(g)
                    so = nstg()
                    for ch in range(4):
                        pn, psw = self.rot(), self.rot()
                        fm(wn, ch * 128, 128, g, pn)
                        fm(ws, ch * 128, 128, g, psw)
                        rotary(pn, psw, g, so[:, ch, :], so, ch)
                    c.dma("sp", self.qaT.rearrange("(c p) t -> p c t", p=128)[:, :, tsl], so[:, :, :], reads=[so], writes=[self.db("qaT", g)])
                wn = load_w([(w_in[:, O_IQ:O_IQ + 256], 0, 256), (w_in[:, O_AK:O_AK + 64], 256, 64), (w_in[:, O_IK:O_IK + 64], 320, 64)])
                ws = load_w([(w_sw[:, 576:832], 0, 256), (w_sw[:, 512:576], 256, 64), (w_sw[:, 832:896], 320, 64)])
                for g in range(NG):
                    tsl = slice(g * 512, (g + 1) * 512)
                    load_rot(g)
                    so = nstg()
                    for ch in range(3):
                        pn, psw = self.rot(), self.rot()
                        fm(wn, ch * 128, 128, g, pn)
                        fm(ws, ch * 128, 128, g, psw)
                        rotary(pn, psw, g, so[:, ch, :], so, ch)
                    c.dma("sp", self.qiT.rearrange("(c p) t -> p c t", p=128)[:, :, tsl], so[:, 0:2, :], reads=[so], writes=[self.db("qiT", g)])
                    c.dma("sp", self.kaT[:, tsl], so[0:64, 2, :], reads=[so], writes=[self.db("kaT", g)])
                    c.dma("sp", self.kiT[:, tsl], so[64:128, 2, :], reads=[so], writes=[self.db("kiT", g)])
                for (c0, dst) in ((O_BQKV, self.qbT), (O_BQKV + 512, self.kbT)):
                    wn = load_w([(w_in[:, c0:c0 + 512], 0, 512)])
                    for g in range(NG):
                        tsl = slice(g * 512, (g + 1) * 512)
                        so = nstg()
                        for ch in range(4):
                            pn = self.rot()
                            fm(wn, ch * 128, 128, g, pn)
                            c.op("act", lambda h, pn=pn, ch=ch, so=so: h.activation(out=so[:, ch, :], in_=pn[:, :], func=AF.Copy), reads=[pn], writes=[so])
                        c.dma("sp", dst.rearrange("(c p) t -> p c t", p=128)[:, :, tsl], so[:, :, :], reads=[so], writes=[self.db(dst.name, g)])
                wn = load_w([(w_in[:, O_BQKV + 1024:O_BQKV + 1536], 0, 512)])
                for g in range(NG):
                    so = nstg()
                    for tt in range(4):
                        pn = self.rot()
                        t0 = g * 512 + tt * 128
                        for k in range(8):
                            c.op("pe", lambda h, k=k, pn=pn, t0=t0: h.matmul(pn[:, :], lhsT=hT[:, k, t0:t0 + 128], rhs=wn[:, k, :], start=(k == 0), stop=(k == 7)),
                                 reads=[wn, hT], writes=[pn], sig=(k == 7))
                        c.op("act", lambda h, pn=pn, tt=tt, so=so: h.activation(out=so[:, tt, :], in_=pn[:, :], func=AF.Copy), reads=[pn], writes=[so])
                    c.dma("sp", self.vB.rearrange("(n p) d -> p n d", p=128)[:, g * 4:(g + 1) * 4, :], so[:, :, :], reads=[so], writes=[self.db("vB", g)])
                with ExitStack() as es3:
                    xcs = [c.sb(es3, "xc", [128, 515], F32) for _ in range(4)]
                    accs = [c.sb(es3, "acc", [128, 512], F32) for _ in range(4)]
                    sls = [c.sb(es3, "sl", [128, 512], F32) for _ in range(4)]
                    sqb = [c.sb(es3, "sqb", [128, 512], BF16) for _ in range(4)]
                    rr = [c.sb(es3, "rr", [128, 512], F32) for _ in range(4)]
                    tok = [c.sb(es3, "tok", [128, 4, 512], BF16) for _ in range(2)]
                    for sec, (dstT, dstTok) in enumerate(((self.qcT, None), (self.kcT, self.kC), (None, self.vC))):
                        c0 = O_CQKV + sec * 512
                        wn = load_w([(w_in[:, c0:c0 + 512], 0, 512)])
                        for j in range(4):
                            c.op("pool", lambda h, j=j: h.memset(xcs[j][:, 0:3], 0.0), writes=[xcs[j]])
                        for g in range(NG):
                            tsl = slice(g * 512, (g + 1) * 512)
                            so = nstg()
                            for j in range(4):
                                ch = sec * 4 + j
                                pn = self.rot()
                                fm(wn, j * 128, 128, g, pn)
                                xc = xcs[j]
                                acc, sl = accs[j % 4], sls[j % 4]
                                wc = cvb + CV_CONV + ch * 4
                                c.op("act", lambda h, pn=pn, xc=xc: h.activation(out=xc[:, 3:515], in_=pn[:, :], func=AF.Copy), reads=[pn], writes=[xc])
                                c.op("dve", lambda h, xc=xc, acc=acc, wc=wc: h.tensor_scalar(out=acc[:, :], in0=xc[:, 3:515], scalar1=self.cvec[:, wc + 3:wc + 4], scalar2=None, op0=ALU.mult),
                                     reads=[xc, self.cvec], writes=[acc])
                                for tap in (2, 1, 0):
                                    c.op("dve", lambda h, xc=xc, acc=acc, wc=wc, tap=tap: h.scalar_tensor_tensor(out=acc[:, :], in0=xc[:, tap:tap + 512], scalar=self.cvec[:, wc + tap:wc + tap + 1],
                                                                                                         in1=acc[:, :], op0=ALU.mult, op1=ALU.add),
                                         reads=[xc, acc, self.cvec], writes=[acc])
                                c.op("act", lambda h, xc=xc: h.activation(out=xc[:, 0:3], in_=xc[:, 512:515], func=AF.Copy), reads=[xc], writes=[xc])
                                if sec == 2:
                                    c.op("act", lambda h, acc=acc, so=so, j=j: h.activation(out=so[:, j, :], in_=acc[:, :], func=AF.Silu), reads=[acc], writes=[so])
                                else:
                                    c.op("act", lambda h, acc=acc, sl=sl: h.activation(out=sl[:, :], in_=acc[:, :], func=AF.Silu), reads=[acc], writes=[sl])
                                    sb_, r_ = sqb[j % 4], rr[j % 4]
                                    c.op("act", lambda h, sl=sl, sb_=sb_: h.activation(out=sb_[:, :], in_=sl[:, :], func=AF.Square), reads=[sl], writes=[sb_])
                                    p2 = self.rot()
                                    c.op("pe", lambda h, p2=p2, sb_=sb_: h.matmul(p2[:, :], lhsT=self.ones_b[:, :], rhs=sb_[:, :], start=True, stop=True), reads=[self.ones_b, sb_], writes=[p2])
                                    c.op("dve", lambda h, p2=p2, r_=r_: h.tensor_scalar(out=r_[:, :], in0=p2[:, :], scalar1=1e-6, scalar2=None, op0=ALU.add), reads=[p2], writes=[r_])
                                    c.op("act", lambda h, r_=r_: h.activation(out=r_[:, :], in_=r_[:, :], func=AF.Ln), reads=[r_], writes=[r_])
                                    c.op("act", lambda h, r_=r_: h.activation(out=r_[:, :], in_=r_[:, :], func=AF.Exp, scale=-0.5), reads=[r_], writes=[r_])
                                    qs = float(128 ** -0.5) if sec == 0 else 1.0
                                    c.op("dve", lambda h, sl=sl, r_=r_, so=so, j=j, qs=qs: h.scalar_tensor_tensor(out=so[:, j, :], in0=sl[:, :], scalar=qs, in1=r_[:, :], op0=ALU.mult, op1=ALU.mult),
                                         reads=[sl, r_], writes=[so])
                            if dstT is not None:
                                c.dma("sp", dstT.rearrange("(c p) t -> p c t", p=128)[:, :, tsl], so[:, :, :], reads=[so], writes=[self.db(dstT.name, g)])
                            if dstTok is not None:
                                tk = tok[g % 2]
                                for tt in range(4):
                                    pt = self.rot()
                                    for j in range(4):
                                        c.op("pe", lambda h, pt=pt, j=j, tt=tt, so=so: h.matmul(pt[:, j * 128:(j + 1) * 128], lhsT=so[:, j, tt * 128:(tt + 1) * 128], rhs=self.ident_b[:, :], start=True, stop=True),
                                             reads=[so, self.ident_b], writes=[pt], sig=(j == 3))
                                    c.op("act", lambda h, pt=pt, tk=tk, tt=tt: h.activation(out=tk[:, tt, :], in_=pt[:, :], func=AF.Copy), reads=[pt], writes=[tk])
                                c.dma("sp", dstTok.rearrange("(n p) d -> p n d", p=128)[:, g * 4:(g + 1) * 4, :], tk[:, :, :], reads=[tk], writes=[self.db(dstTok.name, g)])
                wn = load_w([(w_in[:, O_CZ:O_CZ + 512], 0, 512)])
                for g in range(NG):
                    tsl = slice(g * 512, (g + 1) * 512)
                    so = nstg()
                    for ch in range(4):
                        pn = self.rot()
                        fm(wn, ch * 128, 128, g, pn)
                        c.op("act", lambda h, pn=pn, ch=ch, so=so: h.activation(out=so[:, ch, :], in_=pn[:, :], func=AF.Silu), reads=[pn], writes=[so])
                    c.dma("sp", self.szT.rearrange("(c p) t -> p c t", p=128)[:, :, tsl], so[:, :, :], reads=[so], writes=[self.db("szT", g)])
                for sec in range(6):
                    wn = load_w([(w_in[:, O_G + sec * 512:O_G + (sec + 1) * 512], 0, 512)])
                    for g in range(NG):
                        tsl = slice(g * 512, (g + 1) * 512)
                        so = nstg()
                        for ch in range(4):
                            pn = self.rot()
                            fm(wn, ch * 128, 128, g, pn)
                            bc = cvb + CV_BG + sec * 4 + ch
                            c.op("act", lambda h, pn=pn, ch=ch, so=so, bc=bc: h.activation(out=so[:, ch, :], in_=pn[:, :], func=AF.Sigmoid, bias=self.cvec[:, bc:bc + 1]),
                                 reads=[pn, self.cvec], writes=[so])
                        c.dma("sp", self.gT.rearrange("(c p) t -> p c t", p=128)[:, sec * 4:(sec + 1) * 4, tsl], so[:, :, :], reads=[so], writes=[self.db("gT", sec * 100 + g)])
                with ExitStack() as es3:
                    wsm = c.sb(es3, "wsm", [128, 8, 84], BF16)
                    c.dma("pool", wsm[:, :, :], w_small.rearrange("(k p) n -> p k n", p=128), reads=[wb], writes=[wsm])
                    negA = c.sb(es3, "negA", [128, 4], F32)
                    c.op("act", lambda h: h.activation(out=negA[:, :], in_=self.cvec[:, cvb + CV_ALOG:cvb + CV_ALOG + 4], func=AF.Exp), reads=[self.cvec], writes=[negA])
                    c.op("dve", lambda h: h.tensor_scalar(out=negA[:, :], in0=negA[:, :], scalar1=-1.0, scalar2=None, op0=ALU.mult), reads=[negA], writes=[negA])
                    sms = [c.sb(es3, "sm", [128, 4, 24], F32) for _ in range(2)]
                    vas = [c.sb(es3, "vas", [128, 4, 64], BF16) for _ in range(2)]
                    tmp = [c.sb(es3, "tmps", [128, 16], F32) for _ in range(2)]
                    for g in range(NG):
                        sm, va = sms[g % 2], vas[g % 2]
                        for tt in range(4):
                            pn = self.rot()
                            t0 = g * 512 + tt * 128
                            tp = tmp[tt % 2]
                            for k in range(8):
                                c.op("pe", lambda h, k=k, pn=pn, t0=t0: h.matmul(pn[:, 0:84], lhsT=hT[:, k, t0:t0 + 128], rhs=wsm[:, k, :], start=(k == 0), stop=(k == 7)),
                                     reads=[wsm, hT], writes=[pn], sig=(k == 7))
                            c.op("act", lambda h, pn=pn, va=va, tt=tt: h.activation(out=va[:, tt, :], in_=pn[:, 0:64], func=AF.Copy), reads=[pn], writes=[va])
                            c.op("dve", lambda h, pn=pn, sm=sm, tt=tt: h.tensor_scalar(out=sm[:, tt, 0:4], in0=pn[:, 64:68], scalar1=1.0 / 16.0, scalar2=None, op0=ALU.mult), reads=[pn], writes=[sm])
                            c.op("dve", lambda h, pn=pn, tp=tp: h.tensor_tensor(out=tp[:, 0:8], in0=pn[:, 68:76], in1=self.cvec[:, cvb + CV_BF:cvb + CV_BF + 8], op=ALU.add), reads=[pn, self.cvec], writes=[tp])
                            c.op("dve", lambda h, pn=pn, tp=tp: h.tensor_tensor(out=tp[:, 8:12], in0=pn[:, 80:84], in1=self.cvec[:, cvb + CV_DT:cvb + CV_DT + 4], op=ALU.add), reads=[pn, self.cvec, tp], writes=[tp])
                            c.op("act", lambda h, tp=tp: h.activation(out=tp[:, 0:8], in_=tp[:, 0:8], func=AF.Exp, scale=-1.0), reads=[tp], writes=[tp])
                            c.op("act", lambda h, tp=tp: h.activation(out=tp[:, 8:12], in_=tp[:, 8:12], func=AF.Exp), reads=[tp], writes=[tp])
                            c.op("act", lambda h, tp=tp: h.activation(out=tp[:, 0:12], in_=tp[:, 0:12], func=AF.Ln, bias=1.0), reads=[tp], writes=[tp])
                            c.op("dve", lambda h, tp=tp, sm=sm, tt=tt: h.tensor_scalar(out=sm[:, tt, 4:12], in0=tp[:, 0:8], scalar1=-1.0, scalar2=None, op0=ALU.mult), reads=[tp], writes=[sm])
                            c.op("dve", lambda h, tp=tp, sm=sm, tt=tt: h.tensor_tensor(out=sm[:, tt, 16:20], in0=tp[:, 8:12], in1=negA[:, :], op=ALU.mult), reads=[tp, negA, sm], writes=[sm])
                            c.op("act", lambda h, pn=pn, sm=sm, tt=tt: h.activation(out=sm[:, tt, 12:16], in_=pn[:, 76:80], func=AF.Sigmoid), reads=[pn, sm], writes=[sm])
                        c.dma("sp", self.smallD.rearrange("(n p) c -> p n c", p=128)[:, g * 4:(g + 1) * 4, 0:20], sm[:, :, 0:20], reads=[sm], writes=[self.db("smallD", g)])
                        c.dma("sp", self.vA.rearrange("(n p) d -> p n d", p=128)[:, g * 4:(g + 1) * 4, :], va[:, :, :], reads=[va], writes=[self.db("vA", g)])
                c.barrier()


class ProgAB(ProgM1):
    def rotset(self, key, banks):
        d = self.__dict__.setdefault("_rs", {})
        d[key] = (d.get(key, -1) + 1) % len(banks)
        return self.ps[banks[d[key]]]

    def _b_setup(self, es, lbanks, pbanks):
        c, S, NT, NG = self.c, self.S, self.NT, self.NG
        qbh = [c.sb(es, "qbh", [128, S], BF16) for _ in range(2)]
        kbh = [c.sb(es, "kbh", [128, S], BF16) for _ in range(2)]
        vext = [c.sb(es, "vext", [128, NT, 128], BF16) for _ in range(2)]
        lf = c.sb(es, "lf", [128, NT, 8], F32)
        lfacc = c.sb(es, "lfacc", [128, NT + 1, 8], F32)
        csb = c.sb(es, "csb", [128, NT, 8], F32)
        carry = c.sb(es, "carry", [128, NT, 8], F32)
        npair = NT * (NT + 1) // 2
        bias = c.sb(es, "biasall", [128, npair, 8], F32)
        pts = [c.sb(es, "pt", [128, 512], BF16) for _ in range(4)]
        ptq = [[Buf(name=f"ptq{a}_{b}") for b in range(4)] for a in range(4)]
        rsum = [c.sb(es, "rsum", [64, 512], F32) for _ in range(2)]
        nums = [c.sb(es, "bnum", [64, 512], F32) for _ in range(2)]
        yst = [c.sb(es, "yst", [64, 512], BF16) for _ in range(2)]
        allg = lambda nm: [self.db(nm, g) for g in range(NG)]
        c.dma("sp", lf[:, :, :], self.smallD.rearrange("(n p) c -> p n c", p=128)[:, :, 4:12], reads=allg("smallD"), writes=[lf])
        for v in vext:
            c.op("pool", lambda h, v=v: h.memset(v[:, :, 64:128], 1.0), writes=[v])
        c.op("pool", lambda h: h.memset(lfacc[:, 0, :], 0.0), writes=[lfacc])
        for n in range(NT):
            c.op("dve", lambda h, n=n: h.tensor_tensor(out=lfacc[:, n + 1, :], in0=lfacc[:, n, :], in1=lf[:, n, :], op=ALU.add), reads=[lfacc, lf], writes=[lfacc])
        for n in range(NT):
            pb = self.rotset("bpl", lbanks)
            c.op("pe", lambda h, n=n, pb=pb: h.matmul(pb[:, 0:8], lhsT=self.triu_f[:, :], rhs=lf[:, n, :], start=True, stop=False), reads=[self.triu_f, lf], writes=[pb], sig=False)
            c.op("pe", lambda h, n=n, pb=pb: h.matmul(pb[:, 0:8], lhsT=self.ones_f[:, :], rhs=lfacc[:, n, :], start=False, stop=True), reads=[self.ones_f, lfacc], writes=[pb])
            c.op("act", lambda h, n=n, pb=pb: h.activation(out=csb[:, n, :], in_=pb[:, 0:8], func=AF.Copy), reads=[pb], writes=[csb])
            pb = self.rotset("bpl", lbanks)
            c.op("pe", lambda h, n=n, pb=pb: h.matmul(pb[:, 0:8], lhsT=self.ones_f[:, :], rhs=lfacc[:, n, :], start=True, stop=True), reads=[self.ones_f, lfacc], writes=[pb])
            c.op("act", lambda h, n=n, pb=pb: h.activation(out=carry[:, n, :], in_=pb[:, 0:8], func=AF.Copy), reads=[pb], writes=[carry])
        pidx = {}
        pi = 0
        for i in range(NT):
            for j in range(i + 1):
                pidx[(i, j)] = pi
                c.op("pool", lambda h, i=i, j=j, pi=pi: h.tensor_tensor(out=bias[:, pi, :], in0=carry[:, i, :], in1=csb[:, j, :], op=ALU.subtract), reads=[carry, csb], writes=[bias])
                pi += 1
        vB3 = self.vB.rearrange("(n p) d -> p n d", p=128)
        yb = self.yT[1]
        steps = [(hh, g, j) for hh in range(8) for g in range(NG) for j in range(4 * g + 4)]
        pls = {}
        loaded = set()

        def load_pair(hp):
            if hp in loaded or hp >= 4:
                return
            loaded.add(hp)
            c.dma("pool", qbh[hp % 2][:, :], self.qbT[hp * 128:(hp + 1) * 128, :], reads=allg("qbT"), writes=[qbh[hp % 2]])
            c.dma("pool", kbh[hp % 2][:, :], self.kbT[hp * 128:(hp + 1) * 128, :], reads=allg("kbT"), writes=[kbh[hp % 2]])

        def logits(k):
            hh, g, j = steps[k]
            hb, hp = (hh % 2) * 64, hh // 2
            load_pair(hp)
            qb, kb = qbh[hp % 2], kbh[hp % 2]
            col0 = max(j - 4 * g, 0) * 128
            pl = self.rotset("bpl", lbanks)
            pls[k] = pl
            c.op("pe", lambda h: h.matmul(pl[:, col0:512], lhsT=kb[hb:hb + 64, j * 128:(j + 1) * 128],
                                          rhs=qb[hb:hb + 64, g * 512 + col0:(g + 1) * 512], start=True, stop=True),
                 reads=[kb, qb], writes=[pl])

        def gen():
            po = None
            logits(0)
            for k, (hh, g, j) in enumerate(steps):
                ve = vext[hh % 2]
                if g == 0 and j == 0:
                    c.dma("pool", ve[:, :, 0:64], vB3[:, :, hh * 64:(hh + 1) * 64], reads=allg("vB"), writes=[ve])
                if j == 0:
                    po = self.rotset("bpo", pbanks)
                if k + 1 < len(steps):
                    logits(k + 1)
                nj = 4 * g + 4
                r = j - 4 * g
                col0 = max(r, 0) * 128
                pl = pls.pop(k)
                pt = pts[j % 4]
                for qq in range(max(r, 0), 4):
                    pi = pidx[(4 * g + qq, j)]
                    qs_ = slice(qq * 128, (qq + 1) * 128)
                    c.op("act", lambda h, pl=pl, pt=pt, qs_=qs_, pi=pi, hh=hh: h.activation(out=pt[:, qs_], in_=pl[:, qs_], func=AF.Exp, scale=0.125, bias=bias[:, pi, hh:hh + 1]),
                         reads=[pl, bias], writes=[ptq[j % 4][qq]])
                if r >= 0:
                    c.op("pool", lambda h, pt=pt, col0=col0: h.tensor_tensor(out=pt[:, col0:col0 + 128], in0=pt[:, col0:col0 + 128], in1=self.tri01_b[:, :], op=ALU.mult),
                         reads=[ptq[j % 4][r], self.tri01_b], writes=[ptq[j % 4][r]])
                c.op("pe", lambda h, po=po, pt=pt, j=j, col0=col0, nj=nj, ve=ve: h.matmul(po[:, col0:512], lhsT=ve[:, j, :], rhs=pt[:, col0:512], start=(j == 0), stop=(j == nj - 1)),
                     reads=[ve] + ptq[j % 4][max(r, 0):4], writes=[po], sig=(j == nj - 1))
                if j == nj - 1:
                    rs_, ys_, nm_ = rsum[g % 2], yst[g % 2], nums[g % 2]
                    c.op("act", lambda h, po=po, rs_=rs_: h.activation(out=rs_[:, :], in_=po[64:128, :], func=AF.Copy), reads=[po], writes=[rs_])
                    c.op("act", lambda h, po=po, nm_=nm_: h.activation(out=nm_[:, :], in_=po[0:64, :], func=AF.Copy), reads=[po], writes=[nm_])
                    c.op("act", lambda h, rs_=rs_: h.activation(out=rs_[:, :], in_=rs_[:, :], func=AF.Ln), reads=[rs_], writes=[rs_])
                    c.op("act", lambda h, rs_=rs_: h.activation(out=rs_[:, :], in_=rs_[:, :], func=AF.Exp, scale=-1.0), reads=[rs_], writes=[rs_])
                    c.op("pool", lambda h, nm_=nm_, rs_=rs_, ys_=ys_: h.tensor_tensor(out=ys_[:, :], in0=nm_[:, :], in1=rs_[:, :], op=ALU.mult), reads=[nm_, rs_], writes=[ys_])
                    c.dma("pool", yb[hh * 64:(hh + 1) * 64, g * 512:(g + 1) * 512], ys_[:, :], reads=[ys_], writes=[self.db("yT1", hh * 100 + g)])
                yield k
        return gen(), len(steps)

    def b_phase(self):
        with ExitStack() as es:
            g, n = self._b_setup(es, [0, 1, 2, 3, 4, 5], [6, 7])
            for _ in g:
                pass
            self.c.barrier()

    NITER = 16
    TIE = True

    def _a_setup(self, es, sbanks, lpairs, pvpair):
        c, S, NT, NG = self.c, self.S, self.NT, self.NG
        NIT = self.NITER
        qi = c.sb(es, "qi", [128, 2, S], BF16)
        ki = c.sb(es, "ki", [128, S], BF16)
        ka = c.sb(es, "ka", [64, S], BF16)
        vext = c.sb(es, "vexta", [128, NT, 128], BF16)
        wi = c.sb(es, "wi", [128, NT, 4], F32)
        qat = [c.sb(es, "qat", [64, 1024], BF16) for _ in range(2)]
        score = c.sb(es, "score", [128, S], F32)
        isz = c.sb(es, "isz", [128, S], BF16)
        zrk = c.sb(es, "zrk", [128, S], BF16)
        maskb = [c.sb(es, "maskb", [128, S], BF16) for _ in range(2)]
        irep = c.sb(es, "irep", [128, 512], BF16)
        negc = c.sb(es, "negc", [128, 128], F32)
        pow2 = c.sb(es, "pow2", [128, NIT], F32)
        rts = [c.sb(es, "rt", [128, 512], F32) for _ in range(3)]
        pts = [c.sb(es, "pta", [128, 1024], BF16) for _ in range(3)]
        sm = c.sb(es, "bis", [128, 16], F32)
        steps = c.sb(es, "steps", [128, NIT], F32)
        rsum = c.sb(es, "rsuma", [64, 1024], F32)
        rsb = [Buf(name="rsa0"), Buf(name="rsa1")]
        yst = [c.sb(es, "ysta", [64, 1024], BF16) for _ in range(2)]
        cm = self.dram["cmat"]
        cb = self.db("cmat")
        for r in range(4):
            c.dma("pool", irep[:, r * 128:(r + 1) * 128], cm[0], reads=[cb], writes=[irep])
        c.dma("sp", negc[:, :], cm[4], reads=[cb], writes=[negc])
        c.dma("sp", pow2[:, :], self.dram["pow2"][:, 0:NIT], reads=[self.db("pow2")], writes=[pow2])
        allg = lambda nm: [self.db(nm, g) for g in range(NG)]
        c.dma("sp", qi[:, :, :], self.qiT.rearrange("(hp p) t -> p hp t", p=128), reads=allg("qiT"), writes=[qi])
        c.dma("sp", ki[0:64, :], self.kiT[:, :], reads=allg("kiT"), writes=[ki])
        c.dma("sp", ki[64:128, :], self.kiT[:, :], reads=allg("kiT"), writes=[ki])
        c.dma("sp", ka[:, :], self.kaT[:, :], reads=allg("kaT"), writes=[ka])
        c.dma("sp", vext[:, :, 0:64], self.vA.rearrange("(n p) d -> p n d", p=128), reads=allg("vA"), writes=[vext])
        c.op("pool", lambda h: h.memset(vext[:, :, 64:128], 1.0), writes=[vext])
        c.dma("sp", wi[:, :, :], self.smallD.rearrange("(n p) c -> p n c", p=128)[:, :, 0:4], reads=allg("smallD"), writes=[wi])
        qa3 = self.qaT.rearrange("(h d) t -> d h t", d=64)
        ya = self.yT[0].rearrange("(h d) t -> d h t", d=64)
        NEGM = -32768.0

        def stage1(i):
            ncols = (i + 1) * 128
            qt = qat[i % 2]
            mb = maskb[i % 2]
            c.dma("sp", qt[:, :].rearrange("d (h t) -> d h t", h=8), qa3[:, :, i * 128:(i + 1) * 128], reads=allg("qaT"), writes=[qt])
            for s0 in range(0, ncols, 512):
                w = min(512, ncols - s0)
                for hd in range(4):
                    pl = self.rotset("apl", sbanks)
                    hb, hp = (hd % 2) * 64, hd // 2
                    c.op("pe", lambda h, pl=pl, hb=hb, hp=hp, s0=s0, w=w: h.matmul(pl[:, 0:w], lhsT=qi[hb:hb + 64, hp, i * 128:(i + 1) * 128], rhs=ki[hb:hb + 64, s0:s0 + w], start=True, stop=True),
                         reads=[qi, ki], writes=[pl])
                    if hd == 0:
                        c.op("dve", lambda h, pl=pl, s0=s0, w=w: h.tensor_scalar(out=score[:, s0:s0 + w], in0=pl[:, 0:w], scalar1=0.0, scalar2=wi[:, i, 0:1], op0=ALU.max, op1=ALU.mult),
                             reads=[pl, wi], writes=[score])
                    else:
                        rt = rts[hd - 1]
                        c.op("act", lambda h, pl=pl, rt=rt, w=w: h.activation(out=rt[:, 0:w], in_=pl[:, 0:w], func=AF.Relu), reads=[pl], writes=[rt])
                        c.op("dve", lambda h, rt=rt, hd=hd, s0=s0, w=w: h.scalar_tensor_tensor(out=score[:, s0:s0 + w], in0=rt[:, 0:w], scalar=wi[:, i, hd:hd + 1], in1=score[:, s0:s0 + w],
                                                                                            op0=ALU.mult, op1=ALU.add),
                             reads=[rt, wi, score], writes=[score])
            sc = score[:, 0:ncols]
            c.op("dve", lambda h: h.tensor_reduce(out=sm[:, 5:6], in_=sc, axis=AX.X, op=ALU.max), reads=[score], writes=[sm])
            c.op("dve", lambda h: h.tensor_reduce(out=sm[:, 6:7], in_=sc, axis=AX.X, op=ALU.min), reads=[score, sm], writes=[sm])
            c.op("dve", lambda h: h.tensor_tensor(out=score[:, i * 128:ncols], in0=score[:, i * 128:ncols], in1=negc[:, :], op=ALU.add), reads=[score, negc], writes=[score])
            c.op("dve", lambda h: h.tensor_scalar(out=sm[:, 0:1], in0=sm[:, 6:7], scalar1=-1.0, scalar2=None, op0=ALU.add), reads=[sm], writes=[sm])
            c.op("dve", lambda h: h.scalar_tensor_tensor(out=sm[:, 1:2], in0=sm[:, 5:6], scalar=1.0, in1=sm[:, 0:1], op0=ALU.add, op1=ALU.subtract), reads=[sm], writes=[sm])
            c.op("dve", lambda h: h.tensor_scalar(out=steps[:, :], in0=pow2[:, :], scalar1=sm[:, 1:2], scalar2=None, op0=ALU.mult), reads=[sm, pow2], writes=[steps])
            for k in range(NIT):
                c.op("dve", lambda h, k=k: h.tensor_tensor(out=sm[:, 2:3], in0=sm[:, 0:1], in1=steps[:, k:k + 1], op=ALU.add), reads=[sm, steps], writes=[sm])
                c.op("dve", lambda h: h.tensor_scalar(out=isz[:, 0:ncols], in0=sc, scalar1=sm[:, 2:3], scalar2=0.0, op0=ALU.is_ge, op1=ALU.add, accum_out=sm[:, 3:4]),
                     reads=[score, sm], writes=[isz, sm])
                c.op("dve", lambda h, k=k: h.scalar_tensor_tensor(out=sm[:, 4:5], in0=sm[:, 3:4], scalar=255.5, in1=steps[:, k:k + 1], op0=ALU.is_ge, op1=ALU.mult), reads=[sm, steps], writes=[sm])
                c.op("dve", lambda h: h.tensor_tensor(out=sm[:, 0:1], in0=sm[:, 0:1], in1=sm[:, 4:5], op=ALU.add), reads=[sm], writes=[sm])
            c.op("dve", lambda h: h.tensor_scalar(out=isz[:, 0:ncols], in0=sc, scalar1=0.0, scalar2=0.0, op0=ALU.is_gt, op1=ALU.add, accum_out=sm[:, 7:8]), reads=[score, sm], writes=[isz, sm])
            c.op("dve", lambda h: h.tensor_scalar(out=isz[:, 0:ncols], in0=sc, scalar1=0.0, scalar2=0.0, op0=ALU.is_equal, op1=ALU.add, accum_out=sm[:, 8:9]), reads=[score, sm], writes=[isz, sm])
            c.op("dve", lambda h: h.tensor_tensor(out=sm[:, 8:9], in0=sm[:, 8:9], in1=sm[:, 7:8], op=ALU.add), reads=[sm], writes=[sm])
            c.op("dve", lambda h: h.tensor_scalar(out=sm[:, 13:14], in0=sm[:, 7:8], scalar1=255.5, scalar2=None, op0=ALU.is_lt), reads=[sm], writes=[sm])
            c.op("dve", lambda h: h.scalar_tensor_tensor(out=sm[:, 9:10], in0=sm[:, 8:9], scalar=255.5, in1=sm[:, 13:14], op0=ALU.is_ge, op1=ALU.mult), reads=[sm], writes=[sm])
            c.op("dve", lambda h: h.tensor_scalar(out=sm[:, 10:11], in0=sm[:, 7:8], scalar1=-1.0, scalar2=256.5, op0=ALU.mult, op1=ALU.add), reads=[sm], writes=[sm])
            c.op("dve", lambda h: h.tensor_scalar(out=sm[:, 13:14], in0=sm[:, 9:10], scalar1=-1.0, scalar2=1.0, op0=ALU.mult, op1=ALU.add), reads=[sm], writes=[sm])
            c.op("dve", lambda h: h.tensor_tensor(out=sm[:, 13:14], in0=sm[:, 13:14], in1=sm[:, 0:1], op=ALU.mult), reads=[sm], writes=[sm])
            c.op("dve", lambda h: h.scalar_tensor_tensor(out=sm[:, 11:12], in0=sm[:, 9:10], scalar=1e-30, in1=sm[:, 13:14], op0=ALU.mult, op1=ALU.add), reads=[sm], writes=[sm])
            c.op("dve", lambda h: h.tensor_scalar(out=sm[:, 12:13], in0=sm[:, 9:10], scalar1=-NEGM, scalar2=None, op0=ALU.mult), reads=[sm], writes=[sm])
            c.op("dve", lambda h: h.tensor_tensor_scan(out=zrk[:, 0:ncols], data0=isz[:, 0:ncols], data1=isz[:, 0:ncols], initial=0.0, op0=ALU.add, op1=ALU.max), reads=[isz], writes=[zrk])
            c.op("dve", lambda h: h.scalar_tensor_tensor(out=isz[:, 0:ncols], in0=zrk[:, 0:ncols], scalar=sm[:, 10:11], in1=isz[:, 0:ncols], op0=ALU.is_le, op1=ALU.mult), reads=[zrk, sm, isz], writes=[isz])
            c.op("dve", lambda h, mb=mb: h.tensor_scalar(out=mb[:, 0:ncols], in0=sc, scalar1=sm[:, 11:12], scalar2=NEGM, op0=ALU.is_lt, op1=ALU.mult), reads=[score, sm], writes=[mb])
            if self.TIE:
                c.op("dve", lambda h, mb=mb: h.scalar_tensor_tensor(out=mb[:, 0:ncols], in0=isz[:, 0:ncols], scalar=sm[:, 12:13], in1=mb[:, 0:ncols], op0=ALU.mult, op1=ALU.add), reads=[isz, sm, mb], writes=[mb])

        def stage2(i):
            qt = qat[i % 2]
            mb = maskb[i % 2]
            po = (self.ps[pvpair[0]], self.ps[pvpair[1]])
            lb = [b_ for pr in lpairs for b_ in pr]
            nlb = len(lb)
            hsteps = [(j, half) for j in range(i + 1) for half in range(2)]

            def alog(k):
                j, half = hsteps[k]
                pl = self.ps[lb[k % nlb]]
                c.op("pe", lambda h: h.matmul(pl[:, :], lhsT=ka[:, j * 128:(j + 1) * 128], rhs=qt[:, half * 512:(half + 1) * 512], start=True, stop=False),
                     reads=[ka, qt], writes=[pl], sig=False)
                c.op("pe", lambda h: h.matmul(pl[:, :], lhsT=mb[:, j * 128:(j + 1) * 128], rhs=irep[:, :], start=False, stop=True),
                     reads=[mb, irep], writes=[pl])

            ptb = [[Buf(name=f"apt{a_}_{b_}") for b_ in range(2)] for a_ in range(3)]
            la = min(nlb - 1, 3)
            for k in range(min(la, len(hsteps))):
                alog(k)
            for k, (j, half) in enumerate(hsteps):
                if k + la < len(hsteps):
                    alog(k + la)
                pl = self.ps[lb[k % nlb]]
                pt = pts[j % 3]
                c.op("act", lambda h, pl=pl, half=half, pt=pt: h.activation(out=pt[:, half * 512:(half + 1) * 512], in_=pl[:, :], func=AF.Exp, scale=0.125), reads=[pl], writes=[self._aptb(pt, half)])
                c.op("pe", lambda h, half=half, j=j, pt=pt, po=po: h.matmul(po[half][:, :], lhsT=vext[:, j, :], rhs=pt[:, half * 512:(half + 1) * 512], start=(j == 0), stop=(j == i)),
                     reads=[vext, self._aptb(pt, half)], writes=[po[half]], sig=(j == i))
            ys_ = yst[i % 2]
            for half in range(2):
                hs = slice(half * 512, (half + 1) * 512)
                rb = rsb[half]
                c.op("act", lambda h, half=half, hs=hs: h.activation(out=rsum[:, hs], in_=po[half][64:128, :], func=AF.Copy), reads=[po[half]], writes=[rb])
                c.op("act", lambda h, hs=hs: h.activation(out=rsum[:, hs], in_=rsum[:, hs], func=AF.Ln), reads=[rb], writes=[rb])
                c.op("act", lambda h, hs=hs: h.activation(out=rsum[:, hs], in_=rsum[:, hs], func=AF.Exp, scale=-1.0), reads=[rb], writes=[rb])
            for half in range(2):
                hs = slice(half * 512, (half + 1) * 512)
                c.op("dve", lambda h, half=half, hs=hs, ys_=ys_: h.tensor_tensor(out=ys_[:, hs], in0=po[half][0:64, :], in1=rsum[:, hs], op=ALU.mult), reads=[po[half], rsb[half]], writes=[ys_])
            c.dma("sp", ya[:, :, i * 128:(i + 1) * 128], ys_[:, :].rearrange("d (h t) -> d h t", h=8), reads=[ys_], writes=[self.db("yT0", i)])

        return stage1, stage2

    def _aptb(self, pt, half):
        d = self.__dict__.setdefault("_aptbufs", {})
        k = (id(pt), half)
        if k not in d:
            d[k] = Buf(name=f"aptb{len(d)}")
        return d[k]

    def a_phase(self):
        NT = self.NT
        with ExitStack() as es:
            stage1, stage2 = self._a_setup(es, [0, 1], [(2, 3), (4, 5)], (6, 7))
            stage1(0)
            for i in range(NT):
                if i + 1 < NT:
                    stage1(i + 1)
                stage2(i)
            self.c.barrier()

    def ab_phase(self):
        NT = self.NT
        with ExitStack() as es:
            stage1, stage2 = self._a_setup(es, [0, 1, 2], [(1, 2)], (3, 4))
            bgen, nb = self._b_setup(es, [5, 6], [7])
            done = 0

            def advance(upto):
                nonlocal done
                while done < min(upto, nb):
                    next(bgen)
                    done += 1
            stage1(0)
            for i in range(NT):
                if i + 1 < NT:
                    stage1(i + 1)
                stage2(i)
                advance((nb * (i + 1) + NT - 1) // NT)
            advance(nb)
            self.c.barrier()


def build_gmask():
    i = np.arange(128)
    out = np.zeros((14, 128, 512), np.float32)
    for l in range(7):
        b = 1 << l
        t, tp = i[:, None], i[None, :]
        m = ((t // (2 * b)) == (tp // (2 * b))) & ((t % (2 * b)) >= b) & ((tp % (2 * b)) < b)
        m = m.astype(np.float32)
        out[l] = np.tile(m, (1, 4))
        out[7 + l] = np.tile(m.T, (1, 4))
    return out


class ProgC(ProgAB):
    def c_phase(self, cvb):
        c, S, NT = self.c, self.S, self.NT
        with ExitStack() as es:
            def T(name, shape, dt, n=1):
                return [c.sb(es, name, shape, dt) for _ in range(n)]
            gm = T("gm", [128, 14, 512], BF16)[0]
            identrep = T("identrep", [128, 512], BF16)[0]
            negm4 = T("negm4", [128, 512], F32)[0]
            strict4 = T("strict4", [128, 512], BF16)[0]
            cm = self.dram["cmat"]
            cb = self.db("cmat")
            c.dma("pool", gm[:, :, :], self.dram["gmask"].rearrange("l p n -> p l n"), reads=[self.db("gmask")], writes=[gm])
            for r in range(4):
                c.dma("pool", identrep[:, r * 128:(r + 1) * 128], cm[0], reads=[cb], writes=[identrep])
                c.dma("sp", negm4[:, r * 128:(r + 1) * 128], cm[3], reads=[cb], writes=[negm4])
                c.dma("pool", strict4[:, r * 128:(r + 1) * 128], cm[5], reads=[cb], writes=[strict4])
            kTs = T("kTc", [128, 4, 128], BF16, 2); qTs = T("qTc", [128, 4, 128], BF16, 2)
            ktoks = T("ktok", [128, 512], BF16, 2); vtoks = T("vtok", [128, 512], BF16, 2)
            szs = T("szc", [128, 4, 128], BF16, 2); gbs = T("gb", [128, 8], F32, 2)
            gtri = T("gtri", [128, 4, 128], F32)[0]; ngtri = T("ngtri", [128, 4, 128], F32)[0]; bdiag = T("bdiag", [128, 4, 128], F32)[0]
            dm = T("dm", [128, 512], F32)[0]; decT = T("decT", [128, 512], F32)[0]; egcb = T("egcb", [128, 512], F32)[0]
            bbc = T("bbc", [128, 512], F32)[0]; dsb = T("dsb", [128, 512], F32)[0]
            ngb = T("ngb", [128, 4], F32)[0]
            gcs = T("gcs", [128, 8], F32)[0]; sm = T("csm", [128, 16], F32)[0]
            ATb = T("ATb", [128, 4, 128], BF16)[0]; Ab = T("Ab", [128, 4, 128], BF16)[0]; qkb = T("qkb", [128, 4, 128], BF16)[0]
            Ts = T("Tm", [128, 4, 128], BF16, 2); Us = T("Um", [128, 4, 128], BF16, 2)
            Xb = T("Xb", [128, 4, 128], BF16)[0]; Xpb = T("Xpb", [128, 4, 128], BF16)[0]; tmpb = T("tmpb", [128, 512], BF16, 2)
            kbg = T("kbg", [128, 512], BF16)[0]; vb = T("vb", [128, 512], BF16)[0]; kdec = T("kdec", [128, 512], BF16)[0]
            qg = T("qg", [128, 4, 128], BF16)[0]; nwT = T("nwT", [128, 4, 128], BF16)[0]; vnew = T("vnew", [128, 512], BF16)[0]
            state_f = T("state_f", [128, 4, 128], F32)[0]; state_b = T("state_b", [128, 4, 128], BF16)[0]
            osq = T("osq", [128, 4, 128], BF16)[0]; rstd = T("crstd", [128, 512], F32)[0]; yn = T("yn", [128, 512], F32)[0]
            yst = T("ystc", [128, 4, 128], BF16, 2)
            c.op("pool", lambda h: h.memset(state_f[:, :, :], 0.0), writes=[state_f])
            c.op("pool", lambda h: h.memset(state_b[:, :, :], 0.0), writes=[state_b])
            qc3 = self.qcT.rearrange("(h d) t -> d h t", d=128); kc3 = self.kcT.rearrange("(h d) t -> d h t", d=128)
            sz3 = self.szT.rearrange("(h d) t -> d h t", d=128); yc3 = self.yT[2].rearrange("(h d) t -> d h t", d=128)
            allg = lambda nm: [self.db(nm, g) for g in range(self.NG)]
            f2 = lambda b: b[:, :, :].rearrange("p h t -> p (h t)")
            for n in range(NT):
                cs = slice(n * 128, (n + 1) * 128)
                kT, qT, ktok, vtok, sz, gb = kTs[n % 2], qTs[n % 2], ktoks[n % 2], vtoks[n % 2], szs[n % 2], gbs[n % 2]
                c.dma("sp", kT[:, :, :], kc3[:, :, cs], reads=allg("kcT"), writes=[kT])
                c.dma("sp", qT[:, :, :], qc3[:, :, cs], reads=allg("qcT"), writes=[qT])
                c.dma("sp", sz[:, :, :], sz3[:, :, cs], reads=allg("szT"), writes=[sz])
                c.dma("sp", ktok[:, :], self.kC[cs, :], reads=allg("kC"), writes=[ktok])
                c.dma("sp", vtok[:, :], self.vC[cs, :], reads=allg("vC"), writes=[vtok])
                c.dma("sp", gb[:, :], self.smallD[cs, 12:20], reads=allg("smallD"), writes=[gb])
                c.op("dve", lambda h: h.tensor_scalar(out=ngb[:, :], in0=gb[:, 4:8], scalar1=-1.0, scalar2=None, op0=ALU.mult), reads=[gb], writes=[ngb])
                for hd in range(4):
                    c.op("dve", lambda h, hd=hd: h.tensor_scalar(out=gtri[:, hd, :], in0=self.triu_f[:, :], scalar1=gb[:, 4 + hd:5 + hd], scalar2=None, op0=ALU.mult), reads=[self.triu_f, gb], writes=[gtri])
                    c.op("act", lambda h, hd=hd: h.activation(out=ngtri[:, hd, :], in_=self.triu_f[:, :], func=AF.Copy, scale=ngb[:, hd:hd + 1]), reads=[self.triu_f, ngb], writes=[ngtri])
                    c.op("act", lambda h, hd=hd: h.activation(out=bdiag[:, hd, :], in_=self.ident_f[:, :], func=AF.Copy, scale=gb[:, hd:hd + 1]), reads=[self.ident_f, gb], writes=[bdiag])
                pD, pG, pB, pS = self.rot(), self.rot(), self.rot(), self.rot()
                for hd in range(4):
                    hs = slice(hd * 128, (hd + 1) * 128)
                    c.op("pe", lambda h, hd=hd, hs=hs: h.matmul(pD[:, hs], lhsT=self.ones_f[:, :], rhs=gtri[:, hd, :], start=True, stop=False), reads=[self.ones_f, gtri], writes=[pD], sig=False)
                    c.op("pe", lambda h, hd=hd, hs=hs: h.matmul(pD[:, hs], lhsT=ngtri[:, hd, :], rhs=self.ones_f[:, :], start=False, stop=True), reads=[self.ones_f, ngtri], writes=[pD], sig=(hd == 3))
                for hd in range(4):
                    hs = slice(hd * 128, (hd + 1) * 128)
                    c.op("pe", lambda h, hd=hd, hs=hs: h.matmul(pG[:, hs], lhsT=self.ones_f[:, :], rhs=gtri[:, hd, :], start=True, stop=True), reads=[self.ones_f, gtri], writes=[pG], sig=(hd == 3))
                for hd in range(4):
                    hs = slice(hd * 128, (hd + 1) * 128)
                    c.op("pe", lambda h, hd=hd, hs=hs: h.matmul(pB[:, hs], lhsT=self.ones_f[:, :], rhs=bdiag[:, hd, :], start=True, stop=True), reads=[self.ones_f, bdiag], writes=[pB], sig=(hd == 3))
                c.op("pe", lambda h: h.matmul(pS[:, 0:4], lhsT=self.triu_f[:, :], rhs=gb[:, 4:8], start=True, stop=True), reads=[self.triu_f, gb], writes=[pS], sig=False)
                c.op("pe", lambda h: h.matmul(pS[:, 4:8], lhsT=self.ones_f[:, :], rhs=gb[:, 4:8], start=True, stop=True), reads=[self.ones_f, gb], writes=[pS])
                c.op("dve", lambda h: h.scalar_tensor_tensor(out=dm[:, :], in0=pD[:, :], scalar=0.0, in1=negm4[:, :], op0=ALU.min, op1=ALU.add), reads=[pD, negm4], writes=[dm])
                c.op("act", lambda h: h.activation(out=decT[:, :], in_=dm[:, :], func=AF.Exp), reads=[dm], writes=[decT])
                c.op("act", lambda h: h.activation(out=egcb[:, :], in_=pG[:, :], func=AF.Exp), reads=[pG], writes=[egcb])
                c.op("act", lambda h: h.activation(out=bbc[:, :], in_=pB[:, :], func=AF.Copy), reads=[pB], writes=[bbc])
                c.op("act", lambda h: h.activation(out=gcs[:, :], in_=pS[:, 0:8], func=AF.Copy), reads=[pS], writes=[gcs])
                c.op("act", lambda h: h.activation(out=sm[:, 0:4], in_=gcs[:, 0:4], func=AF.Exp), reads=[gcs], writes=[sm])
                c.op("dve", lambda h: h.tensor_tensor(out=sm[:, 0:4], in0=sm[:, 0:4], in1=gb[:, 0:4], op=ALU.mult), reads=[sm, gb], writes=[sm])
                c.op("dve", lambda h: h.tensor_tensor(out=sm[:, 12:16], in0=gcs[:, 4:8], in1=gcs[:, 0:4], op=ALU.subtract), reads=[gcs, sm], writes=[sm])
                c.op("act", lambda h: h.activation(out=sm[:, 4:8], in_=sm[:, 12:16], func=AF.Exp), reads=[sm], writes=[sm])
                c.op("act", lambda h: h.activation(out=sm[:, 8:12], in_=gcs[:, 4:8], func=AF.Exp), reads=[gcs, sm], writes=[sm])
                pK, pQ = self.rot(), self.rot()
                for hd in range(4):
                    hs = slice(hd * 128, (hd + 1) * 128)
                    c.op("pe", lambda h, hd=hd, hs=hs: h.matmul(pK[:, hs], lhsT=kT[:, hd, :], rhs=kT[:, hd, :], start=True, stop=True), reads=[kT], writes=[pK], sig=(hd == 3))
                for hd in range(4):
                    hs = slice(hd * 128, (hd + 1) * 128)
                    c.op("pe", lambda h, hd=hd, hs=hs: h.matmul(pQ[:, hs], lhsT=kT[:, hd, :], rhs=qT[:, hd, :], start=True, stop=True), reads=[kT, qT], writes=[pQ], sig=(hd == 3))
                c.op("dve", lambda h: h.tensor_tensor(out=dsb[:, :], in0=decT[:, :], in1=bbc[:, :], op=ALU.mult), reads=[decT, bbc], writes=[dsb])
                c.op("dve", lambda h: h.tensor_tensor(out=dsb[:, :], in0=dsb[:, :], in1=strict4[:, :], op=ALU.mult), reads=[dsb, strict4], writes=[dsb])
                c.op("dve", lambda h: h.tensor_tensor(out=f2(ATb), in0=pK[:, :], in1=dsb[:, :], op=ALU.mult), reads=[pK, dsb], writes=[ATb])
                c.op("dve", lambda h: h.tensor_tensor(out=f2(qkb), in0=pQ[:, :], in1=decT[:, :], op=ALU.mult), reads=[pQ, decT], writes=[qkb])
                pA = self.rot()
                for hd in range(4):
                    hs = slice(hd * 128, (hd + 1) * 128)
                    c.op("pe", lambda h, hd=hd, hs=hs: h.matmul(pA[:, hs], lhsT=ATb[:, hd, :], rhs=self.ident_b[:, :], start=True, stop=True), reads=[ATb, self.ident_b], writes=[pA], sig=(hd == 3))
                c.op("act", lambda h: h.activation(out=f2(Ab), in_=pA[:, :], func=AF.Copy), reads=[pA], writes=[Ab])
                Tc, Uc = Ts[0], Us[0]
                c.op("dve", lambda h: h.tensor_tensor(out=tmpb[0][:, :], in0=f2(Ab), in1=gm[:, 0, :], op=ALU.mult), reads=[Ab, gm], writes=[tmpb[0]])
                c.op("dve", lambda h: h.tensor_tensor(out=f2(Tc), in0=identrep[:, :], in1=tmpb[0][:, :], op=ALU.subtract), reads=[tmpb[0], identrep], writes=[Tc])
                c.op("dve", lambda h: h.tensor_tensor(out=tmpb[1][:, :], in0=f2(ATb), in1=gm[:, 7, :], op=ALU.mult), reads=[ATb, gm], writes=[tmpb[1]])
                c.op("dve", lambda h: h.tensor_tensor(out=f2(Uc), in0=identrep[:, :], in1=tmpb[1][:, :], op=ALU.subtract), reads=[tmpb[1], identrep], writes=[Uc])
                for l in range(1, 7):
                    Tn, Un = Ts[l % 2], Us[l % 2]
                    pXp = self.rot()
                    for hd in range(4):
                        hs = slice(hd * 128, (hd + 1) * 128)
                        c.op("pe", lambda h, hd=hd, hs=hs, Uc=Uc: h.matmul(pXp[:, hs], lhsT=Ab[:, hd, :], rhs=Uc[:, hd, :], start=True, stop=True), reads=[Ab, Uc], writes=[pXp], sig=(hd == 3))
                    c.op("dve", lambda h, l=l: h.tensor_tensor(out=f2(Xpb), in0=pXp[:, :], in1=gm[:, 7 + l, :], op=ALU.mult), reads=[pXp, gm], writes=[Xpb])
                    if l < 6:
                        pX = self.rot()
                        for hd in range(4):
                            hs = slice(hd * 128, (hd + 1) * 128)
                            c.op("pe", lambda h, hd=hd, hs=hs, Tc=Tc: h.matmul(pX[:, hs], lhsT=ATb[:, hd, :], rhs=Tc[:, hd, :], start=True, stop=True), reads=[ATb, Tc], writes=[pX], sig=(hd == 3))
                        c.op("dve", lambda h, l=l: h.tensor_tensor(out=f2(Xb), in0=pX[:, :], in1=gm[:, l, :], op=ALU.mult), reads=[pX, gm], writes=[Xb])
                    pYp = self.rot()
                    for hd in range(4):
                        hs = slice(hd * 128, (hd + 1) * 128)
                        c.op("pe", lambda h, hd=hd, hs=hs, Tc=Tc: h.matmul(pYp[:, hs], lhsT=Tc[:, hd, :], rhs=Xpb[:, hd, :], start=True, stop=True), reads=[Tc, Xpb], writes=[pYp], sig=(hd == 3))
                    c.op("dve", lambda h, Uc=Uc, Un=Un: h.tensor_tensor(out=f2(Un), in0=f2(Uc), in1=pYp[:, :], op=ALU.subtract), reads=[Uc, pYp], writes=[Un])
                    if l < 6:
                        pY = self.rot()
                        for hd in range(4):
                            hs = slice(hd * 128, (hd + 1) * 128)
                            c.op("pe", lambda h, hd=hd, hs=hs, Uc=Uc: h.matmul(pY[:, hs], lhsT=Uc[:, hd, :], rhs=Xb[:, hd, :], start=True, stop=True), reads=[Uc, Xb], writes=[pY], sig=(hd == 3))
                        c.op("dve", lambda h, Tc=Tc, Tn=Tn: h.tensor_tensor(out=f2(Tn), in0=f2(Tc), in1=pY[:, :], op=ALU.subtract), reads=[Tc, pY], writes=[Tn])
                    Tc, Uc = Tn, Un
                U = Uc
                for hd in range(4):
                    hs = slice(hd * 128, (hd + 1) * 128)
                    c.op("act", lambda h, hd=hd, hs=hs: h.activation(out=kbg[:, hs], in_=ktok[:, hs], func=AF.Copy, scale=sm[:, hd:hd + 1]), reads=[ktok, sm], writes=[kbg])
                    c.op("act", lambda h, hd=hd, hs=hs: h.activation(out=vb[:, hs], in_=vtok[:, hs], func=AF.Copy, scale=gb[:, hd:hd + 1]), reads=[vtok, gb], writes=[vb])
                    c.op("act", lambda h, hd=hd, hs=hs: h.activation(out=kdec[:, hs], in_=ktok[:, hs], func=AF.Copy, scale=sm[:, 4 + hd:5 + hd]), reads=[ktok, sm], writes=[kdec])
                c.op("dve", lambda h: h.tensor_tensor(out=f2(qg), in0=f2(qT), in1=egcb[:, :], op=ALU.mult), reads=[qT, egcb], writes=[qg])
                pW = self.rot()
                for hd in range(4):
                    hs = slice(hd * 128, (hd + 1) * 128)
                    c.op("pe", lambda h, hd=hd, hs=hs: h.matmul(pW[:, hs], lhsT=kbg[:, hs], rhs=U[:, hd, :], start=True, stop=True), reads=[kbg, U], writes=[pW], sig=(hd == 3))
                c.op("act", lambda h: h.activation(out=f2(nwT), in_=pW[:, :], func=AF.Copy, scale=-1.0), reads=[pW], writes=[nwT])
                pV = self.rot()
                for hd in range(4):
                    hs = slice(hd * 128, (hd + 1) * 128)
                    c.op("pe", lambda h, hd=hd, hs=hs: h.matmul(pV[:, hs], lhsT=U[:, hd, :], rhs=vb[:, hs], start=True, stop=False), reads=[U, vb], writes=[pV], sig=False)
                    c.op("pe", lambda h, hd=hd, hs=hs: h.matmul(pV[:, hs], lhsT=nwT[:, hd, :], rhs=state_b[:, hd, :], start=False, stop=True), reads=[nwT, state_b], writes=[pV], sig=(hd == 3))
                c.op("act", lambda h: h.activation(out=vnew[:, :], in_=pV[:, :], func=AF.Copy), reads=[pV], writes=[vnew])
                pO = self.rot()
                for hd in range(4):
                    hs = slice(hd * 128, (hd + 1) * 128)
                    c.op("pe", lambda h, hd=hd, hs=hs: h.matmul(pO[:, hs], lhsT=state_b[:, hd, :], rhs=qg[:, hd, :], start=True, stop=False), reads=[state_b, qg], writes=[pO], sig=False)
                    c.op("pe", lambda h, hd=hd, hs=hs: h.matmul(pO[:, hs], lhsT=vnew[:, hs], rhs=qkb[:, hd, :], start=False, stop=True), reads=[vnew, qkb], writes=[pO], sig=(hd == 3))
                pS2 = self.rot()
                for hd in range(4):
                    hs = slice(hd * 128, (hd + 1) * 128)
                    c.op("pe", lambda h, hd=hd, hs=hs: h.matmul(pS2[:, hs], lhsT=kdec[:, hs], rhs=vnew[:, hs], start=True, stop=True), reads=[kdec, vnew], writes=[pS2], sig=(hd == 3))
                for hd in range(4):
                    hs = slice(hd * 128, (hd + 1) * 128)
                    c.op("dve", lambda h, hd=hd, hs=hs: h.scalar_tensor_tensor(out=state_f[:, hd, :], in0=state_f[:, hd, :], scalar=sm[:, 8 + hd:9 + hd], in1=pS2[:, hs], op0=ALU.mult, op1=ALU.add),
                         reads=[state_f, sm, pS2], writes=[state_f])
                c.op("act", lambda h: h.activation(out=f2(state_b), in_=f2(state_f), func=AF.Copy), reads=[state_f], writes=[state_b])
                c.op("act", lambda h: h.activation(out=f2(osq), in_=pO[:, :], func=AF.Square), reads=[pO], writes=[osq])
                pN = self.rot()
                for hd in range(4):
                    hs = slice(hd * 128, (hd + 1) * 128)
                    c.op("pe", lambda h, hd=hd, hs=hs: h.matmul(pN[:, hs], lhsT=self.ones_b[:, :], rhs=osq[:, hd, :], start=True, stop=True), reads=[self.ones_b, osq], writes=[pN], sig=(hd == 3))
                c.op("dve", lambda h: h.tensor_scalar(out=rstd[:, :], in0=pN[:, :], scalar1=1.0 / 128, scalar2=1e-6, op0=ALU.mult, op1=ALU.add), reads=[pN], writes=[rstd])
                c.op("act", lambda h: h.activation(out=rstd[:, :], in_=rstd[:, :], func=AF.Ln), reads=[rstd], writes=[rstd])
                c.op("act", lambda h: h.activation(out=rstd[:, :], in_=rstd[:, :], func=AF.Exp, scale=-0.5), reads=[rstd], writes=[rstd])
                c.op("dve", lambda h: h.tensor_tensor(out=yn[:, :], in0=pO[:, :], in1=rstd[:, :], op=ALU.mult), reads=[pO, rstd], writes=[yn])
                ys_ = yst[n % 2]
                c.op("dve", lambda h, ys_=ys_: h.scalar_tensor_tensor(out=f2(ys_), in0=yn[:, :], scalar=self.cvec[:, cvb + CV_DN:cvb + CV_DN + 1], in1=f2(sz), op0=ALU.mult, op1=ALU.mult),
                     reads=[yn, self.cvec, sz], writes=[ys_])
                c.dma("sp", yc3[:, :, cs], ys_[:, :, :], reads=[ys_], writes=[self.db("yT2", n)])
            c.barrier()


class ProgFull(ProgC):
    def merge_phase(self, xsrc, xdst, wbr, w_out):
        c, S, NG = self.c, self.S, self.NG
        with ExitStack() as es:
            wbs = [c.sb(es, "wbr", [128, 4, 1024], BF16) for _ in range(3)]
            wo = c.sb(es, "wo", [128, 8, 1024], BF16)
            wb = self.db("w")
            for i in range(3):
                c.dma("pool", wbs[i][:, :, :], wbr[i].rearrange("(k p) n -> p k n", p=128), reads=[wb], writes=[wbs[i]])
            c.dma("pool", wo[:, :, :], w_out.rearrange("(k p) n -> p k n", p=128), reads=[wb], writes=[wo])
            ys = [c.sb(es, "ymg", [128, 3, 4, 512], BF16) for _ in range(2)]
            gts = [c.sb(es, "gmg", [128, 24, 512], BF16) for _ in range(2)]
            xgs = [c.sb(es, "xmg", [128, 8, 512], F32) for _ in range(2)]
            mg = c.sb(es, "mg", [128, 8, 512], BF16)
            t1 = [c.sb(es, "mt1", [128, 512], F32) for _ in range(2)]
            t2 = [c.sb(es, "mt2", [128, 512], F32) for _ in range(2)]
            xs3 = xsrc.rearrange("(c p) t -> p c t", p=128)
            xd3 = xdst.rearrange("(c p) t -> p c t", p=128)
            def ldm(g):
                tsl = slice(g * 512, (g + 1) * 512)
                y, gt, xg = ys[g % 2], gts[g % 2], xgs[g % 2]
                deps = [self.db("yT0", i) for i in range(g * 4, g * 4 + 4)] + [self.db("yT1", hh * 100 + g) for hh in range(8)] + [self.db("yT2", n) for n in range(g * 4, g * 4 + 4)]
                for br in range(3):
                    c.dma("sp", y[:, br, :, :], self.yT[br].rearrange("(k p) t -> p k t", p=128)[:, :, tsl], reads=deps, writes=[y])
                c.dma("sp", gt[:, :, :], self.gT.rearrange("(k p) t -> p k t", p=128)[:, :, tsl], reads=[self.db("gT", sec * 100 + g) for sec in range(6)], writes=[gt])
                c.dma("sp", xg[:, :, :], xs3[:, :, tsl], reads=[self.db(xsrc.name, g)], writes=[xg])
            ldm(0)
            for g in range(NG):
                tsl = slice(g * 512, (g + 1) * 512)
                y, gt, xg = ys[g % 2], gts[g % 2], xgs[g % 2]
                if g + 1 < NG:
                    ldm(g + 1)
                for d in range(8):
                    pbs = []
                    for br in range(3):
                        pb = self.rot()
                        pbs.append(pb)
                        for k in range(4):
                            c.op("pe", lambda h, br=br, k=k, d=d, pb=pb: h.matmul(pb[:, :], lhsT=wbs[br][:, k, d * 128:(d + 1) * 128], rhs=y[:, br, k, :], start=(k == 0), stop=(k == 3)),
                                 reads=[wbs[br], y], writes=[pb], sig=(k == 3))
                    a, b = t1[d % 2], t2[d % 2]
                    c.op("dve", lambda h, d=d, a=a: h.tensor_tensor(out=a[:, :], in0=pbs[0][:, :], in1=gt[:, d, :], op=ALU.mult), reads=[pbs[0], gt], writes=[a])
                    c.op("dve", lambda h, d=d, b=b: h.tensor_tensor(out=b[:, :], in0=pbs[1][:, :], in1=gt[:, 8 + d, :], op=ALU.mult), reads=[pbs[1], gt], writes=[b])
                    c.op("dve", lambda h, a=a, b=b: h.tensor_tensor(out=a[:, :], in0=a[:, :], in1=b[:, :], op=ALU.add), reads=[a, b], writes=[a])
                    c.op("dve", lambda h, d=d, b=b: h.tensor_tensor(out=b[:, :], in0=pbs[2][:, :], in1=gt[:, 16 + d, :], op=ALU.mult), reads=[pbs[2], gt], writes=[b])
                    c.op("dve", lambda h, d=d, a=a, b=b: h.tensor_tensor(out=mg[:, d, :], in0=a[:, :], in1=b[:, :], op=ALU.add), reads=[a, b], writes=[mg])
                for d in range(8):
                    po = self.rot()
                    for k in range(8):
                        c.op("pe", lambda h, d=d, k=k, po=po: h.matmul(po[:, :], lhsT=wo[:, k, d * 128:(d + 1) * 128], rhs=mg[:, k, :], start=(k == 0), stop=(k == 7)),
                             reads=[wo, mg], writes=[po], sig=(k == 7))
                    c.op("dve", lambda h, d=d, po=po, xg=xg: h.tensor_tensor(out=xg[:, d, :], in0=po[:, :], in1=xg[:, d, :], op=ALU.add), reads=[po, xg], writes=[xg])
                c.dma("sp", xd3[:, :, tsl], xg[:, :, :], reads=[xg], writes=[self.db(xdst.name, g)])
            c.barrier()

    def final_phase(self, xsrc, out):
        c, S, NG = self.c, self.S, self.NG
        with ExitStack() as es:
            xgs = [c.sb(es, "xf", [128, 8, 512], F32) for _ in range(2)]
            ogs = [c.sb(es, "of", [128, 8, 512], F32) for _ in range(2)]
            sq = c.sb(es, "sqf", [128, 8, 512], BF16)
            rstd = c.sb(es, "rstdf", [128, 512], F32)
            xs3 = xsrc.rearrange("(c p) t -> p c t", p=128)
            o3 = out.rearrange("(c p) t -> p c t", p=128)
            for g in range(NG):
                tsl = slice(g * 512, (g + 1) * 512)
                xg, og = xgs[g % 2], ogs[g % 2]
                c.dma("sp", xg[:, :, :], xs3[:, :, tsl], reads=[self.db(xsrc.name, g)], writes=[xg])
                self.rmsnorm_group(xg, sq, lambda k, og=og: og[:, k, :], og, rstd, DEPTH * CV_PER_LAYER, self.rot())
                c.dma("sp", o3[:, :, tsl], og[:, :, :], reads=[og], writes=[self.db("out", g)])
            c.barrier()


def build_full(S=4096):
    P = ProgFull(S)
    es = ExitStack()
    P.setup_consts(es)
    P.declare_scratch()
    d = P.dr
    EI = "ExternalInput"
    d("rotC", [128, S], F32, kind=EI); d("rotS", [128, S], F32, kind=EI)
    d("pow2", [128, 26], F32, kind=EI); d("gmask", [14, 128, 512], F32, kind=EI)
    xin = d("xT_in", [1024, S], F32, kind=EI)
    out = d("outT", [1024, S], F32, kind="ExternalOutput")
    xa = d("xTa", [1024, S], F32); xb = d("xTb", [1024, S], F32)
    f1i = d("ffn1_w_in", [DEPTH, 1024, 4096], F32, kind=EI); f1o = d("ffn1_w_out", [DEPTH, 2048, 1024], F32, kind=EI)
    f2i = d("ffn2_w_in", [DEPTH, 1024, 4096], F32, kind=EI); f2o = d("ffn2_w_out", [DEPTH, 2048, 1024], F32, kind=EI)
    w = d("w_in", [DEPTH, 1024, 7636], F32, kind=EI); wsw = d("w_sw", [DEPTH, 1024, 896], F32, kind=EI); wsm = d("w_small", [DEPTH, 1024, 84], F32, kind=EI)
    wba = d("w_branch_a", [DEPTH, 512, 1024], F32, kind=EI); wbb = d("w_branch_b", [DEPTH, 512, 1024], F32, kind=EI); wbc = d("w_branch_c", [DEPTH, 512, 1024], F32, kind=EI)
    wo = d("w_out", [DEPTH, 1024, 1024], F32, kind=EI)
    cur = xin
    for l in range(DEPTH):
        cvb = l * CV_PER_LAYER
        P.ffn_phase(cur, xa, f1i[l], f1o[l], cvb + CV_FFN1)
        P.m1_phase(xa, w[l], wsw[l], wsm[l], cvb)
        P.ab_phase()
        P.c_phase(cvb)
        P.merge_phase(xa, xb, [wba[l], wbb[l], wbc[l]], wo[l])
        P.ffn_phase(xb, xa, f2i[l], f2o[l], cvb + CV_FFN2)
        cur = xa
    P.final_phase(xa, out)
    es.close()
    return P


_CACHE = {}


def kernel(**inputs):
    S = 4096
    inp = {k: np.asarray(v) for k, v in inputs.items()}
    if "prog" not in _CACHE:
        _CACHE["prog"] = build_full(S)
    P = _CACHE["prog"]
    rotC, rotS = build_rot(S)
    shared = {
        "cmat": build_cmat(), "cvec": build_cvec(inp), "rotC": rotC, "rotS": rotS, "pow2": build_pow2(), "gmask": build_gmask(),
        "ffn1_w_in": inp["ffn1_w_in"], "ffn1_w_out": inp["ffn1_w_out"], "ffn2_w_in": inp["ffn2_w_in"], "ffn2_w_out": inp["ffn2_w_out"],
        "w_in": inp["w_in"], "w_sw": np.ascontiguousarray(inp["w_in"][:, :, swap_cols()]), "w_small": np.ascontiguousarray(inp["w_in"][:, :, small_cols()]),
        "w_branch_a": inp["w_branch_a"], "w_branch_b": inp["w_branch_b"], "w_branch_c": inp["w_branch_c"], "w_out": inp["w_out"],
    }
    in_maps = []
    for b in range(NB):
        m = dict(shared)
        m["xT_in"] = np.ascontiguousarray(inp["x"][b].T)
        in_maps.append(m)
    res = run_bass_kernel_spmd(P.nc, in_maps, core_ids=list(range(NB)))
    out = np.stack([np.ascontiguousarray(r["outT"].T) for r in res.results], axis=0)
    return out.astype(np.float32)
```
